# Optimizing a Trainium2 kernel written in Bass

```python
import math
import jax, jax.numpy as jnp
from jax import lax
import numpy as np

D_MODEL = 2048
BATCH = 1
SEQ = 8192
DEPTH = 2

HEAD_DIM = 128
FOX_HEADS = 6
GLA_HEADS = 4
DSA_HEADS = 6
FOX_W = FOX_HEADS * HEAD_DIM
GLA_DK = HEAD_DIM
GLA_DV = HEAD_DIM
GLA_W = GLA_HEADS * GLA_DV
DSA_W = DSA_HEADS * HEAD_DIM
MIX_W = FOX_W + GLA_W + DSA_W
Q_BLOCK = 128
GLA_GATE_RANK = 16
GLA_GATE_TAU = 16.0
GLA_CHUNK = 64
IDX_HEADS = 16
IDX_DIM = 64
DSA_MAX_TOPK = 256
PEER_HEADS = 8
PEER_DQ = 256
PEER_DHALF = PEER_DQ // 2
PEER_NKEYS = 128
PEER_EXPERTS = PEER_NKEYS * PEER_NKEYS
PEER_TOPK = 16
PEER_BLOCK = 64
RMS_EPS = 1e-6
N_MOD = 6

IN_SIZES = (
    FOX_W, FOX_W, FOX_W, FOX_HEADS,
    GLA_HEADS * GLA_DK, GLA_HEADS * GLA_DK, GLA_W, GLA_W, GLA_GATE_RANK,
    DSA_W, DSA_W, DSA_W,
    IDX_HEADS * IDX_DIM, IDX_DIM, IDX_HEADS,
)
N_IN = sum(IN_SIZES)

kernel_name = 'hybrid_fox_gla_dsa_peer'


def rmsnorm(x, g):
    xf = x.astype(jnp.float32)
    y = xf * lax.rsqrt(jnp.mean(xf * xf, axis=-1, keepdims=True) + RMS_EPS)
    return (y * g.astype(jnp.float32)).astype(x.dtype)


def split_cols(z):
    parts, o = [], 0
    for n in IN_SIZES:
        parts.append(z[..., o:o + n])
        o += n
    return parts


def fox_attention(q, k, v, f_logit, f_bias):
    B, S, H, dh = q.shape
    logf = jax.nn.log_sigmoid(f_logit.astype(jnp.float32) + f_bias.astype(jnp.float32))
    F = jnp.cumsum(logf, axis=1).transpose(0, 2, 1)
    kh = k.transpose(0, 2, 1, 3)
    vh = v.transpose(0, 2, 1, 3)
    scale = dh ** -0.5
    kpos = jnp.arange(S)

    def block(i):
        start = i * Q_BLOCK
        qb = lax.dynamic_slice_in_dim(q, start, Q_BLOCK, axis=1)
        Fq = lax.dynamic_slice_in_dim(F, start, Q_BLOCK, axis=2)
        s = jnp.einsum('bthd,bhsd->bhts', qb, kh).astype(jnp.float32) * scale
        s = s + Fq[..., :, None] - F[..., None, :]
        qpos = start + jnp.arange(Q_BLOCK)
        causal = kpos[None, :] <= qpos[:, None]
        s = jnp.where(causal, s, -jnp.inf)
        p = jax.nn.softmax(s, axis=-1).astype(v.dtype)
        return jnp.einsum('bhts,bhsd->bthd', p, vh)

    out = lax.map(block, jnp.arange(S // Q_BLOCK))
    return out.transpose(1, 0, 2, 3, 4).reshape(B, S, H * dh)


def gla_attention(q, k, v, log_a):
    B, S, H, dk = q.shape
    dv = v.shape[-1]
    C = GLA_CHUNK
    n = S // C

    def chunks(t):
        return t.astype(jnp.float32).reshape(B, n, C, H, t.shape[-1]).transpose(1, 0, 3, 2, 4)

    qc = chunks(q) * (dk ** -0.5)
    kc = chunks(k)
    vc = chunks(v)
    gc = jnp.cumsum(chunks(log_a), axis=3)
    tri = jnp.tril(jnp.ones((C, C), dtype=bool))

    def step(state, inp):
        qi, ki, vi, gi = inp
        inter = jnp.einsum('bhtd,bhde->bhte', qi * jnp.exp(gi), state)
        diff = gi[:, :, :, None, :] - gi[:, :, None, :, :]
        decay = jnp.exp(jnp.where(tri[:, :, None], diff, -jnp.inf))
        A = jnp.einsum('bhtd,bhsd,bhtsd->bhts', qi, ki, decay)
        intra = jnp.einsum('bhts,bhse->bhte', A, vi)
        glast = gi[:, :, -1:, :]
        state = jnp.exp(glast[:, :, 0, :])[..., None] * state + jnp.einsum(
            'bhsd,bhse->bhde', ki * jnp.exp(glast - gi), vi)
        return state, inter + intra

    state0 = jnp.zeros((B, H, dk, dv), jnp.float32)
    _, out = lax.scan(step, state0, (qc, kc, vc, gc))
    return out.transpose(1, 0, 3, 2, 4).reshape(B, S, H, dv)


def dsa_attention(q, k, v, iq, ik, iw):
    B, S, H, dh = q.shape
    topk = min(DSA_MAX_TOPK, S // 4)
    scale = dh ** -0.5
    w = iw.astype(jnp.float32) * (IDX_HEADS ** -0.5)
    kpos = jnp.arange(S)
    gather = jax.vmap(lambda t, i: t[i])

    def block(i):
        start = i * Q_BLOCK
        qpos = start + jnp.arange(Q_BLOCK)
        iqb = lax.dynamic_slice_in_dim(iq, start, Q_BLOCK, axis=1)
        wb = lax.dynamic_slice_in_dim(w, start, Q_BLOCK, axis=1)
        rel = jax.nn.relu(jnp.einsum('bthd,bsd->bths', iqb, ik).astype(jnp.float32) * (IDX_DIM ** -0.5))
        score = jnp.einsum('bths,bth->bts', rel, wb)
        causal = kpos[None, :] <= qpos[:, None]
        score = jnp.where(causal[None], score, -jnp.inf)
        _, idx = lax.top_k(score, topk)
        valid = idx <= qpos[None, :, None]
        kg = gather(k, idx)
        vg = gather(v, idx)
        qb = lax.dynamic_slice_in_dim(q, start, Q_BLOCK, axis=1)
        s = jnp.einsum('bthd,btkhd->bhtk', qb, kg).astype(jnp.float32) * scale
        s = jnp.where(valid[:, None], s, -jnp.inf)
        p = jax.nn.softmax(s, axis=-1).astype(v.dtype)
        return jnp.einsum('bhtk,btkhd->bthd', p, vg)

    out = lax.map(block, jnp.arange(S // Q_BLOCK))
    return out.transpose(1, 0, 2, 3, 4).reshape(B, S, H * dh)


def peer_ffn(h, wq, k1, k2, u, v):
    B, S, D = h.shape
    q = jnp.einsum('bsd,de->bse', h, wq).reshape(B, S, PEER_HEADS, 2, PEER_DHALF)
    s1 = jnp.einsum('bshd,hnd->bshn', q[..., 0, :], k1).astype(jnp.float32)
    s2 = jnp.einsum('bshd,hnd->bshn', q[..., 1, :], k2).astype(jnp.float32)
    v1, i1 = lax.top_k(s1, PEER_TOPK)
    v2, i2 = lax.top_k(s2, PEER_TOPK)
    cand = (v1[..., :, None] + v2[..., None, :]).reshape(B, S, PEER_HEADS, PEER_TOPK * PEER_TOPK)
    cidx = (i1[..., :, None] * PEER_NKEYS + i2[..., None, :]).reshape(B, S, PEER_HEADS, PEER_TOPK * PEER_TOPK)
    best, pos = lax.top_k(cand, PEER_TOPK)
    eidx = jnp.take_along_axis(cidx, pos, axis=-1)
    gate = jax.nn.softmax(best, axis=-1)

    def block(i):
        start = i * PEER_BLOCK
        hb = lax.dynamic_slice_in_dim(h, start, PEER_BLOCK, axis=1)
        eb = lax.dynamic_slice_in_dim(eidx, start, PEER_BLOCK, axis=1)
        gb = lax.dynamic_slice_in_dim(gate, start, PEER_BLOCK, axis=1)
        ub = u[eb]
        vb = v[eb]
        a = jax.nn.gelu(jnp.einsum('btd,bthkd->bthk', hb, ub).astype(jnp.float32))
        return jnp.einsum('bthk,bthkd->btd', (gb * a).astype(h.dtype), vb)

    out = lax.map(block, jnp.arange(S // PEER_BLOCK))
    return out.transpose(1, 0, 2, 3).reshape(B, S, D)


def setup_inputs(seed: int = 0) -> dict:
    key = jax.random.key(seed)
    ks = jax.random.split(key, 20)
    f32 = jnp.float32
    L, D = DEPTH, D_MODEL
    nrm = lambda k, shape, s: jax.random.normal(k, shape, f32) * s
    return {
        'x': nrm(ks[0], (BATCH, SEQ, D), 1.0),
        'c': nrm(ks[1], (BATCH, D), 1.0),
        'ada_w': nrm(ks[2], (L, D, N_MOD * D), 0.5 * D ** -0.5),
        'ada_b': nrm(ks[3], (L, N_MOD * D), 0.01),
        'norm1_g': 1.0 + nrm(ks[4], (L, D), 0.01),
        'norm2_g': 1.0 + nrm(ks[5], (L, D), 0.01),
        'final_g': 1.0 + nrm(ks[6], (D,), 0.01),
        'w_in': nrm(ks[7], (L, D, N_IN), D ** -0.5),
        'fox_fbias': 3.0 + nrm(ks[8], (L, FOX_HEADS), 0.5),
        'gla_wa2': nrm(ks[9], (L, GLA_GATE_RANK, GLA_HEADS * GLA_DK), GLA_GATE_RANK ** -0.5),
        'gla_ba': nrm(ks[10], (L, GLA_HEADS * GLA_DK), 0.01),
        'gla_norm_g': 1.0 + nrm(ks[11], (L, GLA_DV), 0.01),
        'w_out': nrm(ks[12], (L, MIX_W, D), MIX_W ** -0.5),
        'peer_wq': nrm(ks[13], (L, D, PEER_HEADS * PEER_DQ), D ** -0.5),
        'peer_k1': nrm(ks[14], (L, PEER_HEADS, PEER_NKEYS, PEER_DHALF), PEER_DHALF ** -0.5),
        'peer_k2': nrm(ks[15], (L, PEER_HEADS, PEER_NKEYS, PEER_DHALF), PEER_DHALF ** -0.5),
        'peer_u': nrm(ks[16], (L, PEER_EXPERTS, D), D ** -0.5),
        'peer_v': nrm(ks[17], (L, PEER_EXPERTS, D), PEER_HEADS ** -0.5),
    }


def reference(x, c, ada_w, ada_b, norm1_g, norm2_g, final_g, w_in, fox_fbias, gla_wa2, gla_ba,
              gla_norm_g, w_out, peer_wq, peer_k1, peer_k2, peer_u, peer_v):
    B, S, D = x.shape
    c_act = jax.nn.silu(c)
    for l in range(DEPTH):
        mod = jnp.einsum('bd,de->be', c_act, ada_w[l]) + ada_b[l]
        sh1, sc1, g1, sh2, sc2, g2 = [m[:, None, :] for m in jnp.split(mod, N_MOD, axis=-1)]

        h = rmsnorm(x, norm1_g[l]) * (1.0 + sc1) + sh1
        z = jnp.einsum('bsd,de->bse', h, w_in[l])
        (fq, fk, fv, ff, gq, gk, gv, gr, ga, dq, dk, dv, iq, ik, iw) = split_cols(z)

        fox = fox_attention(fq.reshape(B, S, FOX_HEADS, HEAD_DIM), fk.reshape(B, S, FOX_HEADS, HEAD_DIM),
                            fv.reshape(B, S, FOX_HEADS, HEAD_DIM), ff, fox_fbias[l])

        log_a = jax.nn.log_sigmoid(jnp.einsum('bsr,re->bse', ga, gla_wa2[l]).astype(jnp.float32)
                                   + gla_ba[l].astype(jnp.float32)) / GLA_GATE_TAU
        go = gla_attention(gq.reshape(B, S, GLA_HEADS, GLA_DK), gk.reshape(B, S, GLA_HEADS, GLA_DK),
                           gv.reshape(B, S, GLA_HEADS, GLA_DV), log_a.reshape(B, S, GLA_HEADS, GLA_DK))
        gla = (rmsnorm(go, gla_norm_g[l]).reshape(B, S, GLA_W)
               * jax.nn.silu(gr.astype(jnp.float32))).astype(x.dtype)

        dsa = dsa_attention(dq.reshape(B, S, DSA_HEADS, HEAD_DIM), dk.reshape(B, S, DSA_HEADS, HEAD_DIM),
                            dv.reshape(B, S, DSA_HEADS, HEAD_DIM), iq.reshape(B, S, IDX_HEADS, IDX_DIM), ik, iw)

        mix = jnp.concatenate([fox, gla, dsa], axis=-1)
        x = x + g1 * jnp.einsum('bse,ed->bsd', mix, w_out[l])

        h = rmsnorm(x, norm2_g[l]) * (1.0 + sc2) + sh2
        x = x + g2 * peer_ffn(h, peer_wq[l], peer_k1[l], peer_k2[l], peer_u[l], peer_v[l])
    return rmsnorm(x, final_g)
```

```python
import numpy as np
import ml_dtypes
from contextlib import ExitStack
import concourse.bass as bass
import concourse.mybir as mybir
from concourse.bass_utils import run_bass_kernel_spmd

F32 = mybir.dt.float32
BF16 = mybir.dt.bfloat16
U32 = mybir.dt.uint32
ALU = mybir.AluOpType
AF = mybir.ActivationFunctionType
AX = mybir.AxisListType
NPBF = ml_dtypes.bfloat16

ENGS = ("pe", "act", "dve", "pool", "sp")


class Buf:
    __slots__ = ("t", "name", "w", "r", "pr", "dsem")

    def __init__(self, t, name):
        self.t = t
        self.name = name
        self.w = {}
        self.r = {}
        self.pr = {}
        self.dsem = None

    def __getitem__(self, idx):
        return self.t[idx]


class Prog:
    def __init__(self, nc, n_dma_sems=80):
        self.nc = nc
        self.stack = ExitStack()
        self.esem = {}
        for e in ENGS[:4]:
            self.esem[e] = self.stack.enter_context(nc.semaphore("sem_" + e))
        self.ecnt = {e: 0 for e in ENGS[:4]}
        self.dpool = [self.stack.enter_context(nc.semaphore("dsem%d" % i)) for i in range(n_dma_sems)]
        self.dfree = list(range(n_dma_sems))
        self.dcnt = [0] * n_dma_sems
        self.semobj = {}
        for e in ENGS[:4]:
            self.semobj[("e", e)] = self.esem[e]
        for i, s in enumerate(self.dpool):
            self.semobj[("d", i)] = s
        self.seen = {e: {} for e in ENGS}
        self.ops = {e: [] for e in ENGS}
        self.pstack = None
        self.pbufs = []
        self.nbuf = 0
        self.n_instr = 0

    def begin_phase(self):
        self.pstack = ExitStack()
        self.pbufs = []

    def sb(self, shape, dtype, name=None):
        self.nbuf += 1
        name = (name or "sb") + "_%d" % self.nbuf
        t = self.pstack.enter_context(self.nc.sbuf_tensor(name, list(shape), dtype))
        b = Buf(t, name)
        self.pbufs.append(b)
        return b

    def ps(self, shape, dtype, name=None):
        self.nbuf += 1
        name = (name or "ps") + "_%d" % self.nbuf
        t = self.pstack.enter_context(self.nc.psum_tensor(name, list(shape), dtype))
        b = Buf(t, name)
        self.pbufs.append(b)
        return b

    def sbp(self, shape, dtype, name=None):
        self.nbuf += 1
        name = (name or "sbp") + "_%d" % self.nbuf
        t = self.stack.enter_context(self.nc.sbuf_tensor(name, list(shape), dtype))
        return Buf(t, name)

    def scope_begin(self):
        self.sstack = ExitStack()
        self.sbufs = []

    def sbs(self, shape, dtype, name=None):
        self.nbuf += 1
        name = (name or "sbs") + "_%d" % self.nbuf
        t = self.sstack.enter_context(self.nc.sbuf_tensor(name, list(shape), dtype))
        b = Buf(t, name)
        self.sbufs.append(b)
        return b

    def scope_end(self):
        self.begin_phase()
        self.pbufs = list(self.sbufs)
        self.end_phase()
        self.sbufs = []
        self.sstack.close()

    def dram(self, name, shape, dtype, kind):
        t = self.nc.dram_tensor(name, list(shape), dtype, kind=kind).ap()
        return Buf(t, name)

    def _dsem_of(self, buf):
        if buf.dsem is None:
            buf.dsem = self.dfree.pop()
        return buf.dsem

    def op(self, eng, fn, reads=(), writes=(), dma=None):
        kind = "dma" if dma is not None else "compute"
        deps = {}

        def add(key, val, src):
            if kind == "compute" and src == eng:
                return
            if key not in deps or deps[key] < val:
                deps[key] = val

        def add_raw(key, val, src):
            if kind == "compute" and src == eng and eng == "pe":
                return
            if key not in deps or deps[key] < val:
                deps[key] = val

        for b in reads:
            for key, (val, src) in b.w.items():
                add_raw(key, val, src)
        for b in writes:
            for key, (val, src) in b.r.items():
                add(key, val, src)
            for key, (val, src) in b.pr.items():
                add(key, val, src)
            for key, (val, src) in b.w.items():
                if kind == "dma" and key[0] == "d":
                    continue
                add(key, val, src)
        if kind == "compute":
            self.ecnt[eng] += 1
            key, val, src = ("e", eng), self.ecnt[eng], eng
        else:
            i = self._dsem_of(dma)
            self.dcnt[i] += 16
            key, val, src = ("d", i), self.dcnt[i], None
        waits = []
        seen = self.seen[eng]
        for k, v in deps.items():
            if seen.get(k, 0) < v:
                seen[k] = v
                waits.append((k, v))
        for b in reads:
            if b.r.get(key, (0, None))[0] < val:
                b.r[key] = (val, src)
        for b in writes:
            if b.r:
                b.pr = dict(b.r)
                b.w.clear()
                b.r.clear()
            if b.w.get(key, (0, None))[0] < val:
                b.w[key] = (val, src)
        self.ops[eng].append((waits, fn, key))
        self.n_instr += 1

    def load(self, q, dstbuf, out_ap, in_ap, dram=None, **kw):
        self.op(q, lambda e: e.dma_start(out=out_ap, in_=in_ap, **kw), reads=([dram] if dram is not None else []),
                writes=[dstbuf], dma=dstbuf)

    def store(self, q, srcbuf, out_ap, in_ap, dram=None, **kw):
        self.op(q, lambda e: e.dma_start(out=out_ap, in_=in_ap, **kw), reads=[srcbuf],
                writes=([dram] if dram is not None else []), dma=srcbuf)

    def mm(self, out_ap, lhsT, rhs, start, stop, reads, writes):
        self.op("pe", lambda e: e.matmul(out_ap, lhsT, rhs, start=start, stop=stop), reads=reads, writes=writes)

    def end_phase(self, final=False):
        nc = self.nc
        ops = self.ops
        semobj = self.semobj
        esem = self.esem
        finalwaits = []
        if final:
            for i, c in enumerate(self.dcnt):
                if c > 0:
                    finalwaits.append((("d", i), c))
        else:
            for b in self.pbufs:
                if b.dsem is not None and self.dcnt[b.dsem] > 0:
                    finalwaits.append((("d", b.dsem), self.dcnt[b.dsem]))

        def emit(e, name):
            for waits, fn, key in ops[name]:
                for k, v in waits:
                    e.wait_ge(semobj[k], v)
                ins = fn(e)
                if key[0] == "e":
                    ins.then_inc(esem[key[1]], 1)
                else:
                    ins.then_inc(semobj[key], 16)
            if name == "sp":
                for k, v in finalwaits:
                    e.wait_ge(semobj[k], v)

        with nc.Block() as block:
            if ops["sp"] or finalwaits:
                @block.sync
                def _(e):
                    emit(e, "sp")
            if ops["pe"]:
                @block.tensor
                def _(e):
                    emit(e, "pe")
            if ops["act"]:
                @block.scalar
                def _(e):
                    emit(e, "act")
            if ops["dve"]:
                @block.vector
                def _(e):
                    emit(e, "dve")
            if ops["pool"]:
                @block.gpsimd
                def _(e):
                    emit(e, "pool")
        self.ops = {e: [] for e in ENGS}
        for b in self.pbufs:
            if b.dsem is not None:
                self.dfree.append(b.dsem)
        self.pbufs = []
        self.pstack.close()
        self.pstack = None

    def finish(self):
        self.stack.close()


D = 2048
EPS = 1e-6
NTOK = 1024
NB = 8
O_FQ, O_FK, O_FV, O_FF = 0, 768, 1536, 2304
O_GQ, O_GK, O_GV, O_GR, O_GA = 2310, 2822, 3334, 3846, 4358
O_DQ, O_DK, O_DV = 4374, 5142, 5910
O_IQ, O_IK, O_IW = 6678, 7702, 7766
FM_COLS = (list(range(O_FQ, O_FQ + 768)) + list(range(O_FK, O_FK + 768)) + list(range(O_DQ, O_DQ + 768))
           + list(range(O_DK, O_DK + 768)) + list(range(O_GQ, O_GQ + 512)) + list(range(O_GK, O_GK + 512))
           + list(range(O_IQ, O_IQ + 1024)) + list(range(O_IK, O_IK + 64)) + list(range(O_GA, O_GA + 16)))
NFM = 5248
R_FQ, R_FK, R_DQ, R_DK, R_GQ, R_GK, R_IQ, R_IK, R_GA = 0, 768, 1536, 2304, 3072, 3584, 4096, 5120, 5184
TM_COLS = (list(range(O_FV, O_FV + 768)) + list(range(O_DV, O_DV + 768)) + list(range(O_GK, O_GK + 512))
           + list(range(O_GV, O_GV + 512)) + list(range(O_GR, O_GR + 512)) + list(range(O_FF, O_FF + 6))
           + list(range(O_IW, O_IW + 16)))
NTM = 3200
C_FV, C_DV, C_GK, C_GV = 0, 768, 1536, 2048
C_GR, C_FF, C_IW = 0, 512, 518


def emit_mod(P, c_d, adaw_d, adab_d, ncols, modbc):
    P.begin_phase()
    ct = P.sb([128, 16], F32, "sb_ct")
    ca = P.sb([128, 16], F32, "sb_ca")
    crep = P.sb([128, 16, 128], F32, "sb_crep")
    adab = P.sb([128, ncols], F32, "sb_adab")
    wch = [P.sb([128, 16, 512], F32, "sb_wch") for _ in range(2)]
    pm = [P.ps([128, 512], F32, "ps_mod") for _ in range(2)]
    P.load("sp", ct, ct[:, :], c_d[:, :])
    P.load("act", adab, adab[:, :], adab_d[:, :])
    P.op("act", lambda e: e.activation(out=ca[:, :], in_=ct[:, :], func=AF.Silu), reads=[ct], writes=[ca])
    P.op("dve", lambda e: e.tensor_copy(out=crep[:, :, :], in_=ca[:, :].unsqueeze(2).to_broadcast([128, 16, 128])),
         reads=[ca], writes=[crep])
    wv = adaw_d.t.rearrange("(k p) n -> p k n", p=128)
    for ci in range(ncols // 512):
        w = wch[ci % 2]
        q = "sp" if ci % 2 == 0 else "act"
        for kh in range(2):
            P.load(q, w, w[:, kh * 8:(kh + 1) * 8, :], wv[:, kh * 8:(kh + 1) * 8, ci * 512:(ci + 1) * 512])
        ps = pm[ci % 2]
        for k in range(16):
            P.mm(ps[:, :], crep[:, k, :], w[:, k, :], k == 0, k == 15, reads=[crep, w], writes=[ps])
        P.op("dve", lambda e, ps=ps, ci=ci: e.tensor_tensor(out=modbc[:, ci * 512:(ci + 1) * 512], in0=ps[:, :],
                                                          in1=adab[:, ci * 512:(ci + 1) * 512], op=ALU.add),
             reads=[ps, adab], writes=[modbc])
    P.end_phase()


def emit_norm_T(P, xs_d, A_bc, B_bc, ident, hT, xkeep=None, xdram=None):
    P.begin_phase()
    xt = [P.sb([128, D], F32, "sb_x") for _ in range(2)]
    junk = P.sb([128, D], BF16, "sb_junk")
    tmp = [P.sb([128, D], F32, "sb_tmp") for _ in range(2)]
    hb = [P.sb([128, D], BF16, "sb_hb") for _ in range(2)]
    st = [P.sb([128, 4], F32, "sb_st") for _ in range(2)]
    pT = [P.ps([128, 8, 128], BF16, "ps_T") for _ in range(2)]
    for b in range(NB):
        if xkeep is None:
            x = xt[b % 2]
            P.load("sp" if b % 2 == 0 else "act", x, x[:, :], xs_d[b, :, :], dram=xdram)
            xa = x[:, :]
            xr = [x]
        else:
            xa = xkeep[:, b, :]
            xr = [xkeep]
        s = st[b % 2]
        t = tmp[b % 2]
        h = hb[b % 2]
        P.op("act", lambda e, xa=xa, s=s: e.activation(out=junk[:, :], in_=xa, func=AF.Square, accum_out=s[:, 0:1]),
             reads=xr, writes=[junk, s])
        P.op("act", lambda e, s=s: e.activation(out=s[:, 1:2], in_=s[:, 0:1], func=AF.Sqrt, scale=1.0 / D, bias=EPSB[0][:, 0:1]),
             reads=[s, EPSB[0]], writes=[s])
        P.op("dve", lambda e, s=s: e.reciprocal(out=s[:, 2:3], in_=s[:, 1:2]), reads=[s], writes=[s])
        P.op("dve", lambda e, xa=xa, s=s, t=t: e.scalar_tensor_tensor(out=t[:, :], in0=xa, scalar=s[:, 2:3], in1=A_bc[:, :],
                                                                   op0=ALU.mult, op1=ALU.mult),
             reads=xr + [s, A_bc], writes=[t])
        P.op("pool", lambda e, t=t, h=h: e.tensor_tensor(out=h[:, :], in0=t[:, :], in1=B_bc[:, :], op=ALU.add),
             reads=[t, B_bc], writes=[h])
        for half in range(2):
            pt = pT[half]
            for kk in range(8):
                k = half * 8 + kk
                P.op("pe", lambda e, pt=pt, kk=kk, k=k, h=h: e.transpose(out=pt[:, kk, :], in_=h[:, k * 128:(k + 1) * 128],
                                                                       identity=ident[:, :]),
                     reads=[h, ident], writes=[pt])
            P.op("act", lambda e, pt=pt, half=half, b=b: e.activation(
                out=hT[:, half * 8:(half + 1) * 8, b * 128:(b + 1) * 128], in_=pt[:, :, :], func=AF.Copy),
                 reads=[pt], writes=[hT])
    P.end_phase()


EPSB = [None]


def emit_consts(P, ident_d):
    ident = P.sbp([128, 128], BF16, "sbp_ident")
    epsb = P.sbp([128, 1], F32, "sbp_eps")
    EPSB[0] = epsb
    P.begin_phase()
    P.load("sp", ident, ident[:, :], ident_d[:, :])
    P.op("dve", lambda e: e.memset(epsb[:, :], EPS), writes=[epsb])
    P.end_phase()
    return ident


def emit_proj(P, hT, specs):
    P.begin_phase()
    wf = [P.sb([128, 16, 512], F32, "sb_wf") for _ in range(2)]
    wb = [P.sb([128, 16, 512], BF16, "sb_wb") for _ in range(2)]
    pp = [P.ps([128, 512], F32, "ps_pp") for _ in range(4)]
    ofm = [P.sb([128, 1024], BF16, "sb_ofm") for _ in range(2)]
    otb = [P.sb([128, 512], BF16, "sb_otb") for _ in range(2)]
    otf = [P.sb([128, 512], F32, "sb_otf") for _ in range(2)]
    ci_g = 0
    pi = 0
    oi = 0
    for sp in specs:
        N = sp["w"].t.shape[1]
        wv = sp["w"].t.rearrange("(k p) n -> p k n", p=128)
        for c0 in range(0, N, 512):
            cw = min(512, N - c0)
            w32 = wf[ci_g % 2]
            w16 = wb[ci_g % 2]
            for kh in range(2):
                P.load("sp" if kh == 0 else "act", w32, w32[:, kh * 8:(kh + 1) * 8, 0:cw], wv[:, kh * 8:(kh + 1) * 8, c0:c0 + cw])
            P.op("dve", lambda e, w32=w32, w16=w16, cw=cw: e.tensor_copy(out=w16[:, 0:8, 0:cw], in_=w32[:, 0:8, 0:cw]),
                 reads=[w32], writes=[w16])
            P.op("pool", lambda e, w32=w32, w16=w16, cw=cw: e.tensor_copy(out=w16[:, 8:16, 0:cw], in_=w32[:, 8:16, 0:cw]),
                 reads=[w32], writes=[w16])
            ci_g += 1
            if sp["kind"] == "fm":
                od = sp["outs"][0][0]
                for g0 in range(0, cw, 128):
                    o = ofm[oi % 2]
                    oi += 1
                    for th in range(2):
                        ps = pp[pi % 4]
                        pi += 1
                        for k in range(16):
                            P.mm(ps[:, :], w16[:, k, g0:g0 + 128], hT[:, k, th * 512:(th + 1) * 512], k == 0, k == 15,
                                 reads=[w16, hT], writes=[ps])
                        eng = "act" if th == 0 else "dve"
                        if eng == "act":
                            P.op("act", lambda e, o=o, ps=ps, th=th: e.activation(out=o[:, th * 512:(th + 1) * 512], in_=ps[:, :], func=AF.Copy),
                                 reads=[ps], writes=[o])
                        else:
                            P.op("dve", lambda e, o=o, ps=ps, th=th: e.tensor_copy(out=o[:, th * 512:(th + 1) * 512], in_=ps[:, :]),
                                 reads=[ps], writes=[o])
                    r0 = c0 + g0
                    P.store("pool", o, od.t[r0:r0 + 128, :], o[:, :])
            else:
                tgt = None
                for (od, a, b_, dt) in sp["outs"]:
                    if a <= c0 and c0 + cw <= b_:
                        tgt = (od, a, dt)
                od, a, dt = tgt
                for b in range(NB):
                    ps = pp[pi % 4]
                    pi += 1
                    for k in range(16):
                        P.mm(ps[:, 0:cw], hT[:, k, b * 128:(b + 1) * 128], w16[:, k, 0:cw], k == 0, k == 15,
                             reads=[w16, hT], writes=[ps])
                    o = (otb if dt == BF16 else otf)[oi % 2]
                    oi += 1
                    if b % 2 == 0:
                        P.op("act", lambda e, o=o, ps=ps, cw=cw: e.activation(out=o[:, 0:cw], in_=ps[:, 0:cw], func=AF.Copy),
                             reads=[ps], writes=[o])
                    else:
                        P.op("dve", lambda e, o=o, ps=ps, cw=cw: e.tensor_copy(out=o[:, 0:cw], in_=ps[:, 0:cw]),
                             reads=[ps], writes=[o])
                    P.store("pool", o, od.t[b * 128:(b + 1) * 128, c0 - a:c0 - a + cw], o[:, 0:cw])
    P.end_phase()


def build_A():
    nc = bass.Bass("TRN2", target_bir_lowering=False)
    P = Prog(nc)
    xs = P.dram("xs", [NB, 128, D], F32, "ExternalInput")
    c_d = P.dram("c_pk", [128, 16], F32, "ExternalInput")
    adaw = P.dram("adaw", [D, 4096], F32, "ExternalInput")
    adab = P.dram("adab", [128, 4096], F32, "ExternalInput")
    g_d = P.dram("g_bc", [128, D], F32, "ExternalInput")
    ident_d = P.dram("ident", [128, 128], BF16, "ExternalInput")
    wfm = P.dram("wfm", [D, NFM], F32, "ExternalInput")
    wtm = P.dram("wtm", [D, NTM], F32, "ExternalInput")
    zfm = P.dram("zfm", [NFM, NTOK], BF16, "ExternalOutput")
    ztb = P.dram("ztb", [NTOK, 2560], BF16, "ExternalOutput")
    ztf = P.dram("ztf", [NTOK, 640], F32, "ExternalOutput")
    ident = emit_consts(P, ident_d)
    modbc = P.sbp([128, 4096], F32, "sbp_mod")
    A1 = P.sbp([128, D], F32, "sbp_A1")
    hT = P.sbp([128, 16, NTOK], BF16, "sbp_hT")
    emit_mod(P, c_d, adaw, adab, 4096, modbc)
    P.begin_phase()
    gb = P.sb([128, D], F32, "sb_g")
    P.load("sp", gb, gb[:, :], g_d[:, :])
    P.op("dve", lambda e: e.scalar_tensor_tensor(out=A1[:, :], in0=modbc[:, D:2 * D], scalar=1.0, in1=gb[:, :],
                                                 op0=ALU.add, op1=ALU.mult), reads=[modbc, gb], writes=[A1])
    P.end_phase()
    emit_norm_T(P, xs, A1, modbc_view(modbc, 0, D), ident, hT)
    emit_proj(P, hT, [dict(w=wfm, kind="fm", outs=[(zfm, 0, NFM, BF16)]),
                      dict(w=wtm, kind="tm", outs=[(ztb, 0, 2560, BF16), (ztf, 2560, 3200, F32)])])
    P.begin_phase()
    P.end_phase(final=True)
    P.finish()
    return nc


class View:
    def __init__(self, buf, a, b):
        self.buf = buf
        self.a = a
        self.b = b


def modbc_view(buf, a, b):
    v = Buf(buf.t[:, a:b], buf.name + "_v")
    v.w = buf.w
    v.r = buf.r
    v.pr = buf.pr
    return v


def own_blocks(arr, c):
    a = arr.reshape((64, 128) + arr.shape[1:])
    return np.ascontiguousarray(a[c::8])


def bc128(v):
    return np.ascontiguousarray(np.broadcast_to(v[None, :], (128, v.shape[0]))).astype(np.float32)


def host_A_inputs(x, c, ada_w_l, ada_b_l, norm_g_l, w_in_l):
    wfm = np.zeros((D, NFM), np.float32)
    wfm[:, :len(FM_COLS)] = w_in_l[:, FM_COLS]
    wtm = np.zeros((D, NTM), np.float32)
    wtm[:, :len(TM_COLS)] = w_in_l[:, TM_COLS]
    common = dict(c_pk=np.ascontiguousarray(c.reshape(16, 128).T), adaw=np.ascontiguousarray(ada_w_l[:, 0:4096]),
                  adab=bc128(ada_b_l[0:4096]), g_bc=bc128(norm_g_l), ident=np.eye(128, dtype=NPBF), wfm=wfm, wtm=wtm)
    x2 = x.reshape(8192, D)
    return [dict(common, xs=own_blocks(x2, cc)) for cc in range(8)]


S = 8192
NKB = 64
SCALE = 128 ** -0.5
NEG = -1.0e30
NBIS = 18
TOPK = 256


def emit_attn(P, KT_d, Vg_d, QT_d, nheads, out_stage, col0, bias=None, maskT=None, cm=None):
    P.begin_phase()
    KT = [P.sb([128, S], BF16, "sb_KT") for _ in range(2)]
    Vg = [P.sb([128, NKB, 129], BF16, "sb_Vg") for _ in range(2)]
    QT = [P.sb([128, 1024], BF16, "sb_QT") for _ in range(2)]
    pO = [P.ps([128, 512], F32, "ps_O") for _ in range(2)]
    pS = []
    for _ in range(2):
        bank = P.ps([128, 4, 128], F32, "ps_S")
        for q in range(4):
            pS.append(Buf(bank.t[:, q, :], bank.name + "_q%d" % q))
    Pt = [P.sb([128, 128], BF16, "sb_Pt") for _ in range(6)]
    rc = [P.sb([128, 1], F32, "sb_rc") for _ in range(2)]
    zero_b = P.sb([128, 1], F32, "sb_zb")
    P.op("dve", lambda e: e.memset(zero_b[:, :], 0.0), writes=[zero_b])
    si = 0
    pi = 0
    oi = 0
    for h in range(nheads):
        kt, vg, qt = KT[h % 2], Vg[h % 2], QT[h % 2]
        for q4 in range(4):
            P.load("sp" if q4 % 2 == 0 else "act", kt, kt[:, q4 * 2048:(q4 + 1) * 2048],
                   KT_d.t[h * 128:(h + 1) * 128, q4 * 2048:(q4 + 1) * 2048])
        for q2 in range(2):
            P.load("sp" if q2 == 0 else "act", vg, vg[:, q2 * 32:(q2 + 1) * 32, :], Vg_d.t[h, :, q2 * 32:(q2 + 1) * 32, :])
        P.load("pool", qt, qt[:, :], QT_d.t[h * 128:(h + 1) * 128, :])
        for j in range(8):
            nkb = 8 * j + 8
            po = pO[oi % 2]
            oi += 1
            for kb in range(nkb):
                ps = pS[si % 8]
                si += 1
                pt = Pt[pi % 6]
                pi += 1
                P.mm(ps[:, :], kt[:, kb * 128:(kb + 1) * 128], qt[:, j * 128:(j + 1) * 128], True, True,
                     reads=[kt, qt], writes=[ps])
                if bias is not None:
                    P.op("act", lambda e, pt=pt, ps=ps, h=h, j=j, kb=kb: e.activation(
                        out=pt[:, :], in_=ps[:, :], func=AF.Exp, scale=SCALE, bias=bias[:, h, j, kb:kb + 1]),
                        reads=[ps, bias], writes=[pt])
                else:
                    P.op("act", lambda e, pt=pt, ps=ps: e.activation(
                        out=pt[:, :], in_=ps[:, :], func=AF.Exp, scale=SCALE, bias=zero_b[:, 0:1]),
                        reads=[ps, zero_b], writes=[pt])
                if maskT is not None:
                    mt = maskT[j]
                    P.op("dve", lambda e, pt=pt, mt=mt, kb=kb: e.tensor_tensor(out=pt[:, :], in0=pt[:, :], in1=mt[:, kb, :], op=ALU.mult),
                         reads=[pt, mt], writes=[pt])
                elif kb >= 8 * j:
                    r = kb - 8 * j
                    P.op("dve", lambda e, pt=pt, r=r: e.tensor_tensor(out=pt[:, :], in0=pt[:, :], in1=cm[:, r, :], op=ALU.mult),
                         reads=[pt, cm], writes=[pt])
                P.mm(po[:, 0:129], pt[:, :], vg[:, kb, :], kb == 0, kb == nkb - 1, reads=[pt, vg], writes=[po])
            r_ = rc[oi % 2]
            P.op("dve", lambda e, r_=r_, po=po: e.reciprocal(out=r_[:, 0:1], in_=po[:, 128:129]), reads=[po], writes=[r_])
            P.op("dve", lambda e, r_=r_, po=po, j=j, h=h: e.tensor_scalar(
                out=out_stage[:, j, col0 + h * 128:col0 + (h + 1) * 128], in0=po[:, 0:128], scalar1=r_[:, 0:1], scalar2=None,
                op0=ALU.mult), reads=[po, r_], writes=[out_stage])
    P.end_phase()


def emit_fox_bias(P, ff_d, fb_d, tri_d, sel_d, bias):
    P.begin_phase()
    ff = P.sb([128, 6, 64], F32, "sb_ff")
    fb = P.sb([128, 6], F32, "sb_fb")
    tri = P.sb([128, 128], F32, "sb_tri")
    ones = P.sb([128, 128], F32, "sb_ones")
    onec = P.sb([128, 1], F32, "sb_onec")
    sel = P.sb([128, 8], F32, "sb_sel")
    nlf = P.sb([128, 6, 64], F32, "sb_nlf")
    tot = P.sb([128, 6, 64], F32, "sb_tot")
    incl = P.sb([128, 6, 64], F32, "sb_incl")
    NF = P.sb([128, 6, 64], F32, "sb_NF")
    tmp = P.sb([128, 6, 8, 8], F32, "sb_tmp")
    nfe = P.sb([128, 6, 8], F32, "sb_nfe")
    pw = P.ps([128, 384], F32, "ps_w")
    pt_ = P.ps([128, 384], F32, "ps_t")
    P.load("sp", ff, ff[:, :, :], ff_d[:, :, :])
    P.load("act", fb, fb[:, :], fb_d[:, :])
    P.load("sp", tri, tri[:, :], tri_d[:, :])
    P.load("act", sel, sel[:, :], sel_d[:, :])
    P.op("dve", lambda e: e.memset(ones[:, :], 1.0), writes=[ones])
    P.op("dve", lambda e: e.memset(onec[:, :], 1.0), writes=[onec])
    P.op("dve", lambda e: e.tensor_tensor(out=nlf[:, :, :], in0=ff[:, :, :], in1=fb[:, :].unsqueeze(2).to_broadcast([128, 6, 64]),
                                          op=ALU.add), reads=[ff, fb], writes=[nlf])
    nlf2 = nlf.t.rearrange("p h k -> p (h k)")
    P.op("act", lambda e: e.activation(out=nlf2, in_=nlf2, func=AF.Exp, scale=-1.0), reads=[nlf], writes=[nlf])
    P.op("act", lambda e: e.activation(out=nlf2, in_=nlf2, func=AF.Ln, bias=onec[:, 0:1]), reads=[nlf, onec], writes=[nlf])
    P.mm(pw[:, :], tri[:, :], nlf2, True, True, reads=[tri, nlf], writes=[pw])
    P.mm(pt_[:, :], ones[:, :], nlf2, True, True, reads=[ones, nlf], writes=[pt_])
    tot2 = tot.t.rearrange("p h k -> p (h k)")
    P.op("dve", lambda e: e.tensor_copy(out=tot2, in_=pt_[:, :]), reads=[pt_], writes=[tot])
    for h in range(6):
        P.op("dve", lambda e, h=h: e.tensor_tensor_scan(out=incl[:, h, :], data0=ones[:, 0:64], data1=tot[:, h, :], initial=0.0,
                                                        op0=ALU.mult, op1=ALU.add), reads=[ones, tot], writes=[incl])
    NF2 = NF.t.rearrange("p h k -> p (h k)")
    incl2 = incl.t.rearrange("p h k -> p (h k)")
    P.op("dve", lambda e: e.tensor_tensor(out=NF2, in0=pw[:, :], in1=incl2, op=ALU.add), reads=[pw, incl], writes=[NF])
    P.op("dve", lambda e: e.tensor_tensor(out=NF2, in0=NF2, in1=tot2, op=ALU.subtract), reads=[NF, tot], writes=[NF])
    P.op("dve", lambda e: e.tensor_tensor(out=tmp[:, :, :, :], in0=incl.t.rearrange("p h (j r) -> p h j r", r=8),
                                          in1=sel[:, :].unsqueeze(1).unsqueeze(1).to_broadcast([128, 6, 8, 8]), op=ALU.mult),
         reads=[incl, sel], writes=[tmp])
    P.op("dve", lambda e: e.tensor_reduce(out=nfe[:, :, :], in_=tmp[:, :, :, :], axis=AX.X, op=ALU.add), reads=[tmp], writes=[nfe])
    for h in range(6):
        for j in range(8):
            P.op("dve", lambda e, h=h, j=j: e.tensor_scalar(out=bias[:, h, j, :], in0=NF[:, h, :], scalar1=nfe[:, h, j:j + 1],
                                                            scalar2=0.0, op0=ALU.subtract, op1=ALU.min),
                 reads=[NF, nfe], writes=[bias])
    P.end_phase()


def vg_layout(v_all, nheads):
    v = v_all.reshape(64, 128, nheads, 128).transpose(2, 1, 0, 3)
    o = np.ones((nheads, 128, 64, 129), NPBF)
    o[:, :, :, :128] = v
    return o


def band_masks(c):
    sp = np.arange(128)[:, None]
    t = np.arange(128)[None, :]
    cm = np.zeros((128, 8, 128), np.float32)
    am = np.full((128, 8, 128), NEG, np.float32)
    for r in range(8):
        if r < c:
            cm[:, r, :] = 1.0
            am[:, r, :] = 0.0
        elif r == c:
            cm[:, r, :] = (sp <= t)
            am[:, r, :] = np.where(sp.T <= t.T, 0.0, NEG)
    sel = np.zeros((128, 8), np.float32)
    sel[:, c] = 1.0
    return cm.astype(NPBF), am, sel


TRI = np.triu(np.ones((128, 128), np.float32))


def assemble_tokens(parts, axis):
    shp = list(parts[0].shape)
    n = shp[axis]
    assert n == 1024
    st = np.stack([np.moveaxis(p, axis, 0).reshape((8, 128) + tuple(np.moveaxis(p, axis, 0).shape[1:])) for p in parts], axis=1)
    g = st.reshape((8192,) + st.shape[3:])
    return np.moveaxis(g, 0, axis)


def emit_dsa_select(P, iqT_d, ikT_d, iw_d, am_d, ident, maskT):
    P.begin_phase()
    score = P.sb([128, S], F32, "sb_score")
    junk = P.sb([128, S], BF16, "sb_junk")
    ikT = P.sb([64, S], BF16, "sb_ikT")
    iq = [P.sb([64, 16, 128], BF16, "sb_iq") for _ in range(2)]
    rr = [P.sb([128, 512], F32, "sb_rr") for _ in range(3)]
    am = P.sb([128, 8, 128], F32, "sb_am")
    iw = P.sb([128, 8, 16], F32, "sb_iw")
    wsc = P.sb([128, 8, 16], F32, "sb_wsc")
    pI = [P.ps([128, 512], F32, "ps_I") for _ in range(4)]
    pT = [P.ps([128, 8, 128], BF16, "ps_mT") for _ in range(2)]
    mch = [P.sb([128, 1024], BF16, "sb_mch") for _ in range(2)]
    sms = [P.sb([128, 8], F32, "sb_sm") for _ in range(2)]
    for q4 in range(4):
        P.load("sp" if q4 % 2 == 0 else "act", ikT, ikT[:, q4 * 2048:(q4 + 1) * 2048], ikT_d.t[:, q4 * 2048:(q4 + 1) * 2048])
    P.load("sp", am, am[:, :, :], am_d[:, :, :])
    P.load("act", iw, iw[:, :, :], iw_d[:, :, :])
    P.op("dve", lambda e: e.tensor_scalar(out=wsc[:, :, :], in0=iw[:, :, :], scalar1=(64 ** -0.5) * (16 ** -0.5), scalar2=None,
                                          op0=ALU.mult), reads=[iw], writes=[wsc])
    ii = 0
    ti = 0
    for j in range(8):
        L = (8 * j + 8) * 128
        iqj = iq[j % 2]
        P.load("pool", iqj, iqj[:, :, :], iqT_d.t[:, :, j * 128:(j + 1) * 128])
        sm = sms[j % 2]
        for ck in range(L // 512):
            sc = score.t[:, ck * 512:(ck + 1) * 512]
            for h in range(16):
                ps = pI[ii % 4]
                r = rr[ii % 3]
                ii += 1
                P.mm(ps[:, :], iqj[:, h, :], ikT[:, ck * 512:(ck + 1) * 512], True, True, reads=[iqj, ikT], writes=[ps])
                P.op("act", lambda e, r=r, ps=ps: e.activation(out=r[:, :], in_=ps[:, :], func=AF.Relu), reads=[ps], writes=[r])
                if h == 0:
                    P.op("dve", lambda e, sc=sc, r=r, j=j: e.tensor_scalar(out=sc, in0=r[:, :], scalar1=wsc[:, j, 0:1], scalar2=None,
                                                                         op0=ALU.mult), reads=[r, wsc], writes=[score])
                else:
                    P.op("dve", lambda e, sc=sc, r=r, j=j, h=h: e.scalar_tensor_tensor(
                        out=sc, in0=r[:, :], scalar=wsc[:, j, h:h + 1], in1=sc, op0=ALU.mult, op1=ALU.add),
                        reads=[r, wsc, score], writes=[score])
        sL = score.t[:, 0:L]
        P.op("dve", lambda e, sm=sm, sL=sL: e.tensor_reduce(out=sm[:, 0:1], in_=sL, axis=AX.X, op=ALU.max, apply_absolute_value=True),
             reads=[score], writes=[sm])
        P.op("dve", lambda e, sm=sm: e.tensor_scalar(out=sm[:, 0:1], in0=sm[:, 0:1], scalar1=1.001, scalar2=1e-3, op0=ALU.mult, op1=ALU.add),
             reads=[sm], writes=[sm])
        P.op("dve", lambda e, sm=sm: e.tensor_scalar(out=sm[:, 1:2], in0=sm[:, 0:1], scalar1=-1.0, scalar2=None, op0=ALU.mult),
             reads=[sm], writes=[sm])
        sB = score.t[:, L - 1024:L]
        P.op("dve", lambda e, sB=sB: e.tensor_tensor(out=sB, in0=sB, in1=am.t.rearrange("p r s -> p (r s)"), op=ALU.add),
             reads=[score, am], writes=[score])
        for k in range(1, NBIS + 1):
            f = 2.0 ** (1 - k)
            P.op("dve", lambda e, sm=sm, f=f: e.tensor_scalar(out=sm[:, 2:3], in0=sm[:, 0:1], scalar1=f, scalar2=sm[:, 1:2],
                                                            op0=ALU.mult, op1=ALU.add), reads=[sm], writes=[sm])
            P.op("dve", lambda e, sm=sm, sL=sL, L=L: e.tensor_scalar(out=junk[:, 0:L], in0=sL, scalar1=sm[:, 2:3], scalar2=0.0,
                                                                   op0=ALU.is_ge, op1=ALU.add, accum_out=sm[:, 3:4]),
                 reads=[score, sm], writes=[junk, sm])
            P.op("dve", lambda e, sm=sm, f=f: e.tensor_scalar(out=sm[:, 4:5], in0=sm[:, 3:4], scalar1=TOPK - 0.5, scalar2=f,
                                                            op0=ALU.is_ge, op1=ALU.mult), reads=[sm], writes=[sm])
            P.op("dve", lambda e, sm=sm: e.scalar_tensor_tensor(out=sm[:, 1:2], in0=sm[:, 4:5], scalar=sm[:, 0:1], in1=sm[:, 1:2],
                                                              op0=ALU.mult, op1=ALU.add), reads=[sm], writes=[sm])
        for g in range(L // 1024):
            mc = mch[ti % 2]
            pt = pT[ti % 2]
            ti += 1
            P.op("dve", lambda e, mc=mc, g=g, sm=sm: e.tensor_scalar(out=mc[:, :], in0=score[:, g * 1024:(g + 1) * 1024],
                                                                   scalar1=sm[:, 1:2], scalar2=None, op0=ALU.is_ge),
                 reads=[score, sm], writes=[mc])
            for q in range(8):
                P.op("pe", lambda e, pt=pt, mc=mc, q=q: e.transpose(out=pt[:, q, :], in_=mc[:, q * 128:(q + 1) * 128], identity=ident[:, :]),
                     reads=[mc, ident], writes=[pt])
            mt = maskT[j]
            P.op("act", lambda e, mt=mt, pt=pt, g=g: e.activation(out=mt[:, g * 8:(g + 1) * 8, :], in_=pt[:, :, :], func=AF.Copy),
                 reads=[pt], writes=[mt])
    P.end_phase()


def emit_gla(P, gqT_d, gkT_d, gktm_d, gvtm_d, grtm_d, gaT_d, wa2_d, nbacol_d, barow_d, gn_d, tri2_d, suf2_d, rmask_d, out_d):
    P.scope_begin()

    def keep(shape, dt, name):
        return P.sbs(shape, dt, name)
    qtT = keep([128, S], BF16, "sbk_qtT")
    ktT = keep([128, S], BF16, "sbk_ktT")
    dn = keep([128, 128], F32, "sbk_dn")
    Sb = keep([128, 128, 128], BF16, "sbk_Sb")
    vtm = keep([128, 64, 128], BF16, "sbk_v")
    gs = keep([128, 64, 128], F32, "sbk_gs")
    wa2b = keep([16, 128], BF16, "sbk_wa2b")
    gaT = keep([16, S], BF16, "sbk_gaT")
    tri2 = keep([128, 128], F32, "sbk_tri2")
    onec = keep([128, 1], F32, "sbk_onec")
    epsc = keep([128, 1], F32, "sbk_epsc")

    P.begin_phase()
    wa2f = P.sb([16, 128], F32, "sb_wa2f")
    nbac = P.sb([128, 1], F32, "sb_nbac")
    rmask = P.sb([128, 512], F32, "sb_rmask")
    gq = [P.sb([128, 512], BF16, "sb_gq") for _ in range(2)]
    gk = [P.sb([128, 512], BF16, "sb_gk") for _ in range(2)]
    e1 = [P.sb([128, 512], F32, "sb_e1") for _ in range(2)]
    cs = [P.sb([128, 512], F32, "sb_cs") for _ in range(2)]
    eg = [P.sb([128, 512], F32, "sb_eg") for _ in range(2)]
    en = [P.sb([128, 512], F32, "sb_en") for _ in range(2)]
    pg = [P.ps([128, 512], F32, "ps_g") for _ in range(2)]
    P.load("sp", wa2f, wa2f[:, :], wa2_d[:, :])
    P.load("act", nbac, nbac[:, :], nbacol_d[:, :])
    P.load("sp", rmask, rmask[:, :], rmask_d[:, :])
    P.load("act", tri2, tri2[:, :], tri2_d[:, :])
    for q4 in range(4):
        P.load("sp" if q4 % 2 == 0 else "act", gaT, gaT[:, q4 * 2048:(q4 + 1) * 2048], gaT_d.t[:, q4 * 2048:(q4 + 1) * 2048])
    for q2 in range(2):
        P.load("pool", vtm, vtm[:, q2 * 32:(q2 + 1) * 32, :], gvtm_d.t[:, q2 * 32:(q2 + 1) * 32, :])
    P.op("dve", lambda e: e.tensor_copy(out=wa2b[:, :], in_=wa2f[:, :]), reads=[wa2f], writes=[wa2b])
    P.op("dve", lambda e: e.memset(onec[:, :], 1.0), writes=[onec])
    P.op("dve", lambda e: e.memset(epsc[:, :], 1e-6), writes=[epsc])
    for tc in range(16):
        sl = slice(tc * 512, (tc + 1) * 512)
        a, b_ = gq[tc % 2], gk[tc % 2]
        P.load("sp", a, a[:, :], gqT_d.t[:, sl])
        P.load("act", b_, b_[:, :], gkT_d.t[:, sl])
        ps = pg[tc % 2]
        x1, c1, g1, n1 = e1[tc % 2], cs[tc % 2], eg[tc % 2], en[tc % 2]
        P.mm(ps[:, :], wa2b[:, :], gaT[:, sl], True, True, reads=[wa2b, gaT], writes=[ps])
        P.op("act", lambda e, x1=x1, ps=ps: e.activation(out=x1[:, :], in_=ps[:, :], func=AF.Exp, scale=-1.0, bias=nbac[:, 0:1]),
             reads=[ps, nbac], writes=[x1])
        P.op("act", lambda e, x1=x1: e.activation(out=x1[:, :], in_=x1[:, :], func=AF.Ln, bias=onec[:, 0:1]), reads=[x1, onec], writes=[x1])
        P.op("dve", lambda e, x1=x1, c1=c1: e.tensor_tensor_scan(out=c1[:, :], data0=rmask[:, :], data1=x1[:, :], initial=0.0,
                                                               op0=ALU.mult, op1=ALU.add), reads=[rmask, x1], writes=[c1])
        P.op("act", lambda e, c1=c1, g1=g1: e.activation(out=g1[:, :], in_=c1[:, :], func=AF.Exp, scale=-1.0 / 16, bias=ZB[0][:, 0:1]),
             reads=[c1, ZB[0]], writes=[g1])
        P.op("act", lambda e, c1=c1, n1=n1: e.activation(out=n1[:, :], in_=c1[:, :], func=AF.Exp, scale=1.0 / 16, bias=ZB[0][:, 0:1]),
             reads=[c1, ZB[0]], writes=[n1])
        P.op("dve", lambda e, a=a, g1=g1, sl=sl: e.scalar_tensor_tensor(out=qtT[:, sl], in0=a[:, :], scalar=SCALE, in1=g1[:, :],
                                                                      op0=ALU.mult, op1=ALU.mult), reads=[a, g1], writes=[qtT])
        P.op("pool", lambda e, b_=b_, n1=n1, sl=sl: e.tensor_tensor(out=ktT[:, sl], in0=b_[:, :], in1=n1[:, :], op=ALU.mult),
             reads=[b_, n1], writes=[ktT])
        P.op("dve", lambda e, g1=g1, tc=tc: e.tensor_copy(out=dn[:, tc * 8:(tc + 1) * 8],
                                                        in_=g1.t.rearrange("p (n c) -> p n c", c=64)[:, :, 63]),
             reads=[g1], writes=[dn])
    P.end_phase()

    P.begin_phase()
    barow = P.sb([128, 128], F32, "sb_barow")
    gn = P.sb([128, 128], F32, "sb_gn")
    suf2 = P.sb([128, 128], F32, "sb_suf2")
    ktm = P.sb([128, 64, 128], BF16, "sb_ktm")
    Sst = P.sb([128, 128], F32, "sb_Sst")
    xg = [P.sb([128, 128], F32, "sb_xg") for _ in range(2)]
    fk = [P.sb([128, 128], F32, "sb_fk") for _ in range(2)]
    kh = [P.sb([128, 128], BF16, "sb_kh") for _ in range(2)]
    grt = [P.sb([128, 8, 128], F32, "sb_grt") for _ in range(2)]
    pl = [P.ps([128, 128], F32, "ps_l") for _ in range(2)]
    pf = [P.ps([128, 128], F32, "ps_f") for _ in range(2)]
    pU = [P.ps([128, 128], F32, "ps_U") for _ in range(4)]
    P.load("sp", barow, barow[:, :], barow_d[:, :])
    P.load("act", gn, gn[:, :], gn_d[:, :])
    P.load("sp", suf2, suf2[:, :], suf2_d[:, :])
    for q2 in range(2):
        P.load("pool", ktm, ktm[:, q2 * 32:(q2 + 1) * 32, :], gktm_d.t[:, q2 * 32:(q2 + 1) * 32, :])
    P.op("dve", lambda e: e.memset(Sst[:, :], 0.0), writes=[Sst])
    for g8 in range(8):
        gt = grt[g8 % 2]
        P.load("sp" if g8 % 2 == 0 else "act", gt, gt[:, :, :], grtm_d.t[:, g8 * 8:(g8 + 1) * 8, :])
        P.op("act", lambda e, gt=gt: e.activation(out=gt[:, :, :], in_=gt[:, :, :], func=AF.Silu), reads=[gt], writes=[gt])
        P.op("pool", lambda e, gt=gt, g8=g8: e.tensor_tensor(out=gs[:, g8 * 8:(g8 + 1) * 8, :], in0=gt[:, :, :],
                                                           in1=gn[:, :].unsqueeze(1).to_broadcast([128, 8, 128]), op=ALU.mult),
             reads=[gt, gn], writes=[gs])
    for blk in range(64):
        x, f, k2 = xg[blk % 2], fk[blk % 2], kh[blk % 2]
        p1, p2 = pl[blk % 2], pf[blk % 2]
        P.mm(p1[:, :], gaT[:, blk * 128:(blk + 1) * 128], wa2b[:, :], True, True, reads=[gaT, wa2b], writes=[p1])
        P.op("dve", lambda e, x=x, p1=p1: e.tensor_tensor(out=x[:, :], in0=p1[:, :], in1=barow[:, :], op=ALU.add),
             reads=[p1, barow], writes=[x])
        P.op("act", lambda e, x=x: e.activation(out=x[:, :], in_=x[:, :], func=AF.Exp, scale=-1.0, bias=ZB[0][:, 0:1]),
             reads=[x, ZB[0]], writes=[x])
        P.op("act", lambda e, x=x: e.activation(out=x[:, :], in_=x[:, :], func=AF.Ln, bias=onec[:, 0:1]), reads=[x, onec], writes=[x])
        P.mm(p2[:, :], suf2[:, :], x[:, :], True, True, reads=[suf2, x], writes=[p2])
        P.op("act", lambda e, f=f, p2=p2: e.activation(out=f[:, :], in_=p2[:, :], func=AF.Exp, scale=-1.0 / 16, bias=ZB[0][:, 0:1]),
             reads=[p2, ZB[0]], writes=[f])
        P.op("pool", lambda e, k2=k2, f=f, blk=blk: e.tensor_tensor(out=k2[:, :], in0=ktm[:, blk, :], in1=f[:, :], op=ALU.mult),
             reads=[ktm, f], writes=[k2])
        for hf in range(2):
            n = 2 * blk + hf
            pu = pU[n % 4]
            P.mm(pu[:, :], k2[hf * 64:(hf + 1) * 64, :], vtm[hf * 64:(hf + 1) * 64, blk, :], True, True, reads=[k2, vtm], writes=[pu])
            P.op("dve", lambda e, pu=pu, n=n: e.scalar_tensor_tensor(out=Sst[:, :], in0=Sst[:, :], scalar=dn[:, n:n + 1], in1=pu[:, :],
                                                                   op0=ALU.mult, op1=ALU.add), reads=[Sst, dn, pu], writes=[Sst])
            P.op("act", lambda e, n=n: e.activation(out=Sb[:, n, :], in_=Sst[:, :], func=AF.Copy), reads=[Sst], writes=[Sb])
    P.end_phase()

    P.begin_phase()
    ost = P.sb([128, 64, 128], BF16, "sb_gost")
    At = [P.sb([128, 128], BF16, "sb_At") for _ in range(2)]
    st = [P.sb([128, 4], F32, "sb_gst") for _ in range(2)]
    jk = P.sb([128, 128], F32, "sb_gjk")
    pA = [P.ps([128, 128], F32, "ps_A") for _ in range(2)]
    pO = [P.ps([128, 128], F32, "ps_GO") for _ in range(2)]
    for blk in range(64):
        sl = slice(blk * 128, (blk + 1) * 128)
        pa, po, at, s = pA[blk % 2], pO[blk % 2], At[blk % 2], st[blk % 2]
        P.mm(pa[:, :], ktT[:, sl], qtT[:, sl], True, True, reads=[ktT, qtT], writes=[pa])
        P.op("dve", lambda e, at=at, pa=pa: e.tensor_tensor(out=at[:, :], in0=pa[:, :], in1=tri2[:, :], op=ALU.mult),
             reads=[pa, tri2], writes=[at])
        P.mm(po[:, :], at[:, :], vtm[:, blk, :], True, False, reads=[at, vtm], writes=[po])
        if blk > 0:
            P.mm(po[0:64, :], qtT[:, blk * 128:blk * 128 + 64], Sb[:, 2 * blk - 1, :], False, False, reads=[qtT, Sb], writes=[po])
        P.mm(po[64:128, :], qtT[:, blk * 128 + 64:blk * 128 + 128], Sb[:, 2 * blk, :], False, True, reads=[qtT, Sb], writes=[po])
        P.op("act", lambda e, po=po, s=s: e.activation(out=jk[:, :], in_=po[:, :], func=AF.Square, accum_out=s[:, 0:1]),
             reads=[po], writes=[jk, s])
        P.op("act", lambda e, s=s: e.activation(out=s[:, 1:2], in_=s[:, 0:1], func=AF.Sqrt, scale=1.0 / 128, bias=epsc[:, 0:1]),
             reads=[s, epsc], writes=[s])
        P.op("dve", lambda e, s=s: e.reciprocal(out=s[:, 2:3], in_=s[:, 1:2]), reads=[s], writes=[s])
        P.op("dve", lambda e, po=po, s=s, blk=blk: e.scalar_tensor_tensor(out=ost[:, blk, :], in0=po[:, :], scalar=s[:, 2:3],
                                                                        in1=gs[:, blk, :], op0=ALU.mult, op1=ALU.mult),
             reads=[po, s, gs], writes=[ost])
    P.store("sp", ost, out_d.t.rearrange("b p e -> p b e"), ost[:, :, :])
    P.end_phase()
    P.scope_end()


ZB = [None]


def emit_zero(P):
    zb = P.sbp([128, 1], F32, "sbp_zero")
    ZB[0] = zb
    P.begin_phase()
    P.op("dve", lambda e: e.memset(zb[:, :], 0.0), writes=[zb])
    P.end_phase()


def gla_consts():
    s = np.arange(128)[:, None]
    t = np.arange(128)[None, :]
    same = (s // 64) == (t // 64)
    tri2 = (same & (s <= t)).astype(np.float32)
    suf2 = (same & (s > t)).astype(np.float32)
    rmask = np.ones((128, 512), np.float32)
    rmask[:, ::64] = 0.0
    return tri2, suf2, rmask


def tm_layout(a):
    return np.ascontiguousarray(a.reshape(64, 128, a.shape[1]).transpose(1, 0, 2))


def gla_inputs(hg, gq, gk, gv, gr, ga, wa2_l, ba_l, gng_l):
    sl = slice(hg * 128, (hg + 1) * 128)
    tri2, suf2, rmask = gla_consts()
    return dict(gqT=np.ascontiguousarray(gq[:, sl].T).astype(NPBF), gkT=np.ascontiguousarray(gk[:, sl].T).astype(NPBF),
                gktm=tm_layout(gk[:, sl]).astype(NPBF), gvtm=tm_layout(gv[:, sl]).astype(NPBF),
                grtm=tm_layout(gr[:, sl]).astype(np.float32), gaT=np.ascontiguousarray(ga.T).astype(NPBF),
                wa2=np.ascontiguousarray(wa2_l[:, sl]), nbacol=np.ascontiguousarray(-ba_l[sl][:, None]),
                barow=bc128(ba_l[sl]), gnb=bc128(gng_l), tri2=tri2, suf2=suf2, rmask=rmask)


def build_B():
    nc = bass.Bass("TRN2", target_bir_lowering=False)
    P = Prog(nc)
    Dm = {}
    for name, shp, dt in [("fkT", [768, S], BF16), ("fvg", [6, 128, NKB, 129], BF16), ("fqT", [768, 1024], BF16), ("ffp", [128, 6, 64], F32),
                          ("fbb", [128, 6], F32), ("tri", [128, 128], F32), ("sel", [128, 8], F32), ("cm", [128, 8, 128], BF16),
                          ("dkT", [768, S], BF16), ("dvg", [6, 128, NKB, 129], BF16), ("dqT", [768, 1024], BF16), ("iqT", [64, 16, 1024], BF16),
                          ("ikT", [64, S], BF16), ("iwp", [128, 8, 16], F32), ("am", [128, 8, 128], F32), ("ident", [128, 128], BF16),
                          ("gqT", [128, S], BF16), ("gkT", [128, S], BF16), ("gktm", [128, 64, 128], BF16), ("gvtm", [128, 64, 128], BF16),
                          ("grtm", [128, 64, 128], F32), ("gaT", [16, S], BF16), ("wa2", [16, 128], F32), ("nbacol", [128, 1], F32),
                          ("barow", [128, 128], F32), ("gnb", [128, 128], F32), ("tri2", [128, 128], F32), ("suf2", [128, 128], F32),
                          ("rmask", [128, 512], F32)]:
        Dm[name] = P.dram(name, shp, dt, "ExternalInput")
    foxo = P.dram("foxo", [8, 128, 768], BF16, "ExternalOutput")
    dsao = P.dram("dsao", [8, 128, 768], BF16, "ExternalOutput")
    glao = P.dram("glao", [64, 128, 128], BF16, "ExternalOutput")
    emit_zero(P)
    ident = P.sbp([128, 128], BF16, "sbp_ident")
    P.begin_phase()
    P.load("sp", ident, ident[:, :], Dm["ident"][:, :])
    P.end_phase()
    P.scope_begin()
    bias = P.sbs([128, 6, 8, 64], F32, "sbs_bias")
    cm = P.sbs([128, 8, 128], BF16, "sbs_cm")
    ostF = P.sbs([128, 8, 768], BF16, "sbs_ostF")
    P.begin_phase()
    P.load("sp", cm, cm[:, :, :], Dm["cm"][:, :, :])
    P.end_phase()
    emit_fox_bias(P, Dm["ffp"], Dm["fbb"], Dm["tri"], Dm["sel"], bias)
    emit_attn(P, Dm["fkT"], Dm["fvg"], Dm["fqT"], 6, ostF, 0, bias=bias, cm=cm)
    P.begin_phase()
    P.store("sp", ostF, foxo.t.rearrange("j p w -> p j w"), ostF[:, :, :])
    P.end_phase()
    P.scope_end()
    P.scope_begin()
    ostD = P.sbs([128, 8, 768], BF16, "sbs_ostD")
    maskT = [P.sbs([128, 8 * j + 8, 128], BF16, "sbs_mT") for j in range(8)]
    emit_dsa_select(P, Dm["iqT"], Dm["ikT"], Dm["iwp"], Dm["am"], ident, maskT)
    emit_attn(P, Dm["dkT"], Dm["dvg"], Dm["dqT"], 6, ostD, 0, maskT=maskT)
    P.begin_phase()
    P.store("sp", ostD, dsao.t.rearrange("j p w -> p j w"), ostD[:, :, :])
    P.end_phase()
    P.scope_end()
    emit_gla(P, Dm["gqT"], Dm["gkT"], Dm["gktm"], Dm["gvtm"], Dm["grtm"], Dm["gaT"], Dm["wa2"], Dm["nbacol"], Dm["barow"], Dm["gnb"],
             Dm["tri2"], Dm["suf2"], Dm["rmask"], glao)
    P.begin_phase()
    P.end_phase(final=True)
    P.finish()
    return nc


NE = 16384


def build_C1():
    nc = bass.Bass("TRN2", target_bir_lowering=False)
    P = Prog(nc)
    xs = P.dram("xs", [NB, 128, D], F32, "ExternalInput")
    mixT_d = P.dram("mixT", [D, NTOK], BF16, "ExternalInput")
    c_d = P.dram("c_pk", [128, 16], F32, "ExternalInput")
    adaw = P.dram("adaw", [D, 8192], F32, "ExternalInput")
    adab = P.dram("adab", [128, 8192], F32, "ExternalInput")
    g_d = P.dram("g_bc", [128, D], F32, "ExternalInput")
    ident_d = P.dram("ident", [128, 128], BF16, "ExternalInput")
    wo_d = P.dram("wo", [D, D], F32, "ExternalInput")
    wq_d = P.dram("wq", [D, D], F32, "ExternalInput")
    kT_d = P.dram("kT", [16, 128, 128], F32, "ExternalInput")
    xmid = P.dram("xmid", [NB, 128, D], F32, "ExternalOutput")
    h2T_d = P.dram("h2T", [16, 128, NTOK], BF16, "ExternalOutput")
    s12_d = P.dram("s12", [NB, 128, 16, 128], F32, "ExternalOutput")
    g2_d = P.dram("g2bc", [128, D], F32, "ExternalOutput")
    ident = emit_consts(P, ident_d)
    modbc = P.sbp([128, 8192], F32, "sbp_mod2")
    emit_mod(P, c_d, adaw, adab, 8192, modbc)

    P.begin_phase()
    mixT = P.sb([128, 16, NTOK], BF16, "sb_mixT")
    wf = P.sb([128, 16, 512], F32, "sb_wof")
    wb = [P.sb([128, 16, 512], BF16, "sb_wob") for _ in range(2)]
    xt = [P.sb([128, 512], F32, "sb_xt") for _ in range(3)]
    tm = [P.sb([128, 512], F32, "sb_tm") for _ in range(2)]
    pp = [P.ps([128, 512], F32, "ps_wo") for _ in range(3)]
    mv = mixT_d.t.rearrange("(k p) t -> p k t", p=128)
    for kh in range(2):
        P.load("sp" if kh == 0 else "act", mixT, mixT[:, kh * 8:(kh + 1) * 8, :], mv[:, kh * 8:(kh + 1) * 8, :])
    wv = wo_d.t.rearrange("(k p) n -> p k n", p=128)
    i = 0
    for dc in range(4):
        w16 = wb[dc % 2]
        for kh in range(2):
            P.load("sp" if kh == 0 else "act", wf, wf[:, kh * 8:(kh + 1) * 8, :], wv[:, kh * 8:(kh + 1) * 8, dc * 512:(dc + 1) * 512])
        P.op("dve", lambda e, w16=w16: e.tensor_copy(out=w16[:, 0:8, :], in_=wf[:, 0:8, :]), reads=[wf], writes=[w16])
        P.op("pool", lambda e, w16=w16: e.tensor_copy(out=w16[:, 8:16, :], in_=wf[:, 8:16, :]), reads=[wf], writes=[w16])
        for b in range(NB):
            ps = pp[i % 3]
            x = xt[i % 3]
            t = tm[i % 2]
            i += 1
            P.load("pool", x, x[:, :], xs[b, :, dc * 512:(dc + 1) * 512])
            for k in range(16):
                P.mm(ps[:, :], mixT[:, k, b * 128:(b + 1) * 128], w16[:, k, :], k == 0, k == 15, reads=[mixT, w16], writes=[ps])
            P.op("dve", lambda e, t=t, ps=ps, dc=dc: e.tensor_tensor(out=t[:, :], in0=ps[:, :], in1=modbc[:, dc * 512:(dc + 1) * 512], op=ALU.mult),
                 reads=[ps, modbc], writes=[t])
            P.op("dve", lambda e, t=t, x=x: e.tensor_tensor(out=x[:, :], in0=x[:, :], in1=t[:, :], op=ALU.add), reads=[x, t], writes=[x])
            P.store("sp", x, xmid[b, :, dc * 512:(dc + 1) * 512], x[:, :], dram=xmid)
    P.end_phase()

    A2 = P.sbp([128, D], F32, "sbp_A2")
    h2T = P.sbp([128, 16, NTOK], BF16, "sbp_h2T")
    P.begin_phase()
    gb = P.sb([128, D], F32, "sb_g")
    P.load("sp", gb, gb[:, :], g_d[:, :])
    P.op("dve", lambda e: e.scalar_tensor_tensor(out=A2[:, :], in0=modbc[:, 2 * D:3 * D], scalar=1.0, in1=gb[:, :],
                                                 op0=ALU.add, op1=ALU.mult), reads=[modbc, gb], writes=[A2])
    P.store("act", modbc, g2_d[:, :], modbc[:, 3 * D:4 * D])
    P.end_phase()
    emit_norm_T(P, xmid, A2, modbc_view(modbc, D, 2 * D), ident, h2T, xdram=xmid)

    P.begin_phase()
    P.store("sp", h2T, h2T_d.t.rearrange("k p t -> p k t"), h2T[:, :, :])
    qT = P.sb([128, 16, NTOK], BF16, "sb_qT")
    kf = P.sb([128, 16, 128], F32, "sb_kf")
    kb_ = P.sb([128, 16, 128], BF16, "sb_kb")
    wf = P.sb([128, 16, 512], F32, "sb_wqf")
    wb = [P.sb([128, 16, 512], BF16, "sb_wqb") for _ in range(2)]
    pp = [P.ps([128, 512], F32, "ps_q") for _ in range(3)]
    pq = [P.ps([128, 4, 128], F32, "ps_s") for _ in range(2)]
    sst = [P.sb([128, 16, 128], F32, "sb_sst") for _ in range(2)]
    P.load("pool", kf, kf[:, :, :], kT_d.t.rearrange("g d n -> d g n"))
    P.op("dve", lambda e: e.tensor_copy(out=kb_[:, :, :], in_=kf[:, :, :]), reads=[kf], writes=[kb_])
    wv = wq_d.t.rearrange("(k p) n -> p k n", p=128)
    i = 0
    for dc in range(4):
        w16 = wb[dc % 2]
        for kh in range(2):
            P.load("sp" if kh == 0 else "act", wf, wf[:, kh * 8:(kh + 1) * 8, :], wv[:, kh * 8:(kh + 1) * 8, dc * 512:(dc + 1) * 512])
        P.op("dve", lambda e, w16=w16: e.tensor_copy(out=w16[:, 0:8, :], in_=wf[:, 0:8, :]), reads=[wf], writes=[w16])
        P.op("pool", lambda e, w16=w16: e.tensor_copy(out=w16[:, 8:16, :], in_=wf[:, 8:16, :]), reads=[wf], writes=[w16])
        for g in range(4):
            gidx = dc * 4 + g
            for th in range(2):
                ps = pp[i % 3]
                i += 1
                for k in range(16):
                    P.mm(ps[:, :], w16[:, k, g * 128:(g + 1) * 128], h2T[:, k, th * 512:(th + 1) * 512], k == 0, k == 15,
                         reads=[w16, h2T], writes=[ps])
                if th == 0:
                    P.op("act", lambda e, ps=ps, gidx=gidx, th=th: e.activation(out=qT[:, gidx, th * 512:(th + 1) * 512], in_=ps[:, :], func=AF.Copy),
                         reads=[ps], writes=[qT])
                else:
                    P.op("dve", lambda e, ps=ps, gidx=gidx, th=th: e.tensor_copy(out=qT[:, gidx, th * 512:(th + 1) * 512], in_=ps[:, :]),
                         reads=[ps], writes=[qT])
    i = 0
    for b in range(NB):
        st = sst[b % 2]
        for g4 in range(4):
            ps = pq[i % 2]
            i += 1
            for q in range(4):
                gidx = g4 * 4 + q
                P.mm(ps[:, q, :], qT[:, gidx, b * 128:(b + 1) * 128], kb_[:, gidx, :], True, True, reads=[qT, kb_], writes=[ps])
            if g4 % 2 == 0:
                P.op("act", lambda e, ps=ps, st=st, g4=g4: e.activation(out=st[:, g4 * 4:(g4 + 1) * 4, :], in_=ps[:, :, :], func=AF.Copy),
                     reads=[ps], writes=[st])
            else:
                P.op("dve", lambda e, ps=ps, st=st, g4=g4: e.tensor_copy(out=st[:, g4 * 4:(g4 + 1) * 4, :], in_=ps[:, :, :]),
                     reads=[ps], writes=[st])
        P.store("pool", st, s12_d[b, :, :, :], st[:, :, :])
    P.end_phase()
    P.begin_phase()
    P.end_phase(final=True)
    P.finish()
    return nc


def host_C1_inputs(xs_list, mix_list, c, ada_w_l, ada_b_l, norm2_g_l, w_out_l, wq_l, k1_l, k2_l):
    kT = np.zeros((16, 128, 128), np.float32)
    for h in range(8):
        kT[2 * h] = k1_l[h].T
        kT[2 * h + 1] = k2_l[h].T
    common = dict(c_pk=np.ascontiguousarray(c.reshape(16, 128).T), adaw=np.ascontiguousarray(ada_w_l[:, 4096:12288]),
                  adab=bc128(ada_b_l[4096:12288]), g_bc=bc128(norm2_g_l), ident=np.eye(128, dtype=NPBF),
                  wo=np.ascontiguousarray(w_out_l), wq=np.ascontiguousarray(wq_l), kT=kT)
    return [dict(common, xs=xs_list[cc], mixT=np.ascontiguousarray(mix_list[cc].T)) for cc in range(8)]


def build_C2(final):
    nc = bass.Bass("TRN2", target_bir_lowering=False)
    P = Prog(nc)
    h2T_d = P.dram("h2T", [16, 128, NTOK], BF16, "ExternalInput")
    s12_d = P.dram("s12", [NB, 128, 16, 128], F32, "ExternalInput")
    xmid = P.dram("xmid", [NB, 128, D], F32, "ExternalInput")
    g2_d = P.dram("g2bc", [128, D], F32, "ExternalInput")
    uT_d = P.dram("uTt", [128, 128, 16, 128], F32, "ExternalInput")
    v_d = P.dram("v", [NE, D], F32, "ExternalInput")
    ident_d = P.dram("ident", [128, 128], BF16, "ExternalInput")
    fg_d = P.dram("fg_bc", [128, D], F32, "ExternalInput") if final else None
    xo = P.dram("xo", [NB, 128, D], F32, "ExternalOutput")
    ident = emit_consts(P, ident_d)
    kB_zero = P.sbp([128, 1], F32, "sbp_zero")
    g2 = P.sbp([128, D], F32, "sbp_g2")
    stats = P.sbp([128, 4, 8, 2], F32, "sbp_stats")
    P.begin_phase()
    P.load("sp", g2, g2[:, :], g2_d[:, :])
    P.op("dve", lambda e: e.memset(kB_zero[:, :], 0.0), writes=[kB_zero])
    P.end_phase()
    for ps_ in range(2):
        P.begin_phase()
        stt_ = [P.sb([128, 16, 128], F32, "sb_st") for _ in range(2)]
        v16 = [P.sb([128, 2, 16], F32, "sb_v16") for _ in range(2)]
        scr = [P.sb([128, 128], F32, "sb_scr") for _ in range(2)]
        cand = [P.sb([128, 16, 16], F32, "sb_cand") for _ in range(2)]
        scr2 = [P.sb([128, 256], F32, "sb_scr2") for _ in range(2)]
        ez = [P.sb([128, 256], F32, "sb_ez") for _ in range(2)]
        c8 = [P.sb([128, 16], F32, "sb_c8") for _ in range(2)]
        sm = [P.sb([128, 4], F32, "sb_sm") for _ in range(2)]
        i = 0
        for bl in range(4):
            b = ps_ * 4 + bl
            st = stt_[bl % 2]
            P.load("sp" if bl % 2 == 0 else "act", st, st[:, :, :], s12_d[b, :, :, :])
            for h in range(8):
                vv, sc, cd, s2_, ez_, c8_, sm_ = v16[i % 2], scr[i % 2], cand[i % 2], scr2[i % 2], ez[i % 2], c8[i % 2], sm[i % 2]
                i += 1
                for half in range(2):
                    src = st.t[:, 2 * h + half, :]
                    P.op("dve", lambda e, vv=vv, src=src, half=half: e.max(out=vv[:, half, 0:8], in_=src), reads=[st], writes=[vv])
                    P.op("dve", lambda e, vv=vv, src=src, half=half, sc=sc: e.match_replace(out=sc[:, :], in_to_replace=vv[:, half, 0:8],
                                                                                        in_values=src, imm_value=-1e30),
                         reads=[st, vv], writes=[sc])
                    P.op("dve", lambda e, vv=vv, half=half, sc=sc: e.max(out=vv[:, half, 8:16], in_=sc[:, :]), reads=[sc], writes=[vv])
                P.op("dve", lambda e, vv=vv, cd=cd: e.tensor_tensor(out=cd[:, :, :], in0=vv[:, 0, :].unsqueeze(2).to_broadcast([128, 16, 16]),
                                                                  in1=vv[:, 1, :].unsqueeze(1).to_broadcast([128, 16, 16]), op=ALU.add),
                     reads=[vv], writes=[cd])
                cf = cd.t.rearrange("p a b -> p (a b)")
                P.op("dve", lambda e, c8_=c8_, cf=cf: e.max(out=c8_[:, 0:8], in_=cf), reads=[cd], writes=[c8_])
                P.op("dve", lambda e, c8_=c8_, cf=cf, s2_=s2_: e.match_replace(out=s2_[:, :], in_to_replace=c8_[:, 0:8], in_values=cf, imm_value=-1e30),
                     reads=[cd, c8_], writes=[s2_])
                P.op("dve", lambda e, c8_=c8_, s2_=s2_: e.max(out=c8_[:, 8:16], in_=s2_[:, :]), reads=[s2_], writes=[c8_])
                P.op("dve", lambda e, c8_=c8_, sm_=sm_: e.tensor_scalar(out=sm_[:, 0:1], in0=c8_[:, 0:1], scalar1=-1.0, scalar2=None, op0=ALU.mult),
                     reads=[c8_], writes=[sm_])
                P.op("act", lambda e, ez_=ez_, cf=cf, sm_=sm_: e.activation(out=ez_[:, :], in_=cf, func=AF.Exp, bias=sm_[:, 0:1]),
                     reads=[cd, sm_], writes=[ez_])
                P.op("dve", lambda e, s2_=s2_, cf=cf, c8_=c8_, ez_=ez_, sm_=sm_: e.scalar_tensor_tensor(
                    out=s2_[:, :], in0=cf, scalar=c8_[:, 15:16], in1=ez_[:, :], op0=ALU.is_ge, op1=ALU.mult, accum_out=sm_[:, 1:2]),
                    reads=[cd, c8_, ez_], writes=[s2_, sm_])
                P.op("act", lambda e, sm_=sm_: e.activation(out=sm_[:, 2:3], in_=sm_[:, 1:2], func=AF.Ln, bias=kB_zero[:, 0:1]),
                     reads=[sm_, kB_zero], writes=[sm_])
                P.op("dve", lambda e, sm_=sm_, c8_=c8_, bl=bl, h=h: e.tensor_scalar(out=stats[:, bl, h, 1:2], in0=sm_[:, 2:3], scalar1=c8_[:, 0:1],
                                                                                  scalar2=-1.0, op0=ALU.add, op1=ALU.mult),
                     reads=[sm_, c8_], writes=[stats])
                P.op("dve", lambda e, c8_=c8_, bl=bl, h=h: e.tensor_copy(out=stats[:, bl, h, 0:1], in_=c8_[:, 15:16]), reads=[c8_], writes=[stats])
        P.end_phase()

        P.begin_phase()
        h2p = P.sb([128, 16, 512], BF16, "sb_h2p")
        oacc = P.sb([128, 4, D], F32, "sb_oacc")
        s12t = [P.sb([128, 16, 128], F32, "sb_s12t") for _ in range(2)]
        Xb = [P.sb([128, 4, 128], F32, "sb_X") for _ in range(2)]
        Eb = [P.sb([128, 4, 128], F32, "sb_E") for _ in range(2)]
        Tb = [P.sb([128, 8, 4, 128], BF16, "sb_T") for _ in range(2)]
        GT = [P.sb([128, 4, 512], BF16, "sb_GT") for _ in range(2)]
        GAT = [P.sb([128, 4, 512], BF16, "sb_GAT") for _ in range(2)]
        uf = [P.sb([128, 16, 128], F32, "sb_uf") for _ in range(2)]
        ub = [P.sb([128, 16, 128], BF16, "sb_ub") for _ in range(2)]
        vf = [P.sb([128, D], F32, "sb_vf") for _ in range(2)]
        vb = [P.sb([128, 4, D], BF16, "sb_vb") for _ in range(1)]
        ge = [P.sb([128, 512], F32, "sb_ge") for _ in range(2)]
        pG = [P.ps([128, 4, 128], F32, "ps_G") for _ in range(2)]
        pA = [P.ps([128, 512], F32, "ps_A") for _ in range(2)]
        pD = [P.ps([128, 512], F32, "ps_D") for _ in range(3)]
        hv = h2T_d.t.rearrange("k p t -> p k t")
        for kh in range(2):
            P.load("sp" if kh == 0 else "act", h2p, h2p[:, kh * 8:(kh + 1) * 8, :], hv[:, kh * 8:(kh + 1) * 8, ps_ * 512:(ps_ + 1) * 512])
        si = 0
        xi = 0
        di = 0
        for g in range(32):
            gt, gat, vb_ = GT[g % 2], GAT[g % 2], vb[0]
            for bl in range(4):
                b = ps_ * 4 + bl
                st = s12t[si % 2]
                T = Tb[si % 2]
                pg = pG[si % 2]
                si += 1
                P.load("sp" if si % 2 == 0 else "act", st, st[:, :, :], s12_d[b, :, :, :])
                for h in range(8):
                    X, E = Xb[xi % 2], Eb[xi % 2]
                    xi += 1
                    P.op("dve", lambda e, X=X, st=st, h=h, g=g: e.tensor_tensor(
                        out=X[:, :, :], in0=st[:, 2 * h + 1, :].unsqueeze(1).to_broadcast([128, 4, 128]),
                        in1=st[:, 2 * h, 4 * g:4 * g + 4].unsqueeze(2).to_broadcast([128, 4, 128]), op=ALU.add), reads=[st], writes=[X])
                    P.op("act", lambda e, X=X, E=E, bl=bl, h=h: e.activation(out=E[:, :, :], in_=X[:, :, :], func=AF.Exp, bias=stats[:, bl, h, 1:2]),
                         reads=[X, stats], writes=[E])
                    P.op("dve", lambda e, X=X, E=E, T=T, bl=bl, h=h: e.scalar_tensor_tensor(
                        out=T[:, h, :, :], in0=X[:, :, :], scalar=stats[:, bl, h, 0:1], in1=E[:, :, :], op0=ALU.is_ge, op1=ALU.mult),
                        reads=[X, E, stats], writes=[T])
                for a in range(4):
                    for h in range(8):
                        P.mm(pg[:, a, :], T[:, h, a, :], ident[:, :], h == 0, h == 7, reads=[T, ident], writes=[pg])
                P.op("act", lambda e, gt=gt, pg=pg, bl=bl: e.activation(out=gt[:, :, bl * 128:(bl + 1) * 128], in_=pg[:, :, :], func=AF.Copy),
                     reads=[pg], writes=[gt])
            for a in range(4):
                ec = 4 * g + a
                u32, u16, v32, pa, gel = uf[ec % 2], ub[ec % 2], vf[ec % 2], pA[ec % 2], ge[ec % 2]
                P.load("sp", u32, u32[:, :, :], uT_d[ec, :, :, :])
                P.load("act", v32, v32[:, :], v_d[ec * 128:(ec + 1) * 128, :])
                P.op("pool", lambda e, u32=u32, u16=u16: e.tensor_copy(out=u16[:, :, :], in_=u32[:, :, :]), reads=[u32], writes=[u16])
                for k in range(16):
                    P.mm(pa[:, :], u16[:, k, :], h2p[:, k, :], k == 0, k == 15, reads=[u16, h2p], writes=[pa])
                P.op("act", lambda e, gel=gel, pa=pa: e.activation(out=gel[:, :], in_=pa[:, :], func=AF.Gelu_apprx_tanh), reads=[pa], writes=[gel])
                P.op("dve", lambda e, gat=gat, gel=gel, gt=gt, a=a: e.tensor_tensor(out=gat[:, a, :], in0=gel[:, :], in1=gt[:, a, :], op=ALU.mult),
                     reads=[gel, gt], writes=[gat])
                P.op("pool", lambda e, v32=v32, vb_=vb_, a=a: e.tensor_copy(out=vb_[:, a, 0:1024], in_=v32[:, 0:1024]), reads=[v32], writes=[vb_])
                P.op("act", lambda e, v32=v32, vb_=vb_, a=a: e.activation(out=vb_[:, a, 1024:2048], in_=v32[:, 1024:2048], func=AF.Copy),
                     reads=[v32], writes=[vb_])
            for bl in range(4):
                for dc in range(4):
                    pd = pD[di % 3]
                    di += 1
                    for a in range(4):
                        P.mm(pd[:, :], gat[:, a, bl * 128:(bl + 1) * 128], vb_[:, a, dc * 512:(dc + 1) * 512], a == 0, a == 3,
                             reads=[gat, vb_], writes=[pd])
                    if g == 0:
                        P.op("act", lambda e, pd=pd, bl=bl, dc=dc: e.activation(out=oacc[:, bl, dc * 512:(dc + 1) * 512], in_=pd[:, :], func=AF.Copy),
                             reads=[pd], writes=[oacc])
                    else:
                        P.op("dve", lambda e, pd=pd, bl=bl, dc=dc: e.tensor_tensor(out=oacc[:, bl, dc * 512:(dc + 1) * 512],
                                                                                 in0=oacc[:, bl, dc * 512:(dc + 1) * 512], in1=pd[:, :], op=ALU.add),
                             reads=[pd, oacc], writes=[oacc])
        xt = [P.sb([128, D], F32, "sb_xf") for _ in range(2)]
        jk = P.sb([128, D], BF16, "sb_jk")
        s4 = [P.sb([128, 4], F32, "sb_s4") for _ in range(2)]
        if final:
            fg = P.sb([128, D], F32, "sb_fg")
            P.load("sp", fg, fg[:, :], fg_d[:, :])
        for bl in range(4):
            b = ps_ * 4 + bl
            x = xt[bl % 2]
            s = s4[bl % 2]
            P.load("sp" if bl % 2 == 0 else "act", x, x[:, :], xmid[b, :, :])
            P.op("dve", lambda e, bl=bl: e.tensor_tensor(out=oacc[:, bl, :], in0=oacc[:, bl, :], in1=g2[:, :], op=ALU.mult),
                 reads=[oacc, g2], writes=[oacc])
            P.op("pool", lambda e, x=x, bl=bl: e.tensor_tensor(out=x[:, :], in0=x[:, :], in1=oacc[:, bl, :], op=ALU.add),
                 reads=[x, oacc], writes=[x])
            if final:
                P.op("act", lambda e, x=x, s=s: e.activation(out=jk[:, :], in_=x[:, :], func=AF.Square, accum_out=s[:, 0:1]),
                     reads=[x], writes=[jk, s])
                P.op("act", lambda e, s=s: e.activation(out=s[:, 1:2], in_=s[:, 0:1], func=AF.Sqrt, scale=1.0 / D, bias=EPSB[0][:, 0:1]),
                     reads=[s, EPSB[0]], writes=[s])
                P.op("dve", lambda e, s=s: e.reciprocal(out=s[:, 2:3], in_=s[:, 1:2]), reads=[s], writes=[s])
                P.op("dve", lambda e, x=x, s=s: e.scalar_tensor_tensor(out=x[:, :], in0=x[:, :], scalar=s[:, 2:3], in1=fg[:, :],
                                                                     op0=ALU.mult, op1=ALU.mult), reads=[x, s, fg], writes=[x])
            P.store("pool", x, xo[b, :, :], x[:, :])
        P.end_phase(final=(ps_ == 1))
    P.finish()
    return nc


def uT_tiles(u_l):
    return np.ascontiguousarray(u_l.reshape(128, 128, 16, 128).transpose(0, 3, 2, 1))


_NC_CACHE = {}


def _get_nc(name, fn):
    if name not in _NC_CACHE:
        _NC_CACHE[name] = fn()
    return _NC_CACHE[name]


def _run(nc, ins):
    return run_bass_kernel_spmd(nc, ins, core_ids=list(range(8))).results


def kernel(x, c, ada_w, ada_b, norm1_g, norm2_g, final_g, w_in, fox_fbias, gla_wa2, gla_ba, gla_norm_g, w_out,
           peer_wq, peer_k1, peer_k2, peer_u, peer_v):
    f32 = np.float32
    x2 = np.asarray(x, f32).reshape(S, D)
    c1 = np.asarray(c, f32).reshape(D)
    xs_list = [own_blocks(x2, cc) for cc in range(8)]
    ident = np.eye(128, dtype=NPBF)
    tri2, suf2, rmask = gla_consts()
    masks = [band_masks(cc) for cc in range(8)]
    c_pk = np.ascontiguousarray(c1.reshape(16, 128).T)
    for l in range(2):
        w_in_l = np.asarray(w_in[l], f32)
        wfm = np.zeros((D, NFM), f32)
        wfm[:, :len(FM_COLS)] = w_in_l[:, FM_COLS]
        wtm = np.zeros((D, NTM), f32)
        wtm[:, :len(TM_COLS)] = w_in_l[:, TM_COLS]
        common = dict(c_pk=c_pk, adaw=np.ascontiguousarray(ada_w[l][:, 0:4096]), adab=bc128(np.asarray(ada_b[l][0:4096], f32)),
                      g_bc=bc128(np.asarray(norm1_g[l], f32)), ident=ident, wfm=wfm, wtm=wtm)
        rA = _run(_get_nc("A", build_A), [dict(common, xs=xs_list[cc]) for cc in range(8)])
        del wfm, wtm, common
        ZFM = assemble_tokens([r["zfm"] for r in rA], 1)
        ZTB = assemble_tokens([r["ztb"] for r in rA], 0)
        ZTF = assemble_tokens([r["ztf"] for r in rA], 0)
        fkT = np.ascontiguousarray(ZFM[R_FK:R_FK + 768])
        dkT = np.ascontiguousarray(ZFM[R_DK:R_DK + 768])
        ikT = np.ascontiguousarray(ZFM[R_IK:R_IK + 64])
        gaT = np.ascontiguousarray(ZFM[R_GA:R_GA + 16])
        fvg = vg_layout(ZTB[:, C_FV:C_FV + 768], 6)
        dvg = vg_layout(ZTB[:, C_DV:C_DV + 768], 6)
        ffp = np.ascontiguousarray(ZTF[:, 512 + 0:512 + 6].reshape(64, 128, 6).transpose(1, 2, 0))
        fbb = bc128(np.asarray(fox_fbias[l], f32))
        wa2_l = np.asarray(gla_wa2[l], f32)
        ba_l = np.asarray(gla_ba[l], f32)
        gnb = bc128(np.asarray(gla_norm_g[l], f32))
        gl = []
        for hg in range(4):
            sl = slice(hg * 128, (hg + 1) * 128)
            gl.append(dict(gqT=np.ascontiguousarray(ZFM[R_GQ + hg * 128:R_GQ + (hg + 1) * 128]),
                           gkT=np.ascontiguousarray(ZFM[R_GK + hg * 128:R_GK + (hg + 1) * 128]),
                           gktm=tm_layout(ZTB[:, C_GK + hg * 128:C_GK + (hg + 1) * 128]),
                           gvtm=tm_layout(ZTB[:, C_GV + hg * 128:C_GV + (hg + 1) * 128]),
                           grtm=tm_layout(ZTF[:, hg * 128:(hg + 1) * 128]), gaT=gaT,
                           wa2=np.ascontiguousarray(wa2_l[:, sl]), nbacol=np.ascontiguousarray(-ba_l[sl][:, None]),
                           barow=bc128(ba_l[sl]), gnb=gnb, tri2=tri2, suf2=suf2, rmask=rmask))
        insB = []
        for cc in range(8):
            cm, am, sel = masks[cc]
            zo = rA[cc]["zfm"]
            iq = zo[R_IQ:R_IQ + 1024].reshape(16, 64, 1024).transpose(1, 0, 2)
            iw = rA[cc]["ztf"][:, 518:534].reshape(8, 128, 16).transpose(1, 0, 2)
            dct = dict(fkT=fkT, fvg=fvg, fqT=np.ascontiguousarray(zo[R_FQ:R_FQ + 768]), ffp=ffp, fbb=fbb, tri=TRI, sel=sel, cm=cm,
                       dkT=dkT, dvg=dvg, dqT=np.ascontiguousarray(zo[R_DQ:R_DQ + 768]), iqT=np.ascontiguousarray(iq), ikT=ikT,
                       iwp=np.ascontiguousarray(iw), am=am, ident=ident)
            dct.update(gl[cc % 4])
            insB.append(dct)
        rB = _run(_get_nc("B", build_B), insB)
        del insB, fvg, dvg, gl
        gla_full = np.concatenate([rB[hg]["glao"].reshape(S, 128) for hg in range(4)], axis=1)
        mix_list = []
        for cc in range(8):
            g_own = own_blocks(gla_full, cc)
            mix = np.concatenate([rB[cc]["foxo"], g_own, rB[cc]["dsao"]], axis=2)
            mix_list.append(mix.reshape(NTOK, D))
        kT = np.zeros((16, 128, 128), f32)
        for h in range(8):
            kT[2 * h] = np.asarray(peer_k1[l][h], f32).T
            kT[2 * h + 1] = np.asarray(peer_k2[l][h], f32).T
        common = dict(c_pk=c_pk, adaw=np.ascontiguousarray(ada_w[l][:, 4096:12288]), adab=bc128(np.asarray(ada_b[l][4096:12288], f32)),
                      g_bc=bc128(np.asarray(norm2_g[l], f32)), ident=ident, wo=np.ascontiguousarray(w_out[l], dtype=f32),
                      wq=np.ascontiguousarray(peer_wq[l], dtype=f32), kT=kT)
        rC1 = _run(_get_nc("C1", build_C1), [dict(common, xs=xs_list[cc], mixT=np.ascontiguousarray(mix_list[cc].T)) for cc in range(8)])
        del common
        final = (l == 1)
        uTt = uT_tiles(np.asarray(peer_u[l], f32))
        vv = np.ascontiguousarray(peer_v[l], dtype=f32)
        insC2 = []
        for cc in range(8):
            dct = dict(h2T=rC1[cc]["h2T"], s12=rC1[cc]["s12"], xmid=rC1[cc]["xmid"], g2bc=rC1[cc]["g2bc"], uTt=uTt, v=vv, ident=ident)
            if final:
                dct["fg_bc"] = bc128(np.asarray(final_g, f32))
            insC2.append(dct)
        rC2 = _run(_get_nc("C2f" if final else "C2", (lambda: build_C2(True)) if final else (lambda: build_C2(False))), insC2)
        del insC2, uTt, vv
        xs_list = [r["xo"] for r in rC2]
    out = assemble_tokens([xx.reshape(NTOK, D) for xx in xs_list], 0)
    return np.ascontiguousarray(out.reshape(1, S, D).astype(np.float32))
```

```python
import numpy as np
import ml_dtypes
from contextlib import ExitStack
import concourse.bass as bass
import concourse.mybir as mybir
from concourse.bass_utils import run_bass_kernel_spmd

F32 = mybir.dt.float32
BF16 = mybir.dt.bfloat16
U32 = mybir.dt.uint32
ALU = mybir.AluOpType
AF = mybir.ActivationFunctionType
AX = mybir.AxisListType
NPBF = ml_dtypes.bfloat16

ENGS = ("pe", "act", "dve", "pool", "sp")


class Buf:
    __slots__ = ("t", "name", "w", "r", "pr", "dsem")

    def __init__(self, t, name):
        self.t = t
        self.name = name
        self.w = {}
        self.r = {}
        self.pr = {}
        self.dsem = None

    def __getitem__(self, idx):
        return self.t[idx]


class Prog:
    def __init__(self, nc, n_dma_sems=80):
        self.nc = nc
        self.stack = ExitStack()
        self.esem = {}
        for e in ENGS[:4]:
            self.esem[e] = self.stack.enter_context(nc.semaphore("sem_" + e))
        self.ecnt = {e: 0 for e in ENGS[:4]}
        self.dpool = [self.stack.enter_context(nc.semaphore("dsem%d" % i)) for i in range(n_dma_sems)]
        self.dfree = list(range(n_dma_sems))
        self.dcnt = [0] * n_dma_sems
        self.semobj = {}
        for e in ENGS[:4]:
            self.semobj[("e", e)] = self.esem[e]
        for i, s in enumerate(self.dpool):
            self.semobj[("d", i)] = s
        self.seen = {e: {} for e in ENGS}
        self.ops = {e: [] for e in ENGS}
        self.pstack = None
        self.pbufs = []
        self.nbuf = 0
        self.n_instr = 0

    def begin_phase(self):
        self.pstack = ExitStack()
        self.pbufs = []

    def sb(self, shape, dtype, name=None):
        self.nbuf += 1
        name = (name or "sb") + "_%d" % self.nbuf
        t = self.pstack.enter_context(self.nc.sbuf_tensor(name, list(shape), dtype))
        b = Buf(t, name)
        self.pbufs.append(b)
        return b

    def ps(self, shape, dtype, name=None):
        self.nbuf += 1
        name = (name or "ps") + "_%d" % self.nbuf
        t = self.pstack.enter_context(self.nc.psum_tensor(name, list(shape), dtype))
        b = Buf(t, name)
        self.pbufs.append(b)
        return b

    def sbp(self, shape, dtype, name=None):
        self.nbuf += 1
        name = (name or "sbp") + "_%d" % self.nbuf
        t = self.stack.enter_context(self.nc.sbuf_tensor(name, list(shape), dtype))
        return Buf(t, name)

    def scope_begin(self):
        if not hasattr(self, "scopes"):
            self.scopes = []
        self.scopes.append((ExitStack(), []))

    def sbs(self, shape, dtype, name=None):
        self.nbuf += 1
        name = (name or "sbs") + "_%d" % self.nbuf
        stack, bufs = self.scopes[-1]
        t = stack.enter_context(self.nc.sbuf_tensor(name, list(shape), dtype))
        b = Buf(t, name)
        bufs.append(b)
        return b

    def scope_end(self):
        stack, bufs = self.scopes.pop()
        self.begin_phase()
        self.pbufs = list(bufs)
        self.end_phase()
        stack.close()

    def dram(self, name, shape, dtype, kind):
        t = self.nc.dram_tensor(name, list(shape), dtype, kind=kind).ap()
        return Buf(t, name)

    def scratch(self, name, shape, dtype):
        t = self.nc.dram_tensor(name, list(shape), dtype).ap()
        return Buf(t, name)

    def allgather(self, snd, rcv):
        rg = [list(range(8))]
        self.op("pool", lambda e: e.collective_compute("AllGather", ALU.bypass, replica_groups=rg, ins=[snd.t.opt()], outs=[rcv.t.opt()]),
                reads=[snd], writes=[rcv], dma=rcv, inc=1)

    def _dsem_of(self, buf):
        if buf.dsem is None:
            buf.dsem = self.dfree.pop()
        return buf.dsem

    def op(self, eng, fn, reads=(), writes=(), dma=None, inc=16):
        kind = "dma" if dma is not None else "compute"
        deps = {}

        def add(key, val, src):
            if kind == "compute" and src == eng:
                return
            if key not in deps or deps[key] < val:
                deps[key] = val

        def add_raw(key, val, src):
            if kind == "compute" and src == eng and eng == "pe":
                return
            if key not in deps or deps[key] < val:
                deps[key] = val

        for b in reads:
            for key, (val, src) in b.w.items():
                add_raw(key, val, src)
        for b in writes:
            for key, (val, src) in b.r.items():
                add(key, val, src)
            for key, (val, src) in b.pr.items():
                add(key, val, src)
            for key, (val, src) in b.w.items():
                if kind == "dma" and key[0] == "d":
                    continue
                add(key, val, src)
        if kind == "compute":
            self.ecnt[eng] += 1
            key, val, src = ("e", eng), self.ecnt[eng], eng
        else:
            i = self._dsem_of(dma)
            self.dcnt[i] += inc
            key, val, src = ("d", i), self.dcnt[i], None
        waits = []
        seen = self.seen[eng]
        for k, v in deps.items():
            if seen.get(k, 0) < v:
                seen[k] = v
                waits.append((k, v))
        for b in reads:
            if b.r.get(key, (0, None))[0] < val:
                b.r[key] = (val, src)
        for b in writes:
            if b.r:
                b.pr = dict(b.r)
                b.w.clear()
                b.r.clear()
            if b.w.get(key, (0, None))[0] < val:
                b.w[key] = (val, src)
        self.ops[eng].append((waits, fn, key, inc))
        self.n_instr += 1

    def load(self, q, dstbuf, out_ap, in_ap, dram=None, **kw):
        self.op(q, lambda e: e.dma_start(out=out_ap, in_=in_ap, **kw), reads=([dram] if dram is not None else []),
                writes=[dstbuf], dma=dstbuf)

    def store(self, q, srcbuf, out_ap, in_ap, dram=None, **kw):
        self.op(q, lambda e: e.dma_start(out=out_ap, in_=in_ap, **kw), reads=[srcbuf],
                writes=([dram] if dram is not None else []), dma=srcbuf)

    def mm(self, out_ap, lhsT, rhs, start, stop, reads, writes):
        self.op("pe", lambda e: e.matmul(out_ap, lhsT, rhs, start=start, stop=stop), reads=reads, writes=writes)

    def end_phase(self, final=False):
        nc = self.nc
        ops = self.ops
        semobj = self.semobj
        esem = self.esem
        finalwaits = []
        if final:
            for i, c in enumerate(self.dcnt):
                if c > 0:
                    finalwaits.append((("d", i), c))
        else:
            for b in self.pbufs:
                if b.dsem is not None and self.dcnt[b.dsem] > 0:
                    finalwaits.append((("d", b.dsem), self.dcnt[b.dsem]))

        def emit(e, name):
            for waits, fn, key, inc in ops[name]:
                for k, v in waits:
                    e.wait_ge(semobj[k], v)
                ins = fn(e)
                if key[0] == "e":
                    ins.then_inc(esem[key[1]], 1)
                else:
                    ins.then_inc(semobj[key], inc)
            if name == "sp":
                for k, v in finalwaits:
                    e.wait_ge(semobj[k], v)

        with nc.Block() as block:
            if ops["sp"] or finalwaits:
                @block.sync
                def _(e):
                    emit(e, "sp")
            if ops["pe"]:
                @block.tensor
                def _(e):
                    emit(e, "pe")
            if ops["act"]:
                @block.scalar
                def _(e):
                    emit(e, "act")
            if ops["dve"]:
                @block.vector
                def _(e):
                    emit(e, "dve")
            if ops["pool"]:
                @block.gpsimd
                def _(e):
                    emit(e, "pool")
        self.ops = {e: [] for e in ENGS}
        for b in self.pbufs:
            if b.dsem is not None:
                self.dfree.append(b.dsem)
        self.pbufs = []
        self.pstack.close()
        self.pstack = None

    def finish(self):
        self.stack.close()


D = 2048
EPS = 1e-6
NTOK = 1024
NB = 8
O_FQ, O_FK, O_FV, O_FF = 0, 768, 1536, 2304
O_GQ, O_GK, O_GV, O_GR, O_GA = 2310, 2822, 3334, 3846, 4358
O_DQ, O_DK, O_DV = 4374, 5142, 5910
O_IQ, O_IK, O_IW = 6678, 7702, 7766
FM_COLS = (list(range(O_FQ, O_FQ + 768)) + list(range(O_FK, O_FK + 768)) + list(range(O_DQ, O_DQ + 768))
           + list(range(O_DK, O_DK + 768)) + list(range(O_GQ, O_GQ + 512)) + list(range(O_GK, O_GK + 512))
           + list(range(O_IQ, O_IQ + 1024)) + list(range(O_IK, O_IK + 64)) + list(range(O_GA, O_GA + 16)))
NFM = 5248
R_FQ, R_FK, R_DQ, R_DK, R_GQ, R_GK, R_IQ, R_IK, R_GA = 0, 768, 1536, 2304, 3072, 3584, 4096, 5120, 5184
TM_COLS = (list(range(O_FV, O_FV + 768)) + list(range(O_DV, O_DV + 768)) + list(range(O_GK, O_GK + 512))
           + list(range(O_GV, O_GV + 512)) + list(range(O_GR, O_GR + 512)) + list(range(O_FF, O_FF + 6))
           + list(range(O_IW, O_IW + 16)))
NTM = 3200
C_FV, C_DV, C_GK, C_GV = 0, 768, 1536, 2048
C_GR, C_FF, C_IW = 0, 512, 518


def emit_mod(P, c_d, adaw_d, adab_d, ncols, modbc):
    P.begin_phase()
    ct = P.sb([128, 16], F32, "sb_ct")
    ca = P.sb([128, 16], F32, "sb_ca")
    crep = P.sb([128, 16, 128], F32, "sb_crep")
    adab = P.sb([128, ncols], F32, "sb_adab")
    wch = [P.sb([128, 16, 512], F32, "sb_wch") for _ in range(2)]
    pm = [P.ps([128, 512], F32, "ps_mod") for _ in range(2)]
    P.load("sp", ct, ct[:, :], c_d[:, :])
    P.load("act", adab, adab[:, :], adab_d[:, :])
    P.op("act", lambda e: e.activation(out=ca[:, :], in_=ct[:, :], func=AF.Silu), reads=[ct], writes=[ca])
    P.op("dve", lambda e: e.tensor_copy(out=crep[:, :, :], in_=ca[:, :].unsqueeze(2).to_broadcast([128, 16, 128])),
         reads=[ca], writes=[crep])
    wv = adaw_d.t.rearrange("(k p) n -> p k n", p=128)
    for ci in range(ncols // 512):
        w = wch[ci % 2]
        q = "sp" if ci % 2 == 0 else "act"
        for kh in range(2):
            P.load(q, w, w[:, kh * 8:(kh + 1) * 8, :], wv[:, kh * 8:(kh + 1) * 8, ci * 512:(ci + 1) * 512])
        ps = pm[ci % 2]
        for k in range(16):
            P.mm(ps[:, :], crep[:, k, :], w[:, k, :], k == 0, k == 15, reads=[crep, w], writes=[ps])
        P.op("dve", lambda e, ps=ps, ci=ci: e.tensor_tensor(out=modbc[:, ci * 512:(ci + 1) * 512], in0=ps[:, :],
                                                          in1=adab[:, ci * 512:(ci + 1) * 512], op=ALU.add),
             reads=[ps, adab], writes=[modbc])
    P.end_phase()


def emit_norm_T(P, xs_d, A_bc, B_bc, ident, hT, xkeep=None, xdram=None):
    P.begin_phase()
    xt = [P.sb([128, D], F32, "sb_x") for _ in range(2)]
    junk = P.sb([128, D], BF16, "sb_junk")
    tmp = [P.sb([128, D], F32, "sb_tmp") for _ in range(2)]
    hb = [P.sb([128, D], BF16, "sb_hb") for _ in range(2)]
    st = [P.sb([128, 4], F32, "sb_st") for _ in range(2)]
    pT = [P.ps([128, 8, 128], BF16, "ps_T") for _ in range(2)]
    for b in range(NB):
        if xkeep is None:
            x = xt[b % 2]
            P.load("sp" if b % 2 == 0 else "act", x, x[:, :], xs_d[b, :, :], dram=xdram)
            xa = x[:, :]
            xr = [x]
        else:
            xa = xkeep[:, b, :]
            xr = [xkeep]
        s = st[b % 2]
        t = tmp[b % 2]
        h = hb[b % 2]
        P.op("act", lambda e, xa=xa, s=s: e.activation(out=junk[:, :], in_=xa, func=AF.Square, accum_out=s[:, 0:1]),
             reads=xr, writes=[junk, s])
        P.op("act", lambda e, s=s: e.activation(out=s[:, 1:2], in_=s[:, 0:1], func=AF.Sqrt, scale=1.0 / D, bias=EPSB[0][:, 0:1]),
             reads=[s, EPSB[0]], writes=[s])
        P.op("dve", lambda e, s=s: e.reciprocal(out=s[:, 2:3], in_=s[:, 1:2]), reads=[s], writes=[s])
        P.op("dve", lambda e, xa=xa, s=s, t=t: e.scalar_tensor_tensor(out=t[:, :], in0=xa, scalar=s[:, 2:3], in1=A_bc[:, :],
                                                                   op0=ALU.mult, op1=ALU.mult),
             reads=xr + [s, A_bc], writes=[t])
        P.op("pool", lambda e, t=t, h=h: e.tensor_tensor(out=h[:, :], in0=t[:, :], in1=B_bc[:, :], op=ALU.add),
             reads=[t, B_bc], writes=[h])
        for half in range(2):
            pt = pT[half]
            for kk in range(8):
                k = half * 8 + kk
                P.op("pe", lambda e, pt=pt, kk=kk, k=k, h=h: e.transpose(out=pt[:, kk, :], in_=h[:, k * 128:(k + 1) * 128],
                                                                       identity=ident[:, :]),
                     reads=[h, ident], writes=[pt])
            P.op("act", lambda e, pt=pt, half=half, b=b: e.activation(
                out=hT[:, half * 8:(half + 1) * 8, b * 128:(b + 1) * 128], in_=pt[:, :, :], func=AF.Copy),
                 reads=[pt], writes=[hT])
    P.end_phase()


EPSB = [None]


def emit_consts(P, ident_d):
    ident = P.sbp([128, 128], BF16, "sbp_ident")
    epsb = P.sbp([128, 1], F32, "sbp_eps")
    EPSB[0] = epsb
    P.begin_phase()
    P.load("sp", ident, ident[:, :], ident_d[:, :])
    P.op("dve", lambda e: e.memset(epsb[:, :], EPS), writes=[epsb])
    P.end_phase()
    return ident


def emit_proj(P, hT, specs):
    P.begin_phase()
    wf = [P.sb([128, 16, 512], F32, "sb_wf") for _ in range(2)]
    wb = [P.sb([128, 16, 512], BF16, "sb_wb") for _ in range(2)]
    pp = [P.ps([128, 512], F32, "ps_pp") for _ in range(4)]
    ofm = [P.sb([128, 1024], BF16, "sb_ofm") for _ in range(2)]
    otb = [P.sb([128, 512], BF16, "sb_otb") for _ in range(2)]
    otf = [P.sb([128, 512], F32, "sb_otf") for _ in range(2)]
    ci_g = 0
    pi = 0
    oi = 0
    for sp in specs:
        N = sp["w"].t.shape[1]
        wv = sp["w"].t.rearrange("(k p) n -> p k n", p=128)
        for c0 in range(0, N, 512):
            cw = min(512, N - c0)
            w32 = wf[ci_g % 2]
            w16 = wb[ci_g % 2]
            for kh in range(2):
                P.load("sp" if kh == 0 else "act", w32, w32[:, kh * 8:(kh + 1) * 8, 0:cw], wv[:, kh * 8:(kh + 1) * 8, c0:c0 + cw])
            P.op("dve", lambda e, w32=w32, w16=w16, cw=cw: e.tensor_copy(out=w16[:, 0:8, 0:cw], in_=w32[:, 0:8, 0:cw]),
                 reads=[w32], writes=[w16])
            P.op("pool", lambda e, w32=w32, w16=w16, cw=cw: e.tensor_copy(out=w16[:, 8:16, 0:cw], in_=w32[:, 8:16, 0:cw]),
                 reads=[w32], writes=[w16])
            ci_g += 1
            if sp["kind"] == "fm":
                od = sp["outs"][0][0]
                for g0 in range(0, cw, 128):
                    o = ofm[oi % 2]
                    oi += 1
                    for th in range(2):
                        ps = pp[pi % 4]
                        pi += 1
                        for k in range(16):
                            P.mm(ps[:, :], w16[:, k, g0:g0 + 128], hT[:, k, th * 512:(th + 1) * 512], k == 0, k == 15,
                                 reads=[w16, hT], writes=[ps])
                        eng = "act" if th == 0 else "dve"
                        if eng == "act":
                            P.op("act", lambda e, o=o, ps=ps, th=th: e.activation(out=o[:, th * 512:(th + 1) * 512], in_=ps[:, :], func=AF.Copy),
                                 reads=[ps], writes=[o])
                        else:
                            P.op("dve", lambda e, o=o, ps=ps, th=th: e.tensor_copy(out=o[:, th * 512:(th + 1) * 512], in_=ps[:, :]),
                                 reads=[ps], writes=[o])
                    r0 = c0 + g0
                    P.store("pool", o, od.t[r0:r0 + 128, :], o[:, :], dram=od)
            else:
                tgt = None
                for (od, a, b_, dt) in sp["outs"]:
                    if a <= c0 and c0 + cw <= b_:
                        tgt = (od, a, dt)
                od, a, dt = tgt
                for b in range(NB):
                    ps = pp[pi % 4]
                    pi += 1
                    for k in range(16):
                        P.mm(ps[:, 0:cw], hT[:, k, b * 128:(b + 1) * 128], w16[:, k, 0:cw], k == 0, k == 15,
                             reads=[w16, hT], writes=[ps])
                    o = (otb if dt == BF16 else otf)[oi % 2]
                    oi += 1
                    if b % 2 == 0:
                        P.op("act", lambda e, o=o, ps=ps, cw=cw: e.activation(out=o[:, 0:cw], in_=ps[:, 0:cw], func=AF.Copy),
                             reads=[ps], writes=[o])
                    else:
                        P.op("dve", lambda e, o=o, ps=ps, cw=cw: e.tensor_copy(out=o[:, 0:cw], in_=ps[:, 0:cw]),
                             reads=[ps], writes=[o])
                    P.store("pool", o, od.t[b * 128:(b + 1) * 128, c0 - a:c0 - a + cw], o[:, 0:cw], dram=od)
    P.end_phase()


def build_A():
    nc = bass.Bass("TRN2", target_bir_lowering=False)
    P = Prog(nc)
    xs = P.dram("xs", [NB, 128, D], F32, "ExternalInput")
    c_d = P.dram("c_pk", [128, 16], F32, "ExternalInput")
    adaw = P.dram("adaw", [D, 4096], F32, "ExternalInput")
    adab = P.dram("adab", [128, 4096], F32, "ExternalInput")
    g_d = P.dram("g_bc", [128, D], F32, "ExternalInput")
    ident_d = P.dram("ident", [128, 128], BF16, "ExternalInput")
    wfm = P.dram("wfm", [D, NFM], F32, "ExternalInput")
    wtm = P.dram("wtm", [D, NTM], F32, "ExternalInput")
    zfm = P.dram("zfm", [NFM, NTOK], BF16, "ExternalOutput")
    ztb = P.dram("ztb", [NTOK, 2560], BF16, "ExternalOutput")
    ztf = P.dram("ztf", [NTOK, 640], F32, "ExternalOutput")
    ident = emit_consts(P, ident_d)
    modbc = P.sbp([128, 4096], F32, "sbp_mod")
    A1 = P.sbp([128, D], F32, "sbp_A1")
    hT = P.sbp([128, 16, NTOK], BF16, "sbp_hT")
    emit_mod(P, c_d, adaw, adab, 4096, modbc)
    P.begin_phase()
    gb = P.sb([128, D], F32, "sb_g")
    P.load("sp", gb, gb[:, :], g_d[:, :])
    P.op("dve", lambda e: e.scalar_tensor_tensor(out=A1[:, :], in0=modbc[:, D:2 * D], scalar=1.0, in1=gb[:, :],
                                                 op0=ALU.add, op1=ALU.mult), reads=[modbc, gb], writes=[A1])
    P.end_phase()
    emit_norm_T(P, xs, A1, modbc_view(modbc, 0, D), ident, hT)
    emit_proj(P, hT, [dict(w=wfm, kind="fm", outs=[(zfm, 0, NFM, BF16)]),
                      dict(w=wtm, kind="tm", outs=[(ztb, 0, 2560, BF16), (ztf, 2560, 3200, F32)])])
    P.begin_phase()
    P.end_phase(final=True)
    P.finish()
    return nc


class View:
    def __init__(self, buf, a, b):
        self.buf = buf
        self.a = a
        self.b = b


def modbc_view(buf, a, b):
    v = Buf(buf.t[:, a:b], buf.name + "_v")
    v.w = buf.w
    v.r = buf.r
    v.pr = buf.pr
    return v


def own_blocks(arr, c):
    a = arr.reshape((64, 128) + arr.shape[1:])
    return np.ascontiguousarray(a[c::8])


def bc128(v):
    return np.ascontiguousarray(np.broadcast_to(v[None, :], (128, v.shape[0]))).astype(np.float32)


def host_A_inputs(x, c, ada_w_l, ada_b_l, norm_g_l, w_in_l):
    wfm = np.zeros((D, NFM), np.float32)
    wfm[:, :len(FM_COLS)] = w_in_l[:, FM_COLS]
    wtm = np.zeros((D, NTM), np.float32)
    wtm[:, :len(TM_COLS)] = w_in_l[:, TM_COLS]
    common = dict(c_pk=np.ascontiguousarray(c.reshape(16, 128).T), adaw=np.ascontiguousarray(ada_w_l[:, 0:4096]),
                  adab=bc128(ada_b_l[0:4096]), g_bc=bc128(norm_g_l), ident=np.eye(128, dtype=NPBF), wfm=wfm, wtm=wtm)
    x2 = x.reshape(8192, D)
    return [dict(common, xs=own_blocks(x2, cc)) for cc in range(8)]


S = 8192
NKB = 64
SCALE = 128 ** -0.5
NEG = -1.0e30
NBIS = 18
TOPK = 256


def emit_attn(P, KT_d, Vg_d, QT_d, nheads, out_stage, col0, bias=None, maskT=None, cm=None):
    P.begin_phase()
    KT = [P.sb([128, S], BF16, "sb_KT") for _ in range(2)]
    Vg = [P.sb([128, NKB, 129], BF16, "sb_Vg") for _ in range(2)]
    QT = [P.sb([128, 1024], BF16, "sb_QT") for _ in range(2)]
    pO = [P.ps([128, 512], F32, "ps_O") for _ in range(2)]
    pS = []
    for _ in range(2):
        bank = P.ps([128, 4, 128], F32, "ps_S")
        for q in range(4):
            pS.append(Buf(bank.t[:, q, :], bank.name + "_q%d" % q))
    Pt = [P.sb([128, 128], BF16, "sb_Pt") for _ in range(6)]
    rc = [P.sb([128, 1], F32, "sb_rc") for _ in range(2)]
    zero_b = P.sb([128, 1], F32, "sb_zb")
    P.op("dve", lambda e: e.memset(zero_b[:, :], 0.0), writes=[zero_b])
    si = 0
    pi = 0
    oi = 0
    for h in range(nheads):
        kt, vg, qt = KT[h % 2], Vg[h % 2], QT[h % 2]
        for q4 in range(4):
            P.load("sp" if q4 % 2 == 0 else "act", kt, kt[:, q4 * 2048:(q4 + 1) * 2048],
                   KT_d.t[h * 128:(h + 1) * 128, q4 * 2048:(q4 + 1) * 2048])
        for q2 in range(2):
            P.load("sp" if q2 == 0 else "act", vg, vg[:, q2 * 32:(q2 + 1) * 32, :], Vg_d.t[h, :, q2 * 32:(q2 + 1) * 32, :])
        P.load("pool", qt, qt[:, :], QT_d.t[h * 128:(h + 1) * 128, :])
        for j in range(8):
            nkb = 8 * j + 8
            po = pO[oi % 2]
            oi += 1
            for kb in range(nkb):
                ps = pS[si % 8]
                si += 1
                pt = Pt[pi % 6]
                pi += 1
                P.mm(ps[:, :], kt[:, kb * 128:(kb + 1) * 128], qt[:, j * 128:(j + 1) * 128], True, True,
                     reads=[kt, qt], writes=[ps])
                if bias is not None:
                    P.op("act", lambda e, pt=pt, ps=ps, h=h, j=j, kb=kb: e.activation(
                        out=pt[:, :], in_=ps[:, :], func=AF.Exp, scale=SCALE, bias=bias[:, h, j, kb:kb + 1]),
                        reads=[ps, bias], writes=[pt])
                else:
                    P.op("act", lambda e, pt=pt, ps=ps: e.activation(
                        out=pt[:, :], in_=ps[:, :], func=AF.Exp, scale=SCALE, bias=zero_b[:, 0:1]),
                        reads=[ps, zero_b], writes=[pt])
                if maskT is not None:
                    mt = maskT[j]
                    P.op("dve", lambda e, pt=pt, mt=mt, kb=kb: e.tensor_tensor(out=pt[:, :], in0=pt[:, :], in1=mt[:, kb, :], op=ALU.mult),
                         reads=[pt, mt], writes=[pt])
                elif kb >= 8 * j:
                    r = kb - 8 * j
                    P.op("dve", lambda e, pt=pt, r=r: e.tensor_tensor(out=pt[:, :], in0=pt[:, :], in1=cm[:, r, :], op=ALU.mult),
                         reads=[pt, cm], writes=[pt])
                P.mm(po[:, 0:129], pt[:, :], vg[:, kb, :], kb == 0, kb == nkb - 1, reads=[pt, vg], writes=[po])
            r_ = rc[oi % 2]
            P.op("dve", lambda e, r_=r_, po=po: e.reciprocal(out=r_[:, 0:1], in_=po[:, 128:129]), reads=[po], writes=[r_])
            P.op("dve", lambda e, r_=r_, po=po, j=j, h=h: e.tensor_scalar(
                out=out_stage[:, j, col0 + h * 128:col0 + (h + 1) * 128], in0=po[:, 0:128], scalar1=r_[:, 0:1], scalar2=None,
                op0=ALU.mult), reads=[po, r_], writes=[out_stage])
    P.end_phase()


def emit_fox_bias(P, ff_d, fb_d, tri_d, sel_d, bias):
    P.begin_phase()
    ff = P.sb([128, 6, 64], F32, "sb_ff")
    fb = P.sb([128, 6], F32, "sb_fb")
    tri = P.sb([128, 128], F32, "sb_tri")
    ones = P.sb([128, 128], F32, "sb_ones")
    onec = P.sb([128, 1], F32, "sb_onec")
    sel = P.sb([128, 8], F32, "sb_sel")
    nlf = P.sb([128, 6, 64], F32, "sb_nlf")
    tot = P.sb([128, 6, 64], F32, "sb_tot")
    incl = P.sb([128, 6, 64], F32, "sb_incl")
    NF = P.sb([128, 6, 64], F32, "sb_NF")
    tmp = P.sb([128, 6, 8, 8], F32, "sb_tmp")
    nfe = P.sb([128, 6, 8], F32, "sb_nfe")
    pw = P.ps([128, 384], F32, "ps_w")
    pt_ = P.ps([128, 384], F32, "ps_t")
    P.load("sp", ff, ff[:, :, :], ff_d[:, :, :])
    P.load("act", fb, fb[:, :], fb_d[:, :])
    P.load("sp", tri, tri[:, :], tri_d[:, :])
    P.load("act", sel, sel[:, :], sel_d[:, :])
    P.op("dve", lambda e: e.memset(ones[:, :], 1.0), writes=[ones])
    P.op("dve", lambda e: e.memset(onec[:, :], 1.0), writes=[onec])
    P.op("dve", lambda e: e.tensor_tensor(out=nlf[:, :, :], in0=ff[:, :, :], in1=fb[:, :].unsqueeze(2).to_broadcast([128, 6, 64]),
                                          op=ALU.add), reads=[ff, fb], writes=[nlf])
    nlf2 = nlf.t.rearrange("p h k -> p (h k)")
    P.op("act", lambda e: e.activation(out=nlf2, in_=nlf2, func=AF.Exp, scale=-1.0), reads=[nlf], writes=[nlf])
    P.op("act", lambda e: e.activation(out=nlf2, in_=nlf2, func=AF.Ln, bias=onec[:, 0:1]), reads=[nlf, onec], writes=[nlf])
    P.mm(pw[:, :], tri[:, :], nlf2, True, True, reads=[tri, nlf], writes=[pw])
    P.mm(pt_[:, :], ones[:, :], nlf2, True, True, reads=[ones, nlf], writes=[pt_])
    tot2 = tot.t.rearrange("p h k -> p (h k)")
    P.op("dve", lambda e: e.tensor_copy(out=tot2, in_=pt_[:, :]), reads=[pt_], writes=[tot])
    for h in range(6):
        P.op("dve", lambda e, h=h: e.tensor_tensor_scan(out=incl[:, h, :], data0=ones[:, 0:64], data1=tot[:, h, :], initial=0.0,
                                                        op0=ALU.mult, op1=ALU.add), reads=[ones, tot], writes=[incl])
    NF2 = NF.t.rearrange("p h k -> p (h k)")
    incl2 = incl.t.rearrange("p h k -> p (h k)")
    P.op("dve", lambda e: e.tensor_tensor(out=NF2, in0=pw[:, :], in1=incl2, op=ALU.add), reads=[pw, incl], writes=[NF])
    P.op("dve", lambda e: e.tensor_tensor(out=NF2, in0=NF2, in1=tot2, op=ALU.subtract), reads=[NF, tot], writes=[NF])
    P.op("dve", lambda e: e.tensor_tensor(out=tmp[:, :, :, :], in0=incl.t.rearrange("p h (j r) -> p h j r", r=8),
                                          in1=sel[:, :].unsqueeze(1).unsqueeze(1).to_broadcast([128, 6, 8, 8]), op=ALU.mult),
         reads=[incl, sel], writes=[tmp])
    P.op("dve", lambda e: e.tensor_reduce(out=nfe[:, :, :], in_=tmp[:, :, :, :], axis=AX.X, op=ALU.add), reads=[tmp], writes=[nfe])
    for h in range(6):
        for j in range(8):
            P.op("dve", lambda e, h=h, j=j: e.tensor_scalar(out=bias[:, h, j, :], in0=NF[:, h, :], scalar1=nfe[:, h, j:j + 1],
                                                            scalar2=0.0, op0=ALU.subtract, op1=ALU.min),
                 reads=[NF, nfe], writes=[bias])
    P.end_phase()


def vg_layout(v_all, nheads):
    v = v_all.reshape(64, 128, nheads, 128).transpose(2, 1, 0, 3)
    o = np.ones((nheads, 128, 64, 129), NPBF)
    o[:, :, :, :128] = v
    return o


def band_masks(c):
    sp = np.arange(128)[:, None]
    t = np.arange(128)[None, :]
    cm = np.zeros((128, 8, 128), np.float32)
    am = np.full((128, 8, 128), NEG, np.float32)
    for r in range(8):
        if r < c:
            cm[:, r, :] = 1.0
            am[:, r, :] = 0.0
        elif r == c:
            cm[:, r, :] = (sp <= t)
            am[:, r, :] = np.where(sp.T <= t.T, 0.0, NEG)
    sel = np.zeros((128, 8), np.float32)
    sel[:, c] = 1.0
    return cm.astype(NPBF), am, sel


TRI = np.triu(np.ones((128, 128), np.float32))


def assemble_tokens(parts, axis):
    shp = list(parts[0].shape)
    n = shp[axis]
    assert n == 1024
    st = np.stack([np.moveaxis(p, axis, 0).reshape((8, 128) + tuple(np.moveaxis(p, axis, 0).shape[1:])) for p in parts], axis=1)
    g = st.reshape((8192,) + st.shape[3:])
    return np.moveaxis(g, 0, axis)


def emit_dsa_select(P, iqT_d, ikT_d, iw_d, am_d, ident, maskT):
    P.begin_phase()
    score = P.sb([128, S], F32, "sb_score")
    junk = P.sb([128, S], BF16, "sb_junk")
    ikT = P.sb([64, S], BF16, "sb_ikT")
    iq = [P.sb([64, 16, 128], BF16, "sb_iq") for _ in range(2)]
    rr = [P.sb([128, 512], F32, "sb_rr") for _ in range(3)]
    am = P.sb([128, 8, 128], F32, "sb_am")
    iw = P.sb([128, 8, 16], F32, "sb_iw")
    wsc = P.sb([128, 8, 16], F32, "sb_wsc")
    pI = [P.ps([128, 512], F32, "ps_I") for _ in range(4)]
    pT = [P.ps([128, 8, 128], BF16, "ps_mT") for _ in range(2)]
    mch = [P.sb([128, 1024], BF16, "sb_mch") for _ in range(2)]
    sms = [P.sb([128, 8], F32, "sb_sm") for _ in range(2)]
    for q4 in range(4):
        P.load("sp" if q4 % 2 == 0 else "act", ikT, ikT[:, q4 * 2048:(q4 + 1) * 2048], ikT_d.t[:, q4 * 2048:(q4 + 1) * 2048])
    P.load("sp", am, am[:, :, :], am_d[:, :, :])
    P.load("act", iw, iw[:, :, :], iw_d[:, :, :])
    P.op("dve", lambda e: e.tensor_scalar(out=wsc[:, :, :], in0=iw[:, :, :], scalar1=(64 ** -0.5) * (16 ** -0.5), scalar2=None,
                                          op0=ALU.mult), reads=[iw], writes=[wsc])
    ii = 0
    ti = 0
    for j in range(8):
        L = (8 * j + 8) * 128
        iqj = iq[j % 2]
        P.load("pool", iqj, iqj[:, :, :], iqT_d.t[:, :, j * 128:(j + 1) * 128])
        sm = sms[j % 2]
        for ck in range(L // 512):
            sc = score.t[:, ck * 512:(ck + 1) * 512]
            for h in range(16):
                ps = pI[ii % 4]
                r = rr[ii % 3]
                ii += 1
                P.mm(ps[:, :], iqj[:, h, :], ikT[:, ck * 512:(ck + 1) * 512], True, True, reads=[iqj, ikT], writes=[ps])
                P.op("act", lambda e, r=r, ps=ps: e.activation(out=r[:, :], in_=ps[:, :], func=AF.Relu), reads=[ps], writes=[r])
                if h == 0:
                    P.op("dve", lambda e, sc=sc, r=r, j=j: e.tensor_scalar(out=sc, in0=r[:, :], scalar1=wsc[:, j, 0:1], scalar2=None,
                                                                         op0=ALU.mult), reads=[r, wsc], writes=[score])
                else:
                    P.op("dve", lambda e, sc=sc, r=r, j=j, h=h: e.scalar_tensor_tensor(
                        out=sc, in0=r[:, :], scalar=wsc[:, j, h:h + 1], in1=sc, op0=ALU.mult, op1=ALU.add),
                        reads=[r, wsc, score], writes=[score])
        sL = score.t[:, 0:L]
        P.op("dve", lambda e, sm=sm, sL=sL: e.tensor_reduce(out=sm[:, 0:1], in_=sL, axis=AX.X, op=ALU.max, apply_absolute_value=True),
             reads=[score], writes=[sm])
        P.op("dve", lambda e, sm=sm: e.tensor_scalar(out=sm[:, 0:1], in0=sm[:, 0:1], scalar1=1.001, scalar2=1e-3, op0=ALU.mult, op1=ALU.add),
             reads=[sm], writes=[sm])
        P.op("dve", lambda e, sm=sm: e.tensor_scalar(out=sm[:, 1:2], in0=sm[:, 0:1], scalar1=-1.0, scalar2=None, op0=ALU.mult),
             reads=[sm], writes=[sm])
        sB = score.t[:, L - 1024:L]
        P.op("dve", lambda e, sB=sB: e.tensor_tensor(out=sB, in0=sB, in1=am.t.rearrange("p r s -> p (r s)"), op=ALU.add),
             reads=[score, am], writes=[score])
        for k in range(1, NBIS + 1):
            f = 2.0 ** (1 - k)
            P.op("dve", lambda e, sm=sm, f=f: e.tensor_scalar(out=sm[:, 2:3], in0=sm[:, 0:1], scalar1=f, scalar2=sm[:, 1:2],
                                                            op0=ALU.mult, op1=ALU.add), reads=[sm], writes=[sm])
            P.op("dve", lambda e, sm=sm, sL=sL, L=L: e.tensor_scalar(out=junk[:, 0:L], in0=sL, scalar1=sm[:, 2:3], scalar2=0.0,
                                                                   op0=ALU.is_ge, op1=ALU.add, accum_out=sm[:, 3:4]),
                 reads=[score, sm], writes=[junk, sm])
            P.op("dve", lambda e, sm=sm, f=f: e.tensor_scalar(out=sm[:, 4:5], in0=sm[:, 3:4], scalar1=TOPK - 0.5, scalar2=f,
                                                            op0=ALU.is_ge, op1=ALU.mult), reads=[sm], writes=[sm])
            P.op("dve", lambda e, sm=sm: e.scalar_tensor_tensor(out=sm[:, 1:2], in0=sm[:, 4:5], scalar=sm[:, 0:1], in1=sm[:, 1:2],
                                                              op0=ALU.mult, op1=ALU.add), reads=[sm], writes=[sm])
        for g in range(L // 1024):
            mc = mch[ti % 2]
            pt = pT[ti % 2]
            ti += 1
            P.op("dve", lambda e, mc=mc, g=g, sm=sm: e.tensor_scalar(out=mc[:, :], in0=score[:, g * 1024:(g + 1) * 1024],
                                                                   scalar1=sm[:, 1:2], scalar2=None, op0=ALU.is_ge),
                 reads=[score, sm], writes=[mc])
            for q in range(8):
                P.op("pe", lambda e, pt=pt, mc=mc, q=q: e.transpose(out=pt[:, q, :], in_=mc[:, q * 128:(q + 1) * 128], identity=ident[:, :]),
                     reads=[mc, ident], writes=[pt])
            mt = maskT[j]
            P.op("act", lambda e, mt=mt, pt=pt, g=g: e.activation(out=mt[:, g * 8:(g + 1) * 8, :], in_=pt[:, :, :], func=AF.Copy),
                 reads=[pt], writes=[mt])
    P.end_phase()


def emit_gla(P, gqT_d, gkT_d, gktm_d, gvtm_d, grtm_d, gaT_d, wa2_d, nbacol_d, barow_d, gn_d, tri2_d, suf2_d, rmask_d, out_d):
    P.scope_begin()

    def keep(shape, dt, name):
        return P.sbs(shape, dt, name)
    qtT = keep([128, S], BF16, "sbk_qtT")
    ktT = keep([128, S], BF16, "sbk_ktT")
    dn = keep([128, 128], F32, "sbk_dn")
    Sb = keep([128, 128, 128], BF16, "sbk_Sb")
    vtm = keep([128, 64, 128], BF16, "sbk_v")
    gs = keep([128, 64, 128], F32, "sbk_gs")
    wa2b = keep([16, 128], BF16, "sbk_wa2b")
    gaT = keep([16, S], BF16, "sbk_gaT")
    tri2 = keep([128, 128], F32, "sbk_tri2")
    onec = keep([128, 1], F32, "sbk_onec")
    epsc = keep([128, 1], F32, "sbk_epsc")

    P.begin_phase()
    wa2f = P.sb([16, 128], F32, "sb_wa2f")
    nbac = P.sb([128, 1], F32, "sb_nbac")
    rmask = P.sb([128, 512], F32, "sb_rmask")
    gq = [P.sb([128, 512], BF16, "sb_gq") for _ in range(2)]
    gk = [P.sb([128, 512], BF16, "sb_gk") for _ in range(2)]
    e1 = [P.sb([128, 512], F32, "sb_e1") for _ in range(2)]
    cs = [P.sb([128, 512], F32, "sb_cs") for _ in range(2)]
    eg = [P.sb([128, 512], F32, "sb_eg") for _ in range(2)]
    en = [P.sb([128, 512], F32, "sb_en") for _ in range(2)]
    pg = [P.ps([128, 512], F32, "ps_g") for _ in range(2)]
    P.load("sp", wa2f, wa2f[:, :], wa2_d[:, :])
    P.load("act", nbac, nbac[:, :], nbacol_d[:, :])
    P.load("sp", rmask, rmask[:, :], rmask_d[:, :])
    P.load("act", tri2, tri2[:, :], tri2_d[:, :])
    for q4 in range(4):
        P.load("sp" if q4 % 2 == 0 else "act", gaT, gaT[:, q4 * 2048:(q4 + 1) * 2048], gaT_d.t[:, q4 * 2048:(q4 + 1) * 2048])
    for q2 in range(2):
        P.load("pool", vtm, vtm[:, q2 * 32:(q2 + 1) * 32, :], gvtm_d.t[:, q2 * 32:(q2 + 1) * 32, :])
    P.op("dve", lambda e: e.tensor_copy(out=wa2b[:, :], in_=wa2f[:, :]), reads=[wa2f], writes=[wa2b])
    P.op("dve", lambda e: e.memset(onec[:, :], 1.0), writes=[onec])
    P.op("dve", lambda e: e.memset(epsc[:, :], 1e-6), writes=[epsc])
    for tc in range(16):
        sl = slice(tc * 512, (tc + 1) * 512)
        a, b_ = gq[tc % 2], gk[tc % 2]
        P.load("sp", a, a[:, :], gqT_d.t[:, sl])
        P.load("act", b_, b_[:, :], gkT_d.t[:, sl])
        ps = pg[tc % 2]
        x1, c1, g1, n1 = e1[tc % 2], cs[tc % 2], eg[tc % 2], en[tc % 2]
        P.mm(ps[:, :], wa2b[:, :], gaT[:, sl], True, True, reads=[wa2b, gaT], writes=[ps])
        P.op("act", lambda e, x1=x1, ps=ps: e.activation(out=x1[:, :], in_=ps[:, :], func=AF.Exp, scale=-1.0, bias=nbac[:, 0:1]),
             reads=[ps, nbac], writes=[x1])
        P.op("act", lambda e, x1=x1: e.activation(out=x1[:, :], in_=x1[:, :], func=AF.Ln, bias=onec[:, 0:1]), reads=[x1, onec], writes=[x1])
        P.op("dve", lambda e, x1=x1, c1=c1: e.tensor_tensor_scan(out=c1[:, :], data0=rmask[:, :], data1=x1[:, :], initial=0.0,
                                                               op0=ALU.mult, op1=ALU.add), reads=[rmask, x1], writes=[c1])
        P.op("act", lambda e, c1=c1, g1=g1: e.activation(out=g1[:, :], in_=c1[:, :], func=AF.Exp, scale=-1.0 / 16, bias=ZB[0][:, 0:1]),
             reads=[c1, ZB[0]], writes=[g1])
        P.op("act", lambda e, c1=c1, n1=n1: e.activation(out=n1[:, :], in_=c1[:, :], func=AF.Exp, scale=1.0 / 16, bias=ZB[0][:, 0:1]),
             reads=[c1, ZB[0]], writes=[n1])
        P.op("dve", lambda e, a=a, g1=g1, sl=sl: e.scalar_tensor_tensor(out=qtT[:, sl], in0=a[:, :], scalar=SCALE, in1=g1[:, :],
                                                                      op0=ALU.mult, op1=ALU.mult), reads=[a, g1], writes=[qtT])
        P.op("pool", lambda e, b_=b_, n1=n1, sl=sl: e.tensor_tensor(out=ktT[:, sl], in0=b_[:, :], in1=n1[:, :], op=ALU.mult),
             reads=[b_, n1], writes=[ktT])
        P.op("dve", lambda e, g1=g1, tc=tc: e.tensor_copy(out=dn[:, tc * 8:(tc + 1) * 8],
                                                        in_=g1.t.rearrange("p (n c) -> p n c", c=64)[:, :, 63]),
             reads=[g1], writes=[dn])
    P.end_phase()

    P.begin_phase()
    barow = P.sb([128, 128], F32, "sb_barow")
    gn = P.sb([128, 128], F32, "sb_gn")
    suf2 = P.sb([128, 128], F32, "sb_suf2")
    ktm = P.sb([128, 64, 128], BF16, "sb_ktm")
    Sst = P.sb([128, 128], F32, "sb_Sst")
    xg = [P.sb([128, 128], F32, "sb_xg") for _ in range(2)]
    fk = [P.sb([128, 128], F32, "sb_fk") for _ in range(2)]
    kh = [P.sb([128, 128], BF16, "sb_kh") for _ in range(2)]
    grt = [P.sb([128, 8, 128], F32, "sb_grt") for _ in range(2)]
    pl = [P.ps([128, 128], F32, "ps_l") for _ in range(2)]
    pf = [P.ps([128, 128], F32, "ps_f") for _ in range(2)]
    pU = [P.ps([128, 128], F32, "ps_U") for _ in range(4)]
    P.load("sp", barow, barow[:, :], barow_d[:, :])
    P.load("act", gn, gn[:, :], gn_d[:, :])
    P.load("sp", suf2, suf2[:, :], suf2_d[:, :])
    for q2 in range(2):
        P.load("pool", ktm, ktm[:, q2 * 32:(q2 + 1) * 32, :], gktm_d.t[:, q2 * 32:(q2 + 1) * 32, :])
    P.op("dve", lambda e: e.memset(Sst[:, :], 0.0), writes=[Sst])
    for g8 in range(8):
        gt = grt[g8 % 2]
        P.load("sp" if g8 % 2 == 0 else "act", gt, gt[:, :, :], grtm_d.t[:, g8 * 8:(g8 + 1) * 8, :])
        P.op("act", lambda e, gt=gt: e.activation(out=gt[:, :, :], in_=gt[:, :, :], func=AF.Silu), reads=[gt], writes=[gt])
        P.op("pool", lambda e, gt=gt, g8=g8: e.tensor_tensor(out=gs[:, g8 * 8:(g8 + 1) * 8, :], in0=gt[:, :, :],
                                                           in1=gn[:, :].unsqueeze(1).to_broadcast([128, 8, 128]), op=ALU.mult),
             reads=[gt, gn], writes=[gs])
    for blk in range(64):
        x, f, k2 = xg[blk % 2], fk[blk % 2], kh[blk % 2]
        p1, p2 = pl[blk % 2], pf[blk % 2]
        P.mm(p1[:, :], gaT[:, blk * 128:(blk + 1) * 128], wa2b[:, :], True, True, reads=[gaT, wa2b], writes=[p1])
        P.op("dve", lambda e, x=x, p1=p1: e.tensor_tensor(out=x[:, :], in0=p1[:, :], in1=barow[:, :], op=ALU.add),
             reads=[p1, barow], writes=[x])
        P.op("act", lambda e, x=x: e.activation(out=x[:, :], in_=x[:, :], func=AF.Exp, scale=-1.0, bias=ZB[0][:, 0:1]),
             reads=[x, ZB[0]], writes=[x])
        P.op("act", lambda e, x=x: e.activation(out=x[:, :], in_=x[:, :], func=AF.Ln, bias=onec[:, 0:1]), reads=[x, onec], writes=[x])
        P.mm(p2[:, :], suf2[:, :], x[:, :], True, True, reads=[suf2, x], writes=[p2])
        P.op("act", lambda e, f=f, p2=p2: e.activation(out=f[:, :], in_=p2[:, :], func=AF.Exp, scale=-1.0 / 16, bias=ZB[0][:, 0:1]),
             reads=[p2, ZB[0]], writes=[f])
        P.op("pool", lambda e, k2=k2, f=f, blk=blk: e.tensor_tensor(out=k2[:, :], in0=ktm[:, blk, :], in1=f[:, :], op=ALU.mult),
             reads=[ktm, f], writes=[k2])
        for hf in range(2):
            n = 2 * blk + hf
            pu = pU[n % 4]
            P.mm(pu[:, :], k2[hf * 64:(hf + 1) * 64, :], vtm[hf * 64:(hf + 1) * 64, blk, :], True, True, reads=[k2, vtm], writes=[pu])
            P.op("dve", lambda e, pu=pu, n=n: e.scalar_tensor_tensor(out=Sst[:, :], in0=Sst[:, :], scalar=dn[:, n:n + 1], in1=pu[:, :],
                                                                   op0=ALU.mult, op1=ALU.add), reads=[Sst, dn, pu], writes=[Sst])
            P.op("act", lambda e, n=n: e.activation(out=Sb[:, n, :], in_=Sst[:, :], func=AF.Copy), reads=[Sst], writes=[Sb])
    P.end_phase()

    P.begin_phase()
    ost = P.sb([128, 64, 128], BF16, "sb_gost")
    At = [P.sb([128, 128], BF16, "sb_At") for _ in range(2)]
    st = [P.sb([128, 4], F32, "sb_gst") for _ in range(2)]
    jk = P.sb([128, 128], F32, "sb_gjk")
    pA = [P.ps([128, 128], F32, "ps_A") for _ in range(2)]
    pO = [P.ps([128, 128], F32, "ps_GO") for _ in range(2)]
    for blk in range(64):
        sl = slice(blk * 128, (blk + 1) * 128)
        pa, po, at, s = pA[blk % 2], pO[blk % 2], At[blk % 2], st[blk % 2]
        P.mm(pa[:, :], ktT[:, sl], qtT[:, sl], True, True, reads=[ktT, qtT], writes=[pa])
        P.op("dve", lambda e, at=at, pa=pa: e.tensor_tensor(out=at[:, :], in0=pa[:, :], in1=tri2[:, :], op=ALU.mult),
             reads=[pa, tri2], writes=[at])
        P.mm(po[:, :], at[:, :], vtm[:, blk, :], True, False, reads=[at, vtm], writes=[po])
        if blk > 0:
            P.mm(po[0:64, :], qtT[:, blk * 128:blk * 128 + 64], Sb[:, 2 * blk - 1, :], False, False, reads=[qtT, Sb], writes=[po])
        P.mm(po[64:128, :], qtT[:, blk * 128 + 64:blk * 128 + 128], Sb[:, 2 * blk, :], False, True, reads=[qtT, Sb], writes=[po])
        P.op("act", lambda e, po=po, s=s: e.activation(out=jk[:, :], in_=po[:, :], func=AF.Square, accum_out=s[:, 0:1]),
             reads=[po], writes=[jk, s])
        P.op("act", lambda e, s=s: e.activation(out=s[:, 1:2], in_=s[:, 0:1], func=AF.Sqrt, scale=1.0 / 128, bias=epsc[:, 0:1]),
             reads=[s, epsc], writes=[s])
        P.op("dve", lambda e, s=s: e.reciprocal(out=s[:, 2:3], in_=s[:, 1:2]), reads=[s], writes=[s])
        P.op("dve", lambda e, po=po, s=s, blk=blk: e.scalar_tensor_tensor(out=ost[:, blk, :], in0=po[:, :], scalar=s[:, 2:3],
                                                                        in1=gs[:, blk, :], op0=ALU.mult, op1=ALU.mult),
             reads=[po, s, gs], writes=[ost])
    P.store("sp", ost, out_d.t.rearrange("b p e -> p b e"), ost[:, :, :])
    P.end_phase()
    P.scope_end()


ZB = [None]


def emit_zero(P):
    zb = P.sbp([128, 1], F32, "sbp_zero")
    ZB[0] = zb
    P.begin_phase()
    P.op("dve", lambda e: e.memset(zb[:, :], 0.0), writes=[zb])
    P.end_phase()


def gla_consts():
    s = np.arange(128)[:, None]
    t = np.arange(128)[None, :]
    same = (s // 64) == (t // 64)
    tri2 = (same & (s <= t)).astype(np.float32)
    suf2 = (same & (s > t)).astype(np.float32)
    rmask = np.ones((128, 512), np.float32)
    rmask[:, ::64] = 0.0
    return tri2, suf2, rmask


def tm_layout(a):
    return np.ascontiguousarray(a.reshape(64, 128, a.shape[1]).transpose(1, 0, 2))


def gla_inputs(hg, gq, gk, gv, gr, ga, wa2_l, ba_l, gng_l):
    sl = slice(hg * 128, (hg + 1) * 128)
    tri2, suf2, rmask = gla_consts()
    return dict(gqT=np.ascontiguousarray(gq[:, sl].T).astype(NPBF), gkT=np.ascontiguousarray(gk[:, sl].T).astype(NPBF),
                gktm=tm_layout(gk[:, sl]).astype(NPBF), gvtm=tm_layout(gv[:, sl]).astype(NPBF),
                grtm=tm_layout(gr[:, sl]).astype(np.float32), gaT=np.ascontiguousarray(ga.T).astype(NPBF),
                wa2=np.ascontiguousarray(wa2_l[:, sl]), nbacol=np.ascontiguousarray(-ba_l[sl][:, None]),
                barow=bc128(ba_l[sl]), gnb=bc128(gng_l), tri2=tri2, suf2=suf2, rmask=rmask)


def build_B():
    nc = bass.Bass("TRN2", target_bir_lowering=False)
    P = Prog(nc)
    Dm = {}
    for name, shp, dt in [("fkT", [768, S], BF16), ("fvg", [6, 128, NKB, 129], BF16), ("fqT", [768, 1024], BF16), ("ffp", [128, 6, 64], F32),
                          ("fbb", [128, 6], F32), ("tri", [128, 128], F32), ("sel", [128, 8], F32), ("cm", [128, 8, 128], BF16),
                          ("dkT", [768, S], BF16), ("dvg", [6, 128, NKB, 129], BF16), ("dqT", [768, 1024], BF16), ("iqT", [64, 16, 1024], BF16),
                          ("ikT", [64, S], BF16), ("iwp", [128, 8, 16], F32), ("am", [128, 8, 128], F32), ("ident", [128, 128], BF16),
                          ("gqT", [128, S], BF16), ("gkT", [128, S], BF16), ("gktm", [128, 64, 128], BF16), ("gvtm", [128, 64, 128], BF16),
                          ("grtm", [128, 64, 128], F32), ("gaT", [16, S], BF16), ("wa2", [16, 128], F32), ("nbacol", [128, 1], F32),
                          ("barow", [128, 128], F32), ("gnb", [128, 128], F32), ("tri2", [128, 128], F32), ("suf2", [128, 128], F32),
                          ("rmask", [128, 512], F32)]:
        Dm[name] = P.dram(name, shp, dt, "ExternalInput")
    foxo = P.dram("foxo", [8, 128, 768], BF16, "ExternalOutput")
    dsao = P.dram("dsao", [8, 128, 768], BF16, "ExternalOutput")
    glao = P.dram("glao", [64, 128, 128], BF16, "ExternalOutput")
    emit_zero(P)
    ident = P.sbp([128, 128], BF16, "sbp_ident")
    P.begin_phase()
    P.load("sp", ident, ident[:, :], Dm["ident"][:, :])
    P.end_phase()
    P.scope_begin()
    bias = P.sbs([128, 6, 8, 64], F32, "sbs_bias")
    cm = P.sbs([128, 8, 128], BF16, "sbs_cm")
    ostF = P.sbs([128, 8, 768], BF16, "sbs_ostF")
    P.begin_phase()
    P.load("sp", cm, cm[:, :, :], Dm["cm"][:, :, :])
    P.end_phase()
    emit_fox_bias(P, Dm["ffp"], Dm["fbb"], Dm["tri"], Dm["sel"], bias)
    emit_attn(P, Dm["fkT"], Dm["fvg"], Dm["fqT"], 6, ostF, 0, bias=bias, cm=cm)
    P.begin_phase()
    P.store("sp", ostF, foxo.t.rearrange("j p w -> p j w"), ostF[:, :, :])
    P.end_phase()
    P.scope_end()
    P.scope_begin()
    ostD = P.sbs([128, 8, 768], BF16, "sbs_ostD")
    maskT = [P.sbs([128, 8 * j + 8, 128], BF16, "sbs_mT") for j in range(8)]
    emit_dsa_select(P, Dm["iqT"], Dm["ikT"], Dm["iwp"], Dm["am"], ident, maskT)
    emit_attn(P, Dm["dkT"], Dm["dvg"], Dm["dqT"], 6, ostD, 0, maskT=maskT)
    P.begin_phase()
    P.store("sp", ostD, dsao.t.rearrange("j p w -> p j w"), ostD[:, :, :])
    P.end_phase()
    P.scope_end()
    emit_gla(P, Dm["gqT"], Dm["gkT"], Dm["gktm"], Dm["gvtm"], Dm["grtm"], Dm["gaT"], Dm["wa2"], Dm["nbacol"], Dm["barow"], Dm["gnb"],
             Dm["tri2"], Dm["suf2"], Dm["rmask"], glao)
    P.begin_phase()
    P.end_phase(final=True)
    P.finish()
    return nc


NE = 16384


def build_C1():
    nc = bass.Bass("TRN2", target_bir_lowering=False)
    P = Prog(nc)
    xs = P.dram("xs", [NB, 128, D], F32, "ExternalInput")
    mixT_d = P.dram("mixT", [D, NTOK], BF16, "ExternalInput")
    c_d = P.dram("c_pk", [128, 16], F32, "ExternalInput")
    adaw = P.dram("adaw", [D, 8192], F32, "ExternalInput")
    adab = P.dram("adab", [128, 8192], F32, "ExternalInput")
    g_d = P.dram("g_bc", [128, D], F32, "ExternalInput")
    ident_d = P.dram("ident", [128, 128], BF16, "ExternalInput")
    wo_d = P.dram("wo", [D, D], F32, "ExternalInput")
    wq_d = P.dram("wq", [D, D], F32, "ExternalInput")
    kT_d = P.dram("kT", [16, 128, 128], F32, "ExternalInput")
    xmid = P.dram("xmid", [NB, 128, D], F32, "ExternalOutput")
    h2T_d = P.dram("h2T", [16, 128, NTOK], BF16, "ExternalOutput")
    s12_d = P.dram("s12", [NB, 128, 16, 128], F32, "ExternalOutput")
    g2_d = P.dram("g2bc", [128, D], F32, "ExternalOutput")
    ident = emit_consts(P, ident_d)
    emit_C1(P, ident, xs, None, mixT_d, None, c_d, adaw, adab, g_d, wo_d, wq_d, kT_d, xmid, h2T_d, s12_d, g2_d)
    P.begin_phase()
    P.end_phase(final=True)
    P.finish()
    return nc


def emit_C1(P, ident, xs, xs_dram, mixT_d, mixT_sb, c_d, adaw, adab, g_d, wo_d, wq_d, kT_d, xmid, h2T_d, s12_d, g2_d):
    P.scope_begin()
    h2T = P.sbs([128, 16, NTOK], BF16, "sbs_h2T")
    P.scope_begin()
    modbc = P.sbs([128, 8192], F32, "sbs_mod2")
    emit_mod(P, c_d, adaw, adab, 8192, modbc)
    P.begin_phase()
    mixT = mixT_sb if mixT_sb is not None else P.sb([128, 16, NTOK], BF16, "sb_mixT")
    wf = P.sb([128, 16, 512], F32, "sb_wof")
    wb = [P.sb([128, 16, 512], BF16, "sb_wob") for _ in range(2)]
    xt = [P.sb([128, 512], F32, "sb_xt") for _ in range(3)]
    tm = [P.sb([128, 512], F32, "sb_tm") for _ in range(2)]
    pp = [P.ps([128, 512], F32, "ps_wo") for _ in range(3)]
    if mixT_sb is None:
        mv = mixT_d.t.rearrange("(k p) t -> p k t", p=128)
        for kh in range(2):
            P.load("sp" if kh == 0 else "act", mixT, mixT[:, kh * 8:(kh + 1) * 8, :], mv[:, kh * 8:(kh + 1) * 8, :])
    wv = wo_d.t.rearrange("(k p) n -> p k n", p=128)
    i = 0
    for dc in range(4):
        w16 = wb[dc % 2]
        for kh in range(2):
            P.load("sp" if kh == 0 else "act", wf, wf[:, kh * 8:(kh + 1) * 8, :], wv[:, kh * 8:(kh + 1) * 8, dc * 512:(dc + 1) * 512])
        P.op("dve", lambda e, w16=w16: e.tensor_copy(out=w16[:, 0:8, :], in_=wf[:, 0:8, :]), reads=[wf], writes=[w16])
        P.op("pool", lambda e, w16=w16: e.tensor_copy(out=w16[:, 8:16, :], in_=wf[:, 8:16, :]), reads=[wf], writes=[w16])
        for b in range(NB):
            ps = pp[i % 3]
            x = xt[i % 3]
            t = tm[i % 2]
            i += 1
            P.load("pool", x, x[:, :], xs[b, :, dc * 512:(dc + 1) * 512], dram=xs_dram)
            for k in range(16):
                P.mm(ps[:, :], mixT[:, k, b * 128:(b + 1) * 128], w16[:, k, :], k == 0, k == 15, reads=[mixT, w16], writes=[ps])
            P.op("dve", lambda e, t=t, ps=ps, dc=dc: e.tensor_tensor(out=t[:, :], in0=ps[:, :], in1=modbc[:, dc * 512:(dc + 1) * 512], op=ALU.mult),
                 reads=[ps, modbc], writes=[t])
            P.op("dve", lambda e, t=t, x=x: e.tensor_tensor(out=x[:, :], in0=x[:, :], in1=t[:, :], op=ALU.add), reads=[x, t], writes=[x])
            P.store("sp", x, xmid[b, :, dc * 512:(dc + 1) * 512], x[:, :], dram=xmid)
    P.end_phase()

    A2 = P.sbs([128, D], F32, "sbs_A2")
    P.begin_phase()
    gb = P.sb([128, D], F32, "sb_g")
    P.load("sp", gb, gb[:, :], g_d[:, :])
    P.op("dve", lambda e: e.scalar_tensor_tensor(out=A2[:, :], in0=modbc[:, 2 * D:3 * D], scalar=1.0, in1=gb[:, :],
                                                 op0=ALU.add, op1=ALU.mult), reads=[modbc, gb], writes=[A2])
    P.store("act", modbc, g2_d[:, :], modbc[:, 3 * D:4 * D], dram=g2_d)
    P.end_phase()
    emit_norm_T(P, xmid, A2, modbc_view(modbc, D, 2 * D), ident, h2T, xdram=xmid)
    P.scope_end()

    P.begin_phase()
    P.store("sp", h2T, h2T_d.t.rearrange("k p t -> p k t"), h2T[:, :, :], dram=h2T_d)
    qT = P.sb([128, 16, NTOK], BF16, "sb_qT")
    kf = P.sb([128, 16, 128], F32, "sb_kf")
    kb_ = P.sb([128, 16, 128], BF16, "sb_kb")
    wf = P.sb([128, 16, 512], F32, "sb_wqf")
    wb = [P.sb([128, 16, 512], BF16, "sb_wqb") for _ in range(2)]
    pp = [P.ps([128, 512], F32, "ps_q") for _ in range(3)]
    pq = [P.ps([128, 4, 128], F32, "ps_s") for _ in range(2)]
    sst = [P.sb([128, 16, 128], F32, "sb_sst") for _ in range(2)]
    P.load("pool", kf, kf[:, :, :], kT_d.t.rearrange("g d n -> d g n"))
    P.op("dve", lambda e: e.tensor_copy(out=kb_[:, :, :], in_=kf[:, :, :]), reads=[kf], writes=[kb_])
    wv = wq_d.t.rearrange("(k p) n -> p k n", p=128)
    i = 0
    for dc in range(4):
        w16 = wb[dc % 2]
        for kh in range(2):
            P.load("sp" if kh == 0 else "act", wf, wf[:, kh * 8:(kh + 1) * 8, :], wv[:, kh * 8:(kh + 1) * 8, dc * 512:(dc + 1) * 512])
        P.op("dve", lambda e, w16=w16: e.tensor_copy(out=w16[:, 0:8, :], in_=wf[:, 0:8, :]), reads=[wf], writes=[w16])
        P.op("pool", lambda e, w16=w16: e.tensor_copy(out=w16[:, 8:16, :], in_=wf[:, 8:16, :]), reads=[wf], writes=[w16])
        for g in range(4):
            gidx = dc * 4 + g
            for th in range(2):
                ps = pp[i % 3]
                i += 1
                for k in range(16):
                    P.mm(ps[:, :], w16[:, k, g * 128:(g + 1) * 128], h2T[:, k, th * 512:(th + 1) * 512], k == 0, k == 15,
                         reads=[w16, h2T], writes=[ps])
                if th == 0:
                    P.op("act", lambda e, ps=ps, gidx=gidx, th=th: e.activation(out=qT[:, gidx, th * 512:(th + 1) * 512], in_=ps[:, :], func=AF.Copy),
                         reads=[ps], writes=[qT])
                else:
                    P.op("dve", lambda e, ps=ps, gidx=gidx, th=th: e.tensor_copy(out=qT[:, gidx, th * 512:(th + 1) * 512], in_=ps[:, :]),
                         reads=[ps], writes=[qT])
    i = 0
    for b in range(NB):
        st = sst[b % 2]
        for g4 in range(4):
            ps = pq[i % 2]
            i += 1
            for q in range(4):
                gidx = g4 * 4 + q
                P.mm(ps[:, q, :], qT[:, gidx, b * 128:(b + 1) * 128], kb_[:, gidx, :], True, True, reads=[qT, kb_], writes=[ps])
            if g4 % 2 == 0:
                P.op("act", lambda e, ps=ps, st=st, g4=g4: e.activation(out=st[:, g4 * 4:(g4 + 1) * 4, :], in_=ps[:, :, :], func=AF.Copy),
                     reads=[ps], writes=[st])
            else:
                P.op("dve", lambda e, ps=ps, st=st, g4=g4: e.tensor_copy(out=st[:, g4 * 4:(g4 + 1) * 4, :], in_=ps[:, :, :]),
                     reads=[ps], writes=[st])
        P.store("pool", st, s12_d[b, :, :, :], st[:, :, :], dram=s12_d)
    P.end_phase()
    P.scope_end()


def host_C1_inputs(xs_list, mix_list, c, ada_w_l, ada_b_l, norm2_g_l, w_out_l, wq_l, k1_l, k2_l):
    kT = np.zeros((16, 128, 128), np.float32)
    for h in range(8):
        kT[2 * h] = k1_l[h].T
        kT[2 * h + 1] = k2_l[h].T
    common = dict(c_pk=np.ascontiguousarray(c.reshape(16, 128).T), adaw=np.ascontiguousarray(ada_w_l[:, 4096:12288]),
                  adab=bc128(ada_b_l[4096:12288]), g_bc=bc128(norm2_g_l), ident=np.eye(128, dtype=NPBF),
                  wo=np.ascontiguousarray(w_out_l), wq=np.ascontiguousarray(wq_l), kT=kT)
    return [dict(common, xs=xs_list[cc], mixT=np.ascontiguousarray(mix_list[cc].T)) for cc in range(8)]


def build_C2(final):
    nc = bass.Bass("TRN2", target_bir_lowering=False)
    P = Prog(nc)
    h2T_d = P.dram("h2T", [16, 128, NTOK], BF16, "ExternalInput")
    s12_d = P.dram("s12", [NB, 128, 16, 128], F32, "ExternalInput")
    xmid = P.dram("xmid", [NB, 128, D], F32, "ExternalInput")
    g2_d = P.dram("g2bc", [128, D], F32, "ExternalInput")
    uT_d = P.dram("uTt", [128, 128, 16, 128], F32, "ExternalInput")
    v_d = P.dram("v", [NE, D], F32, "ExternalInput")
    ident_d = P.dram("ident", [128, 128], BF16, "ExternalInput")
    fg_d = P.dram("fg_bc", [128, D], F32, "ExternalInput") if final else None
    xo = P.dram("xo", [NB, 128, D], F32, "ExternalOutput")
    ident = emit_consts(P, ident_d)
    emit_C2(P, ident, h2T_d, s12_d, xmid, g2_d, uT_d, v_d, fg_d, xo, final, True)
    P.finish()
    return nc


def emit_C2(P, ident, h2T_d, s12_d, xmid, g2_d, uT_d, v_d, fg_d, xo, final, last):
    P.scope_begin()
    kB_zero = P.sbs([128, 1], F32, "sbs_zero")
    g2 = P.sbs([128, D], F32, "sbs_g2")
    stats = P.sbs([128, 4, 8, 2], F32, "sbs_stats")
    P.begin_phase()
    P.load("sp", g2, g2[:, :], g2_d[:, :], dram=g2_d)
    P.op("dve", lambda e: e.memset(kB_zero[:, :], 0.0), writes=[kB_zero])
    P.end_phase()
    for ps_ in range(2):
        P.begin_phase()
        stt_ = [P.sb([128, 16, 128], F32, "sb_st") for _ in range(2)]
        v16 = [P.sb([128, 2, 16], F32, "sb_v16") for _ in range(2)]
        scr = [P.sb([128, 128], F32, "sb_scr") for _ in range(2)]
        cand = [P.sb([128, 16, 16], F32, "sb_cand") for _ in range(2)]
        scr2 = [P.sb([128, 256], F32, "sb_scr2") for _ in range(2)]
        ez = [P.sb([128, 256], F32, "sb_ez") for _ in range(2)]
        c8 = [P.sb([128, 16], F32, "sb_c8") for _ in range(2)]
        sm = [P.sb([128, 4], F32, "sb_sm") for _ in range(2)]
        i = 0
        for bl in range(4):
            b = ps_ * 4 + bl
            st = stt_[bl % 2]
            P.load("sp" if bl % 2 == 0 else "act", st, st[:, :, :], s12_d[b, :, :, :], dram=s12_d)
            for h in range(8):
                vv, sc, cd, s2_, ez_, c8_, sm_ = v16[i % 2], scr[i % 2], cand[i % 2], scr2[i % 2], ez[i % 2], c8[i % 2], sm[i % 2]
                i += 1
                for half in range(2):
                    src = st.t[:, 2 * h + half, :]
                    P.op("dve", lambda e, vv=vv, src=src, half=half: e.max(out=vv[:, half, 0:8], in_=src), reads=[st], writes=[vv])
                    P.op("dve", lambda e, vv=vv, src=src, half=half, sc=sc: e.match_replace(out=sc[:, :], in_to_replace=vv[:, half, 0:8],
                                                                                        in_values=src, imm_value=-1e30),
                         reads=[st, vv], writes=[sc])
                    P.op("dve", lambda e, vv=vv, half=half, sc=sc: e.max(out=vv[:, half, 8:16], in_=sc[:, :]), reads=[sc], writes=[vv])
                P.op("dve", lambda e, vv=vv, cd=cd: e.tensor_tensor(out=cd[:, :, :], in0=vv[:, 0, :].unsqueeze(2).to_broadcast([128, 16, 16]),
                                                                  in1=vv[:, 1, :].unsqueeze(1).to_broadcast([128, 16, 16]), op=ALU.add),
                     reads=[vv], writes=[cd])
                cf = cd.t.rearrange("p a b -> p (a b)")
                P.op("dve", lambda e, c8_=c8_, cf=cf: e.max(out=c8_[:, 0:8], in_=cf), reads=[cd], writes=[c8_])
                P.op("dve", lambda e, c8_=c8_, cf=cf, s2_=s2_: e.match_replace(out=s2_[:, :], in_to_replace=c8_[:, 0:8], in_values=cf, imm_value=-1e30),
                     reads=[cd, c8_], writes=[s2_])
                P.op("dve", lambda e, c8_=c8_, s2_=s2_: e.max(out=c8_[:, 8:16], in_=s2_[:, :]), reads=[s2_], writes=[c8_])
                P.op("dve", lambda e, c8_=c8_, sm_=sm_: e.tensor_scalar(out=sm_[:, 0:1], in0=c8_[:, 0:1], scalar1=-1.0, scalar2=None, op0=ALU.mult),
                     reads=[c8_], writes=[sm_])
                P.op("act", lambda e, ez_=ez_, cf=cf, sm_=sm_: e.activation(out=ez_[:, :], in_=cf, func=AF.Exp, bias=sm_[:, 0:1]),
                     reads=[cd, sm_], writes=[ez_])
                P.op("dve", lambda e, s2_=s2_, cf=cf, c8_=c8_, ez_=ez_, sm_=sm_: e.scalar_tensor_tensor(
                    out=s2_[:, :], in0=cf, scalar=c8_[:, 15:16], in1=ez_[:, :], op0=ALU.is_ge, op1=ALU.mult, accum_out=sm_[:, 1:2]),
                    reads=[cd, c8_, ez_], writes=[s2_, sm_])
                P.op("act", lambda e, sm_=sm_: e.activation(out=sm_[:, 2:3], in_=sm_[:, 1:2], func=AF.Ln, bias=kB_zero[:, 0:1]),
                     reads=[sm_, kB_zero], writes=[sm_])
                P.op("dve", lambda e, sm_=sm_, c8_=c8_, bl=bl, h=h: e.tensor_scalar(out=stats[:, bl, h, 1:2], in0=sm_[:, 2:3], scalar1=c8_[:, 0:1],
                                                                                  scalar2=-1.0, op0=ALU.add, op1=ALU.mult),
                     reads=[sm_, c8_], writes=[stats])
                P.op("dve", lambda e, c8_=c8_, bl=bl, h=h: e.tensor_copy(out=stats[:, bl, h, 0:1], in_=c8_[:, 15:16]), reads=[c8_], writes=[stats])
        P.end_phase()

        P.begin_phase()
        h2p = P.sb([128, 16, 512], BF16, "sb_h2p")
        oacc = P.sb([128, 4, D], F32, "sb_oacc")
        s12t = [P.sb([128, 16, 128], F32, "sb_s12t") for _ in range(2)]
        Xb = [P.sb([128, 4, 128], F32, "sb_X") for _ in range(2)]
        Eb = [P.sb([128, 4, 128], F32, "sb_E") for _ in range(2)]
        Tb = [P.sb([128, 8, 4, 128], BF16, "sb_T") for _ in range(2)]
        GT = [P.sb([128, 4, 512], BF16, "sb_GT") for _ in range(2)]
        GAT = [P.sb([128, 4, 512], BF16, "sb_GAT") for _ in range(2)]
        uf = [P.sb([128, 16, 128], F32, "sb_uf") for _ in range(2)]
        ub = [P.sb([128, 16, 128], BF16, "sb_ub") for _ in range(2)]
        vf = [P.sb([128, D], F32, "sb_vf") for _ in range(2)]
        vb = [P.sb([128, 4, D], BF16, "sb_vb") for _ in range(1)]
        ge = [P.sb([128, 512], F32, "sb_ge") for _ in range(2)]
        pG = [P.ps([128, 4, 128], F32, "ps_G") for _ in range(2)]
        pA = [P.ps([128, 512], F32, "ps_A") for _ in range(2)]
        pD = [P.ps([128, 512], F32, "ps_D") for _ in range(3)]
        hv = h2T_d.t.rearrange("k p t -> p k t")
        for kh in range(2):
            P.load("sp" if kh == 0 else "act", h2p, h2p[:, kh * 8:(kh + 1) * 8, :], hv[:, kh * 8:(kh + 1) * 8, ps_ * 512:(ps_ + 1) * 512], dram=h2T_d)
        si = 0
        xi = 0
        di = 0
        for g in range(32):
            gt, gat, vb_ = GT[g % 2], GAT[g % 2], vb[0]
            for bl in range(4):
                b = ps_ * 4 + bl
                st = s12t[si % 2]
                T = Tb[si % 2]
                pg = pG[si % 2]
                si += 1
                P.load("sp" if si % 2 == 0 else "act", st, st[:, :, :], s12_d[b, :, :, :], dram=s12_d)
                for h in range(8):
                    X, E = Xb[xi % 2], Eb[xi % 2]
                    xi += 1
                    P.op("dve", lambda e, X=X, st=st, h=h, g=g: e.tensor_tensor(
                        out=X[:, :, :], in0=st[:, 2 * h + 1, :].unsqueeze(1).to_broadcast([128, 4, 128]),
                        in1=st[:, 2 * h, 4 * g:4 * g + 4].unsqueeze(2).to_broadcast([128, 4, 128]), op=ALU.add), reads=[st], writes=[X])
                    P.op("act", lambda e, X=X, E=E, bl=bl, h=h: e.activation(out=E[:, :, :], in_=X[:, :, :], func=AF.Exp, bias=stats[:, bl, h, 1:2]),
                         reads=[X, stats], writes=[E])
                    P.op("dve", lambda e, X=X, E=E, T=T, bl=bl, h=h: e.scalar_tensor_tensor(
                        out=T[:, h, :, :], in0=X[:, :, :], scalar=stats[:, bl, h, 0:1], in1=E[:, :, :], op0=ALU.is_ge, op1=ALU.mult),
                        reads=[X, E, stats], writes=[T])
                for a in range(4):
                    for h in range(8):
                        P.mm(pg[:, a, :], T[:, h, a, :], ident[:, :], h == 0, h == 7, reads=[T, ident], writes=[pg])
                P.op("act", lambda e, gt=gt, pg=pg, bl=bl: e.activation(out=gt[:, :, bl * 128:(bl + 1) * 128], in_=pg[:, :, :], func=AF.Copy),
                     reads=[pg], writes=[gt])
            for a in range(4):
                ec = 4 * g + a
                u32, u16, v32, pa, gel = uf[ec % 2], ub[ec % 2], vf[ec % 2], pA[ec % 2], ge[ec % 2]
                P.load("sp", u32, u32[:, :, :], uT_d[ec, :, :, :])
                P.load("act", v32, v32[:, :], v_d[ec * 128:(ec + 1) * 128, :])
                P.op("pool", lambda e, u32=u32, u16=u16: e.tensor_copy(out=u16[:, :, :], in_=u32[:, :, :]), reads=[u32], writes=[u16])
                for k in range(16):
                    P.mm(pa[:, :], u16[:, k, :], h2p[:, k, :], k == 0, k == 15, reads=[u16, h2p], writes=[pa])
                P.op("act", lambda e, gel=gel, pa=pa: e.activation(out=gel[:, :], in_=pa[:, :], func=AF.Gelu_apprx_tanh), reads=[pa], writes=[gel])
                P.op("dve", lambda e, gat=gat, gel=gel, gt=gt, a=a: e.tensor_tensor(out=gat[:, a, :], in0=gel[:, :], in1=gt[:, a, :], op=ALU.mult),
                     reads=[gel, gt], writes=[gat])
                P.op("pool", lambda e, v32=v32, vb_=vb_, a=a: e.tensor_copy(out=vb_[:, a, 0:1024], in_=v32[:, 0:1024]), reads=[v32], writes=[vb_])
                P.op("act", lambda e, v32=v32, vb_=vb_, a=a: e.activation(out=vb_[:, a, 1024:2048], in_=v32[:, 1024:2048], func=AF.Copy),
                     reads=[v32], writes=[vb_])
            for bl in range(4):
                for dc in range(4):
                    pd = pD[di % 3]
                    di += 1
                    for a in range(4):
                        P.mm(pd[:, :], gat[:, a, bl * 128:(bl + 1) * 128], vb_[:, a, dc * 512:(dc + 1) * 512], a == 0, a == 3,
                             reads=[gat, vb_], writes=[pd])
                    if g == 0:
                        P.op("act", lambda e, pd=pd, bl=bl, dc=dc: e.activation(out=oacc[:, bl, dc * 512:(dc + 1) * 512], in_=pd[:, :], func=AF.Copy),
                             reads=[pd], writes=[oacc])
                    else:
                        P.op("dve", lambda e, pd=pd, bl=bl, dc=dc: e.tensor_tensor(out=oacc[:, bl, dc * 512:(dc + 1) * 512],
                                                                                 in0=oacc[:, bl, dc * 512:(dc + 1) * 512], in1=pd[:, :], op=ALU.add),
                             reads=[pd, oacc], writes=[oacc])
        xt = [P.sb([128, D], F32, "sb_xf") for _ in range(2)]
        jk = P.sb([128, D], BF16, "sb_jk")
        s4 = [P.sb([128, 4], F32, "sb_s4") for _ in range(2)]
        if final:
            fg = P.sb([128, D], F32, "sb_fg")
            P.load("sp", fg, fg[:, :], fg_d[:, :])
        for bl in range(4):
            b = ps_ * 4 + bl
            x = xt[bl % 2]
            s = s4[bl % 2]
            P.load("sp" if bl % 2 == 0 else "act", x, x[:, :], xmid[b, :, :], dram=xmid)
            P.op("dve", lambda e, bl=bl: e.tensor_tensor(out=oacc[:, bl, :], in0=oacc[:, bl, :], in1=g2[:, :], op=ALU.mult),
                 reads=[oacc, g2], writes=[oacc])
            P.op("pool", lambda e, x=x, bl=bl: e.tensor_tensor(out=x[:, :], in0=x[:, :], in1=oacc[:, bl, :], op=ALU.add),
                 reads=[x, oacc], writes=[x])
            if final:
                P.op("act", lambda e, x=x, s=s: e.activation(out=jk[:, :], in_=x[:, :], func=AF.Square, accum_out=s[:, 0:1]),
                     reads=[x], writes=[jk, s])
                P.op("act", lambda e, s=s: e.activation(out=s[:, 1:2], in_=s[:, 0:1], func=AF.Sqrt, scale=1.0 / D, bias=EPSB[0][:, 0:1]),
                     reads=[s, EPSB[0]], writes=[s])
                P.op("dve", lambda e, s=s: e.reciprocal(out=s[:, 2:3], in_=s[:, 1:2]), reads=[s], writes=[s])
                P.op("dve", lambda e, x=x, s=s: e.scalar_tensor_tensor(out=x[:, :], in0=x[:, :], scalar=s[:, 2:3], in1=fg[:, :],
                                                                     op0=ALU.mult, op1=ALU.mult), reads=[x, s, fg], writes=[x])
            P.store("pool", x, xo[b, :, :], x[:, :], dram=xo)
        P.end_phase(final=(last and ps_ == 1))
    P.scope_end()


def uT_tiles(u_l):
    return np.ascontiguousarray(u_l.reshape(128, 128, 16, 128).transpose(0, 3, 2, 1))


NFMR = NFM
RM = "(c r) t -> r c t"


def emit_A_f(P, xs_src, xdram, c_d, adaw, adab, g_d, wfm, wtm, ident, zfm_s, ztb_s, ztf_s):
    P.scope_begin()
    modbc = P.sbs([128, 4096], F32, "sbs_mod")
    A1 = P.sbs([128, D], F32, "sbs_A1")
    hT = P.sbs([128, 16, NTOK], BF16, "sbs_hT")
    emit_mod(P, c_d, adaw, adab, 4096, modbc)
    P.begin_phase()
    gb = P.sb([128, D], F32, "sb_g")
    P.load("sp", gb, gb[:, :], g_d[:, :])
    P.op("dve", lambda e: e.scalar_tensor_tensor(out=A1[:, :], in0=modbc[:, D:2 * D], scalar=1.0, in1=gb[:, :],
                                                 op0=ALU.add, op1=ALU.mult), reads=[modbc, gb], writes=[A1])
    P.end_phase()
    emit_norm_T(P, xs_src, A1, modbc_view(modbc, 0, D), ident, hT, xdram=xdram)
    emit_proj(P, hT, [dict(w=wfm, kind="fm", outs=[(zfm_s, 0, NFM, BF16)]),
                      dict(w=wtm, kind="tm", outs=[(ztb_s, 0, 2560, BF16), (ztf_s, 2560, 3200, F32)])])
    P.scope_end()


def emit_fox_bias_f(P, ztf_all, fb_d, tri_d, sel_d, bias):
    P.begin_phase()
    ff2 = P.sb([128, 64, 6], F32, "sb_ff2")
    fb = P.sb([128, 6], F32, "sb_fb")
    tri = P.sb([128, 128], F32, "sb_tri")
    ones = P.sb([128, 128], F32, "sb_ones")
    onec = P.sb([128, 1], F32, "sb_onec")
    sel = P.sb([128, 8], F32, "sb_sel")
    nlf = P.sb([128, 6, 64], F32, "sb_nlf")
    tot = P.sb([128, 6, 64], F32, "sb_tot")
    incl = P.sb([128, 6, 64], F32, "sb_incl")
    NF = P.sb([128, 6, 64], F32, "sb_NF")
    tmp = P.sb([128, 6, 8, 8], F32, "sb_tmp")
    nfe = P.sb([128, 6, 8], F32, "sb_nfe")
    pw = P.ps([128, 384], F32, "ps_w")
    pt_ = P.ps([128, 384], F32, "ps_t")
    src = ztf_all.t.rearrange("(c j p) w -> p j c w", c=8, j=8, p=128)
    ffv = ff2.t.rearrange("p (j c) h -> p j c h", c=8)
    for jh in range(8):
        P.load("sp" if jh % 2 == 0 else "act", ff2, ffv[:, jh, :, :], src[:, jh, :, 512:518], dram=ztf_all)
    P.load("act", fb, fb[:, :], fb_d[:, :])
    P.load("sp", tri, tri[:, :], tri_d[:, :])
    P.load("act", sel, sel[:, :], sel_d[:, :])
    P.op("dve", lambda e: e.memset(ones[:, :], 1.0), writes=[ones])
    P.op("dve", lambda e: e.memset(onec[:, :], 1.0), writes=[onec])
    P.op("dve", lambda e: e.tensor_tensor(out=nlf[:, :, :], in0=ff2.t.rearrange("p k h -> p h k"),
                                          in1=fb[:, :].unsqueeze(2).to_broadcast([128, 6, 64]), op=ALU.add), reads=[ff2, fb], writes=[nlf])
    nlf2 = nlf.t.rearrange("p h k -> p (h k)")
    P.op("act", lambda e: e.activation(out=nlf2, in_=nlf2, func=AF.Exp, scale=-1.0), reads=[nlf], writes=[nlf])
    P.op("act", lambda e: e.activation(out=nlf2, in_=nlf2, func=AF.Ln, bias=onec[:, 0:1]), reads=[nlf, onec], writes=[nlf])
    P.mm(pw[:, :], tri[:, :], nlf2, True, True, reads=[tri, nlf], writes=[pw])
    P.mm(pt_[:, :], ones[:, :], nlf2, True, True, reads=[ones, nlf], writes=[pt_])
    tot2 = tot.t.rearrange("p h k -> p (h k)")
    P.op("dve", lambda e: e.tensor_copy(out=tot2, in_=pt_[:, :]), reads=[pt_], writes=[tot])
    for h in range(6):
        P.op("dve", lambda e, h=h: e.tensor_tensor_scan(out=incl[:, h, :], data0=ones[:, 0:64], data1=tot[:, h, :], initial=0.0,
                                                        op0=ALU.mult, op1=ALU.add), reads=[ones, tot], writes=[incl])
    NF2 = NF.t.rearrange("p h k -> p (h k)")
    incl2 = incl.t.rearrange("p h k -> p (h k)")
    P.op("dve", lambda e: e.tensor_tensor(out=NF2, in0=pw[:, :], in1=incl2, op=ALU.add), reads=[pw, incl], writes=[NF])
    P.op("dve", lambda e: e.tensor_tensor(out=NF2, in0=NF2, in1=tot2, op=ALU.subtract), reads=[NF, tot], writes=[NF])
    P.op("dve", lambda e: e.tensor_tensor(out=tmp[:, :, :, :], in0=incl.t.rearrange("p h (j r) -> p h j r", r=8),
                                          in1=sel[:, :].unsqueeze(1).unsqueeze(1).to_broadcast([128, 6, 8, 8]), op=ALU.mult),
         reads=[incl, sel], writes=[tmp])
    P.op("dve", lambda e: e.tensor_reduce(out=nfe[:, :, :], in_=tmp[:, :, :, :], axis=AX.X, op=ALU.add), reads=[tmp], writes=[nfe])
    for h in range(6):
        for j in range(8):
            P.op("dve", lambda e, h=h, j=j: e.tensor_scalar(out=bias[:, h, j, :], in0=NF[:, h, :], scalar1=nfe[:, h, j:j + 1],
                                                            scalar2=0.0, op0=ALU.subtract, op1=ALU.min),
                 reads=[NF, nfe], writes=[bias])
    P.end_phase()


def emit_attn_f(P, zfm_all, krow0, ztb_all, vcol0, zfm_s, qrow0, nheads, out_stage, bias=None, maskT=None, cm=None):
    P.begin_phase()
    KT = [P.sb([128, S], BF16, "sb_KT") for _ in range(2)]
    Vg = [P.sb([128, NKB, 129], BF16, "sb_Vg") for _ in range(2)]
    QT = [P.sb([128, 1024], BF16, "sb_QT") for _ in range(2)]
    pO = [P.ps([128, 512], F32, "ps_O") for _ in range(2)]
    pS = []
    for _ in range(2):
        bank = P.ps([128, 4, 128], F32, "ps_S")
        for q in range(4):
            pS.append(Buf(bank.t[:, q, :], bank.name + "_q%d" % q))
    Pt = [P.sb([128, 128], BF16, "sb_Pt") for _ in range(6)]
    rc = [P.sb([128, 1], F32, "sb_rc") for _ in range(2)]
    zero_b = P.sb([128, 1], F32, "sb_zb")
    P.op("dve", lambda e: e.memset(zero_b[:, :], 0.0), writes=[zero_b])
    for vg in Vg:
        P.op("pool", lambda e, vg=vg: e.memset(vg[:, :, 128:129], 1.0), writes=[vg])
    kv = zfm_all.t.rearrange(RM, c=8)
    vv = ztb_all.t.rearrange("(m p) w -> p m w", p=128)
    si = pi = oi = 0
    for h in range(nheads):
        kt, vg, qt = KT[h % 2], Vg[h % 2], QT[h % 2]
        ktv = kt.t.rearrange("d (c t) -> d c t", c=8)
        for q4 in range(4):
            P.load("sp" if q4 % 2 == 0 else "act", kt, ktv[:, q4 * 2:(q4 + 1) * 2, :],
                   kv[krow0 + h * 128:krow0 + (h + 1) * 128, q4 * 2:(q4 + 1) * 2, :], dram=zfm_all)
        for q8 in range(8):
            P.load("sp" if q8 % 2 == 0 else "act", vg, vg[:, q8 * 8:(q8 + 1) * 8, 0:128],
                   vv[:, q8 * 8:(q8 + 1) * 8, vcol0 + h * 128:vcol0 + (h + 1) * 128], dram=ztb_all)
        P.load("sp", qt, qt[:, :], zfm_s.t[qrow0 + h * 128:qrow0 + (h + 1) * 128, :], dram=zfm_s)
        for j in range(8):
            blocks = [(cp * 8 + jp, 8 * jp + cp, (cp if jp == j else None), cp * (j + 1) + jp) for jp in range(j + 1) for cp in range(8)]
            nkb = len(blocks)
            po = pO[oi % 2]
            oi += 1
            for bi, (m, gb, r, q) in enumerate(blocks):
                ps = pS[si % 8]
                si += 1
                pt = Pt[pi % 6]
                pi += 1
                P.mm(ps[:, :], kt[:, m * 128:(m + 1) * 128], qt[:, j * 128:(j + 1) * 128], True, True, reads=[kt, qt], writes=[ps])
                if bias is not None:
                    P.op("act", lambda e, pt=pt, ps=ps, h=h, j=j, gb=gb: e.activation(
                        out=pt[:, :], in_=ps[:, :], func=AF.Exp, scale=SCALE, bias=bias[:, h, j, gb:gb + 1]),
                        reads=[ps, bias], writes=[pt])
                else:
                    P.op("act", lambda e, pt=pt, ps=ps: e.activation(
                        out=pt[:, :], in_=ps[:, :], func=AF.Exp, scale=SCALE, bias=zero_b[:, 0:1]),
                        reads=[ps, zero_b], writes=[pt])
                if maskT is not None:
                    mt = maskT[j]
                    P.op("dve", lambda e, pt=pt, mt=mt, q=q: e.tensor_tensor(out=pt[:, :], in0=pt[:, :], in1=mt[:, q, :], op=ALU.mult),
                         reads=[pt, mt], writes=[pt])
                elif r is not None:
                    P.op("dve", lambda e, pt=pt, r=r: e.tensor_tensor(out=pt[:, :], in0=pt[:, :], in1=cm[:, r, :], op=ALU.mult),
                         reads=[pt, cm], writes=[pt])
                P.mm(po[:, 0:129], pt[:, :], vg[:, m, :], bi == 0, bi == nkb - 1, reads=[pt, vg], writes=[po])
            r_ = rc[oi % 2]
            P.op("dve", lambda e, r_=r_, po=po: e.reciprocal(out=r_[:, 0:1], in_=po[:, 128:129]), reads=[po], writes=[r_])
            P.op("dve", lambda e, r_=r_, po=po, j=j, h=h: e.tensor_scalar(
                out=out_stage[:, j, h * 128:(h + 1) * 128], in0=po[:, 0:128], scalar1=r_[:, 0:1], scalar2=None,
                op0=ALU.mult), reads=[po, r_], writes=[out_stage])
    P.end_phase()


def emit_dsa_select_f(P, zfm_all, zfm_s, ztf_s, am_d, ident, maskT):
    P.begin_phase()
    score = P.sb([128, S], F32, "sb_score")
    junk = P.sb([128, S], BF16, "sb_junk")
    ikT = P.sb([64, S], BF16, "sb_ikT")
    iq = [P.sb([64, 16, 128], BF16, "sb_iq") for _ in range(2)]
    rr = [P.sb([128, 512], F32, "sb_rr") for _ in range(3)]
    am = P.sb([128, 8, 128], F32, "sb_am")
    iw = P.sb([128, 8, 16], F32, "sb_iw")
    wsc = P.sb([128, 8, 16], F32, "sb_wsc")
    pI = [P.ps([128, 512], F32, "ps_I") for _ in range(4)]
    pT = [P.ps([128, 8, 128], BF16, "ps_mT") for _ in range(2)]
    mch = [P.sb([128, 1024], BF16, "sb_mch") for _ in range(2)]
    sms = [P.sb([128, 8], F32, "sb_sm") for _ in range(2)]
    kv = zfm_all.t.rearrange(RM, c=8)
    ikv = ikT.t.rearrange("d (c t) -> d c t", c=8)
    for q4 in range(4):
        P.load("sp" if q4 % 2 == 0 else "act", ikT, ikv[:, q4 * 2:(q4 + 1) * 2, :], kv[R_IK:R_IK + 64, q4 * 2:(q4 + 1) * 2, :], dram=zfm_all)
    P.load("sp", am, am[:, :, :], am_d[:, :, :])
    P.load("act", iw, iw[:, :, :], ztf_s.t.rearrange("(j p) w -> p j w", p=128)[:, :, 518:534], dram=ztf_s)
    P.op("dve", lambda e: e.tensor_scalar(out=wsc[:, :, :], in0=iw[:, :, :], scalar1=(64 ** -0.5) * (16 ** -0.5), scalar2=None,
                                          op0=ALU.mult), reads=[iw], writes=[wsc])
    iqsrc = zfm_s.t[R_IQ:R_IQ + 1024, :].rearrange("(h d) t -> d h t", d=64)
    ii = ti = 0
    for j in range(8):
        W = (j + 1) * 128
        L = 8 * W
        iqj = iq[j % 2]
        P.load("sp", iqj, iqj[:, :, :], iqsrc[:, :, j * 128:(j + 1) * 128], dram=zfm_s)
        sm = sms[j % 2]
        for cp in range(8):
            for w0 in range(0, W, 512):
                ww = min(512, W - w0)
                sc = score.t[:, cp * W + w0:cp * W + w0 + ww]
                kcol = cp * 1024 + w0
                for h in range(16):
                    ps = pI[ii % 4]
                    r = rr[ii % 3]
                    ii += 1
                    P.mm(ps[:, 0:ww], iqj[:, h, :], ikT[:, kcol:kcol + ww], True, True, reads=[iqj, ikT], writes=[ps])
                    P.op("act", lambda e, r=r, ps=ps, ww=ww: e.activation(out=r[:, 0:ww], in_=ps[:, 0:ww], func=AF.Relu), reads=[ps], writes=[r])
                    if h == 0:
                        P.op("dve", lambda e, sc=sc, r=r, j=j, ww=ww: e.tensor_scalar(out=sc, in0=r[:, 0:ww], scalar1=wsc[:, j, 0:1], scalar2=None,
                                                                                    op0=ALU.mult), reads=[r, wsc], writes=[score])
                    else:
                        P.op("dve", lambda e, sc=sc, r=r, j=j, h=h, ww=ww: e.scalar_tensor_tensor(
                            out=sc, in0=r[:, 0:ww], scalar=wsc[:, j, h:h + 1], in1=sc, op0=ALU.mult, op1=ALU.add),
                            reads=[r, wsc, score], writes=[score])
        sL = score.t[:, 0:L]
        P.op("dve", lambda e, sm=sm, sL=sL: e.tensor_reduce(out=sm[:, 0:1], in_=sL, axis=AX.X, op=ALU.max, apply_absolute_value=True),
             reads=[score], writes=[sm])
        P.op("dve", lambda e, sm=sm: e.tensor_scalar(out=sm[:, 0:1], in0=sm[:, 0:1], scalar1=1.001, scalar2=1e-3, op0=ALU.mult, op1=ALU.add),
             reads=[sm], writes=[sm])
        P.op("dve", lambda e, sm=sm: e.tensor_scalar(out=sm[:, 1:2], in0=sm[:, 0:1], scalar1=-1.0, scalar2=None, op0=ALU.mult),
             reads=[sm], writes=[sm])
        sB = score.t[:, 0:L].rearrange("p (c w) -> p c w", c=8)[:, :, j * 128:(j + 1) * 128]
        P.op("dve", lambda e, sB=sB: e.tensor_tensor(out=sB, in0=sB, in1=am[:, :, :], op=ALU.add), reads=[score, am], writes=[score])
        for k in range(1, NBIS + 1):
            f = 2.0 ** (1 - k)
            P.op("dve", lambda e, sm=sm, f=f: e.tensor_scalar(out=sm[:, 2:3], in0=sm[:, 0:1], scalar1=f, scalar2=sm[:, 1:2],
                                                            op0=ALU.mult, op1=ALU.add), reads=[sm], writes=[sm])
            P.op("dve", lambda e, sm=sm, sL=sL, L=L: e.tensor_scalar(out=junk[:, 0:L], in0=sL, scalar1=sm[:, 2:3], scalar2=0.0,
                                                                   op0=ALU.is_ge, op1=ALU.add, accum_out=sm[:, 3:4]),
                 reads=[score, sm], writes=[junk, sm])
            P.op("dve", lambda e, sm=sm, f=f: e.tensor_scalar(out=sm[:, 4:5], in0=sm[:, 3:4], scalar1=TOPK - 0.5, scalar2=f,
                                                            op0=ALU.is_ge, op1=ALU.mult), reads=[sm], writes=[sm])
            P.op("dve", lambda e, sm=sm: e.scalar_tensor_tensor(out=sm[:, 1:2], in0=sm[:, 4:5], scalar=sm[:, 0:1], in1=sm[:, 1:2],
                                                              op0=ALU.mult, op1=ALU.add), reads=[sm], writes=[sm])
        for g in range(L // 1024):
            mc = mch[ti % 2]
            pt = pT[ti % 2]
            ti += 1
            P.op("dve", lambda e, mc=mc, g=g, sm=sm: e.tensor_scalar(out=mc[:, :], in0=score[:, g * 1024:(g + 1) * 1024],
                                                                   scalar1=sm[:, 1:2], scalar2=None, op0=ALU.is_ge),
                 reads=[score, sm], writes=[mc])
            for q in range(8):
                P.op("pe", lambda e, pt=pt, mc=mc, q=q: e.transpose(out=pt[:, q, :], in_=mc[:, q * 128:(q + 1) * 128], identity=ident[:, :]),
                     reads=[mc, ident], writes=[pt])
            mt = maskT[j]
            P.op("act", lambda e, mt=mt, pt=pt, g=g: e.activation(out=mt[:, g * 8:(g + 1) * 8, :], in_=pt[:, :, :], func=AF.Copy),
                 reads=[pt], writes=[mt])
    P.end_phase()


def emit_gla_f(P, zfm_all, ztb_all, ztf_all, sel4_d, wa2_d, nbacol_d, barow_d, gn_d, tri2_d, suf2_d, rmask_d, gla_s):
    P.scope_begin()
    keep = P.sbs
    qtT = keep([128, S], BF16, "sbk_qtT")
    ktT = keep([128, S], BF16, "sbk_ktT")
    dn = keep([128, 128], F32, "sbk_dn")
    Sb = keep([128, 128, 128], BF16, "sbk_Sb")
    vtm = keep([128, 64, 128], BF16, "sbk_v")
    gs = keep([128, 64, 128], BF16, "sbk_gs")
    wa2b = keep([16, 128], BF16, "sbk_wa2b")
    gaT = keep([16, S], BF16, "sbk_gaT")
    tri2 = keep([128, 128], F32, "sbk_tri2")
    onec = keep([128, 1], F32, "sbk_onec")
    epsc = keep([128, 1], F32, "sbk_epsc")
    sel4 = keep([128, 4], F32, "sbk_sel4")
    kv = zfm_all.t.rearrange(RM, c=8)

    P.begin_phase()
    wa2f = P.sb([16, 128], F32, "sb_wa2f")
    nbac = P.sb([128, 1], F32, "sb_nbac")
    rmask = P.sb([128, 512], F32, "sb_rmask")
    gq4 = [P.sb([128, 4, 512], BF16, "sb_gq4") for _ in range(2)]
    gk4 = [P.sb([128, 4, 512], BF16, "sb_gk4") for _ in range(2)]
    gq = [P.sb([128, 512], F32, "sb_gq") for _ in range(2)]
    gk = [P.sb([128, 512], F32, "sb_gk") for _ in range(2)]
    e1 = [P.sb([128, 512], F32, "sb_e1") for _ in range(2)]
    cs = [P.sb([128, 512], F32, "sb_cs") for _ in range(2)]
    eg = [P.sb([128, 512], F32, "sb_eg") for _ in range(2)]
    en = [P.sb([128, 512], F32, "sb_en") for _ in range(2)]
    pg = [P.ps([128, 512], F32, "ps_g") for _ in range(2)]
    P.load("sp", wa2f, wa2f[:, :], wa2_d[:, :])
    P.load("act", nbac, nbac[:, :], nbacol_d[:, :])
    P.load("sp", rmask, rmask[:, :], rmask_d[:, :])
    P.load("act", tri2, tri2[:, :], tri2_d[:, :])
    P.load("sp", sel4, sel4[:, :], sel4_d[:, :])
    gav = gaT.t.rearrange("r (j c p) -> r j c p", j=8, c=8)
    gas = kv[R_GA:R_GA + 16, :, :].rearrange("r c (j p) -> r j c p", p=128)
    for jh in range(8):
        P.load("sp" if jh % 2 == 0 else "act", gaT, gav[:, jh, :, :], gas[:, jh, :, :], dram=zfm_all)
    P.op("dve", lambda e: e.tensor_copy(out=wa2b[:, :], in_=wa2f[:, :]), reads=[wa2f], writes=[wa2b])
    P.op("dve", lambda e: e.memset(onec[:, :], 1.0), writes=[onec])
    P.op("dve", lambda e: e.memset(epsc[:, :], 1e-6), writes=[epsc])

    def sel_acc(eng, out_ap, src4, nh_view, outbuf, srcbuf):
        for h in range(4):
            if h == 0:
                P.op(eng, lambda e, h=h: e.tensor_scalar(out=out_ap, in0=nh_view(h), scalar1=sel4[:, 0:1], scalar2=None, op0=ALU.mult),
                     reads=[srcbuf, sel4], writes=[outbuf])
            else:
                P.op("dve", lambda e, h=h: e.scalar_tensor_tensor(out=out_ap, in0=nh_view(h), scalar=sel4[:, h:h + 1], in1=out_ap,
                                                                  op0=ALU.mult, op1=ALU.add), reads=[srcbuf, sel4, outbuf], writes=[outbuf])

    for tc in range(16):
        sl = slice(tc * 512, (tc + 1) * 512)
        j_ = tc // 2
        c0 = (tc % 2) * 4
        a4, b4 = gq4[tc % 2], gk4[tc % 2]
        a, b_ = gq[tc % 2], gk[tc % 2]
        for (dst4, row0, q) in ((a4, R_GQ, "sp"), (b4, R_GK, "act")):
            for hh in range(4):
                srcv = kv[row0 + hh * 128:row0 + (hh + 1) * 128, c0:c0 + 4, j_ * 128:(j_ + 1) * 128]
                P.load(q, dst4, dst4.t[:, hh, :].rearrange("d (c p) -> d c p", p=128), srcv, dram=zfm_all)
        sel_acc("dve", a[:, :], a4, lambda h, a4=a4: a4[:, h, :], a, a4)
        sel_acc("dve", b_[:, :], b4, lambda h, b4=b4: b4[:, h, :], b_, b4)
        ps = pg[tc % 2]
        x1, c1, g1, n1 = e1[tc % 2], cs[tc % 2], eg[tc % 2], en[tc % 2]
        P.mm(ps[:, :], wa2b[:, :], gaT[:, sl], True, True, reads=[wa2b, gaT], writes=[ps])
        P.op("act", lambda e, x1=x1, ps=ps: e.activation(out=x1[:, :], in_=ps[:, :], func=AF.Exp, scale=-1.0, bias=nbac[:, 0:1]),
             reads=[ps, nbac], writes=[x1])
        P.op("act", lambda e, x1=x1: e.activation(out=x1[:, :], in_=x1[:, :], func=AF.Ln, bias=onec[:, 0:1]), reads=[x1, onec], writes=[x1])
        P.op("dve", lambda e, x1=x1, c1=c1: e.tensor_tensor_scan(out=c1[:, :], data0=rmask[:, :], data1=x1[:, :], initial=0.0,
                                                               op0=ALU.mult, op1=ALU.add), reads=[rmask, x1], writes=[c1])
        P.op("act", lambda e, c1=c1, g1=g1: e.activation(out=g1[:, :], in_=c1[:, :], func=AF.Exp, scale=-1.0 / 16, bias=ZB[0][:, 0:1]),
             reads=[c1, ZB[0]], writes=[g1])
        P.op("act", lambda e, c1=c1, n1=n1: e.activation(out=n1[:, :], in_=c1[:, :], func=AF.Exp, scale=1.0 / 16, bias=ZB[0][:, 0:1]),
             reads=[c1, ZB[0]], writes=[n1])
        P.op("dve", lambda e, a=a, g1=g1, sl=sl: e.scalar_tensor_tensor(out=qtT[:, sl], in0=a[:, :], scalar=SCALE, in1=g1[:, :],
                                                                      op0=ALU.mult, op1=ALU.mult), reads=[a, g1], writes=[qtT])
        P.op("pool", lambda e, b_=b_, n1=n1, sl=sl: e.tensor_tensor(out=ktT[:, sl], in0=b_[:, :], in1=n1[:, :], op=ALU.mult),
             reads=[b_, n1], writes=[ktT])
        P.op("dve", lambda e, g1=g1, tc=tc: e.tensor_copy(out=dn[:, tc * 8:(tc + 1) * 8],
                                                        in_=g1.t.rearrange("p (n c) -> p n c", c=64)[:, :, 63]),
             reads=[g1], writes=[dn])
    P.end_phase()

    P.begin_phase()
    barow = P.sb([128, 128], F32, "sb_barow")
    gn = P.sb([128, 128], F32, "sb_gn")
    suf2 = P.sb([128, 128], F32, "sb_suf2")
    ktm = P.sb([128, 64, 128], BF16, "sb_ktm")
    Sst = P.sb([128, 128], F32, "sb_Sst")
    xg = [P.sb([128, 128], F32, "sb_xg") for _ in range(2)]
    fk = [P.sb([128, 128], F32, "sb_fk") for _ in range(2)]
    kh = [P.sb([128, 128], BF16, "sb_kh") for _ in range(2)]
    t4 = [P.sb([128, 4, 1024], BF16, "sb_t4") for _ in range(2)]
    g4 = [P.sb([128, 4, 512], F32, "sb_g4") for _ in range(2)]
    grt = [P.sb([128, 4, 128], F32, "sb_grt") for _ in range(2)]
    pl = [P.ps([128, 128], F32, "ps_l") for _ in range(2)]
    pf = [P.ps([128, 128], F32, "ps_f") for _ in range(2)]
    pU = [P.ps([128, 128], F32, "ps_U") for _ in range(4)]
    P.load("sp", barow, barow[:, :], barow_d[:, :])
    P.load("act", gn, gn[:, :], gn_d[:, :])
    P.load("sp", suf2, suf2[:, :], suf2_d[:, :])
    P.op("dve", lambda e: e.memset(Sst[:, :], 0.0), writes=[Sst])
    tbv = ztb_all.t.rearrange("(c j p) w -> p j c w", c=8, j=8, p=128)
    tfv = ztf_all.t.rearrange("(c j p) w -> p j c w", c=8, j=8, p=128)
    for jc in range(16):
        j_, ch_ = jc // 2, jc % 2
        tt, gg, gt = t4[jc % 2], g4[jc % 2], grt[jc % 2]
        P.load("sp", tt, tt[:, :, :], tbv[:, j_, ch_ * 4:(ch_ + 1) * 4, C_GK:C_GK + 1024], dram=ztb_all)
        P.load("act", gg, gg[:, :, :], tfv[:, j_, ch_ * 4:(ch_ + 1) * 4, 0:512], dram=ztf_all)
        bs = slice(j_ * 8 + ch_ * 4, j_ * 8 + ch_ * 4 + 4)
        sel_acc("dve", ktm[:, bs, :], tt, lambda h, tt=tt: tt[:, :, h * 128:(h + 1) * 128], ktm, tt)
        sel_acc("dve", vtm[:, bs, :], tt, lambda h, tt=tt: tt[:, :, 512 + h * 128:512 + (h + 1) * 128], vtm, tt)
        sel_acc("dve", gt[:, :, :], gg, lambda h, gg=gg: gg[:, :, h * 128:(h + 1) * 128], gt, gg)
        P.op("act", lambda e, gt=gt: e.activation(out=gt[:, :, :], in_=gt[:, :, :], func=AF.Silu), reads=[gt], writes=[gt])
        P.op("pool", lambda e, gt=gt, bs=bs: e.tensor_tensor(out=gs[:, bs, :], in0=gt[:, :, :],
                                                           in1=gn[:, :].unsqueeze(1).to_broadcast([128, 4, 128]), op=ALU.mult),
             reads=[gt, gn], writes=[gs])
    for blk in range(64):
        x, f, k2 = xg[blk % 2], fk[blk % 2], kh[blk % 2]
        p1, p2 = pl[blk % 2], pf[blk % 2]
        P.mm(p1[:, :], gaT[:, blk * 128:(blk + 1) * 128], wa2b[:, :], True, True, reads=[gaT, wa2b], writes=[p1])
        P.op("dve", lambda e, x=x, p1=p1: e.tensor_tensor(out=x[:, :], in0=p1[:, :], in1=barow[:, :], op=ALU.add),
             reads=[p1, barow], writes=[x])
        P.op("act", lambda e, x=x: e.activation(out=x[:, :], in_=x[:, :], func=AF.Exp, scale=-1.0, bias=ZB[0][:, 0:1]),
             reads=[x, ZB[0]], writes=[x])
        P.op("act", lambda e, x=x: e.activation(out=x[:, :], in_=x[:, :], func=AF.Ln, bias=onec[:, 0:1]), reads=[x, onec], writes=[x])
        P.mm(p2[:, :], suf2[:, :], x[:, :], True, True, reads=[suf2, x], writes=[p2])
        P.op("act", lambda e, f=f, p2=p2: e.activation(out=f[:, :], in_=p2[:, :], func=AF.Exp, scale=-1.0 / 16, bias=ZB[0][:, 0:1]),
             reads=[p2, ZB[0]], writes=[f])
        P.op("pool", lambda e, k2=k2, f=f, blk=blk: e.tensor_tensor(out=k2[:, :], in0=ktm[:, blk, :], in1=f[:, :], op=ALU.mult),
             reads=[ktm, f], writes=[k2])
        for hf in range(2):
            n = 2 * blk + hf
            pu = pU[n % 4]
            P.mm(pu[:, :], k2[hf * 64:(hf + 1) * 64, :], vtm[hf * 64:(hf + 1) * 64, blk, :], True, True, reads=[k2, vtm], writes=[pu])
            P.op("dve", lambda e, pu=pu, n=n: e.scalar_tensor_tensor(out=Sst[:, :], in0=Sst[:, :], scalar=dn[:, n:n + 1], in1=pu[:, :],
                                                                   op0=ALU.mult, op1=ALU.add), reads=[Sst, dn, pu], writes=[Sst])
            P.op("act", lambda e, n=n: e.activation(out=Sb[:, n, :], in_=Sst[:, :], func=AF.Copy), reads=[Sst], writes=[Sb])
    P.end_phase()

    P.begin_phase()
    ost = P.sb([128, 64, 128], BF16, "sb_gost")
    At = [P.sb([128, 128], BF16, "sb_At") for _ in range(2)]
    st = [P.sb([128, 4], F32, "sb_gst") for _ in range(2)]
    jk = P.sb([128, 128], F32, "sb_gjk")
    pA = [P.ps([128, 128], F32, "ps_A") for _ in range(2)]
    pO = [P.ps([128, 128], F32, "ps_GO") for _ in range(2)]
    for blk in range(64):
        sl = slice(blk * 128, (blk + 1) * 128)
        pa, po, at, s = pA[blk % 2], pO[blk % 2], At[blk % 2], st[blk % 2]
        P.mm(pa[:, :], ktT[:, sl], qtT[:, sl], True, True, reads=[ktT, qtT], writes=[pa])
        P.op("dve", lambda e, at=at, pa=pa: e.tensor_tensor(out=at[:, :], in0=pa[:, :], in1=tri2[:, :], op=ALU.mult),
             reads=[pa, tri2], writes=[at])
        P.mm(po[:, :], at[:, :], vtm[:, blk, :], True, False, reads=[at, vtm], writes=[po])
        if blk > 0:
            P.mm(po[0:64, :], qtT[:, blk * 128:blk * 128 + 64], Sb[:, 2 * blk - 1, :], False, False, reads=[qtT, Sb], writes=[po])
        P.mm(po[64:128, :], qtT[:, blk * 128 + 64:blk * 128 + 128], Sb[:, 2 * blk, :], False, True, reads=[qtT, Sb], writes=[po])
        P.op("act", lambda e, po=po, s=s: e.activation(out=jk[:, :], in_=po[:, :], func=AF.Square, accum_out=s[:, 0:1]),
             reads=[po], writes=[jk, s])
        P.op("act", lambda e, s=s: e.activation(out=s[:, 1:2], in_=s[:, 0:1], func=AF.Sqrt, scale=1.0 / 128, bias=epsc[:, 0:1]),
             reads=[s, epsc], writes=[s])
        P.op("dve", lambda e, s=s: e.reciprocal(out=s[:, 2:3], in_=s[:, 1:2]), reads=[s], writes=[s])
        P.op("dve", lambda e, po=po, s=s, blk=blk: e.scalar_tensor_tensor(out=ost[:, blk, :], in0=po[:, :], scalar=s[:, 2:3],
                                                                        in1=gs[:, blk, :], op0=ALU.mult, op1=ALU.mult),
             reads=[po, s, gs], writes=[ost])
    P.store("sp", ost, gla_s.t.rearrange("(b p) e -> p b e", p=128), ost[:, :, :], dram=gla_s)
    P.end_phase()
    P.scope_end()


def emit_mixT(P, foxo, dsao, gla_all, sel_d, ident, mixT):
    P.begin_phase()
    sel = P.sb([128, 8], F32, "sb_sel8")
    gch = [P.sb([128, 4, 8, 128], BF16, "sb_gch") for _ in range(2)]
    mg = [P.sb([128, 4, 128], BF16, "sb_mg") for _ in range(2)]
    fo = [P.sb([128, 768], BF16, "sb_fo") for _ in range(2)]
    do = [P.sb([128, 768], BF16, "sb_do") for _ in range(2)]
    pT = [P.ps([128, 8, 128], BF16, "ps_xT") for _ in range(2)]
    P.load("sp", sel, sel[:, :], sel_d[:, :])
    gv_ = gla_all.t.rearrange("(r j c p) e -> p j r c e", r=8, j=8, c=8, p=128)
    for j in range(8):
        ch = gch[j % 2]
        m = mg[j % 2]
        ostF, ostD = fo[j % 2], do[j % 2]
        P.load("sp", ostF, ostF[:, :], foxo[j, :, :], dram=foxo)
        P.load("act", ostD, ostD[:, :], dsao[j, :, :], dram=dsao)
        for hh in range(4):
            P.load("sp" if hh % 2 == 0 else "act", ch, ch[:, hh, :, :], gv_[:, j, hh, :, :], dram=gla_all)
        for cp in range(8):
            if cp == 0:
                P.op("dve", lambda e, m=m, ch=ch: e.tensor_scalar(out=m[:, :, :], in0=ch[:, :, 0, :], scalar1=sel[:, 0:1], scalar2=None, op0=ALU.mult),
                     reads=[ch, sel], writes=[m])
            else:
                P.op("dve", lambda e, m=m, ch=ch, cp=cp: e.scalar_tensor_tensor(out=m[:, :, :], in0=ch[:, :, cp, :], scalar=sel[:, cp:cp + 1],
                                                                              in1=m[:, :, :], op0=ALU.mult, op1=ALU.add),
                     reads=[ch, sel, m], writes=[m])
        for half in range(2):
            pt = pT[half]
            for kk in range(8):
                k = half * 8 + kk
                if k < 6:
                    src, sb_ = ostF[:, k * 128:(k + 1) * 128], ostF
                elif k < 10:
                    src, sb_ = m[:, k - 6, :], m
                else:
                    src, sb_ = ostD[:, (k - 10) * 128:(k - 9) * 128], ostD
                P.op("pe", lambda e, pt=pt, kk=kk, src=src: e.transpose(out=pt[:, kk, :], in_=src, identity=ident[:, :]),
                     reads=[sb_, ident], writes=[pt])
            P.op("act", lambda e, pt=pt, half=half, j=j: e.activation(
                out=mixT[:, half * 8:(half + 1) * 8, j * 128:(j + 1) * 128], in_=pt[:, :, :], func=AF.Copy), reads=[pt], writes=[mixT])
    P.end_phase()


def build_fused():
    nc = bass.Bass("TRN2", target_bir_lowering=False)
    P = Prog(nc)
    IN = {}

    def inp(name, shp, dt):
        IN[name] = P.dram(name, shp, dt, "ExternalInput")
        return IN[name]
    xs = inp("xs", [NB, 128, D], F32)
    c_d = inp("c_pk", [128, 16], F32)
    ident_d = inp("ident", [128, 128], BF16)
    tri_d = inp("tri", [128, 128], F32)
    sel_d = inp("sel", [128, 8], F32)
    sel4_d = inp("sel4", [128, 4], F32)
    cm_d = inp("cm", [128, 8, 128], BF16)
    am_d = inp("am", [128, 8, 128], F32)
    tri2_d = inp("tri2", [128, 128], F32)
    suf2_d = inp("suf2", [128, 128], F32)
    rmask_d = inp("rmask", [128, 512], F32)
    fg_d = inp("fg_bc", [128, D], F32)
    L = []
    for l in range(2):
        s_ = "_%d" % l
        L.append(dict(
            adawA=inp("adawA" + s_, [D, 4096], F32), adabA=inp("adabA" + s_, [128, 4096], F32), g1=inp("g1" + s_, [128, D], F32),
            wfm=inp("wfm" + s_, [D, NFM], F32), wtm=inp("wtm" + s_, [D, NTM], F32), fbb=inp("fbb" + s_, [128, 6], F32),
            wa2=inp("wa2" + s_, [16, 128], F32), nbacol=inp("nbacol" + s_, [128, 1], F32), barow=inp("barow" + s_, [128, 128], F32),
            gnb=inp("gnb" + s_, [128, 128], F32), adawC=inp("adawC" + s_, [D, 8192], F32), adabC=inp("adabC" + s_, [128, 8192], F32),
            g2n=inp("g2n" + s_, [128, D], F32), wo=inp("wo" + s_, [D, D], F32), wq=inp("wq" + s_, [D, D], F32),
            kT=inp("kT" + s_, [16, 128, 128], F32), uTt=inp("uTt" + s_, [128, 128, 16, 128], F32), v=inp("v" + s_, [NE, D], F32),
            zfm_s=P.scratch("zfm_s" + s_, [NFM, NTOK], BF16), zfm_all=P.scratch("zfm_all" + s_, [8 * NFM, NTOK], BF16),
            ztb_s=P.scratch("ztb_s" + s_, [NTOK, 2560], BF16), ztb_all=P.scratch("ztb_all" + s_, [8 * NTOK, 2560], BF16),
            ztf_s=P.scratch("ztf_s" + s_, [NTOK, 640], F32), ztf_all=P.scratch("ztf_all" + s_, [8 * NTOK, 640], F32),
            gla_s=P.scratch("gla_s" + s_, [S, 128], BF16), gla_all=P.scratch("gla_all" + s_, [8 * S, 128], BF16),
            xmid=P.scratch("xmid" + s_, [NB, 128, D], F32), h2T=P.scratch("h2T" + s_, [16, 128, NTOK], BF16),
            s12=P.scratch("s12" + s_, [NB, 128, 16, 128], F32), g2=P.scratch("g2" + s_, [128, D], F32),
            xcur=P.scratch("xcur" + s_, [NB, 128, D], F32), foxo=P.scratch("foxo" + s_, [NB, 128, 768], BF16),
            dsao=P.scratch("dsao" + s_, [NB, 128, 768], BF16)))
    xo = P.dram("xo", [NB, 128, D], F32, "ExternalOutput")
    ident = emit_consts(P, ident_d)
    emit_zero(P)
    for l in range(2):
        W = L[l]
        xsrc = xs if l == 0 else L[0]["xcur"]
        xdr = None if l == 0 else L[0]["xcur"]
        emit_A_f(P, xsrc, xdr, c_d, W["adawA"], W["adabA"], W["g1"], W["wfm"], W["wtm"], ident, W["zfm_s"], W["ztb_s"], W["ztf_s"])
        P.begin_phase()
        P.allgather(W["zfm_s"], W["zfm_all"])
        P.allgather(W["ztb_s"], W["ztb_all"])
        P.allgather(W["ztf_s"], W["ztf_all"])
        P.end_phase()
        P.scope_begin()
        bias = P.sbs([128, 6, 8, 64], F32, "sbs_bias")
        cm = P.sbs([128, 8, 128], BF16, "sbs_cm")
        ostF = P.sbs([128, 8, 768], BF16, "sbs_ostF")
        P.begin_phase()
        P.load("sp", cm, cm[:, :, :], cm_d[:, :, :])
        P.end_phase()
        emit_fox_bias_f(P, W["ztf_all"], W["fbb"], tri_d, sel_d, bias)
        emit_attn_f(P, W["zfm_all"], R_FK, W["ztb_all"], C_FV, W["zfm_s"], R_FQ, 6, ostF, bias=bias, cm=cm)
        P.begin_phase()
        P.store("sp", ostF, W["foxo"].t.rearrange("j p w -> p j w"), ostF[:, :, :], dram=W["foxo"])
        P.end_phase()
        P.scope_end()
        P.scope_begin()
        ostD = P.sbs([128, 8, 768], BF16, "sbs_ostD")
        maskT = [P.sbs([128, 8 * j + 8, 128], BF16, "sbs_mT") for j in range(8)]
        emit_dsa_select_f(P, W["zfm_all"], W["zfm_s"], W["ztf_s"], am_d, ident, maskT)
        emit_attn_f(P, W["zfm_all"], R_DK, W["ztb_all"], C_DV, W["zfm_s"], R_DQ, 6, ostD, maskT=maskT)
        P.begin_phase()
        P.store("sp", ostD, W["dsao"].t.rearrange("j p w -> p j w"), ostD[:, :, :], dram=W["dsao"])
        P.end_phase()
        P.scope_end()
        emit_gla_f(P, W["zfm_all"], W["ztb_all"], W["ztf_all"], sel4_d, W["wa2"], W["nbacol"], W["barow"], W["gnb"], tri2_d, suf2_d,
                   rmask_d, W["gla_s"])
        P.begin_phase()
        P.allgather(W["gla_s"], W["gla_all"])
        P.end_phase()
        P.scope_begin()
        mixT = P.sbs([128, 16, NTOK], BF16, "sbs_mixT")
        emit_mixT(P, W["foxo"], W["dsao"], W["gla_all"], sel_d, ident, mixT)
        emit_C1(P, ident, xsrc, xdr, None, mixT, c_d, W["adawC"], W["adabC"], W["g2n"], W["wo"], W["wq"], W["kT"],
                W["xmid"], W["h2T"], W["s12"], W["g2"])
        P.scope_end()
        final = (l == 1)
        emit_C2(P, ident, W["h2T"], W["s12"], W["xmid"], W["g2"], W["uTt"], W["v"], fg_d if final else None,
                xo if final else W["xcur"], final, final)
    P.finish()
    return nc


def fused_inputs(x, c, ada_w, ada_b, norm1_g, norm2_g, final_g, w_in, fox_fbias, gla_wa2, gla_ba, gla_norm_g, w_out,
                 peer_wq, peer_k1, peer_k2, peer_u, peer_v):
    f32 = np.float32
    x2 = np.asarray(x, f32).reshape(S, D)
    c1 = np.asarray(c, f32).reshape(D)
    tri2, suf2, rmask = gla_consts()
    shared = dict(c_pk=np.ascontiguousarray(c1.reshape(16, 128).T), ident=np.eye(128, dtype=NPBF), tri=TRI, tri2=tri2, suf2=suf2,
                  rmask=rmask, fg_bc=bc128(np.asarray(final_g, f32)))
    per_head = [dict() for _ in range(4)]
    for l in range(2):
        s_ = "_%d" % l
        w_in_l = np.asarray(w_in[l], f32)
        wfm = np.zeros((D, NFM), f32)
        wfm[:, :len(FM_COLS)] = w_in_l[:, FM_COLS]
        wtm = np.zeros((D, NTM), f32)
        wtm[:, :len(TM_COLS)] = w_in_l[:, TM_COLS]
        kT = np.zeros((16, 128, 128), f32)
        for h in range(8):
            kT[2 * h] = np.asarray(peer_k1[l][h], f32).T
            kT[2 * h + 1] = np.asarray(peer_k2[l][h], f32).T
        shared.update({
            "adawA" + s_: np.ascontiguousarray(ada_w[l][:, 0:4096]), "adabA" + s_: bc128(np.asarray(ada_b[l][0:4096], f32)),
            "g1" + s_: bc128(np.asarray(norm1_g[l], f32)), "wfm" + s_: wfm, "wtm" + s_: wtm, "fbb" + s_: bc128(np.asarray(fox_fbias[l], f32)),
            "gnb" + s_: bc128(np.asarray(gla_norm_g[l], f32)), "adawC" + s_: np.ascontiguousarray(ada_w[l][:, 4096:12288]),
            "adabC" + s_: bc128(np.asarray(ada_b[l][4096:12288], f32)), "g2n" + s_: bc128(np.asarray(norm2_g[l], f32)),
            "wo" + s_: np.ascontiguousarray(w_out[l], dtype=f32), "wq" + s_: np.ascontiguousarray(peer_wq[l], dtype=f32), "kT" + s_: kT,
            "uTt" + s_: uT_tiles(np.asarray(peer_u[l], f32)), "v" + s_: np.ascontiguousarray(peer_v[l], dtype=f32)})
        wa2_l = np.asarray(gla_wa2[l], f32)
        ba_l = np.asarray(gla_ba[l], f32)
        for hg in range(4):
            sl = slice(hg * 128, (hg + 1) * 128)
            per_head[hg].update({"wa2" + s_: np.ascontiguousarray(wa2_l[:, sl]), "nbacol" + s_: np.ascontiguousarray(-ba_l[sl][:, None]),
                                 "barow" + s_: bc128(ba_l[sl])})
    ins = []
    for cc in range(8):
        cm, am, sel = band_masks(cc)
        sel4 = np.zeros((128, 4), f32)
        sel4[:, cc % 4] = 1.0
        dct = dict(shared, xs=own_blocks(x2, cc), sel=sel, sel4=sel4, cm=cm, am=am)
        dct.update(per_head[cc % 4])
        ins.append(dct)
    return ins


def build_CA(with_next_A, final):
    nc = bass.Bass("TRN2", target_bir_lowering=False)
    P = Prog(nc)
    I = lambda name, shp, dt: P.dram(name, shp, dt, "ExternalInput")
    xs = I("xs", [NB, 128, D], F32)
    mixT_d = I("mixT", [D, NTOK], BF16)
    c_d = I("c_pk", [128, 16], F32)
    adawC = I("adaw", [D, 8192], F32)
    adabC = I("adab", [128, 8192], F32)
    g2n = I("g_bc", [128, D], F32)
    ident_d = I("ident", [128, 128], BF16)
    wo_d = I("wo", [D, D], F32)
    wq_d = I("wq", [D, D], F32)
    kT_d = I("kT", [16, 128, 128], F32)
    uT_d = I("uTt", [128, 128, 16, 128], F32)
    v_d = I("v", [NE, D], F32)
    fg_d = I("fg_bc", [128, D], F32) if final else None
    if with_next_A:
        adawA = I("adawA", [D, 4096], F32)
        adabA = I("adabA", [128, 4096], F32)
        g1 = I("g1_bc", [128, D], F32)
        wfm = I("wfm", [D, NFM], F32)
        wtm = I("wtm", [D, NTM], F32)
        zfm = P.dram("zfm", [NFM, NTOK], BF16, "ExternalOutput")
        ztb = P.dram("ztb", [NTOK, 2560], BF16, "ExternalOutput")
        ztf = P.dram("ztf", [NTOK, 640], F32, "ExternalOutput")
    xo = P.dram("xo", [NB, 128, D], F32, "ExternalOutput")
    xmid = P.scratch("xmid_s", [NB, 128, D], F32)
    h2T = P.scratch("h2T_s", [16, 128, NTOK], BF16)
    s12 = P.scratch("s12_s", [NB, 128, 16, 128], F32)
    g2 = P.scratch("g2_s", [128, D], F32)
    ident = emit_consts(P, ident_d)
    emit_C1(P, ident, xs, None, mixT_d, None, c_d, adawC, adabC, g2n, wo_d, wq_d, kT_d, xmid, h2T, s12, g2)
    emit_C2(P, ident, h2T, s12, xmid, g2, uT_d, v_d, fg_d, xo, final, not with_next_A)
    if with_next_A:
        emit_A_f(P, xo, xo, c_d, adawA, adabA, g1, wfm, wtm, ident, zfm, ztb, ztf)
        P.begin_phase()
        P.end_phase(final=True)
    P.finish()
    return nc


_NC_CACHE = {}


def _get_nc(name, fn):
    if name not in _NC_CACHE:
        _NC_CACHE[name] = fn()
    return _NC_CACHE[name]


def _run(nc, ins):
    return run_bass_kernel_spmd(nc, ins, core_ids=list(range(8))).results


def _a_weights(l, ada_w, ada_b, norm1_g, w_in, f32):
    w_in_l = np.asarray(w_in[l], f32)
    wfm = np.zeros((D, NFM), f32)
    wfm[:, :len(FM_COLS)] = w_in_l[:, FM_COLS]
    wtm = np.zeros((D, NTM), f32)
    wtm[:, :len(TM_COLS)] = w_in_l[:, TM_COLS]
    return dict(adaw=np.ascontiguousarray(ada_w[l][:, 0:4096]), adab=bc128(np.asarray(ada_b[l][0:4096], f32)),
                g_bc=bc128(np.asarray(norm1_g[l], f32)), wfm=wfm, wtm=wtm)


def kernel(x, c, ada_w, ada_b, norm1_g, norm2_g, final_g, w_in, fox_fbias, gla_wa2, gla_ba, gla_norm_g, w_out,
           peer_wq, peer_k1, peer_k2, peer_u, peer_v):
    f32 = np.float32
    x2 = np.asarray(x, f32).reshape(S, D)
    c1 = np.asarray(c, f32).reshape(D)
    xs_list = [own_blocks(x2, cc) for cc in range(8)]
    ident = np.eye(128, dtype=NPBF)
    tri2, suf2, rmask = gla_consts()
    masks = [band_masks(cc) for cc in range(8)]
    c_pk = np.ascontiguousarray(c1.reshape(16, 128).T)
    aw = _a_weights(0, ada_w, ada_b, norm1_g, w_in, f32)
    rA = _run(_get_nc("A", build_A), [dict(aw, c_pk=c_pk, ident=ident, xs=xs_list[cc]) for cc in range(8)])
    del aw
    for l in range(2):
        ZFM = assemble_tokens([r["zfm"] for r in rA], 1)
        ZTB = assemble_tokens([r["ztb"] for r in rA], 0)
        ZTF = assemble_tokens([r["ztf"] for r in rA], 0)
        fkT = np.ascontiguousarray(ZFM[R_FK:R_FK + 768])
        dkT = np.ascontiguousarray(ZFM[R_DK:R_DK + 768])
        ikT = np.ascontiguousarray(ZFM[R_IK:R_IK + 64])
        gaT = np.ascontiguousarray(ZFM[R_GA:R_GA + 16])
        fvg = vg_layout(ZTB[:, C_FV:C_FV + 768], 6)
        dvg = vg_layout(ZTB[:, C_DV:C_DV + 768], 6)
        ffp = np.ascontiguousarray(ZTF[:, 512:518].reshape(64, 128, 6).transpose(1, 2, 0))
        fbb = bc128(np.asarray(fox_fbias[l], f32))
        wa2_l = np.asarray(gla_wa2[l], f32)
        ba_l = np.asarray(gla_ba[l], f32)
        gnb = bc128(np.asarray(gla_norm_g[l], f32))
        gl = []
        for hg in range(4):
            sl = slice(hg * 128, (hg + 1) * 128)
            gl.append(dict(gqT=np.ascontiguousarray(ZFM[R_GQ + hg * 128:R_GQ + (hg + 1) * 128]),
                           gkT=np.ascontiguousarray(ZFM[R_GK + hg * 128:R_GK + (hg + 1) * 128]),
                           gktm=tm_layout(ZTB[:, C_GK + hg * 128:C_GK + (hg + 1) * 128]),
                           gvtm=tm_layout(ZTB[:, C_GV + hg * 128:C_GV + (hg + 1) * 128]),
                           grtm=tm_layout(ZTF[:, hg * 128:(hg + 1) * 128]), gaT=gaT,
                           wa2=np.ascontiguousarray(wa2_l[:, sl]), nbacol=np.ascontiguousarray(-ba_l[sl][:, None]),
                           barow=bc128(ba_l[sl]), gnb=gnb, tri2=tri2, suf2=suf2, rmask=rmask))
        insB = []
        for cc in range(8):
            cm, am, sel = masks[cc]
            zo = rA[cc]["zfm"]
            iq = zo[R_IQ:R_IQ + 1024].reshape(16, 64, 1024).transpose(1, 0, 2)
            iw = rA[cc]["ztf"][:, 518:534].reshape(8, 128, 16).transpose(1, 0, 2)
            dct = dict(fkT=fkT, fvg=fvg, fqT=np.ascontiguousarray(zo[R_FQ:R_FQ + 768]), ffp=ffp, fbb=fbb, tri=TRI, sel=sel, cm=cm,
                       dkT=dkT, dvg=dvg, dqT=np.ascontiguousarray(zo[R_DQ:R_DQ + 768]), iqT=np.ascontiguousarray(iq), ikT=ikT,
                       iwp=np.ascontiguousarray(iw), am=am, ident=ident)
            dct.update(gl[cc % 4])
            insB.append(dct)
        rB = _run(_get_nc("B", build_B), insB)
        del insB, fvg, dvg, gl, ZFM, ZTB, ZTF
        gla_full = np.concatenate([rB[hg]["glao"].reshape(S, 128) for hg in range(4)], axis=1)
        mix_list = []
        for cc in range(8):
            g_own = own_blocks(gla_full, cc)
            mix = np.concatenate([rB[cc]["foxo"], g_own, rB[cc]["dsao"]], axis=2)
            mix_list.append(mix.reshape(NTOK, D))
        final = (l == 1)
        kT = np.zeros((16, 128, 128), f32)
        for h in range(8):
            kT[2 * h] = np.asarray(peer_k1[l][h], f32).T
            kT[2 * h + 1] = np.asarray(peer_k2[l][h], f32).T
        common = dict(c_pk=c_pk, adaw=np.ascontiguousarray(ada_w[l][:, 4096:12288]), adab=bc128(np.asarray(ada_b[l][4096:12288], f32)),
                      g_bc=bc128(np.asarray(norm2_g[l], f32)), ident=ident, wo=np.ascontiguousarray(w_out[l], dtype=f32),
                      wq=np.ascontiguousarray(peer_wq[l], dtype=f32), kT=kT, uTt=uT_tiles(np.asarray(peer_u[l], f32)),
                      v=np.ascontiguousarray(peer_v[l], dtype=f32))
        if final:
            common["fg_bc"] = bc128(np.asarray(final_g, f32))
            nc = _get_nc("Cf", lambda: build_CA(False, True))
        else:
            aw = _a_weights(1, ada_w, ada_b, norm1_g, w_in, f32)
            common.update(adawA=aw["adaw"], adabA=aw["adab"], g1_bc=aw["g_bc"], wfm=aw["wfm"], wtm=aw["wtm"])
            nc = _get_nc("CA", lambda: build_CA(True, False))
        rC = _run(nc, [dict(common, xs=xs_list[cc], mixT=np.ascontiguousarray(mix_list[cc].T)) for cc in range(8)])
        del common
        xs_list = [r["xo"] for r in rC]
        rA = rC
    out = assemble_tokens([xx.reshape(NTOK, D) for xx in xs_list], 0)
    return np.ascontiguousarray(out.reshape(1, S, D).astype(np.float32))
```

```python
import numpy as np
import ml_dtypes
from contextlib import ExitStack
import concourse.bass as bass
import concourse.mybir as mybir
from concourse.bass_utils import run_bass_kernel_spmd

F32 = mybir.dt.float32
BF16 = mybir.dt.bfloat16
U32 = mybir.dt.uint32
ALU = mybir.AluOpType
AF = mybir.ActivationFunctionType
AX = mybir.AxisListType
NPBF = ml_dtypes.bfloat16

ENGS = ("pe", "act", "dve", "pool", "sp")


class Buf:
    __slots__ = ("t", "name", "w", "r", "pr", "dsem")

    def __init__(self, t, name):
        self.t = t
        self.name = name
        self.w = {}
        self.r = {}
        self.pr = {}
        self.dsem = None

    def __getitem__(self, idx):
        return self.t[idx]


class Prog:
    def __init__(self, nc, n_dma_sems=80):
        self.nc = nc
        self.stack = ExitStack()
        self.esem = {}
        for e in ENGS[:4]:
            self.esem[e] = self.stack.enter_context(nc.semaphore("sem_" + e))
        self.ecnt = {e: 0 for e in ENGS[:4]}
        self.dpool = [self.stack.enter_context(nc.semaphore("dsem%d" % i)) for i in range(n_dma_sems)]
        self.dfree = list(range(n_dma_sems))
        self.dcnt = [0] * n_dma_sems
        self.semobj = {}
        for e in ENGS[:4]:
            self.semobj[("e", e)] = self.esem[e]
        for i, s in enumerate(self.dpool):
            self.semobj[("d", i)] = s
        self.seen = {e: {} for e in ENGS}
        self.ops = {e: [] for e in ENGS}
        self.pstack = None
        self.pbufs = []
        self.nbuf = 0
        self.n_instr = 0

    def begin_phase(self):
        self.pstack = ExitStack()
        self.pbufs = []

    def sb(self, shape, dtype, name=None):
        self.nbuf += 1
        name = (name or "sb") + "_%d" % self.nbuf
        t = self.pstack.enter_context(self.nc.sbuf_tensor(name, list(shape), dtype))
        b = Buf(t, name)
        self.pbufs.append(b)
        return b

    def ps(self, shape, dtype, name=None):
        self.nbuf += 1
        name = (name or "ps") + "_%d" % self.nbuf
        t = self.pstack.enter_context(self.nc.psum_tensor(name, list(shape), dtype))
        b = Buf(t, name)
        self.pbufs.append(b)
        return b

    def sbp(self, shape, dtype, name=None):
        self.nbuf += 1
        name = (name or "sbp") + "_%d" % self.nbuf
        t = self.stack.enter_context(self.nc.sbuf_tensor(name, list(shape), dtype))
        return Buf(t, name)

    def scope_begin(self):
        if not hasattr(self, "scopes"):
            self.scopes = []
        self.scopes.append((ExitStack(), []))

    def sbs(self, shape, dtype, name=None):
        self.nbuf += 1
        name = (name or "sbs") + "_%d" % self.nbuf
        stack, bufs = self.scopes[-1]
        t = stack.enter_context(self.nc.sbuf_tensor(name, list(shape), dtype))
        b = Buf(t, name)
        bufs.append(b)
        return b

    def scope_end(self):
        stack, bufs = self.scopes.pop()
        self.begin_phase()
        self.pbufs = list(bufs)
        self.end_phase()
        stack.close()

    def dram(self, name, shape, dtype, kind):
        t = self.nc.dram_tensor(name, list(shape), dtype, kind=kind).ap()
        return Buf(t, name)

    def scratch(self, name, shape, dtype):
        t = self.nc.dram_tensor(name, list(shape), dtype).ap()
        return Buf(t, name)

    def allgather(self, snd, rcv):
        rg = [list(range(8))]
        self.op("pool", lambda e: e.collective_compute("AllGather", ALU.bypass, replica_groups=rg, ins=[snd.t.opt()], outs=[rcv.t.opt()]),
                reads=[snd], writes=[rcv], dma=rcv, inc=1)

    def _dsem_of(self, buf):
        if buf.dsem is None:
            buf.dsem = self.dfree.pop()
        return buf.dsem

    def op(self, eng, fn, reads=(), writes=(), dma=None, inc=16):
        kind = "dma" if dma is not None else "compute"
        deps = {}

        def add(key, val, src):
            if kind == "compute" and src == eng:
                return
            if key not in deps or deps[key] < val:
                deps[key] = val

        def add_raw(key, val, src):
            if kind == "compute" and src == eng and eng == "pe":
                return
            if key not in deps or deps[key] < val:
                deps[key] = val

        for b in reads:
            for key, (val, src) in b.w.items():
                add_raw(key, val, src)
        for b in writes:
            for key, (val, src) in b.r.items():
                add(key, val, src)
            for key, (val, src) in b.pr.items():
                add(key, val, src)
            for key, (val, src) in b.w.items():
                if kind == "dma" and key[0] == "d":
                    continue
                add(key, val, src)
        if kind == "compute":
            self.ecnt[eng] += 1
            key, val, src = ("e", eng), self.ecnt[eng], eng
        else:
            i = self._dsem_of(dma)
            self.dcnt[i] += inc
            key, val, src = ("d", i), self.dcnt[i], None
        waits = []
        seen = self.seen[eng]
        for k, v in deps.items():
            if seen.get(k, 0) < v:
                seen[k] = v
                waits.append((k, v))
        for b in reads:
            if b.r.get(key, (0, None))[0] < val:
                b.r[key] = (val, src)
        for b in writes:
            if b.r:
                b.pr = dict(b.r)
                b.w.clear()
                b.r.clear()
            if b.w.get(key, (0, None))[0] < val:
                b.w[key] = (val, src)
        self.ops[eng].append((waits, fn, key, inc))
        self.n_instr += 1

    def load(self, q, dstbuf, out_ap, in_ap, dram=None, **kw):
        self.op(q, lambda e: e.dma_start(out=out_ap, in_=in_ap, **kw), reads=([dram] if dram is not None else []),
                writes=[dstbuf], dma=dstbuf)

    def store(self, q, srcbuf, out_ap, in_ap, dram=None, **kw):
        self.op(q, lambda e: e.dma_start(out=out_ap, in_=in_ap, **kw), reads=[srcbuf],
                writes=([dram] if dram is not None else []), dma=srcbuf)

    def mm(self, out_ap, lhsT, rhs, start, stop, reads, writes):
        self.op("pe", lambda e: e.matmul(out_ap, lhsT, rhs, start=start, stop=stop), reads=reads, writes=writes)

    def end_phase(self, final=False):
        nc = self.nc
        ops = self.ops
        semobj = self.semobj
        esem = self.esem
        finalwaits = []
        if final:
            for i, c in enumerate(self.dcnt):
                if c > 0:
                    finalwaits.append((("d", i), c))
        else:
            for b in self.pbufs:
                if b.dsem is not None and self.dcnt[b.dsem] > 0:
                    finalwaits.append((("d", b.dsem), self.dcnt[b.dsem]))

        def emit(e, name):
            for waits, fn, key, inc in ops[name]:
                for k, v in waits:
                    e.wait_ge(semobj[k], v)
                ins = fn(e)
                if key[0] == "e":
                    ins.then_inc(esem[key[1]], 1)
                else:
                    ins.then_inc(semobj[key], inc)
            if name == "sp":
                for k, v in finalwaits:
                    e.wait_ge(semobj[k], v)

        with nc.Block() as block:
            if ops["sp"] or finalwaits:
                @block.sync
                def _(e):
                    emit(e, "sp")
            if ops["pe"]:
                @block.tensor
                def _(e):
                    emit(e, "pe")
            if ops["act"]:
                @block.scalar
                def _(e):
                    emit(e, "act")
            if ops["dve"]:
                @block.vector
                def _(e):
                    emit(e, "dve")
            if ops["pool"]:
                @block.gpsimd
                def _(e):
                    emit(e, "pool")
        self.ops = {e: [] for e in ENGS}
        for b in self.pbufs:
            if b.dsem is not None:
                self.dfree.append(b.dsem)
        self.pbufs = []
        self.pstack.close()
        self.pstack = None

    def finish(self):
        self.stack.close()


D = 2048
EPS = 1e-6
NTOK = 1024
NB = 8
O_FQ, O_FK, O_FV, O_FF = 0, 768, 1536, 2304
O_GQ, O_GK, O_GV, O_GR, O_GA = 2310, 2822, 3334, 3846, 4358
O_DQ, O_DK, O_DV = 4374, 5142, 5910
O_IQ, O_IK, O_IW = 6678, 7702, 7766
FM_COLS = (list(range(O_FQ, O_FQ + 768)) + list(range(O_FK, O_FK + 768)) + list(range(O_DQ, O_DQ + 768))
           + list(range(O_DK, O_DK + 768)) + list(range(O_GQ, O_GQ + 512)) + list(range(O_GK, O_GK + 512))
           + list(range(O_IQ, O_IQ + 1024)) + list(range(O_IK, O_IK + 64)) + list(range(O_GA, O_GA + 16)))
NFM = 5248
R_FQ, R_FK, R_DQ, R_DK, R_GQ, R_GK, R_IQ, R_IK, R_GA = 0, 768, 1536, 2304, 3072, 3584, 4096, 5120, 5184
TM_COLS = (list(range(O_FV, O_FV + 768)) + list(range(O_DV, O_DV + 768)) + list(range(O_GK, O_GK + 512))
           + list(range(O_GV, O_GV + 512)) + list(range(O_GR, O_GR + 512)) + list(range(O_FF, O_FF + 6))
           + list(range(O_IW, O_IW + 16)))
NTM = 3200
C_FV, C_DV, C_GK, C_GV = 0, 768, 1536, 2048
C_GR, C_FF, C_IW = 0, 512, 518


def emit_mod(P, c_d, adaw_d, adab_d, ncols, modbc):
    P.begin_phase()
    ct = P.sb([128, 16], F32, "sb_ct")
    ca = P.sb([128, 16], F32, "sb_ca")
    crep = P.sb([128, 16, 128], F32, "sb_crep")
    adab = P.sb([128, ncols], F32, "sb_adab")
    wch = [P.sb([128, 16, 512], F32, "sb_wch") for _ in range(2)]
    pm = [P.ps([128, 512], F32, "ps_mod") for _ in range(2)]
    P.load("sp", ct, ct[:, :], c_d[:, :])
    P.load("act", adab, adab[:, :], adab_d[:, :])
    P.op("act", lambda e: e.activation(out=ca[:, :], in_=ct[:, :], func=AF.Silu), reads=[ct], writes=[ca])
    P.op("dve", lambda e: e.tensor_copy(out=crep[:, :, :], in_=ca[:, :].unsqueeze(2).to_broadcast([128, 16, 128])),
         reads=[ca], writes=[crep])
    wv = adaw_d.t.rearrange("(k p) n -> p k n", p=128)
    for ci in range(ncols // 512):
        w = wch[ci % 2]
        q = "sp" if ci % 2 == 0 else "act"
        for kh in range(2):
            P.load(q, w, w[:, kh * 8:(kh + 1) * 8, :], wv[:, kh * 8:(kh + 1) * 8, ci * 512:(ci + 1) * 512])
        ps = pm[ci % 2]
        for k in range(16):
            P.mm(ps[:, :], crep[:, k, :], w[:, k, :], k == 0, k == 15, reads=[crep, w], writes=[ps])
        P.op("dve", lambda e, ps=ps, ci=ci: e.tensor_tensor(out=modbc[:, ci * 512:(ci + 1) * 512], in0=ps[:, :],
                                                          in1=adab[:, ci * 512:(ci + 1) * 512], op=ALU.add),
             reads=[ps, adab], writes=[modbc])
    P.end_phase()


def emit_norm_T(P, xs_d, A_bc, B_bc, ident, hT, xkeep=None, xdram=None):
    P.begin_phase()
    xt = [P.sb([128, D], F32, "sb_x") for _ in range(2)]
    junk = P.sb([128, D], BF16, "sb_junk")
    tmp = [P.sb([128, D], F32, "sb_tmp") for _ in range(2)]
    hb = [P.sb([128, D], BF16, "sb_hb") for _ in range(2)]
    st = [P.sb([128, 4], F32, "sb_st") for _ in range(2)]
    pT = [P.ps([128, 8, 128], BF16, "ps_T") for _ in range(2)]
    for b in range(NB):
        if xkeep is None:
            x = xt[b % 2]
            P.load("sp" if b % 2 == 0 else "act", x, x[:, :], xs_d[b, :, :], dram=xdram)
            xa = x[:, :]
            xr = [x]
        else:
            xa = xkeep[:, b, :]
            xr = [xkeep]
        s = st[b % 2]
        t = tmp[b % 2]
        h = hb[b % 2]
        P.op("act", lambda e, xa=xa, s=s: e.activation(out=junk[:, :], in_=xa, func=AF.Square, accum_out=s[:, 0:1]),
             reads=xr, writes=[junk, s])
        P.op("act", lambda e, s=s: e.activation(out=s[:, 1:2], in_=s[:, 0:1], func=AF.Sqrt, scale=1.0 / D, bias=EPSB[0][:, 0:1]),
             reads=[s, EPSB[0]], writes=[s])
        P.op("dve", lambda e, s=s: e.reciprocal(out=s[:, 2:3], in_=s[:, 1:2]), reads=[s], writes=[s])
        P.op("dve", lambda e, xa=xa, s=s, t=t: e.scalar_tensor_tensor(out=t[:, :], in0=xa, scalar=s[:, 2:3], in1=A_bc[:, :],
                                                                   op0=ALU.mult, op1=ALU.mult),
             reads=xr + [s, A_bc], writes=[t])
        P.op("pool", lambda e, t=t, h=h: e.tensor_tensor(out=h[:, :], in0=t[:, :], in1=B_bc[:, :], op=ALU.add),
             reads=[t, B_bc], writes=[h])
        for half in range(2):
            pt = pT[half]
            for kk in range(8):
                k = half * 8 + kk
                P.op("pe", lambda e, pt=pt, kk=kk, k=k, h=h: e.transpose(out=pt[:, kk, :], in_=h[:, k * 128:(k + 1) * 128],
                                                                       identity=ident[:, :]),
                     reads=[h, ident], writes=[pt])
            P.op("act", lambda e, pt=pt, half=half, b=b: e.activation(
                out=hT[:, half * 8:(half + 1) * 8, b * 128:(b + 1) * 128], in_=pt[:, :, :], func=AF.Copy),
                 reads=[pt], writes=[hT])
    P.end_phase()


EPSB = [None]


def emit_consts(P, ident_d):
    ident = P.sbp([128, 128], BF16, "sbp_ident")
    epsb = P.sbp([128, 1], F32, "sbp_eps")
    EPSB[0] = epsb
    P.begin_phase()
    P.load("sp", ident, ident[:, :], ident_d[:, :])
    P.op("dve", lambda e: e.memset(epsb[:, :], EPS), writes=[epsb])
    P.end_phase()
    return ident


def emit_proj(P, hT, specs):
    P.begin_phase()
    wf = [P.sb([128, 16, 512], F32, "sb_wf") for _ in range(2)]
    wb = [P.sb([128, 16, 512], BF16, "sb_wb") for _ in range(2)]
    pp = [P.ps([128, 512], F32, "ps_pp") for _ in range(4)]
    ofm = [P.sb([128, 1024], BF16, "sb_ofm") for _ in range(2)]
    otb = [P.sb([128, 512], BF16, "sb_otb") for _ in range(2)]
    otf = [P.sb([128, 512], F32, "sb_otf") for _ in range(2)]
    ci_g = 0
    pi = 0
    oi = 0
    for sp in specs:
        N = sp["w"].t.shape[1]
        wv = sp["w"].t.rearrange("(k p) n -> p k n", p=128)
        for c0 in range(0, N, 512):
            cw = min(512, N - c0)
            w32 = wf[ci_g % 2]
            w16 = wb[ci_g % 2]
            for kh in range(2):
                P.load("sp" if kh == 0 else "act", w32, w32[:, kh * 8:(kh + 1) * 8, 0:cw], wv[:, kh * 8:(kh + 1) * 8, c0:c0 + cw])
            P.op("dve", lambda e, w32=w32, w16=w16, cw=cw: e.tensor_copy(out=w16[:, 0:8, 0:cw], in_=w32[:, 0:8, 0:cw]),
                 reads=[w32], writes=[w16])
            P.op("pool", lambda e, w32=w32, w16=w16, cw=cw: e.tensor_copy(out=w16[:, 8:16, 0:cw], in_=w32[:, 8:16, 0:cw]),
                 reads=[w32], writes=[w16])
            ci_g += 1
            if sp["kind"] == "fm":
                od = sp["outs"][0][0]
                for g0 in range(0, cw, 128):
                    o = ofm[oi % 2]
                    oi += 1
                    for th in range(2):
                        ps = pp[pi % 4]
                        pi += 1
                        for k in range(16):
                            P.mm(ps[:, :], w16[:, k, g0:g0 + 128], hT[:, k, th * 512:(th + 1) * 512], k == 0, k == 15,
                                 reads=[w16, hT], writes=[ps])
                        eng = "act" if th == 0 else "dve"
                        if eng == "act":
                            P.op("act", lambda e, o=o, ps=ps, th=th: e.activation(out=o[:, th * 512:(th + 1) * 512], in_=ps[:, :], func=AF.Copy),
                                 reads=[ps], writes=[o])
                        else:
                            P.op("dve", lambda e, o=o, ps=ps, th=th: e.tensor_copy(out=o[:, th * 512:(th + 1) * 512], in_=ps[:, :]),
                                 reads=[ps], writes=[o])
                    r0 = c0 + g0
                    P.store("pool", o, od.t[r0:r0 + 128, :], o[:, :], dram=od)
            else:
                tgt = None
                for (od, a, b_, dt) in sp["outs"]:
                    if a <= c0 and c0 + cw <= b_:
                        tgt = (od, a, dt)
                od, a, dt = tgt
                for b in range(NB):
                    ps = pp[pi % 4]
                    pi += 1
                    for k in range(16):
                        P.mm(ps[:, 0:cw], hT[:, k, b * 128:(b + 1) * 128], w16[:, k, 0:cw], k == 0, k == 15,
                             reads=[w16, hT], writes=[ps])
                    o = (otb if dt == BF16 else otf)[oi % 2]
                    oi += 1
                    if b % 2 == 0:
                        P.op("act", lambda e, o=o, ps=ps, cw=cw: e.activation(out=o[:, 0:cw], in_=ps[:, 0:cw], func=AF.Copy),
                             reads=[ps], writes=[o])
                    else:
                        P.op("dve", lambda e, o=o, ps=ps, cw=cw: e.tensor_copy(out=o[:, 0:cw], in_=ps[:, 0:cw]),
                             reads=[ps], writes=[o])
                    P.store("pool", o, od.t[b * 128:(b + 1) * 128, c0 - a:c0 - a + cw], o[:, 0:cw], dram=od)
    P.end_phase()


def build_A():
    nc = bass.Bass("TRN2", target_bir_lowering=False)
    P = Prog(nc)
    xs = P.dram("xs", [NB, 128, D], F32, "ExternalInput")
    c_d = P.dram("c_pk", [128, 16], F32, "ExternalInput")
    adaw = P.dram("adaw", [D, 4096], F32, "ExternalInput")
    adab = P.dram("adab", [128, 4096], F32, "ExternalInput")
    g_d = P.dram("g_bc", [128, D], F32, "ExternalInput")
    ident_d = P.dram("ident", [128, 128], BF16, "ExternalInput")
    wfm = P.dram("wfm", [D, NFM], F32, "ExternalInput")
    wtm = P.dram("wtm", [D, NTM], F32, "ExternalInput")
    zfm = P.dram("zfm", [NFM, NTOK], BF16, "ExternalOutput")
    ztb = P.dram("ztb", [NTOK, 2560], BF16, "ExternalOutput")
    ztf = P.dram("ztf", [NTOK, 640], F32, "ExternalOutput")
    ident = emit_consts(P, ident_d)
    modbc = P.sbp([128, 4096], F32, "sbp_mod")
    A1 = P.sbp([128, D], F32, "sbp_A1")
    hT = P.sbp([128, 16, NTOK], BF16, "sbp_hT")
    emit_mod(P, c_d, adaw, adab, 4096, modbc)
    P.begin_phase()
    gb = P.sb([128, D], F32, "sb_g")
    P.load("sp", gb, gb[:, :], g_d[:, :])
    P.op("dve", lambda e: e.scalar_tensor_tensor(out=A1[:, :], in0=modbc[:, D:2 * D], scalar=1.0, in1=gb[:, :],
                                                 op0=ALU.add, op1=ALU.mult), reads=[modbc, gb], writes=[A1])
    P.end_phase()
    emit_norm_T(P, xs, A1, modbc_view(modbc, 0, D), ident, hT)
    emit_proj(P, hT, [dict(w=wfm, kind="fm", outs=[(zfm, 0, NFM, BF16)]),
                      dict(w=wtm, kind="tm", outs=[(ztb, 0, 2560, BF16), (ztf, 2560, 3200, F32)])])
    P.begin_phase()
    P.end_phase(final=True)
    P.finish()
    return nc


class View:
    def __init__(self, buf, a, b):
        self.buf = buf
        self.a = a
        self.b = b


def modbc_view(buf, a, b):
    v = Buf(buf.t[:, a:b], buf.name + "_v")
    v.w = buf.w
    v.r = buf.r
    v.pr = buf.pr
    return v


def own_blocks(arr, c):
    a = arr.reshape((64, 128) + arr.shape[1:])
    return np.ascontiguousarray(a[c::8])


def bc128(v):
    return np.ascontiguousarray(np.broadcast_to(v[None, :], (128, v.shape[0]))).astype(np.float32)


def host_A_inputs(x, c, ada_w_l, ada_b_l, norm_g_l, w_in_l):
    wfm = np.zeros((D, NFM), np.float32)
    wfm[:, :len(FM_COLS)] = w_in_l[:, FM_COLS]
    wtm = np.zeros((D, NTM), np.float32)
    wtm[:, :len(TM_COLS)] = w_in_l[:, TM_COLS]
    common = dict(c_pk=np.ascontiguousarray(c.reshape(16, 128).T), adaw=np.ascontiguousarray(ada_w_l[:, 0:4096]),
                  adab=bc128(ada_b_l[0:4096]), g_bc=bc128(norm_g_l), ident=np.eye(128, dtype=NPBF), wfm=wfm, wtm=wtm)
    x2 = x.reshape(8192, D)
    return [dict(common, xs=own_blocks(x2, cc)) for cc in range(8)]


S = 8192
NKB = 64
SCALE = 128 ** -0.5
NEG = -1.0e30
NBIS = 18
TOPK = 256


def emit_attn(P, KT_d, Vg_d, QT_d, nheads, out_stage, col0, bias=None, maskT=None, cm=None):
    P.begin_phase()
    KT = [P.sb([128, S], BF16, "sb_KT") for _ in range(2)]
    Vg = [P.sb([128, NKB, 129], BF16, "sb_Vg") for _ in range(2)]
    QT = [P.sb([128, 1024], BF16, "sb_QT") for _ in range(2)]
    pO = [P.ps([128, 512], F32, "ps_O") for _ in range(2)]
    pS = []
    for _ in range(2):
        bank = P.ps([128, 4, 128], F32, "ps_S")
        for q in range(4):
            pS.append(Buf(bank.t[:, q, :], bank.name + "_q%d" % q))
    Pt = [P.sb([128, 128], BF16, "sb_Pt") for _ in range(6)]
    rc = [P.sb([128, 1], F32, "sb_rc") for _ in range(2)]
    zero_b = P.sb([128, 1], F32, "sb_zb")
    P.op("dve", lambda e: e.memset(zero_b[:, :], 0.0), writes=[zero_b])
    si = 0
    pi = 0
    oi = 0
    for h in range(nheads):
        kt, vg, qt = KT[h % 2], Vg[h % 2], QT[h % 2]
        for q4 in range(4):
            P.load("sp" if q4 % 2 == 0 else "act", kt, kt[:, q4 * 2048:(q4 + 1) * 2048],
                   KT_d.t[h * 128:(h + 1) * 128, q4 * 2048:(q4 + 1) * 2048])
        for q2 in range(2):
            P.load("sp" if q2 == 0 else "act", vg, vg[:, q2 * 32:(q2 + 1) * 32, :], Vg_d.t[h, :, q2 * 32:(q2 + 1) * 32, :])
        P.load("pool", qt, qt[:, :], QT_d.t[h * 128:(h + 1) * 128, :])
        for j in range(8):
            nkb = 8 * j + 8
            po = pO[oi % 2]
            oi += 1
            for kb in range(nkb):
                ps = pS[si % 8]
                si += 1
                pt = Pt[pi % 6]
                pi += 1
                P.mm(ps[:, :], kt[:, kb * 128:(kb + 1) * 128], qt[:, j * 128:(j + 1) * 128], True, True,
                     reads=[kt, qt], writes=[ps])
                if bias is not None:
                    P.op("act", lambda e, pt=pt, ps=ps, h=h, j=j, kb=kb: e.activation(
                        out=pt[:, :], in_=ps[:, :], func=AF.Exp, scale=SCALE, bias=bias[:, h, j, kb:kb + 1]),
                        reads=[ps, bias], writes=[pt])
                else:
                    P.op("act", lambda e, pt=pt, ps=ps: e.activation(
                        out=pt[:, :], in_=ps[:, :], func=AF.Exp, scale=SCALE, bias=zero_b[:, 0:1]),
                        reads=[ps, zero_b], writes=[pt])
                if maskT is not None:
                    mt = maskT[j]
                    P.op("dve", lambda e, pt=pt, mt=mt, kb=kb: e.tensor_tensor(out=pt[:, :], in0=pt[:, :], in1=mt[:, kb, :], op=ALU.mult),
                         reads=[pt, mt], writes=[pt])
                elif kb >= 8 * j:
                    r = kb - 8 * j
                    P.op("dve", lambda e, pt=pt, r=r: e.tensor_tensor(out=pt[:, :], in0=pt[:, :], in1=cm[:, r, :], op=ALU.mult),
                         reads=[pt, cm], writes=[pt])
                P.mm(po[:, 0:129], pt[:, :], vg[:, kb, :], kb == 0, kb == nkb - 1, reads=[pt, vg], writes=[po])
            r_ = rc[oi % 2]
            P.op("dve", lambda e, r_=r_, po=po: e.reciprocal(out=r_[:, 0:1], in_=po[:, 128:129]), reads=[po], writes=[r_])
            P.op("dve", lambda e, r_=r_, po=po, j=j, h=h: e.tensor_scalar(
                out=out_stage[:, j, col0 + h * 128:col0 + (h + 1) * 128], in0=po[:, 0:128], scalar1=r_[:, 0:1], scalar2=None,
                op0=ALU.mult), reads=[po, r_], writes=[out_stage])
    P.end_phase()


def emit_fox_bias(P, ff_d, fb_d, tri_d, sel_d, bias):
    P.begin_phase()
    ff = P.sb([128, 6, 64], F32, "sb_ff")
    fb = P.sb([128, 6], F32, "sb_fb")
    tri = P.sb([128, 128], F32, "sb_tri")
    ones = P.sb([128, 128], F32, "sb_ones")
    onec = P.sb([128, 1], F32, "sb_onec")
    sel = P.sb([128, 8], F32, "sb_sel")
    nlf = P.sb([128, 6, 64], F32, "sb_nlf")
    tot = P.sb([128, 6, 64], F32, "sb_tot")
    incl = P.sb([128, 6, 64], F32, "sb_incl")
    NF = P.sb([128, 6, 64], F32, "sb_NF")
    tmp = P.sb([128, 6, 8, 8], F32, "sb_tmp")
    nfe = P.sb([128, 6, 8], F32, "sb_nfe")
    pw = P.ps([128, 384], F32, "ps_w")
    pt_ = P.ps([128, 384], F32, "ps_t")
    P.load("sp", ff, ff[:, :, :], ff_d[:, :, :])
    P.load("act", fb, fb[:, :], fb_d[:, :])
    P.load("sp", tri, tri[:, :], tri_d[:, :])
    P.load("act", sel, sel[:, :], sel_d[:, :])
    P.op("dve", lambda e: e.memset(ones[:, :], 1.0), writes=[ones])
    P.op("dve", lambda e: e.memset(onec[:, :], 1.0), writes=[onec])
    P.op("dve", lambda e: e.tensor_tensor(out=nlf[:, :, :], in0=ff[:, :, :], in1=fb[:, :].unsqueeze(2).to_broadcast([128, 6, 64]),
                                          op=ALU.add), reads=[ff, fb], writes=[nlf])
    nlf2 = nlf.t.rearrange("p h k -> p (h k)")
    P.op("act", lambda e: e.activation(out=nlf2, in_=nlf2, func=AF.Exp, scale=-1.0), reads=[nlf], writes=[nlf])
    P.op("act", lambda e: e.activation(out=nlf2, in_=nlf2, func=AF.Ln, bias=onec[:, 0:1]), reads=[nlf, onec], writes=[nlf])
    P.mm(pw[:, :], tri[:, :], nlf2, True, True, reads=[tri, nlf], writes=[pw])
    P.mm(pt_[:, :], ones[:, :], nlf2, True, True, reads=[ones, nlf], writes=[pt_])
    tot2 = tot.t.rearrange("p h k -> p (h k)")
    P.op("dve", lambda e: e.tensor_copy(out=tot2, in_=pt_[:, :]), reads=[pt_], writes=[tot])
    for h in range(6):
        P.op("dve", lambda e, h=h: e.tensor_tensor_scan(out=incl[:, h, :], data0=ones[:, 0:64], data1=tot[:, h, :], initial=0.0,
                                                        op0=ALU.mult, op1=ALU.add), reads=[ones, tot], writes=[incl])
    NF2 = NF.t.rearrange("p h k -> p (h k)")
    incl2 = incl.t.rearrange("p h k -> p (h k)")
    P.op("dve", lambda e: e.tensor_tensor(out=NF2, in0=pw[:, :], in1=incl2, op=ALU.add), reads=[pw, incl], writes=[NF])
    P.op("dve", lambda e: e.tensor_tensor(out=NF2, in0=NF2, in1=tot2, op=ALU.subtract), reads=[NF, tot], writes=[NF])
    P.op("dve", lambda e: e.tensor_tensor(out=tmp[:, :, :, :], in0=incl.t.rearrange("p h (j r) -> p h j r", r=8),
                                          in1=sel[:, :].unsqueeze(1).unsqueeze(1).to_broadcast([128, 6, 8, 8]), op=ALU.mult),
         reads=[incl, sel], writes=[tmp])
    P.op("dve", lambda e: e.tensor_reduce(out=nfe[:, :, :], in_=tmp[:, :, :, :], axis=AX.X, op=ALU.add), reads=[tmp], writes=[nfe])
    for h in range(6):
        for j in range(8):
            P.op("dve", lambda e, h=h, j=j: e.tensor_scalar(out=bias[:, h, j, :], in0=NF[:, h, :], scalar1=nfe[:, h, j:j + 1],
                                                            scalar2=0.0, op0=ALU.subtract, op1=ALU.min),
                 reads=[NF, nfe], writes=[bias])
    P.end_phase()


def vg_layout(v_all, nheads):
    v = v_all.reshape(64, 128, nheads, 128).transpose(2, 1, 0, 3)
    o = np.ones((nheads, 128, 64, 129), NPBF)
    o[:, :, :, :128] = v
    return o


def band_masks(c):
    sp = np.arange(128)[:, None]
    t = np.arange(128)[None, :]
    cm = np.zeros((128, 8, 128), np.float32)
    am = np.full((128, 8, 128), NEG, np.float32)
    for r in range(8):
        if r < c:
            cm[:, r, :] = 1.0
            am[:, r, :] = 0.0
        elif r == c:
            cm[:, r, :] = (sp <= t)
            am[:, r, :] = np.where(sp.T <= t.T, 0.0, NEG)
    sel = np.zeros((128, 8), np.float32)
    sel[:, c] = 1.0
    return cm.astype(NPBF), am, sel


TRI = np.triu(np.ones((128, 128), np.float32))


def assemble_tokens(parts, axis):
    shp = list(parts[0].shape)
    n = shp[axis]
    assert n == 1024
    st = np.stack([np.moveaxis(p, axis, 0).reshape((8, 128) + tuple(np.moveaxis(p, axis, 0).shape[1:])) for p in parts], axis=1)
    g = st.reshape((8192,) + st.shape[3:])
    return np.moveaxis(g, 0, axis)


def emit_dsa_select(P, iqT_d, ikT_d, iw_d, am_d, ident, maskT):
    P.begin_phase()
    score = P.sb([128, S], F32, "sb_score")
    junk = P.sb([128, S], BF16, "sb_junk")
    ikT = P.sb([64, S], BF16, "sb_ikT")
    iq = [P.sb([64, 16, 128], BF16, "sb_iq") for _ in range(2)]
    rr = [P.sb([128, 512], F32, "sb_rr") for _ in range(3)]
    am = P.sb([128, 8, 128], F32, "sb_am")
    iw = P.sb([128, 8, 16], F32, "sb_iw")
    wsc = P.sb([128, 8, 16], F32, "sb_wsc")
    pI = [P.ps([128, 512], F32, "ps_I") for _ in range(4)]
    pT = [P.ps([128, 8, 128], BF16, "ps_mT") for _ in range(2)]
    mch = [P.sb([128, 1024], BF16, "sb_mch") for _ in range(2)]
    sms = [P.sb([128, 8], F32, "sb_sm") for _ in range(2)]
    for q4 in range(4):
        P.load("sp" if q4 % 2 == 0 else "act", ikT, ikT[:, q4 * 2048:(q4 + 1) * 2048], ikT_d.t[:, q4 * 2048:(q4 + 1) * 2048])
    P.load("sp", am, am[:, :, :], am_d[:, :, :])
    P.load("act", iw, iw[:, :, :], iw_d[:, :, :])
    P.op("dve", lambda e: e.tensor_scalar(out=wsc[:, :, :], in0=iw[:, :, :], scalar1=(64 ** -0.5) * (16 ** -0.5), scalar2=None,
                                          op0=ALU.mult), reads=[iw], writes=[wsc])
    ii = 0
    ti = 0
    for j in range(8):
        L = (8 * j + 8) * 128
        iqj = iq[j % 2]
        P.load("pool", iqj, iqj[:, :, :], iqT_d.t[:, :, j * 128:(j + 1) * 128])
        sm = sms[j % 2]
        for ck in range(L // 512):
            sc = score.t[:, ck * 512:(ck + 1) * 512]
            for h in range(16):
                ps = pI[ii % 4]
                r = rr[ii % 3]
                ii += 1
                P.mm(ps[:, :], iqj[:, h, :], ikT[:, ck * 512:(ck + 1) * 512], True, True, reads=[iqj, ikT], writes=[ps])
                P.op("act", lambda e, r=r, ps=ps: e.activation(out=r[:, :], in_=ps[:, :], func=AF.Relu), reads=[ps], writes=[r])
                if h == 0:
                    P.op("dve", lambda e, sc=sc, r=r, j=j: e.tensor_scalar(out=sc, in0=r[:, :], scalar1=wsc[:, j, 0:1], scalar2=None,
                                                                         op0=ALU.mult), reads=[r, wsc], writes=[score])
                else:
                    P.op("dve", lambda e, sc=sc, r=r, j=j, h=h: e.scalar_tensor_tensor(
                        out=sc, in0=r[:, :], scalar=wsc[:, j, h:h + 1], in1=sc, op0=ALU.mult, op1=ALU.add),
                        reads=[r, wsc, score], writes=[score])
        sL = score.t[:, 0:L]
        P.op("dve", lambda e, sm=sm, sL=sL: e.tensor_reduce(out=sm[:, 0:1], in_=sL, axis=AX.X, op=ALU.max, apply_absolute_value=True),
             reads=[score], writes=[sm])
        P.op("dve", lambda e, sm=sm: e.tensor_scalar(out=sm[:, 0:1], in0=sm[:, 0:1], scalar1=1.001, scalar2=1e-3, op0=ALU.mult, op1=ALU.add),
             reads=[sm], writes=[sm])
        P.op("dve", lambda e, sm=sm: e.tensor_scalar(out=sm[:, 1:2], in0=sm[:, 0:1], scalar1=-1.0, scalar2=None, op0=ALU.mult),
             reads=[sm], writes=[sm])
        sB = score.t[:, L - 1024:L]
        P.op("dve", lambda e, sB=sB: e.tensor_tensor(out=sB, in0=sB, in1=am.t.rearrange("p r s -> p (r s)"), op=ALU.add),
             reads=[score, am], writes=[score])
        for k in range(1, NBIS + 1):
            f = 2.0 ** (1 - k)
            P.op("dve", lambda e, sm=sm, f=f: e.tensor_scalar(out=sm[:, 2:3], in0=sm[:, 0:1], scalar1=f, scalar2=sm[:, 1:2],
                                                            op0=ALU.mult, op1=ALU.add), reads=[sm], writes=[sm])
            P.op("dve", lambda e, sm=sm, sL=sL, L=L: e.tensor_scalar(out=junk[:, 0:L], in0=sL, scalar1=sm[:, 2:3], scalar2=0.0,
                                                                   op0=ALU.is_ge, op1=ALU.add, accum_out=sm[:, 3:4]),
                 reads=[score, sm], writes=[junk, sm])
            P.op("dve", lambda e, sm=sm, f=f: e.tensor_scalar(out=sm[:, 4:5], in0=sm[:, 3:4], scalar1=TOPK - 0.5, scalar2=f,
                                                            op0=ALU.is_ge, op1=ALU.mult), reads=[sm], writes=[sm])
            P.op("dve", lambda e, sm=sm: e.scalar_tensor_tensor(out=sm[:, 1:2], in0=sm[:, 4:5], scalar=sm[:, 0:1], in1=sm[:, 1:2],
                                                              op0=ALU.mult, op1=ALU.add), reads=[sm], writes=[sm])
        for g in range(L // 1024):
            mc = mch[ti % 2]
            pt = pT[ti % 2]
            ti += 1
            P.op("dve", lambda e, mc=mc, g=g, sm=sm: e.tensor_scalar(out=mc[:, :], in0=score[:, g * 1024:(g + 1) * 1024],
                                                                   scalar1=sm[:, 1:2], scalar2=None, op0=ALU.is_ge),
                 reads=[score, sm], writes=[mc])
            for q in range(8):
                P.op("pe", lambda e, pt=pt, mc=mc, q=q: e.transpose(out=pt[:, q, :], in_=mc[:, q * 128:(q + 1) * 128], identity=ident[:, :]),
                     reads=[mc, ident], writes=[pt])
            mt = maskT[j]
            P.op("act", lambda e, mt=mt, pt=pt, g=g: e.activation(out=mt[:, g * 8:(g + 1) * 8, :], in_=pt[:, :, :], func=AF.Copy),
                 reads=[pt], writes=[mt])
    P.end_phase()


def emit_gla(P, gqT_d, gkT_d, gktm_d, gvtm_d, grtm_d, gaT_d, wa2_d, nbacol_d, barow_d, gn_d, tri2_d, suf2_d, rmask_d, out_d):
    P.scope_begin()

    def keep(shape, dt, name):
        return P.sbs(shape, dt, name)
    qtT = keep([128, S], BF16, "sbk_qtT")
    ktT = keep([128, S], BF16, "sbk_ktT")
    dn = keep([128, 128], F32, "sbk_dn")
    Sb = keep([128, 128, 128], BF16, "sbk_Sb")
    vtm = keep([128, 64, 128], BF16, "sbk_v")
    gs = keep([128, 64, 128], F32, "sbk_gs")
    wa2b = keep([16, 128], BF16, "sbk_wa2b")
    gaT = keep([16, S], BF16, "sbk_gaT")
    tri2 = keep([128, 128], F32, "sbk_tri2")
    onec = keep([128, 1], F32, "sbk_onec")
    epsc = keep([128, 1], F32, "sbk_epsc")

    P.begin_phase()
    wa2f = P.sb([16, 128], F32, "sb_wa2f")
    nbac = P.sb([128, 1], F32, "sb_nbac")
    rmask = P.sb([128, 512], F32, "sb_rmask")
    gq = [P.sb([128, 512], BF16, "sb_gq") for _ in range(2)]
    gk = [P.sb([128, 512], BF16, "sb_gk") for _ in range(2)]
    e1 = [P.sb([128, 512], F32, "sb_e1") for _ in range(2)]
    cs = [P.sb([128, 512], F32, "sb_cs") for _ in range(2)]
    eg = [P.sb([128, 512], F32, "sb_eg") for _ in range(2)]
    en = [P.sb([128, 512], F32, "sb_en") for _ in range(2)]
    pg = [P.ps([128, 512], F32, "ps_g") for _ in range(2)]
    P.load("sp", wa2f, wa2f[:, :], wa2_d[:, :])
    P.load("act", nbac, nbac[:, :], nbacol_d[:, :])
    P.load("sp", rmask, rmask[:, :], rmask_d[:, :])
    P.load("act", tri2, tri2[:, :], tri2_d[:, :])
    for q4 in range(4):
        P.load("sp" if q4 % 2 == 0 else "act", gaT, gaT[:, q4 * 2048:(q4 + 1) * 2048], gaT_d.t[:, q4 * 2048:(q4 + 1) * 2048])
    for q2 in range(2):
        P.load("pool", vtm, vtm[:, q2 * 32:(q2 + 1) * 32, :], gvtm_d.t[:, q2 * 32:(q2 + 1) * 32, :])
    P.op("dve", lambda e: e.tensor_copy(out=wa2b[:, :], in_=wa2f[:, :]), reads=[wa2f], writes=[wa2b])
    P.op("dve", lambda e: e.memset(onec[:, :], 1.0), writes=[onec])
    P.op("dve", lambda e: e.memset(epsc[:, :], 1e-6), writes=[epsc])
    for tc in range(16):
        sl = slice(tc * 512, (tc + 1) * 512)
        a, b_ = gq[tc % 2], gk[tc % 2]
        P.load("sp", a, a[:, :], gqT_d.t[:, sl])
        P.load("act", b_, b_[:, :], gkT_d.t[:, sl])
        ps = pg[tc % 2]
        x1, c1, g1, n1 = e1[tc % 2], cs[tc % 2], eg[tc % 2], en[tc % 2]
        P.mm(ps[:, :], wa2b[:, :], gaT[:, sl], True, True, reads=[wa2b, gaT], writes=[ps])
        P.op("act", lambda e, x1=x1, ps=ps: e.activation(out=x1[:, :], in_=ps[:, :], func=AF.Exp, scale=-1.0, bias=nbac[:, 0:1]),
             reads=[ps, nbac], writes=[x1])
        P.op("act", lambda e, x1=x1: e.activation(out=x1[:, :], in_=x1[:, :], func=AF.Ln, bias=onec[:, 0:1]), reads=[x1, onec], writes=[x1])
        P.op("dve", lambda e, x1=x1, c1=c1: e.tensor_tensor_scan(out=c1[:, :], data0=rmask[:, :], data1=x1[:, :], initial=0.0,
                                                               op0=ALU.mult, op1=ALU.add), reads=[rmask, x1], writes=[c1])
        P.op("act", lambda e, c1=c1, g1=g1: e.activation(out=g1[:, :], in_=c1[:, :], func=AF.Exp, scale=-1.0 / 16, bias=ZB[0][:, 0:1]),
             reads=[c1, ZB[0]], writes=[g1])
        P.op("act", lambda e, c1=c1, n1=n1: e.activation(out=n1[:, :], in_=c1[:, :], func=AF.Exp, scale=1.0 / 16, bias=ZB[0][:, 0:1]),
             reads=[c1, ZB[0]], writes=[n1])
        P.op("dve", lambda e, a=a, g1=g1, sl=sl: e.scalar_tensor_tensor(out=qtT[:, sl], in0=a[:, :], scalar=SCALE, in1=g1[:, :],
                                                                      op0=ALU.mult, op1=ALU.mult), reads=[a, g1], writes=[qtT])
        P.op("pool", lambda e, b_=b_, n1=n1, sl=sl: e.tensor_tensor(out=ktT[:, sl], in0=b_[:, :], in1=n1[:, :], op=ALU.mult),
             reads=[b_, n1], writes=[ktT])
        P.op("dve", lambda e, g1=g1, tc=tc: e.tensor_copy(out=dn[:, tc * 8:(tc + 1) * 8],
                                                        in_=g1.t.rearrange("p (n c) -> p n c", c=64)[:, :, 63]),
             reads=[g1], writes=[dn])
    P.end_phase()

    P.begin_phase()
    barow = P.sb([128, 128], F32, "sb_barow")
    gn = P.sb([128, 128], F32, "sb_gn")
    suf2 = P.sb([128, 128], F32, "sb_suf2")
    ktm = P.sb([128, 64, 128], BF16, "sb_ktm")
    Sst = P.sb([128, 128], F32, "sb_Sst")
    xg = [P.sb([128, 128], F32, "sb_xg") for _ in range(2)]
    fk = [P.sb([128, 128], F32, "sb_fk") for _ in range(2)]
    kh = [P.sb([128, 128], BF16, "sb_kh") for _ in range(2)]
    grt = [P.sb([128, 8, 128], F32, "sb_grt") for _ in range(2)]
    pl = [P.ps([128, 128], F32, "ps_l") for _ in range(2)]
    pf = [P.ps([128, 128], F32, "ps_f") for _ in range(2)]
    pU = [P.ps([128, 128], F32, "ps_U") for _ in range(4)]
    P.load("sp", barow, barow[:, :], barow_d[:, :])
    P.load("act", gn, gn[:, :], gn_d[:, :])
    P.load("sp", suf2, suf2[:, :], suf2_d[:, :])
    for q2 in range(2):
        P.load("pool", ktm, ktm[:, q2 * 32:(q2 + 1) * 32, :], gktm_d.t[:, q2 * 32:(q2 + 1) * 32, :])
    P.op("dve", lambda e: e.memset(Sst[:, :], 0.0), writes=[Sst])
    for g8 in range(8):
        gt = grt[g8 % 2]
        P.load("sp" if g8 % 2 == 0 else "act", gt, gt[:, :, :], grtm_d.t[:, g8 * 8:(g8 + 1) * 8, :])
        P.op("act", lambda e, gt=gt: e.activation(out=gt[:, :, :], in_=gt[:, :, :], func=AF.Silu), reads=[gt], writes=[gt])
        P.op("pool", lambda e, gt=gt, g8=g8: e.tensor_tensor(out=gs[:, g8 * 8:(g8 + 1) * 8, :], in0=gt[:, :, :],
                                                           in1=gn[:, :].unsqueeze(1).to_broadcast([128, 8, 128]), op=ALU.mult),
             reads=[gt, gn], writes=[gs])
    for blk in range(64):
        x, f, k2 = xg[blk % 2], fk[blk % 2], kh[blk % 2]
        p1, p2 = pl[blk % 2], pf[blk % 2]
        P.mm(p1[:, :], gaT[:, blk * 128:(blk + 1) * 128], wa2b[:, :], True, True, reads=[gaT, wa2b], writes=[p1])
        P.op("dve", lambda e, x=x, p1=p1: e.tensor_tensor(out=x[:, :], in0=p1[:, :], in1=barow[:, :], op=ALU.add),
             reads=[p1, barow], writes=[x])
        P.op("act", lambda e, x=x: e.activation(out=x[:, :], in_=x[:, :], func=AF.Exp, scale=-1.0, bias=ZB[0][:, 0:1]),
             reads=[x, ZB[0]], writes=[x])
        P.op("act", lambda e, x=x: e.activation(out=x[:, :], in_=x[:, :], func=AF.Ln, bias=onec[:, 0:1]), reads=[x, onec], writes=[x])
        P.mm(p2[:, :], suf2[:, :], x[:, :], True, True, reads=[suf2, x], writes=[p2])
        P.op("act", lambda e, f=f, p2=p2: e.activation(out=f[:, :], in_=p2[:, :], func=AF.Exp, scale=-1.0 / 16, bias=ZB[0][:, 0:1]),
             reads=[p2, ZB[0]], writes=[f])
        P.op("pool", lambda e, k2=k2, f=f, blk=blk: e.tensor_tensor(out=k2[:, :], in0=ktm[:, blk, :], in1=f[:, :], op=ALU.mult),
             reads=[ktm, f], writes=[k2])
        for hf in range(2):
            n = 2 * blk + hf
            pu = pU[n % 4]
            P.mm(pu[:, :], k2[hf * 64:(hf + 1) * 64, :], vtm[hf * 64:(hf + 1) * 64, blk, :], True, True, reads=[k2, vtm], writes=[pu])
            P.op("dve", lambda e, pu=pu, n=n: e.scalar_tensor_tensor(out=Sst[:, :], in0=Sst[:, :], scalar=dn[:, n:n + 1], in1=pu[:, :],
                                                                   op0=ALU.mult, op1=ALU.add), reads=[Sst, dn, pu], writes=[Sst])
            P.op("act", lambda e, n=n: e.activation(out=Sb[:, n, :], in_=Sst[:, :], func=AF.Copy), reads=[Sst], writes=[Sb])
    P.end_phase()

    P.begin_phase()
    ost = P.sb([128, 64, 128], BF16, "sb_gost")
    At = [P.sb([128, 128], BF16, "sb_At") for _ in range(2)]
    st = [P.sb([128, 4], F32, "sb_gst") for _ in range(2)]
    jk = P.sb([128, 128], F32, "sb_gjk")
    pA = [P.ps([128, 128], F32, "ps_A") for _ in range(2)]
    pO = [P.ps([128, 128], F32, "ps_GO") for _ in range(2)]
    for blk in range(64):
        sl = slice(blk * 128, (blk + 1) * 128)
        pa, po, at, s = pA[blk % 2], pO[blk % 2], At[blk % 2], st[blk % 2]
        P.mm(pa[:, :], ktT[:, sl], qtT[:, sl], True, True, reads=[ktT, qtT], writes=[pa])
        P.op("dve", lambda e, at=at, pa=pa: e.tensor_tensor(out=at[:, :], in0=pa[:, :], in1=tri2[:, :], op=ALU.mult),
             reads=[pa, tri2], writes=[at])
        P.mm(po[:, :], at[:, :], vtm[:, blk, :], True, False, reads=[at, vtm], writes=[po])
        if blk > 0:
            P.mm(po[0:64, :], qtT[:, blk * 128:blk * 128 + 64], Sb[:, 2 * blk - 1, :], False, False, reads=[qtT, Sb], writes=[po])
        P.mm(po[64:128, :], qtT[:, blk * 128 + 64:blk * 128 + 128], Sb[:, 2 * blk, :], False, True, reads=[qtT, Sb], writes=[po])
        P.op("act", lambda e, po=po, s=s: e.activation(out=jk[:, :], in_=po[:, :], func=AF.Square, accum_out=s[:, 0:1]),
             reads=[po], writes=[jk, s])
        P.op("act", lambda e, s=s: e.activation(out=s[:, 1:2], in_=s[:, 0:1], func=AF.Sqrt, scale=1.0 / 128, bias=epsc[:, 0:1]),
             reads=[s, epsc], writes=[s])
        P.op("dve", lambda e, s=s: e.reciprocal(out=s[:, 2:3], in_=s[:, 1:2]), reads=[s], writes=[s])
        P.op("dve", lambda e, po=po, s=s, blk=blk: e.scalar_tensor_tensor(out=ost[:, blk, :], in0=po[:, :], scalar=s[:, 2:3],
                                                                        in1=gs[:, blk, :], op0=ALU.mult, op1=ALU.mult),
             reads=[po, s, gs], writes=[ost])
    P.store("sp", ost, out_d.t.rearrange("b p e -> p b e"), ost[:, :, :])
    P.end_phase()
    P.scope_end()


ZB = [None]


def emit_zero(P):
    zb = P.sbp([128, 1], F32, "sbp_zero")
    ZB[0] = zb
    P.begin_phase()
    P.op("dve", lambda e: e.memset(zb[:, :], 0.0), writes=[zb])
    P.end_phase()


def gla_consts():
    s = np.arange(128)[:, None]
    t = np.arange(128)[None, :]
    same = (s // 64) == (t // 64)
    tri2 = (same & (s <= t)).astype(np.float32)
    suf2 = (same & (s > t)).astype(np.float32)
    rmask = np.ones((128, 512), np.float32)
    rmask[:, ::64] = 0.0
    return tri2, suf2, rmask


def tm_layout(a):
    return np.ascontiguousarray(a.reshape(64, 128, a.shape[1]).transpose(1, 0, 2))


def gla_inputs(hg, gq, gk, gv, gr, ga, wa2_l, ba_l, gng_l):
    sl = slice(hg * 128, (hg + 1) * 128)
    tri2, suf2, rmask = gla_consts()
    return dict(gqT=np.ascontiguousarray(gq[:, sl].T).astype(NPBF), gkT=np.ascontiguousarray(gk[:, sl].T).astype(NPBF),
                gktm=tm_layout(gk[:, sl]).astype(NPBF), gvtm=tm_layout(gv[:, sl]).astype(NPBF),
                grtm=tm_layout(gr[:, sl]).astype(np.float32), gaT=np.ascontiguousarray(ga.T).astype(NPBF),
                wa2=np.ascontiguousarray(wa2_l[:, sl]), nbacol=np.ascontiguousarray(-ba_l[sl][:, None]),
                barow=bc128(ba_l[sl]), gnb=bc128(gng_l), tri2=tri2, suf2=suf2, rmask=rmask)


def build_B():
    nc = bass.Bass("TRN2", target_bir_lowering=False)
    P = Prog(nc)
    Dm = {}
    for name, shp, dt in [("fkT", [768, S], BF16), ("fvg", [6, 128, NKB, 129], BF16), ("fqT", [768, 1024], BF16), ("ffp", [128, 6, 64], F32),
                          ("fbb", [128, 6], F32), ("tri", [128, 128], F32), ("sel", [128, 8], F32), ("cm", [128, 8, 128], BF16),
                          ("dkT", [768, S], BF16), ("dvg", [6, 128, NKB, 129], BF16), ("dqT", [768, 1024], BF16), ("iqT", [64, 16, 1024], BF16),
                          ("ikT", [64, S], BF16), ("iwp", [128, 8, 16], F32), ("am", [128, 8, 128], F32), ("ident", [128, 128], BF16),
                          ("gqT", [128, S], BF16), ("gkT", [128, S], BF16), ("gktm", [128, 64, 128], BF16), ("gvtm", [128, 64, 128], BF16),
                          ("grtm", [128, 64, 128], F32), ("gaT", [16, S], BF16), ("wa2", [16, 128], F32), ("nbacol", [128, 1], F32),
                          ("barow", [128, 128], F32), ("gnb", [128, 128], F32), ("tri2", [128, 128], F32), ("suf2", [128, 128], F32),
                          ("rmask", [128, 512], F32)]:
        Dm[name] = P.dram(name, shp, dt, "ExternalInput")
    foxo = P.dram("foxo", [8, 128, 768], BF16, "ExternalOutput")
    dsao = P.dram("dsao", [8, 128, 768], BF16, "ExternalOutput")
    glao = P.dram("glao", [64, 128, 128], BF16, "ExternalOutput")
    emit_zero(P)
    ident = P.sbp([128, 128], BF16, "sbp_ident")
    P.begin_phase()
    P.load("sp", ident, ident[:, :], Dm["ident"][:, :])
    P.end_phase()
    P.scope_begin()
    bias = P.sbs([128, 6, 8, 64], F32, "sbs_bias")
    cm = P.sbs([128, 8, 128], BF16, "sbs_cm")
    ostF = P.sbs([128, 8, 768], BF16, "sbs_ostF")
    P.begin_phase()
    P.load("sp", cm, cm[:, :, :], Dm["cm"][:, :, :])
    P.end_phase()
    emit_fox_bias(P, Dm["ffp"], Dm["fbb"], Dm["tri"], Dm["sel"], bias)
    emit_attn(P, Dm["fkT"], Dm["fvg"], Dm["fqT"], 6, ostF, 0, bias=bias, cm=cm)
    P.begin_phase()
    P.store("sp", ostF, foxo.t.rearrange("j p w -> p j w"), ostF[:, :, :])
    P.end_phase()
    P.scope_end()
    P.scope_begin()
    ostD = P.sbs([128, 8, 768], BF16, "sbs_ostD")
    maskT = [P.sbs([128, 8 * j + 8, 128], BF16, "sbs_mT") for j in range(8)]
    emit_dsa_select(P, Dm["iqT"], Dm["ikT"], Dm["iwp"], Dm["am"], ident, maskT)
    emit_attn(P, Dm["dkT"], Dm["dvg"], Dm["dqT"], 6, ostD, 0, maskT=maskT)
    P.begin_phase()
    P.store("sp", ostD, dsao.t.rearrange("j p w -> p j w"), ostD[:, :, :])
    P.end_phase()
    P.scope_end()
    emit_gla(P, Dm["gqT"], Dm["gkT"], Dm["gktm"], Dm["gvtm"], Dm["grtm"], Dm["gaT"], Dm["wa2"], Dm["nbacol"], Dm["barow"], Dm["gnb"],
             Dm["tri2"], Dm["suf2"], Dm["rmask"], glao)
    P.begin_phase()
    P.end_phase(final=True)
    P.finish()
    return nc


NE = 16384


def build_C1():
    nc = bass.Bass("TRN2", target_bir_lowering=False)
    P = Prog(nc)
    xs = P.dram("xs", [NB, 128, D], F32, "ExternalInput")
    mixT_d = P.dram("mixT", [D, NTOK], BF16, "ExternalInput")
    c_d = P.dram("c_pk", [128, 16], F32, "ExternalInput")
    adaw = P.dram("adaw", [D, 8192], F32, "ExternalInput")
    adab = P.dram("adab", [128, 8192], F32, "ExternalInput")
    g_d = P.dram("g_bc", [128, D], F32, "ExternalInput")
    ident_d = P.dram("ident", [128, 128], BF16, "ExternalInput")
    wo_d = P.dram("wo", [D, D], F32, "ExternalInput")
    wq_d = P.dram("wq", [D, D], F32, "ExternalInput")
    kT_d = P.dram("kT", [16, 128, 128], F32, "ExternalInput")
    xmid = P.dram("xmid", [NB, 128, D], F32, "ExternalOutput")
    h2T_d = P.dram("h2T", [16, 128, NTOK], BF16, "ExternalOutput")
    s12_d = P.dram("s12", [NB, 128, 16, 128], F32, "ExternalOutput")
    g2_d = P.dram("g2bc", [128, D], F32, "ExternalOutput")
    ident = emit_consts(P, ident_d)
    emit_C1(P, ident, xs, None, mixT_d, None, c_d, adaw, adab, g_d, wo_d, wq_d, kT_d, xmid, h2T_d, s12_d, g2_d)
    P.begin_phase()
    P.end_phase(final=True)
    P.finish()
    return nc


def emit_C1(P, ident, xs, xs_dram, mixT_d, mixT_sb, c_d, adaw, adab, g_d, wo_d, wq_d, kT_d, xmid, h2T_d, s12_d, g2_d):
    P.scope_begin()
    h2T = P.sbs([128, 16, NTOK], BF16, "sbs_h2T")
    P.scope_begin()
    modbc = P.sbs([128, 8192], F32, "sbs_mod2")
    emit_mod(P, c_d, adaw, adab, 8192, modbc)
    P.begin_phase()
    mixT = mixT_sb if mixT_sb is not None else P.sb([128, 16, NTOK], BF16, "sb_mixT")
    wf = P.sb([128, 16, 512], F32, "sb_wof")
    wb = [P.sb([128, 16, 512], BF16, "sb_wob") for _ in range(2)]
    xt = [P.sb([128, 512], F32, "sb_xt") for _ in range(3)]
    tm = [P.sb([128, 512], F32, "sb_tm") for _ in range(2)]
    pp = [P.ps([128, 512], F32, "ps_wo") for _ in range(3)]
    if mixT_sb is None:
        mv = mixT_d.t.rearrange("(k p) t -> p k t", p=128)
        for kh in range(2):
            P.load("sp" if kh == 0 else "act", mixT, mixT[:, kh * 8:(kh + 1) * 8, :], mv[:, kh * 8:(kh + 1) * 8, :])
    wv = wo_d.t.rearrange("(k p) n -> p k n", p=128)
    i = 0
    for dc in range(4):
        w16 = wb[dc % 2]
        for kh in range(2):
            P.load("sp" if kh == 0 else "act", wf, wf[:, kh * 8:(kh + 1) * 8, :], wv[:, kh * 8:(kh + 1) * 8, dc * 512:(dc + 1) * 512])
        P.op("dve", lambda e, w16=w16: e.tensor_copy(out=w16[:, 0:8, :], in_=wf[:, 0:8, :]), reads=[wf], writes=[w16])
        P.op("pool", lambda e, w16=w16: e.tensor_copy(out=w16[:, 8:16, :], in_=wf[:, 8:16, :]), reads=[wf], writes=[w16])
        for b in range(NB):
            ps = pp[i % 3]
            x = xt[i % 3]
            t = tm[i % 2]
            i += 1
            P.load("pool", x, x[:, :], xs[b, :, dc * 512:(dc + 1) * 512], dram=xs_dram)
            for k in range(16):
                P.mm(ps[:, :], mixT[:, k, b * 128:(b + 1) * 128], w16[:, k, :], k == 0, k == 15, reads=[mixT, w16], writes=[ps])
            P.op("dve", lambda e, t=t, ps=ps, dc=dc: e.tensor_tensor(out=t[:, :], in0=ps[:, :], in1=modbc[:, dc * 512:(dc + 1) * 512], op=ALU.mult),
                 reads=[ps, modbc], writes=[t])
            P.op("dve", lambda e, t=t, x=x: e.tensor_tensor(out=x[:, :], in0=x[:, :], in1=t[:, :], op=ALU.add), reads=[x, t], writes=[x])
            P.store("sp", x, xmid[b, :, dc * 512:(dc + 1) * 512], x[:, :], dram=xmid)
    P.end_phase()

    A2 = P.sbs([128, D], F32, "sbs_A2")
    P.begin_phase()
    gb = P.sb([128, D], F32, "sb_g")
    P.load("sp", gb, gb[:, :], g_d[:, :])
    P.op("dve", lambda e: e.scalar_tensor_tensor(out=A2[:, :], in0=modbc[:, 2 * D:3 * D], scalar=1.0, in1=gb[:, :],
                                                 op0=ALU.add, op1=ALU.mult), reads=[modbc, gb], writes=[A2])
    P.store("act", modbc, g2_d[:, :], modbc[:, 3 * D:4 * D], dram=g2_d)
    P.end_phase()
    emit_norm_T(P, xmid, A2, modbc_view(modbc, D, 2 * D), ident, h2T, xdram=xmid)
    P.scope_end()

    P.begin_phase()
    P.store("sp", h2T, h2T_d.t.rearrange("k p t -> p k t"), h2T[:, :, :], dram=h2T_d)
    qT = P.sb([128, 16, NTOK], BF16, "sb_qT")
    kf = P.sb([128, 16, 128], F32, "sb_kf")
    kb_ = P.sb([128, 16, 128], BF16, "sb_kb")
    wf = P.sb([128, 16, 512], F32, "sb_wqf")
    wb = [P.sb([128, 16, 512], BF16, "sb_wqb") for _ in range(2)]
    pp = [P.ps([128, 512], F32, "ps_q") for _ in range(3)]
    pq = [P.ps([128, 4, 128], F32, "ps_s") for _ in range(2)]
    sst = [P.sb([128, 16, 128], F32, "sb_sst") for _ in range(2)]
    P.load("pool", kf, kf[:, :, :], kT_d.t.rearrange("g d n -> d g n"))
    P.op("dve", lambda e: e.tensor_copy(out=kb_[:, :, :], in_=kf[:, :, :]), reads=[kf], writes=[kb_])
    wv = wq_d.t.rearrange("(k p) n -> p k n", p=128)
    i = 0
    for dc in range(4):
        w16 = wb[dc % 2]
        for kh in range(2):
            P.load("sp" if kh == 0 else "act", wf, wf[:, kh * 8:(kh + 1) * 8, :], wv[:, kh * 8:(kh + 1) * 8, dc * 512:(dc + 1) * 512])
        P.op("dve", lambda e, w16=w16: e.tensor_copy(out=w16[:, 0:8, :], in_=wf[:, 0:8, :]), reads=[wf], writes=[w16])
        P.op("pool", lambda e, w16=w16: e.tensor_copy(out=w16[:, 8:16, :], in_=wf[:, 8:16, :]), reads=[wf], writes=[w16])
        for g in range(4):
            gidx = dc * 4 + g
            for th in range(2):
                ps = pp[i % 3]
                i += 1
                for k in range(16):
                    P.mm(ps[:, :], w16[:, k, g * 128:(g + 1) * 128], h2T[:, k, th * 512:(th + 1) * 512], k == 0, k == 15,
                         reads=[w16, h2T], writes=[ps])
                if th == 0:
                    P.op("act", lambda e, ps=ps, gidx=gidx, th=th: e.activation(out=qT[:, gidx, th * 512:(th + 1) * 512], in_=ps[:, :], func=AF.Copy),
                         reads=[ps], writes=[qT])
                else:
                    P.op("dve", lambda e, ps=ps, gidx=gidx, th=th: e.tensor_copy(out=qT[:, gidx, th * 512:(th + 1) * 512], in_=ps[:, :]),
                         reads=[ps], writes=[qT])
    i = 0
    for b in range(NB):
        st = sst[b % 2]
        for g4 in range(4):
            ps = pq[i % 2]
            i += 1
            for q in range(4):
                gidx = g4 * 4 + q
                P.mm(ps[:, q, :], qT[:, gidx, b * 128:(b + 1) * 128], kb_[:, gidx, :], True, True, reads=[qT, kb_], writes=[ps])
            if g4 % 2 == 0:
                P.op("act", lambda e, ps=ps, st=st, g4=g4: e.activation(out=st[:, g4 * 4:(g4 + 1) * 4, :], in_=ps[:, :, :], func=AF.Copy),
                     reads=[ps], writes=[st])
            else:
                P.op("dve", lambda e, ps=ps, st=st, g4=g4: e.tensor_copy(out=st[:, g4 * 4:(g4 + 1) * 4, :], in_=ps[:, :, :]),
                     reads=[ps], writes=[st])
        P.store("pool", st, s12_d[b, :, :, :], st[:, :, :], dram=s12_d)
    P.end_phase()
    P.scope_end()


def host_C1_inputs(xs_list, mix_list, c, ada_w_l, ada_b_l, norm2_g_l, w_out_l, wq_l, k1_l, k2_l):
    kT = np.zeros((16, 128, 128), np.float32)
    for h in range(8):
        kT[2 * h] = k1_l[h].T
        kT[2 * h + 1] = k2_l[h].T
    common = dict(c_pk=np.ascontiguousarray(c.reshape(16, 128).T), adaw=np.ascontiguousarray(ada_w_l[:, 4096:12288]),
                  adab=bc128(ada_b_l[4096:12288]), g_bc=bc128(norm2_g_l), ident=np.eye(128, dtype=NPBF),
                  wo=np.ascontiguousarray(w_out_l), wq=np.ascontiguousarray(wq_l), kT=kT)
    return [dict(common, xs=xs_list[cc], mixT=np.ascontiguousarray(mix_list[cc].T)) for cc in range(8)]


def build_C2(final):
    nc = bass.Bass("TRN2", target_bir_lowering=False)
    P = Prog(nc)
    h2T_d = P.dram("h2T", [16, 128, NTOK], BF16, "ExternalInput")
    s12_d = P.dram("s12", [NB, 128, 16, 128], F32, "ExternalInput")
    xmid = P.dram("xmid", [NB, 128, D], F32, "ExternalInput")
    g2_d = P.dram("g2bc", [128, D], F32, "ExternalInput")
    uT_d = P.dram("uTt", [128, 128, 16, 128], F32, "ExternalInput")
    v_d = P.dram("v", [NE, D], F32, "ExternalInput")
    ident_d = P.dram("ident", [128, 128], BF16, "ExternalInput")
    fg_d = P.dram("fg_bc", [128, D], F32, "ExternalInput") if final else None
    xo = P.dram("xo", [NB, 128, D], F32, "ExternalOutput")
    ident = emit_consts(P, ident_d)
    emit_C2(P, ident, h2T_d, s12_d, xmid, g2_d, uT_d, v_d, fg_d, xo, final, True)
    P.finish()
    return nc


def emit_C2(P, ident, h2T_d, s12_d, xmid, g2_d, uT_d, v_d, fg_d, xo, final, last):
    P.scope_begin()
    kB_zero = P.sbs([128, 1], F32, "sbs_zero")
    g2 = P.sbs([128, D], F32, "sbs_g2")
    stats = P.sbs([128, 4, 8, 2], F32, "sbs_stats")
    P.begin_phase()
    P.load("sp", g2, g2[:, :], g2_d[:, :], dram=g2_d)
    P.op("dve", lambda e: e.memset(kB_zero[:, :], 0.0), writes=[kB_zero])
    P.end_phase()
    for ps_ in range(2):
        P.begin_phase()
        stt_ = [P.sb([128, 16, 128], F32, "sb_st") for _ in range(2)]
        v16 = [P.sb([128, 2, 16], F32, "sb_v16") for _ in range(2)]
        scr = [P.sb([128, 128], F32, "sb_scr") for _ in range(2)]
        cand = [P.sb([128, 16, 16], F32, "sb_cand") for _ in range(2)]
        scr2 = [P.sb([128, 256], F32, "sb_scr2") for _ in range(2)]
        ez = [P.sb([128, 256], F32, "sb_ez") for _ in range(2)]
        c8 = [P.sb([128, 16], F32, "sb_c8") for _ in range(2)]
        sm = [P.sb([128, 4], F32, "sb_sm") for _ in range(2)]
        i = 0
        for bl in range(4):
            b = ps_ * 4 + bl
            st = stt_[bl % 2]
            P.load("sp" if bl % 2 == 0 else "act", st, st[:, :, :], s12_d[b, :, :, :], dram=s12_d)
            for h in range(8):
                vv, sc, cd, s2_, ez_, c8_, sm_ = v16[i % 2], scr[i % 2], cand[i % 2], scr2[i % 2], ez[i % 2], c8[i % 2], sm[i % 2]
                i += 1
                for half in range(2):
                    src = st.t[:, 2 * h + half, :]
                    P.op("dve", lambda e, vv=vv, src=src, half=half: e.max(out=vv[:, half, 0:8], in_=src), reads=[st], writes=[vv])
                    P.op("dve", lambda e, vv=vv, src=src, half=half, sc=sc: e.match_replace(out=sc[:, :], in_to_replace=vv[:, half, 0:8],
                                                                                        in_values=src, imm_value=-1e30),
                         reads=[st, vv], writes=[sc])
                    P.op("dve", lambda e, vv=vv, half=half, sc=sc: e.max(out=vv[:, half, 8:16], in_=sc[:, :]), reads=[sc], writes=[vv])
                P.op("dve", lambda e, vv=vv, cd=cd: e.tensor_tensor(out=cd[:, :, :], in0=vv[:, 0, :].unsqueeze(2).to_broadcast([128, 16, 16]),
                                                                  in1=vv[:, 1, :].unsqueeze(1).to_broadcast([128, 16, 16]), op=ALU.add),
                     reads=[vv], writes=[cd])
                cf = cd.t.rearrange("p a b -> p (a b)")
                P.op("dve", lambda e, c8_=c8_, cf=cf: e.max(out=c8_[:, 0:8], in_=cf), reads=[cd], writes=[c8_])
                P.op("dve", lambda e, c8_=c8_, cf=cf, s2_=s2_: e.match_replace(out=s2_[:, :], in_to_replace=c8_[:, 0:8], in_values=cf, imm_value=-1e30),
                     reads=[cd, c8_], writes=[s2_])
                P.op("dve", lambda e, c8_=c8_, s2_=s2_: e.max(out=c8_[:, 8:16], in_=s2_[:, :]), reads=[s2_], writes=[c8_])
                P.op("dve", lambda e, c8_=c8_, sm_=sm_: e.tensor_scalar(out=sm_[:, 0:1], in0=c8_[:, 0:1], scalar1=-1.0, scalar2=None, op0=ALU.mult),
                     reads=[c8_], writes=[sm_])
                P.op("act", lambda e, ez_=ez_, cf=cf, sm_=sm_: e.activation(out=ez_[:, :], in_=cf, func=AF.Exp, bias=sm_[:, 0:1]),
                     reads=[cd, sm_], writes=[ez_])
                P.op("dve", lambda e, s2_=s2_, cf=cf, c8_=c8_, ez_=ez_, sm_=sm_: e.scalar_tensor_tensor(
                    out=s2_[:, :], in0=cf, scalar=c8_[:, 15:16], in1=ez_[:, :], op0=ALU.is_ge, op1=ALU.mult, accum_out=sm_[:, 1:2]),
                    reads=[cd, c8_, ez_], writes=[s2_, sm_])
                P.op("act", lambda e, sm_=sm_: e.activation(out=sm_[:, 2:3], in_=sm_[:, 1:2], func=AF.Ln, bias=kB_zero[:, 0:1]),
                     reads=[sm_, kB_zero], writes=[sm_])
                P.op("dve", lambda e, sm_=sm_, c8_=c8_, bl=bl, h=h: e.tensor_scalar(out=stats[:, bl, h, 1:2], in0=sm_[:, 2:3], scalar1=c8_[:, 0:1],
                                                                                  scalar2=-1.0, op0=ALU.add, op1=ALU.mult),
                     reads=[sm_, c8_], writes=[stats])
                P.op("dve", lambda e, c8_=c8_, bl=bl, h=h: e.tensor_copy(out=stats[:, bl, h, 0:1], in_=c8_[:, 15:16]), reads=[c8_], writes=[stats])
        P.end_phase()

        P.scope_begin()
        oacc = P.sbs([128, 4, D], F32, "sbs_oacc")
        P.begin_phase()
        h2p = P.sb([128, 16, 512], BF16, "sb_h2p")
        s12t = [P.sb([128, 16, 128], F32, "sb_s12t") for _ in range(2)]
        Xb = [P.sb([128, 8, 4, 128], F32, "sb_X") for _ in range(1)]
        Eb = [P.sb([128, 8, 4, 128], BF16, "sb_E") for _ in range(1)]
        Mb = [P.sb([128, 8, 4, 128], F32, "sb_Y") for _ in range(1)]
        Tb = [P.sb([128, 8, 4, 128], BF16, "sb_T") for _ in range(2)]
        cexp = P.sb([128, 4, 8], F32, "sb_cexp")
        Dg = P.sb([128, 4, 8, 128], BF16, "sb_Dg")
        GT = [P.sb([128, 4, 512], BF16, "sb_GT") for _ in range(2)]
        GAT = [P.sb([128, 4, 512], BF16, "sb_GAT") for _ in range(2)]
        uf = [P.sb([128, 16, 128], F32, "sb_uf") for _ in range(1)]
        ub = [P.sb([128, 16, 128], BF16, "sb_ub") for _ in range(2)]
        vf = [P.sb([128, D], F32, "sb_vf") for _ in range(2)]
        vb = [P.sb([128, 4, D], BF16, "sb_vb") for _ in range(1)]
        ge = [P.sb([128, 512], F32, "sb_ge") for _ in range(2)]
        pG = [P.ps([128, 4, 128], F32, "ps_G") for _ in range(2)]
        pA = [P.ps([128, 512], F32, "ps_A") for _ in range(2)]
        pD = [P.ps([128, 512], F32, "ps_D") for _ in range(3)]
        hv = h2T_d.t.rearrange("k p t -> p k t")
        for kh in range(2):
            P.load("sp" if kh == 0 else "act", h2p, h2p[:, kh * 8:(kh + 1) * 8, :], hv[:, kh * 8:(kh + 1) * 8, ps_ * 512:(ps_ + 1) * 512], dram=h2T_d)
        P.op("act", lambda e: e.activation(out=cexp[:, :, :], in_=stats[:, :, :, 1], func=AF.Exp, bias=kB_zero[:, 0:1]),
             reads=[stats, kB_zero], writes=[cexp])
        for bl in range(4):
            for h in range(8):
                P.op("dve", lambda e, bl=bl, h=h: e.tensor_scalar(out=Dg[:, bl, h, :], in0=ident[:, :], scalar1=cexp[:, bl, h:h + 1],
                                                                 scalar2=None, op0=ALU.mult), reads=[ident, cexp], writes=[Dg])
        si = 0
        xi = 0
        di = 0
        for g in range(32):
            gt, gat, vb_ = GT[g % 2], GAT[g % 2], vb[0]
            for bl in range(4):
                b = ps_ * 4 + bl
                st = s12t[si % 2]
                T = Tb[si % 2]
                pg = pG[si % 2]
                si += 1
                P.load("sp" if si % 2 == 0 else "act", st, st[:, :, :], s12_d[b, :, :, :], dram=s12_d)
                X, E, M = Xb[0], Eb[0], Mb[0]
                xi += 1
                stv = st.t.rearrange("p (h two) n -> p h two n", two=2)
                P.op("dve", lambda e, X=X, stv=stv, g=g: e.tensor_tensor(
                    out=X[:, :, :, :], in0=stv[:, :, 1, :].unsqueeze(2).to_broadcast([128, 8, 4, 128]),
                    in1=stv[:, :, 0, 4 * g:4 * g + 4].unsqueeze(3).to_broadcast([128, 8, 4, 128]), op=ALU.add), reads=[st], writes=[X])
                P.op("act", lambda e, X=X, E=E: e.activation(out=E[:, :, :, :], in_=X[:, :, :, :], func=AF.Exp, bias=kB_zero[:, 0:1]),
                     reads=[X, kB_zero], writes=[E])
                P.op("pool", lambda e, X=X, M=M, bl=bl: e.tensor_tensor(
                    out=M[:, :, :, :], in0=X[:, :, :, :],
                    in1=stats[:, bl, :, 0].unsqueeze(2).unsqueeze(3).to_broadcast([128, 8, 4, 128]), op=ALU.subtract),
                    reads=[X, stats], writes=[M])
                P.op("dve", lambda e, E=E, M=M, T=T: e.scalar_tensor_tensor(out=T[:, :, :, :], in0=M[:, :, :, :], scalar=0.0, in1=E[:, :, :, :],
                                                                            op0=ALU.is_ge, op1=ALU.mult), reads=[E, M], writes=[T])
                for a in range(4):
                    for h in range(8):
                        P.mm(pg[:, a, :], T[:, h, a, :], Dg[:, bl, h, :], h == 0, h == 7, reads=[T, Dg], writes=[pg])
                P.op("act", lambda e, gt=gt, pg=pg, bl=bl: e.activation(out=gt[:, :, bl * 128:(bl + 1) * 128], in_=pg[:, :, :], func=AF.Copy),
                     reads=[pg], writes=[gt])
            for a in range(4):
                ec = 4 * g + a
                u32, u16, v32, pa, gel = uf[0], ub[ec % 2], vf[ec % 2], pA[ec % 2], ge[ec % 2]
                P.load("sp", u32, u32[:, :, :], uT_d[ec, :, :, :])
                P.load("act", v32, v32[:, :], v_d[ec * 128:(ec + 1) * 128, :])
                P.op("pool", lambda e, u32=u32, u16=u16: e.tensor_copy(out=u16[:, :, :], in_=u32[:, :, :]), reads=[u32], writes=[u16])
                for k in range(16):
                    P.mm(pa[:, :], u16[:, k, :], h2p[:, k, :], k == 0, k == 15, reads=[u16, h2p], writes=[pa])
                P.op("act", lambda e, gel=gel, pa=pa: e.activation(out=gel[:, :], in_=pa[:, :], func=AF.Gelu_apprx_tanh), reads=[pa], writes=[gel])
                P.op("dve", lambda e, gat=gat, gel=gel, gt=gt, a=a: e.tensor_tensor(out=gat[:, a, :], in0=gel[:, :], in1=gt[:, a, :], op=ALU.mult),
                     reads=[gel, gt], writes=[gat])
                P.op("pool", lambda e, v32=v32, vb_=vb_, a=a: e.tensor_copy(out=vb_[:, a, 0:1024], in_=v32[:, 0:1024]), reads=[v32], writes=[vb_])
                P.op("act", lambda e, v32=v32, vb_=vb_, a=a: e.activation(out=vb_[:, a, 1024:2048], in_=v32[:, 1024:2048], func=AF.Copy),
                     reads=[v32], writes=[vb_])
            for bl in range(4):
                for dc in range(4):
                    pd = pD[di % 3]
                    di += 1
                    for a in range(4):
                        P.mm(pd[:, :], gat[:, a, bl * 128:(bl + 1) * 128], vb_[:, a, dc * 512:(dc + 1) * 512], a == 0, a == 3,
                             reads=[gat, vb_], writes=[pd])
                    if g == 0:
                        P.op("act", lambda e, pd=pd, bl=bl, dc=dc: e.activation(out=oacc[:, bl, dc * 512:(dc + 1) * 512], in_=pd[:, :], func=AF.Copy),
                             reads=[pd], writes=[oacc])
                    else:
                        P.op("dve", lambda e, pd=pd, bl=bl, dc=dc: e.tensor_tensor(out=oacc[:, bl, dc * 512:(dc + 1) * 512],
                                                                                 in0=oacc[:, bl, dc * 512:(dc + 1) * 512], in1=pd[:, :], op=ALU.add),
                             reads=[pd, oacc], writes=[oacc])
        P.end_phase()
        P.begin_phase()
        xt = [P.sb([128, D], F32, "sb_xf") for _ in range(2)]
        jk = P.sb([128, D], BF16, "sb_jk")
        s4 = [P.sb([128, 4], F32, "sb_s4") for _ in range(2)]
        if final:
            fg = P.sb([128, D], F32, "sb_fg")
            P.load("sp", fg, fg[:, :], fg_d[:, :])
        for bl in range(4):
            b = ps_ * 4 + bl
            x = xt[bl % 2]
            s = s4[bl % 2]
            P.load("sp" if bl % 2 == 0 else "act", x, x[:, :], xmid[b, :, :], dram=xmid)
            P.op("dve", lambda e, bl=bl: e.tensor_tensor(out=oacc[:, bl, :], in0=oacc[:, bl, :], in1=g2[:, :], op=ALU.mult),
                 reads=[oacc, g2], writes=[oacc])
            P.op("pool", lambda e, x=x, bl=bl: e.tensor_tensor(out=x[:, :], in0=x[:, :], in1=oacc[:, bl, :], op=ALU.add),
                 reads=[x, oacc], writes=[x])
            if final:
                P.op("act", lambda e, x=x, s=s: e.activation(out=jk[:, :], in_=x[:, :], func=AF.Square, accum_out=s[:, 0:1]),
                     reads=[x], writes=[jk, s])
                P.op("act", lambda e, s=s: e.activation(out=s[:, 1:2], in_=s[:, 0:1], func=AF.Sqrt, scale=1.0 / D, bias=EPSB[0][:, 0:1]),
                     reads=[s, EPSB[0]], writes=[s])
                P.op("dve", lambda e, s=s: e.reciprocal(out=s[:, 2:3], in_=s[:, 1:2]), reads=[s], writes=[s])
                P.op("dve", lambda e, x=x, s=s: e.scalar_tensor_tensor(out=x[:, :], in0=x[:, :], scalar=s[:, 2:3], in1=fg[:, :],
                                                                     op0=ALU.mult, op1=ALU.mult), reads=[x, s, fg], writes=[x])
            P.store("pool", x, xo[b, :, :], x[:, :], dram=xo)
        P.end_phase(final=(last and ps_ == 1))
        P.scope_end()
    P.scope_end()


def uT_tiles(u_l):
    return np.ascontiguousarray(u_l.reshape(128, 128, 16, 128).transpose(0, 3, 2, 1))


NFMR = NFM
RM = "(c r) t -> r c t"


def emit_A_f(P, xs_src, xdram, c_d, adaw, adab, g_d, wfm, wtm, ident, zfm_s, ztb_s, ztf_s):
    P.scope_begin()
    modbc = P.sbs([128, 4096], F32, "sbs_mod")
    A1 = P.sbs([128, D], F32, "sbs_A1")
    hT = P.sbs([128, 16, NTOK], BF16, "sbs_hT")
    emit_mod(P, c_d, adaw, adab, 4096, modbc)
    P.begin_phase()
    gb = P.sb([128, D], F32, "sb_g")
    P.load("sp", gb, gb[:, :], g_d[:, :])
    P.op("dve", lambda e: e.scalar_tensor_tensor(out=A1[:, :], in0=modbc[:, D:2 * D], scalar=1.0, in1=gb[:, :],
                                                 op0=ALU.add, op1=ALU.mult), reads=[modbc, gb], writes=[A1])
    P.end_phase()
    emit_norm_T(P, xs_src, A1, modbc_view(modbc, 0, D), ident, hT, xdram=xdram)
    emit_proj(P, hT, [dict(w=wfm, kind="fm", outs=[(zfm_s, 0, NFM, BF16)]),
                      dict(w=wtm, kind="tm", outs=[(ztb_s, 0, 2560, BF16), (ztf_s, 2560, 3200, F32)])])
    P.scope_end()


def emit_fox_bias_f(P, ztf_all, fb_d, tri_d, sel_d, bias):
    P.begin_phase()
    ff2 = P.sb([128, 64, 6], F32, "sb_ff2")
    fb = P.sb([128, 6], F32, "sb_fb")
    tri = P.sb([128, 128], F32, "sb_tri")
    ones = P.sb([128, 128], F32, "sb_ones")
    onec = P.sb([128, 1], F32, "sb_onec")
    sel = P.sb([128, 8], F32, "sb_sel")
    nlf = P.sb([128, 6, 64], F32, "sb_nlf")
    tot = P.sb([128, 6, 64], F32, "sb_tot")
    incl = P.sb([128, 6, 64], F32, "sb_incl")
    NF = P.sb([128, 6, 64], F32, "sb_NF")
    tmp = P.sb([128, 6, 8, 8], F32, "sb_tmp")
    nfe = P.sb([128, 6, 8], F32, "sb_nfe")
    pw = P.ps([128, 384], F32, "ps_w")
    pt_ = P.ps([128, 384], F32, "ps_t")
    src = ztf_all.t.rearrange("(c j p) w -> p j c w", c=8, j=8, p=128)
    ffv = ff2.t.rearrange("p (j c) h -> p j c h", c=8)
    for jh in range(8):
        P.load("sp" if jh % 2 == 0 else "act", ff2, ffv[:, jh, :, :], src[:, jh, :, 512:518], dram=ztf_all)
    P.load("act", fb, fb[:, :], fb_d[:, :])
    P.load("sp", tri, tri[:, :], tri_d[:, :])
    P.load("act", sel, sel[:, :], sel_d[:, :])
    P.op("dve", lambda e: e.memset(ones[:, :], 1.0), writes=[ones])
    P.op("dve", lambda e: e.memset(onec[:, :], 1.0), writes=[onec])
    P.op("dve", lambda e: e.tensor_tensor(out=nlf[:, :, :], in0=ff2.t.rearrange("p k h -> p h k"),
                                          in1=fb[:, :].unsqueeze(2).to_broadcast([128, 6, 64]), op=ALU.add), reads=[ff2, fb], writes=[nlf])
    nlf2 = nlf.t.rearrange("p h k -> p (h k)")
    P.op("act", lambda e: e.activation(out=nlf2, in_=nlf2, func=AF.Exp, scale=-1.0), reads=[nlf], writes=[nlf])
    P.op("act", lambda e: e.activation(out=nlf2, in_=nlf2, func=AF.Ln, bias=onec[:, 0:1]), reads=[nlf, onec], writes=[nlf])
    P.mm(pw[:, :], tri[:, :], nlf2, True, True, reads=[tri, nlf], writes=[pw])
    P.mm(pt_[:, :], ones[:, :], nlf2, True, True, reads=[ones, nlf], writes=[pt_])
    tot2 = tot.t.rearrange("p h k -> p (h k)")
    P.op("dve", lambda e: e.tensor_copy(out=tot2, in_=pt_[:, :]), reads=[pt_], writes=[tot])
    for h in range(6):
        P.op("dve", lambda e, h=h: e.tensor_tensor_scan(out=incl[:, h, :], data0=ones[:, 0:64], data1=tot[:, h, :], initial=0.0,
                                                        op0=ALU.mult, op1=ALU.add), reads=[ones, tot], writes=[incl])
    NF2 = NF.t.rearrange("p h k -> p (h k)")
    incl2 = incl.t.rearrange("p h k -> p (h k)")
    P.op("dve", lambda e: e.tensor_tensor(out=NF2, in0=pw[:, :], in1=incl2, op=ALU.add), reads=[pw, incl], writes=[NF])
    P.op("dve", lambda e: e.tensor_tensor(out=NF2, in0=NF2, in1=tot2, op=ALU.subtract), reads=[NF, tot], writes=[NF])
    P.op("dve", lambda e: e.tensor_tensor(out=tmp[:, :, :, :], in0=incl.t.rearrange("p h (j r) -> p h j r", r=8),
                                          in1=sel[:, :].unsqueeze(1).unsqueeze(1).to_broadcast([128, 6, 8, 8]), op=ALU.mult),
         reads=[incl, sel], writes=[tmp])
    P.op("dve", lambda e: e.tensor_reduce(out=nfe[:, :, :], in_=tmp[:, :, :, :], axis=AX.X, op=ALU.add), reads=[tmp], writes=[nfe])
    for h in range(6):
        for j in range(8):
            P.op("dve", lambda e, h=h, j=j: e.tensor_scalar(out=bias[:, h, j, :], in0=NF[:, h, :], scalar1=nfe[:, h, j:j + 1],
                                                            scalar2=0.0, op0=ALU.subtract, op1=ALU.min),
                 reads=[NF, nfe], writes=[bias])
    P.end_phase()


def emit_attn_f(P, zfm_all, krow0, ztb_all, vcol0, zfm_s, qrow0, nheads, out_stage, bias=None, maskT=None, cm=None):
    P.begin_phase()
    KT = [P.sb([128, S], BF16, "sb_KT") for _ in range(2)]
    Vg = [P.sb([128, NKB, 129], BF16, "sb_Vg") for _ in range(2)]
    QT = [P.sb([128, 1024], BF16, "sb_QT") for _ in range(2)]
    pO = [P.ps([128, 512], F32, "ps_O") for _ in range(2)]
    pS = []
    for _ in range(2):
        bank = P.ps([128, 4, 128], F32, "ps_S")
        for q in range(4):
            pS.append(Buf(bank.t[:, q, :], bank.name + "_q%d" % q))
    Pt = [P.sb([128, 128], BF16, "sb_Pt") for _ in range(6)]
    rc = [P.sb([128, 1], F32, "sb_rc") for _ in range(2)]
    zero_b = P.sb([128, 1], F32, "sb_zb")
    P.op("dve", lambda e: e.memset(zero_b[:, :], 0.0), writes=[zero_b])
    for vg in Vg:
        P.op("pool", lambda e, vg=vg: e.memset(vg[:, :, 128:129], 1.0), writes=[vg])
    kv = zfm_all.t.rearrange(RM, c=8)
    vv = ztb_all.t.rearrange("(m p) w -> p m w", p=128)
    si = pi = oi = 0
    for h in range(nheads):
        kt, vg, qt = KT[h % 2], Vg[h % 2], QT[h % 2]
        ktv = kt.t.rearrange("d (c t) -> d c t", c=8)
        for q4 in range(4):
            P.load("sp" if q4 % 2 == 0 else "act", kt, ktv[:, q4 * 2:(q4 + 1) * 2, :],
                   kv[krow0 + h * 128:krow0 + (h + 1) * 128, q4 * 2:(q4 + 1) * 2, :], dram=zfm_all)
        for q8 in range(8):
            P.load("sp" if q8 % 2 == 0 else "act", vg, vg[:, q8 * 8:(q8 + 1) * 8, 0:128],
                   vv[:, q8 * 8:(q8 + 1) * 8, vcol0 + h * 128:vcol0 + (h + 1) * 128], dram=ztb_all)
        P.load("sp", qt, qt[:, :], zfm_s.t[qrow0 + h * 128:qrow0 + (h + 1) * 128, :], dram=zfm_s)
        for j in range(8):
            blocks = [(cp * 8 + jp, 8 * jp + cp, (cp if jp == j else None), cp * (j + 1) + jp) for jp in range(j + 1) for cp in range(8)]
            nkb = len(blocks)
            po = pO[oi % 2]
            oi += 1
            for bi, (m, gb, r, q) in enumerate(blocks):
                ps = pS[si % 8]
                si += 1
                pt = Pt[pi % 6]
                pi += 1
                P.mm(ps[:, :], kt[:, m * 128:(m + 1) * 128], qt[:, j * 128:(j + 1) * 128], True, True, reads=[kt, qt], writes=[ps])
                if bias is not None:
                    P.op("act", lambda e, pt=pt, ps=ps, h=h, j=j, gb=gb: e.activation(
                        out=pt[:, :], in_=ps[:, :], func=AF.Exp, scale=SCALE, bias=bias[:, h, j, gb:gb + 1]),
                        reads=[ps, bias], writes=[pt])
                else:
                    P.op("act", lambda e, pt=pt, ps=ps: e.activation(
                        out=pt[:, :], in_=ps[:, :], func=AF.Exp, scale=SCALE, bias=zero_b[:, 0:1]),
                        reads=[ps, zero_b], writes=[pt])
                if maskT is not None:
                    mt = maskT[j]
                    P.op("dve", lambda e, pt=pt, mt=mt, q=q: e.tensor_tensor(out=pt[:, :], in0=pt[:, :], in1=mt[:, q, :], op=ALU.mult),
                         reads=[pt, mt], writes=[pt])
                elif r is not None:
                    P.op("dve", lambda e, pt=pt, r=r: e.tensor_tensor(out=pt[:, :], in0=pt[:, :], in1=cm[:, r, :], op=ALU.mult),
                         reads=[pt, cm], writes=[pt])
                P.mm(po[:, 0:129], pt[:, :], vg[:, m, :], bi == 0, bi == nkb - 1, reads=[pt, vg], writes=[po])
            r_ = rc[oi % 2]
            P.op("dve", lambda e, r_=r_, po=po: e.reciprocal(out=r_[:, 0:1], in_=po[:, 128:129]), reads=[po], writes=[r_])
            P.op("dve", lambda e, r_=r_, po=po, j=j, h=h: e.tensor_scalar(
                out=out_stage[:, j, h * 128:(h + 1) * 128], in0=po[:, 0:128], scalar1=r_[:, 0:1], scalar2=None,
                op0=ALU.mult), reads=[po, r_], writes=[out_stage])
    P.end_phase()


def emit_dsa_select_f(P, zfm_all, zfm_s, ztf_s, am_d, ident, maskT):
    P.begin_phase()
    score = P.sb([128, S], F32, "sb_score")
    junk = P.sb([128, S], BF16, "sb_junk")
    ikT = P.sb([64, S], BF16, "sb_ikT")
    iq = [P.sb([64, 16, 128], BF16, "sb_iq") for _ in range(2)]
    rr = [P.sb([128, 512], F32, "sb_rr") for _ in range(3)]
    am = P.sb([128, 8, 128], F32, "sb_am")
    iw = P.sb([128, 8, 16], F32, "sb_iw")
    wsc = P.sb([128, 8, 16], F32, "sb_wsc")
    pI = [P.ps([128, 512], F32, "ps_I") for _ in range(4)]
    pT = [P.ps([128, 8, 128], BF16, "ps_mT") for _ in range(2)]
    mch = [P.sb([128, 1024], BF16, "sb_mch") for _ in range(2)]
    sms = [P.sb([128, 8], F32, "sb_sm") for _ in range(2)]
    kv = zfm_all.t.rearrange(RM, c=8)
    ikv = ikT.t.rearrange("d (c t) -> d c t", c=8)
    for q4 in range(4):
        P.load("sp" if q4 % 2 == 0 else "act", ikT, ikv[:, q4 * 2:(q4 + 1) * 2, :], kv[R_IK:R_IK + 64, q4 * 2:(q4 + 1) * 2, :], dram=zfm_all)
    P.load("sp", am, am[:, :, :], am_d[:, :, :])
    P.load("act", iw, iw[:, :, :], ztf_s.t.rearrange("(j p) w -> p j w", p=128)[:, :, 518:534], dram=ztf_s)
    P.op("dve", lambda e: e.tensor_scalar(out=wsc[:, :, :], in0=iw[:, :, :], scalar1=(64 ** -0.5) * (16 ** -0.5), scalar2=None,
                                          op0=ALU.mult), reads=[iw], writes=[wsc])
    iqsrc = zfm_s.t[R_IQ:R_IQ + 1024, :].rearrange("(h d) t -> d h t", d=64)
    ii = ti = 0
    for j in range(8):
        W = (j + 1) * 128
        L = 8 * W
        iqj = iq[j % 2]
        P.load("sp", iqj, iqj[:, :, :], iqsrc[:, :, j * 128:(j + 1) * 128], dram=zfm_s)
        sm = sms[j % 2]
        for cp in range(8):
            for w0 in range(0, W, 512):
                ww = min(512, W - w0)
                sc = score.t[:, cp * W + w0:cp * W + w0 + ww]
                kcol = cp * 1024 + w0
                for h in range(16):
                    ps = pI[ii % 4]
                    r = rr[ii % 3]
                    ii += 1
                    P.mm(ps[:, 0:ww], iqj[:, h, :], ikT[:, kcol:kcol + ww], True, True, reads=[iqj, ikT], writes=[ps])
                    P.op("act", lambda e, r=r, ps=ps, ww=ww: e.activation(out=r[:, 0:ww], in_=ps[:, 0:ww], func=AF.Relu), reads=[ps], writes=[r])
                    if h == 0:
                        P.op("dve", lambda e, sc=sc, r=r, j=j, ww=ww: e.tensor_scalar(out=sc, in0=r[:, 0:ww], scalar1=wsc[:, j, 0:1], scalar2=None,
                                                                                    op0=ALU.mult), reads=[r, wsc], writes=[score])
                    else:
                        P.op("dve", lambda e, sc=sc, r=r, j=j, h=h, ww=ww: e.scalar_tensor_tensor(
                            out=sc, in0=r[:, 0:ww], scalar=wsc[:, j, h:h + 1], in1=sc, op0=ALU.mult, op1=ALU.add),
                            reads=[r, wsc, score], writes=[score])
        sL = score.t[:, 0:L]
        P.op("dve", lambda e, sm=sm, sL=sL: e.tensor_reduce(out=sm[:, 0:1], in_=sL, axis=AX.X, op=ALU.max, apply_absolute_value=True),
             reads=[score], writes=[sm])
        P.op("dve", lambda e, sm=sm: e.tensor_scalar(out=sm[:, 0:1], in0=sm[:, 0:1], scalar1=1.001, scalar2=1e-3, op0=ALU.mult, op1=ALU.add),
             reads=[sm], writes=[sm])
        P.op("dve", lambda e, sm=sm: e.tensor_scalar(out=sm[:, 1:2], in0=sm[:, 0:1], scalar1=-1.0, scalar2=None, op0=ALU.mult),
             reads=[sm], writes=[sm])
        sB = score.t[:, 0:L].rearrange("p (c w) -> p c w", c=8)[:, :, j * 128:(j + 1) * 128]
        P.op("dve", lambda e, sB=sB: e.tensor_tensor(out=sB, in0=sB, in1=am[:, :, :], op=ALU.add), reads=[score, am], writes=[score])
        for k in range(1, NBIS + 1):
            f = 2.0 ** (1 - k)
            P.op("dve", lambda e, sm=sm, f=f: e.tensor_scalar(out=sm[:, 2:3], in0=sm[:, 0:1], scalar1=f, scalar2=sm[:, 1:2],
                                                            op0=ALU.mult, op1=ALU.add), reads=[sm], writes=[sm])
            P.op("dve", lambda e, sm=sm, sL=sL, L=L: e.tensor_scalar(out=junk[:, 0:L], in0=sL, scalar1=sm[:, 2:3], scalar2=0.0,
                                                                   op0=ALU.is_ge, op1=ALU.add, accum_out=sm[:, 3:4]),
                 reads=[score, sm], writes=[junk, sm])
            P.op("dve", lambda e, sm=sm, f=f: e.tensor_scalar(out=sm[:, 4:5], in0=sm[:, 3:4], scalar1=TOPK - 0.5, scalar2=f,
                                                            op0=ALU.is_ge, op1=ALU.mult), reads=[sm], writes=[sm])
            P.op("dve", lambda e, sm=sm: e.scalar_tensor_tensor(out=sm[:, 1:2], in0=sm[:, 4:5], scalar=sm[:, 0:1], in1=sm[:, 1:2],
                                                              op0=ALU.mult, op1=ALU.add), reads=[sm], writes=[sm])
        for g in range(L // 1024):
            mc = mch[ti % 2]
            pt = pT[ti % 2]
            ti += 1
            P.op("dve", lambda e, mc=mc, g=g, sm=sm: e.tensor_scalar(out=mc[:, :], in0=score[:, g * 1024:(g + 1) * 1024],
                                                                   scalar1=sm[:, 1:2], scalar2=None, op0=ALU.is_ge),
                 reads=[score, sm], writes=[mc])
            for q in range(8):
                P.op("pe", lambda e, pt=pt, mc=mc, q=q: e.transpose(out=pt[:, q, :], in_=mc[:, q * 128:(q + 1) * 128], identity=ident[:, :]),
                     reads=[mc, ident], writes=[pt])
            mt = maskT[j]
            P.op("act", lambda e, mt=mt, pt=pt, g=g: e.activation(out=mt[:, g * 8:(g + 1) * 8, :], in_=pt[:, :, :], func=AF.Copy),
                 reads=[pt], writes=[mt])
    P.end_phase()


def emit_gla_f(P, zfm_all, ztb_all, ztf_all, sel4_d, wa2_d, nbacol_d, barow_d, gn_d, tri2_d, suf2_d, rmask_d, gla_s):
    P.scope_begin()
    keep = P.sbs
    qtT = keep([128, S], BF16, "sbk_qtT")
    ktT = keep([128, S], BF16, "sbk_ktT")
    dn = keep([128, 128], F32, "sbk_dn")
    Sb = keep([128, 128, 128], BF16, "sbk_Sb")
    vtm = keep([128, 64, 128], BF16, "sbk_v")
    gs = keep([128, 64, 128], BF16, "sbk_gs")
    wa2b = keep([16, 128], BF16, "sbk_wa2b")
    gaT = keep([16, S], BF16, "sbk_gaT")
    tri2 = keep([128, 128], F32, "sbk_tri2")
    onec = keep([128, 1], F32, "sbk_onec")
    epsc = keep([128, 1], F32, "sbk_epsc")
    sel4 = keep([128, 4], F32, "sbk_sel4")
    kv = zfm_all.t.rearrange(RM, c=8)

    P.begin_phase()
    wa2f = P.sb([16, 128], F32, "sb_wa2f")
    nbac = P.sb([128, 1], F32, "sb_nbac")
    rmask = P.sb([128, 512], F32, "sb_rmask")
    gq4 = [P.sb([128, 4, 512], BF16, "sb_gq4") for _ in range(2)]
    gk4 = [P.sb([128, 4, 512], BF16, "sb_gk4") for _ in range(2)]
    gq = [P.sb([128, 512], F32, "sb_gq") for _ in range(2)]
    gk = [P.sb([128, 512], F32, "sb_gk") for _ in range(2)]
    e1 = [P.sb([128, 512], F32, "sb_e1") for _ in range(2)]
    cs = [P.sb([128, 512], F32, "sb_cs") for _ in range(2)]
    eg = [P.sb([128, 512], F32, "sb_eg") for _ in range(2)]
    en = [P.sb([128, 512], F32, "sb_en") for _ in range(2)]
    pg = [P.ps([128, 512], F32, "ps_g") for _ in range(2)]
    P.load("sp", wa2f, wa2f[:, :], wa2_d[:, :])
    P.load("act", nbac, nbac[:, :], nbacol_d[:, :])
    P.load("sp", rmask, rmask[:, :], rmask_d[:, :])
    P.load("act", tri2, tri2[:, :], tri2_d[:, :])
    P.load("sp", sel4, sel4[:, :], sel4_d[:, :])
    gav = gaT.t.rearrange("r (j c p) -> r j c p", j=8, c=8)
    gas = kv[R_GA:R_GA + 16, :, :].rearrange("r c (j p) -> r j c p", p=128)
    for jh in range(8):
        P.load("sp" if jh % 2 == 0 else "act", gaT, gav[:, jh, :, :], gas[:, jh, :, :], dram=zfm_all)
    P.op("dve", lambda e: e.tensor_copy(out=wa2b[:, :], in_=wa2f[:, :]), reads=[wa2f], writes=[wa2b])
    P.op("dve", lambda e: e.memset(onec[:, :], 1.0), writes=[onec])
    P.op("dve", lambda e: e.memset(epsc[:, :], 1e-6), writes=[epsc])

    def sel_acc(eng, out_ap, src4, nh_view, outbuf, srcbuf):
        for h in range(4):
            if h == 0:
                P.op(eng, lambda e, h=h: e.tensor_scalar(out=out_ap, in0=nh_view(h), scalar1=sel4[:, 0:1], scalar2=None, op0=ALU.mult),
                     reads=[srcbuf, sel4], writes=[outbuf])
            else:
                P.op("dve", lambda e, h=h: e.scalar_tensor_tensor(out=out_ap, in0=nh_view(h), scalar=sel4[:, h:h + 1], in1=out_ap,
                                                                  op0=ALU.mult, op1=ALU.add), reads=[srcbuf, sel4, outbuf], writes=[outbuf])

    for tc in range(16):
        sl = slice(tc * 512, (tc + 1) * 512)
        j_ = tc // 2
        c0 = (tc % 2) * 4
        a4, b4 = gq4[tc % 2], gk4[tc % 2]
        a, b_ = gq[tc % 2], gk[tc % 2]
        for (dst4, row0, q) in ((a4, R_GQ, "sp"), (b4, R_GK, "act")):
            for hh in range(4):
                srcv = kv[row0 + hh * 128:row0 + (hh + 1) * 128, c0:c0 + 4, j_ * 128:(j_ + 1) * 128]
                P.load(q, dst4, dst4.t[:, hh, :].rearrange("d (c p) -> d c p", p=128), srcv, dram=zfm_all)
        sel_acc("dve", a[:, :], a4, lambda h, a4=a4: a4[:, h, :], a, a4)
        sel_acc("dve", b_[:, :], b4, lambda h, b4=b4: b4[:, h, :], b_, b4)
        ps = pg[tc % 2]
        x1, c1, g1, n1 = e1[tc % 2], cs[tc % 2], eg[tc % 2], en[tc % 2]
        P.mm(ps[:, :], wa2b[:, :], gaT[:, sl], True, True, reads=[wa2b, gaT], writes=[ps])
        P.op("act", lambda e, x1=x1, ps=ps: e.activation(out=x1[:, :], in_=ps[:, :], func=AF.Exp, scale=-1.0, bias=nbac[:, 0:1]),
             reads=[ps, nbac], writes=[x1])
        P.op("act", lambda e, x1=x1: e.activation(out=x1[:, :], in_=x1[:, :], func=AF.Ln, bias=onec[:, 0:1]), reads=[x1, onec], writes=[x1])
        P.op("dve", lambda e, x1=x1, c1=c1: e.tensor_tensor_scan(out=c1[:, :], data0=rmask[:, :], data1=x1[:, :], initial=0.0,
                                                               op0=ALU.mult, op1=ALU.add), reads=[rmask, x1], writes=[c1])
        P.op("act", lambda e, c1=c1, g1=g1: e.activation(out=g1[:, :], in_=c1[:, :], func=AF.Exp, scale=-1.0 / 16, bias=ZB[0][:, 0:1]),
             reads=[c1, ZB[0]], writes=[g1])
        P.op("act", lambda e, c1=c1, n1=n1: e.activation(out=n1[:, :], in_=c1[:, :], func=AF.Exp, scale=1.0 / 16, bias=ZB[0][:, 0:1]),
             reads=[c1, ZB[0]], writes=[n1])
        P.op("dve", lambda e, a=a, g1=g1, sl=sl: e.scalar_tensor_tensor(out=qtT[:, sl], in0=a[:, :], scalar=SCALE, in1=g1[:, :],
                                                                      op0=ALU.mult, op1=ALU.mult), reads=[a, g1], writes=[qtT])
        P.op("pool", lambda e, b_=b_, n1=n1, sl=sl: e.tensor_tensor(out=ktT[:, sl], in0=b_[:, :], in1=n1[:, :], op=ALU.mult),
             reads=[b_, n1], writes=[ktT])
        P.op("dve", lambda e, g1=g1, tc=tc: e.tensor_copy(out=dn[:, tc * 8:(tc + 1) * 8],
                                                        in_=g1.t.rearrange("p (n c) -> p n c", c=64)[:, :, 63]),
             reads=[g1], writes=[dn])
    P.end_phase()

    P.begin_phase()
    barow = P.sb([128, 128], F32, "sb_barow")
    gn = P.sb([128, 128], F32, "sb_gn")
    suf2 = P.sb([128, 128], F32, "sb_suf2")
    ktm = P.sb([128, 64, 128], BF16, "sb_ktm")
    Sst = P.sb([128, 128], F32, "sb_Sst")
    xg = [P.sb([128, 128], F32, "sb_xg") for _ in range(2)]
    fk = [P.sb([128, 128], F32, "sb_fk") for _ in range(2)]
    kh = [P.sb([128, 128], BF16, "sb_kh") for _ in range(2)]
    t4 = [P.sb([128, 4, 1024], BF16, "sb_t4") for _ in range(2)]
    g4 = [P.sb([128, 4, 512], F32, "sb_g4") for _ in range(2)]
    grt = [P.sb([128, 4, 128], F32, "sb_grt") for _ in range(2)]
    pl = [P.ps([128, 128], F32, "ps_l") for _ in range(2)]
    pf = [P.ps([128, 128], F32, "ps_f") for _ in range(2)]
    pU = [P.ps([128, 128], F32, "ps_U") for _ in range(4)]
    P.load("sp", barow, barow[:, :], barow_d[:, :])
    P.load("act", gn, gn[:, :], gn_d[:, :])
    P.load("sp", suf2, suf2[:, :], suf2_d[:, :])
    P.op("dve", lambda e: e.memset(Sst[:, :], 0.0), writes=[Sst])
    tbv = ztb_all.t.rearrange("(c j p) w -> p j c w", c=8, j=8, p=128)
    tfv = ztf_all.t.rearrange("(c j p) w -> p j c w", c=8, j=8, p=128)
    for jc in range(16):
        j_, ch_ = jc // 2, jc % 2
        tt, gg, gt = t4[jc % 2], g4[jc % 2], grt[jc % 2]
        P.load("sp", tt, tt[:, :, :], tbv[:, j_, ch_ * 4:(ch_ + 1) * 4, C_GK:C_GK + 1024], dram=ztb_all)
        P.load("act", gg, gg[:, :, :], tfv[:, j_, ch_ * 4:(ch_ + 1) * 4, 0:512], dram=ztf_all)
        bs = slice(j_ * 8 + ch_ * 4, j_ * 8 + ch_ * 4 + 4)
        sel_acc("dve", ktm[:, bs, :], tt, lambda h, tt=tt: tt[:, :, h * 128:(h + 1) * 128], ktm, tt)
        sel_acc("dve", vtm[:, bs, :], tt, lambda h, tt=tt: tt[:, :, 512 + h * 128:512 + (h + 1) * 128], vtm, tt)
        sel_acc("dve", gt[:, :, :], gg, lambda h, gg=gg: gg[:, :, h * 128:(h + 1) * 128], gt, gg)
        P.op("act", lambda e, gt=gt: e.activation(out=gt[:, :, :], in_=gt[:, :, :], func=AF.Silu), reads=[gt], writes=[gt])
        P.op("pool", lambda e, gt=gt, bs=bs: e.tensor_tensor(out=gs[:, bs, :], in0=gt[:, :, :],
                                                           in1=gn[:, :].unsqueeze(1).to_broadcast([128, 4, 128]), op=ALU.mult),
             reads=[gt, gn], writes=[gs])
    for blk in range(64):
        x, f, k2 = xg[blk % 2], fk[blk % 2], kh[blk % 2]
        p1, p2 = pl[blk % 2], pf[blk % 2]
        P.mm(p1[:, :], gaT[:, blk * 128:(blk + 1) * 128], wa2b[:, :], True, True, reads=[gaT, wa2b], writes=[p1])
        P.op("dve", lambda e, x=x, p1=p1: e.tensor_tensor(out=x[:, :], in0=p1[:, :], in1=barow[:, :], op=ALU.add),
             reads=[p1, barow], writes=[x])
        P.op("act", lambda e, x=x: e.activation(out=x[:, :], in_=x[:, :], func=AF.Exp, scale=-1.0, bias=ZB[0][:, 0:1]),
             reads=[x, ZB[0]], writes=[x])
        P.op("act", lambda e, x=x: e.activation(out=x[:, :], in_=x[:, :], func=AF.Ln, bias=onec[:, 0:1]), reads=[x, onec], writes=[x])
        P.mm(p2[:, :], suf2[:, :], x[:, :], True, True, reads=[suf2, x], writes=[p2])
        P.op("act", lambda e, f=f, p2=p2: e.activation(out=f[:, :], in_=p2[:, :], func=AF.Exp, scale=-1.0 / 16, bias=ZB[0][:, 0:1]),
             reads=[p2, ZB[0]], writes=[f])
        P.op("pool", lambda e, k2=k2, f=f, blk=blk: e.tensor_tensor(out=k2[:, :], in0=ktm[:, blk, :], in1=f[:, :], op=ALU.mult),
             reads=[ktm, f], writes=[k2])
        for hf in range(2):
            n = 2 * blk + hf
            pu = pU[n % 4]
            P.mm(pu[:, :], k2[hf * 64:(hf + 1) * 64, :], vtm[hf * 64:(hf + 1) * 64, blk, :], True, True, reads=[k2, vtm], writes=[pu])
            P.op("dve", lambda e, pu=pu, n=n: e.scalar_tensor_tensor(out=Sst[:, :], in0=Sst[:, :], scalar=dn[:, n:n + 1], in1=pu[:, :],
                                                                   op0=ALU.mult, op1=ALU.add), reads=[Sst, dn, pu], writes=[Sst])
            P.op("act", lambda e, n=n: e.activation(out=Sb[:, n, :], in_=Sst[:, :], func=AF.Copy), reads=[Sst], writes=[Sb])
    P.end_phase()

    P.begin_phase()
    ost = P.sb([128, 64, 128], BF16, "sb_gost")
    At = [P.sb([128, 128], BF16, "sb_At") for _ in range(2)]
    st = [P.sb([128, 4], F32, "sb_gst") for _ in range(2)]
    jk = P.sb([128, 128], F32, "sb_gjk")
    pA = [P.ps([128, 128], F32, "ps_A") for _ in range(2)]
    pO = [P.ps([128, 128], F32, "ps_GO") for _ in range(2)]
    for blk in range(64):
        sl = slice(blk * 128, (blk + 1) * 128)
        pa, po, at, s = pA[blk % 2], pO[blk % 2], At[blk % 2], st[blk % 2]
        P.mm(pa[:, :], ktT[:, sl], qtT[:, sl], True, True, reads=[ktT, qtT], writes=[pa])
        P.op("dve", lambda e, at=at, pa=pa: e.tensor_tensor(out=at[:, :], in0=pa[:, :], in1=tri2[:, :], op=ALU.mult),
             reads=[pa, tri2], writes=[at])
        P.mm(po[:, :], at[:, :], vtm[:, blk, :], True, False, reads=[at, vtm], writes=[po])
        if blk > 0:
            P.mm(po[0:64, :], qtT[:, blk * 128:blk * 128 + 64], Sb[:, 2 * blk - 1, :], False, False, reads=[qtT, Sb], writes=[po])
        P.mm(po[64:128, :], qtT[:, blk * 128 + 64:blk * 128 + 128], Sb[:, 2 * blk, :], False, True, reads=[qtT, Sb], writes=[po])
        P.op("act", lambda e, po=po, s=s: e.activation(out=jk[:, :], in_=po[:, :], func=AF.Square, accum_out=s[:, 0:1]),
             reads=[po], writes=[jk, s])
        P.op("act", lambda e, s=s: e.activation(out=s[:, 1:2], in_=s[:, 0:1], func=AF.Sqrt, scale=1.0 / 128, bias=epsc[:, 0:1]),
             reads=[s, epsc], writes=[s])
        P.op("dve", lambda e, s=s: e.reciprocal(out=s[:, 2:3], in_=s[:, 1:2]), reads=[s], writes=[s])
        P.op("dve", lambda e, po=po, s=s, blk=blk: e.scalar_tensor_tensor(out=ost[:, blk, :], in0=po[:, :], scalar=s[:, 2:3],
                                                                        in1=gs[:, blk, :], op0=ALU.mult, op1=ALU.mult),
             reads=[po, s, gs], writes=[ost])
    P.store("sp", ost, gla_s.t.rearrange("(b p) e -> p b e", p=128), ost[:, :, :], dram=gla_s)
    P.end_phase()
    P.scope_end()


def emit_mixT(P, foxo, dsao, gla_all, sel_d, ident, mixT):
    P.begin_phase()
    sel = P.sb([128, 8], F32, "sb_sel8")
    gch = [P.sb([128, 4, 8, 128], BF16, "sb_gch") for _ in range(2)]
    mg = [P.sb([128, 4, 128], BF16, "sb_mg") for _ in range(2)]
    fo = [P.sb([128, 768], BF16, "sb_fo") for _ in range(2)]
    do = [P.sb([128, 768], BF16, "sb_do") for _ in range(2)]
    pT = [P.ps([128, 8, 128], BF16, "ps_xT") for _ in range(2)]
    P.load("sp", sel, sel[:, :], sel_d[:, :])
    gv_ = gla_all.t.rearrange("(r j c p) e -> p j r c e", r=8, j=8, c=8, p=128)
    for j in range(8):
        ch = gch[j % 2]
        m = mg[j % 2]
        ostF, ostD = fo[j % 2], do[j % 2]
        P.load("sp", ostF, ostF[:, :], foxo[j, :, :], dram=foxo)
        P.load("act", ostD, ostD[:, :], dsao[j, :, :], dram=dsao)
        for hh in range(4):
            P.load("sp" if hh % 2 == 0 else "act", ch, ch[:, hh, :, :], gv_[:, j, hh, :, :], dram=gla_all)
        for cp in range(8):
            if cp == 0:
                P.op("dve", lambda e, m=m, ch=ch: e.tensor_scalar(out=m[:, :, :], in0=ch[:, :, 0, :], scalar1=sel[:, 0:1], scalar2=None, op0=ALU.mult),
                     reads=[ch, sel], writes=[m])
            else:
                P.op("dve", lambda e, m=m, ch=ch, cp=cp: e.scalar_tensor_tensor(out=m[:, :, :], in0=ch[:, :, cp, :], scalar=sel[:, cp:cp + 1],
                                                                              in1=m[:, :, :], op0=ALU.mult, op1=ALU.add),
                     reads=[ch, sel, m], writes=[m])
        for half in range(2):
            pt = pT[half]
            for kk in range(8):
                k = half * 8 + kk
                if k < 6:
                    src, sb_ = ostF[:, k * 128:(k + 1) * 128], ostF
                elif k < 10:
                    src, sb_ = m[:, k - 6, :], m
                else:
                    src, sb_ = ostD[:, (k - 10) * 128:(k - 9) * 128], ostD
                P.op("pe", lambda e, pt=pt, kk=kk, src=src: e.transpose(out=pt[:, kk, :], in_=src, identity=ident[:, :]),
                     reads=[sb_, ident], writes=[pt])
            P.op("act", lambda e, pt=pt, half=half, j=j: e.activation(
                out=mixT[:, half * 8:(half + 1) * 8, j * 128:(j + 1) * 128], in_=pt[:, :, :], func=AF.Copy), reads=[pt], writes=[mixT])
    P.end_phase()


def build_fused():
    nc = bass.Bass("TRN2", target_bir_lowering=False)
    P = Prog(nc)
    IN = {}

    def inp(name, shp, dt):
        IN[name] = P.dram(name, shp, dt, "ExternalInput")
        return IN[name]
    xs = inp("xs", [NB, 128, D], F32)
    c_d = inp("c_pk", [128, 16], F32)
    ident_d = inp("ident", [128, 128], BF16)
    tri_d = inp("tri", [128, 128], F32)
    sel_d = inp("sel", [128, 8], F32)
    sel4_d = inp("sel4", [128, 4], F32)
    cm_d = inp("cm", [128, 8, 128], BF16)
    am_d = inp("am", [128, 8, 128], F32)
    tri2_d = inp("tri2", [128, 128], F32)
    suf2_d = inp("suf2", [128, 128], F32)
    rmask_d = inp("rmask", [128, 512], F32)
    fg_d = inp("fg_bc", [128, D], F32)
    L = []
    for l in range(2):
        s_ = "_%d" % l
        L.append(dict(
            adawA=inp("adawA" + s_, [D, 4096], F32), adabA=inp("adabA" + s_, [128, 4096], F32), g1=inp("g1" + s_, [128, D], F32),
            wfm=inp("wfm" + s_, [D, NFM], F32), wtm=inp("wtm" + s_, [D, NTM], F32), fbb=inp("fbb" + s_, [128, 6], F32),
            wa2=inp("wa2" + s_, [16, 128], F32), nbacol=inp("nbacol" + s_, [128, 1], F32), barow=inp("barow" + s_, [128, 128], F32),
            gnb=inp("gnb" + s_, [128, 128], F32), adawC=inp("adawC" + s_, [D, 8192], F32), adabC=inp("adabC" + s_, [128, 8192], F32),
            g2n=inp("g2n" + s_, [128, D], F32), wo=inp("wo" + s_, [D, D], F32), wq=inp("wq" + s_, [D, D], F32),
            kT=inp("kT" + s_, [16, 128, 128], F32), uTt=inp("uTt" + s_, [128, 128, 16, 128], F32), v=inp("v" + s_, [NE, D], F32),
            zfm_s=P.scratch("zfm_s" + s_, [NFM, NTOK], BF16), zfm_all=P.scratch("zfm_all" + s_, [8 * NFM, NTOK], BF16),
            ztb_s=P.scratch("ztb_s" + s_, [NTOK, 2560], BF16), ztb_all=P.scratch("ztb_all" + s_, [8 * NTOK, 2560], BF16),
            ztf_s=P.scratch("ztf_s" + s_, [NTOK, 640], F32), ztf_all=P.scratch("ztf_all" + s_, [8 * NTOK, 640], F32),
            gla_s=P.scratch("gla_s" + s_, [S, 128], BF16), gla_all=P.scratch("gla_all" + s_, [8 * S, 128], BF16),
            xmid=P.scratch("xmid" + s_, [NB, 128, D], F32), h2T=P.scratch("h2T" + s_, [16, 128, NTOK], BF16),
            s12=P.scratch("s12" + s_, [NB, 128, 16, 128], F32), g2=P.scratch("g2" + s_, [128, D], F32),
            xcur=P.scratch("xcur" + s_, [NB, 128, D], F32), foxo=P.scratch("foxo" + s_, [NB, 128, 768], BF16),
            dsao=P.scratch("dsao" + s_, [NB, 128, 768], BF16)))
    xo = P.dram("xo", [NB, 128, D], F32, "ExternalOutput")
    ident = emit_consts(P, ident_d)
    emit_zero(P)
    for l in range(2):
        W = L[l]
        xsrc = xs if l == 0 else L[0]["xcur"]
        xdr = None if l == 0 else L[0]["xcur"]
        emit_A_f(P, xsrc, xdr, c_d, W["adawA"], W["adabA"], W["g1"], W["wfm"], W["wtm"], ident, W["zfm_s"], W["ztb_s"], W["ztf_s"])
        P.begin_phase()
        P.allgather(W["zfm_s"], W["zfm_all"])
        P.allgather(W["ztb_s"], W["ztb_all"])
        P.allgather(W["ztf_s"], W["ztf_all"])
        P.end_phase()
        P.scope_begin()
        bias = P.sbs([128, 6, 8, 64], F32, "sbs_bias")
        cm = P.sbs([128, 8, 128], BF16, "sbs_cm")
        ostF = P.sbs([128, 8, 768], BF16, "sbs_ostF")
        P.begin_phase()
        P.load("sp", cm, cm[:, :, :], cm_d[:, :, :])
        P.end_phase()
        emit_fox_bias_f(P, W["ztf_all"], W["fbb"], tri_d, sel_d, bias)
        emit_attn_f(P, W["zfm_all"], R_FK, W["ztb_all"], C_FV, W["zfm_s"], R_FQ, 6, ostF, bias=bias, cm=cm)
        P.begin_phase()
        P.store("sp", ostF, W["foxo"].t.rearrange("j p w -> p j w"), ostF[:, :, :], dram=W["foxo"])
        P.end_phase()
        P.scope_end()
        P.scope_begin()
        ostD = P.sbs([128, 8, 768], BF16, "sbs_ostD")
        maskT = [P.sbs([128, 8 * j + 8, 128], BF16, "sbs_mT") for j in range(8)]
        emit_dsa_select_f(P, W["zfm_all"], W["zfm_s"], W["ztf_s"], am_d, ident, maskT)
        emit_attn_f(P, W["zfm_all"], R_DK, W["ztb_all"], C_DV, W["zfm_s"], R_DQ, 6, ostD, maskT=maskT)
        P.begin_phase()
        P.store("sp", ostD, W["dsao"].t.rearrange("j p w -> p j w"), ostD[:, :, :], dram=W["dsao"])
        P.end_phase()
        P.scope_end()
        emit_gla_f(P, W["zfm_all"], W["ztb_all"], W["ztf_all"], sel4_d, W["wa2"], W["nbacol"], W["barow"], W["gnb"], tri2_d, suf2_d,
                   rmask_d, W["gla_s"])
        P.begin_phase()
        P.allgather(W["gla_s"], W["gla_all"])
        P.end_phase()
        P.scope_begin()
        mixT = P.sbs([128, 16, NTOK], BF16, "sbs_mixT")
        emit_mixT(P, W["foxo"], W["dsao"], W["gla_all"], sel_d, ident, mixT)
        emit_C1(P, ident, xsrc, xdr, None, mixT, c_d, W["adawC"], W["adabC"], W["g2n"], W["wo"], W["wq"], W["kT"],
                W["xmid"], W["h2T"], W["s12"], W["g2"])
        P.scope_end()
        final = (l == 1)
        emit_C2(P, ident, W["h2T"], W["s12"], W["xmid"], W["g2"], W["uTt"], W["v"], fg_d if final else None,
                xo if final else W["xcur"], final, final)
    P.finish()
    return nc


def fused_inputs(x, c, ada_w, ada_b, norm1_g, norm2_g, final_g, w_in, fox_fbias, gla_wa2, gla_ba, gla_norm_g, w_out,
                 peer_wq, peer_k1, peer_k2, peer_u, peer_v):
    f32 = np.float32
    x2 = np.asarray(x, f32).reshape(S, D)
    c1 = np.asarray(c, f32).reshape(D)
    tri2, suf2, rmask = gla_consts()
    shared = dict(c_pk=np.ascontiguousarray(c1.reshape(16, 128).T), ident=np.eye(128, dtype=NPBF), tri=TRI, tri2=tri2, suf2=suf2,
                  rmask=rmask, fg_bc=bc128(np.asarray(final_g, f32)))
    per_head = [dict() for _ in range(4)]
    for l in range(2):
        s_ = "_%d" % l
        w_in_l = np.asarray(w_in[l], f32)
        wfm = np.zeros((D, NFM), f32)
        wfm[:, :len(FM_COLS)] = w_in_l[:, FM_COLS]
        wtm = np.zeros((D, NTM), f32)
        wtm[:, :len(TM_COLS)] = w_in_l[:, TM_COLS]
        kT = np.zeros((16, 128, 128), f32)
        for h in range(8):
            kT[2 * h] = np.asarray(peer_k1[l][h], f32).T
            kT[2 * h + 1] = np.asarray(peer_k2[l][h], f32).T
        shared.update({
            "adawA" + s_: np.ascontiguousarray(ada_w[l][:, 0:4096]), "adabA" + s_: bc128(np.asarray(ada_b[l][0:4096], f32)),
            "g1" + s_: bc128(np.asarray(norm1_g[l], f32)), "wfm" + s_: wfm, "wtm" + s_: wtm, "fbb" + s_: bc128(np.asarray(fox_fbias[l], f32)),
            "gnb" + s_: bc128(np.asarray(gla_norm_g[l], f32)), "adawC" + s_: np.ascontiguousarray(ada_w[l][:, 4096:12288]),
            "adabC" + s_: bc128(np.asarray(ada_b[l][4096:12288], f32)), "g2n" + s_: bc128(np.asarray(norm2_g[l], f32)),
            "wo" + s_: np.ascontiguousarray(w_out[l], dtype=f32), "wq" + s_: np.ascontiguousarray(peer_wq[l], dtype=f32), "kT" + s_: kT,
            "uTt" + s_: uT_tiles(np.asarray(peer_u[l], f32)), "v" + s_: np.ascontiguousarray(peer_v[l], dtype=f32)})
        wa2_l = np.asarray(gla_wa2[l], f32)
        ba_l = np.asarray(gla_ba[l], f32)
        for hg in range(4):
            sl = slice(hg * 128, (hg + 1) * 128)
            per_head[hg].update({"wa2" + s_: np.ascontiguousarray(wa2_l[:, sl]), "nbacol" + s_: np.ascontiguousarray(-ba_l[sl][:, None]),
                                 "barow" + s_: bc128(ba_l[sl])})
    ins = []
    for cc in range(8):
        cm, am, sel = band_masks(cc)
        sel4 = np.zeros((128, 4), f32)
        sel4[:, cc % 4] = 1.0
        dct = dict(shared, xs=own_blocks(x2, cc), sel=sel, sel4=sel4, cm=cm, am=am)
        dct.update(per_head[cc % 4])
        ins.append(dct)
    return ins


def build_CA(with_next_A, final):
    nc = bass.Bass("TRN2", target_bir_lowering=False)
    P = Prog(nc)
    I = lambda name, shp, dt: P.dram(name, shp, dt, "ExternalInput")
    xs = I("xs", [NB, 128, D], F32)
    mixT_d = I("mixT", [D, NTOK], BF16)
    c_d = I("c_pk", [128, 16], F32)
    adawC = I("adaw", [D, 8192], F32)
    adabC = I("adab", [128, 8192], F32)
    g2n = I("g_bc", [128, D], F32)
    ident_d = I("ident", [128, 128], BF16)
    wo_d = I("wo", [D, D], F32)
    wq_d = I("wq", [D, D], F32)
    kT_d = I("kT", [16, 128, 128], F32)
    uT_d = I("uTt", [128, 128, 16, 128], F32)
    v_d = I("v", [NE, D], F32)
    fg_d = I("fg_bc", [128, D], F32) if final else None
    if with_next_A:
        adawA = I("adawA", [D, 4096], F32)
        adabA = I("adabA", [128, 4096], F32)
        g1 = I("g1_bc", [128, D], F32)
        wfm = I("wfm", [D, NFM], F32)
        wtm = I("wtm", [D, NTM], F32)
        zfm = P.dram("zfm", [NFM, NTOK], BF16, "ExternalOutput")
        ztb = P.dram("ztb", [NTOK, 2560], BF16, "ExternalOutput")
        ztf = P.dram("ztf", [NTOK, 640], F32, "ExternalOutput")
    xo = P.dram("xo", [NB, 128, D], F32, "ExternalOutput")
    xmid = P.scratch("xmid_s", [NB, 128, D], F32)
    h2T = P.scratch("h2T_s", [16, 128, NTOK], BF16)
    s12 = P.scratch("s12_s", [NB, 128, 16, 128], F32)
    g2 = P.scratch("g2_s", [128, D], F32)
    ident = emit_consts(P, ident_d)
    emit_C1(P, ident, xs, None, mixT_d, None, c_d, adawC, adabC, g2n, wo_d, wq_d, kT_d, xmid, h2T, s12, g2)
    emit_C2(P, ident, h2T, s12, xmid, g2, uT_d, v_d, fg_d, xo, final, not with_next_A)
    if with_next_A:
        emit_A_f(P, xo, xo, c_d, adawA, adabA, g1, wfm, wtm, ident, zfm, ztb, ztf)
        P.begin_phase()
        P.end_phase(final=True)
    P.finish()
    return nc


_NC_CACHE = {}


def _get_nc(name, fn):
    if name not in _NC_CACHE:
        _NC_CACHE[name] = fn()
    return _NC_CACHE[name]


def _run(nc, ins):
    return run_bass_kernel_spmd(nc, ins, core_ids=list(range(8))).results


def _a_weights(l, ada_w, ada_b, norm1_g, w_in, f32):
    w_in_l = np.asarray(w_in[l], f32)
    wfm = np.zeros((D, NFM), f32)
    wfm[:, :len(FM_COLS)] = w_in_l[:, FM_COLS]
    wtm = np.zeros((D, NTM), f32)
    wtm[:, :len(TM_COLS)] = w_in_l[:, TM_COLS]
    return dict(adaw=np.ascontiguousarray(ada_w[l][:, 0:4096]), adab=bc128(np.asarray(ada_b[l][0:4096], f32)),
                g_bc=bc128(np.asarray(norm1_g[l], f32)), wfm=wfm, wtm=wtm)


def kernel(x, c, ada_w, ada_b, norm1_g, norm2_g, final_g, w_in, fox_fbias, gla_wa2, gla_ba, gla_norm_g, w_out,
           peer_wq, peer_k1, peer_k2, peer_u, peer_v):
    f32 = np.float32
    x2 = np.asarray(x, f32).reshape(S, D)
    c1 = np.asarray(c, f32).reshape(D)
    xs_list = [own_blocks(x2, cc) for cc in range(8)]
    ident = np.eye(128, dtype=NPBF)
    tri2, suf2, rmask = gla_consts()
    masks = [band_masks(cc) for cc in range(8)]
    c_pk = np.ascontiguousarray(c1.reshape(16, 128).T)
    aw = _a_weights(0, ada_w, ada_b, norm1_g, w_in, f32)
    rA = _run(_get_nc("A", build_A), [dict(aw, c_pk=c_pk, ident=ident, xs=xs_list[cc]) for cc in range(8)])
    del aw
    for l in range(2):
        ZFM = assemble_tokens([r["zfm"] for r in rA], 1)
        ZTB = assemble_tokens([r["ztb"] for r in rA], 0)
        ZTF = assemble_tokens([r["ztf"] for r in rA], 0)
        fkT = np.ascontiguousarray(ZFM[R_FK:R_FK + 768])
        dkT = np.ascontiguousarray(ZFM[R_DK:R_DK + 768])
        ikT = np.ascontiguousarray(ZFM[R_IK:R_IK + 64])
        gaT = np.ascontiguousarray(ZFM[R_GA:R_GA + 16])
        fvg = vg_layout(ZTB[:, C_FV:C_FV + 768], 6)
        dvg = vg_layout(ZTB[:, C_DV:C_DV + 768], 6)
        ffp = np.ascontiguousarray(ZTF[:, 512:518].reshape(64, 128, 6).transpose(1, 2, 0))
        fbb = bc128(np.asarray(fox_fbias[l], f32))
        wa2_l = np.asarray(gla_wa2[l], f32)
        ba_l = np.asarray(gla_ba[l], f32)
        gnb = bc128(np.asarray(gla_norm_g[l], f32))
        gl = []
        for hg in range(4):
            sl = slice(hg * 128, (hg + 1) * 128)
            gl.append(dict(gqT=np.ascontiguousarray(ZFM[R_GQ + hg * 128:R_GQ + (hg + 1) * 128]),
                           gkT=np.ascontiguousarray(ZFM[R_GK + hg * 128:R_GK + (hg + 1) * 128]),
                           gktm=tm_layout(ZTB[:, C_GK + hg * 128:C_GK + (hg + 1) * 128]),
                           gvtm=tm_layout(ZTB[:, C_GV + hg * 128:C_GV + (hg + 1) * 128]),
                           grtm=tm_layout(ZTF[:, hg * 128:(hg + 1) * 128]), gaT=gaT,
                           wa2=np.ascontiguousarray(wa2_l[:, sl]), nbacol=np.ascontiguousarray(-ba_l[sl][:, None]),
                           barow=bc128(ba_l[sl]), gnb=gnb, tri2=tri2, suf2=suf2, rmask=rmask))
        insB = []
        for cc in range(8):
            cm, am, sel = masks[cc]
            zo = rA[cc]["zfm"]
            iq = zo[R_IQ:R_IQ + 1024].reshape(16, 64, 1024).transpose(1, 0, 2)
            iw = rA[cc]["ztf"][:, 518:534].reshape(8, 128, 16).transpose(1, 0, 2)
            dct = dict(fkT=fkT, fvg=fvg, fqT=np.ascontiguousarray(zo[R_FQ:R_FQ + 768]), ffp=ffp, fbb=fbb, tri=TRI, sel=sel, cm=cm,
                       dkT=dkT, dvg=dvg, dqT=np.ascontiguousarray(zo[R_DQ:R_DQ + 768]), iqT=np.ascontiguousarray(iq), ikT=ikT,
                       iwp=np.ascontiguousarray(iw), am=am, ident=ident)
            dct.update(gl[cc % 4])
            insB.append(dct)
        rB = _run(_get_nc("B", build_B), insB)
        del insB, fvg, dvg, gl, ZFM, ZTB, ZTF
        gla_full = np.concatenate([rB[hg]["glao"].reshape(S, 128) for hg in range(4)], axis=1)
        mix_list = []
        for cc in range(8):
            g_own = own_blocks(gla_full, cc)
            mix = np.concatenate([rB[cc]["foxo"], g_own, rB[cc]["dsao"]], axis=2)
            mix_list.append(mix.reshape(NTOK, D))
        final = (l == 1)
        kT = np.zeros((16, 128, 128), f32)
        for h in range(8):
            kT[2 * h] = np.asarray(peer_k1[l][h], f32).T
            kT[2 * h + 1] = np.asarray(peer_k2[l][h], f32).T
        common = dict(c_pk=c_pk, adaw=np.ascontiguousarray(ada_w[l][:, 4096:12288]), adab=bc128(np.asarray(ada_b[l][4096:12288], f32)),
                      g_bc=bc128(np.asarray(norm2_g[l], f32)), ident=ident, wo=np.ascontiguousarray(w_out[l], dtype=f32),
                      wq=np.ascontiguousarray(peer_wq[l], dtype=f32), kT=kT, uTt=uT_tiles(np.asarray(peer_u[l], f32)),
                      v=np.ascontiguousarray(peer_v[l], dtype=f32))
        if final:
            common["fg_bc"] = bc128(np.asarray(final_g, f32))
            nc = _get_nc("Cf", lambda: build_CA(False, True))
        else:
            aw = _a_weights(1, ada_w, ada_b, norm1_g, w_in, f32)
            common.update(adawA=aw["adaw"], adabA=aw["adab"], g1_bc=aw["g_bc"], wfm=aw["wfm"], wtm=aw["wtm"])
            nc = _get_nc("CA", lambda: build_CA(True, False))
        rC = _run(nc, [dict(common, xs=xs_list[cc], mixT=np.ascontiguousarray(mix_list[cc].T)) for cc in range(8)])
        del common
        xs_list = [r["xo"] for r in rC]
        rA = rC
    out = assemble_tokens([xx.reshape(NTOK, D) for xx in xs_list], 0)
    return np.ascontiguousarray(out.reshape(1, S, D).astype(np.float32))
```

```python
import numpy as np
import ml_dtypes
from contextlib import ExitStack
import concourse.bass as bass
import concourse.mybir as mybir
from concourse.bass_utils import run_bass_kernel_spmd

F32 = mybir.dt.float32
BF16 = mybir.dt.bfloat16
U32 = mybir.dt.uint32
ALU = mybir.AluOpType
AF = mybir.ActivationFunctionType
AX = mybir.AxisListType
NPBF = ml_dtypes.bfloat16

ENGS = ("pe", "act", "dve", "pool", "sp")


class Buf:
    __slots__ = ("t", "name", "w", "r", "pr", "dsem")

    def __init__(self, t, name):
        self.t = t
        self.name = name
        self.w = {}
        self.r = {}
        self.pr = {}
        self.dsem = None

    def __getitem__(self, idx):
        return self.t[idx]


class Prog:
    def __init__(self, nc, n_dma_sems=80):
        self.nc = nc
        self.stack = ExitStack()
        self.esem = {}
        for e in ENGS[:4]:
            self.esem[e] = self.stack.enter_context(nc.semaphore("sem_" + e))
        self.ecnt = {e: 0 for e in ENGS[:4]}
        self.dpool = [self.stack.enter_context(nc.semaphore("dsem%d" % i)) for i in range(n_dma_sems)]
        self.dfree = list(range(n_dma_sems))
        self.dcnt = [0] * n_dma_sems
        self.semobj = {}
        for e in ENGS[:4]:
            self.semobj[("e", e)] = self.esem[e]
        for i, s in enumerate(self.dpool):
            self.semobj[("d", i)] = s
        self.seen = {e: {} for e in ENGS}
        self.ops = {e: [] for e in ENGS}
        self.pstack = None
        self.pbufs = []
        self.nbuf = 0
        self.n_instr = 0

    def begin_phase(self):
        self.pstack = ExitStack()
        self.pbufs = []

    def sb(self, shape, dtype, name=None):
        self.nbuf += 1
        name = (name or "sb") + "_%d" % self.nbuf
        t = self.pstack.enter_context(self.nc.sbuf_tensor(name, list(shape), dtype))
        b = Buf(t, name)
        self.pbufs.append(b)
        return b

    def ps(self, shape, dtype, name=None):
        self.nbuf += 1
        name = (name or "ps") + "_%d" % self.nbuf
        t = self.pstack.enter_context(self.nc.psum_tensor(name, list(shape), dtype))
        b = Buf(t, name)
        self.pbufs.append(b)
        return b

    def sbp(self, shape, dtype, name=None):
        self.nbuf += 1
        name = (name or "sbp") + "_%d" % self.nbuf
        t = self.stack.enter_context(self.nc.sbuf_tensor(name, list(shape), dtype))
        return Buf(t, name)

    def scope_begin(self):
        if not hasattr(self, "scopes"):
            self.scopes = []
        self.scopes.append((ExitStack(), []))

    def sbs(self, shape, dtype, name=None):
        self.nbuf += 1
        name = (name or "sbs") + "_%d" % self.nbuf
        stack, bufs = self.scopes[-1]
        t = stack.enter_context(self.nc.sbuf_tensor(name, list(shape), dtype))
        b = Buf(t, name)
        bufs.append(b)
        return b

    def scope_end(self):
        stack, bufs = self.scopes.pop()
        self.begin_phase()
        self.pbufs = list(bufs)
        self.end_phase()
        stack.close()

    def dram(self, name, shape, dtype, kind):
        t = self.nc.dram_tensor(name, list(shape), dtype, kind=kind).ap()
        return Buf(t, name)

    def scratch(self, name, shape, dtype):
        t = self.nc.dram_tensor(name, list(shape), dtype).ap()
        return Buf(t, name)

    def allgather(self, snd, rcv):
        rg = [list(range(8))]
        self.op("pool", lambda e: e.collective_compute("AllGather", ALU.bypass, replica_groups=rg, ins=[snd.t.opt()], outs=[rcv.t.opt()]),
                reads=[snd], writes=[rcv], dma=rcv, inc=1)

    def _dsem_of(self, buf):
        if buf.dsem is None:
            buf.dsem = self.dfree.pop()
        return buf.dsem

    def op(self, eng, fn, reads=(), writes=(), dma=None, inc=16):
        kind = "dma" if dma is not None else "compute"
        deps = {}

        def add(key, val, src):
            if kind == "compute" and src == eng:
                return
            if key not in deps or deps[key] < val:
                deps[key] = val

        def add_raw(key, val, src):
            if kind == "compute" and src == eng and eng == "pe":
                return
            if key not in deps or deps[key] < val:
                deps[key] = val

        for b in reads:
            for key, (val, src) in b.w.items():
                add_raw(key, val, src)
        for b in writes:
            for key, (val, src) in b.r.items():
                add(key, val, src)
            for key, (val, src) in b.pr.items():
                add(key, val, src)
            for key, (val, src) in b.w.items():
                if kind == "dma" and key[0] == "d":
                    continue
                add(key, val, src)
        if kind == "compute":
            self.ecnt[eng] += 1
            key, val, src = ("e", eng), self.ecnt[eng], eng
        else:
            i = self._dsem_of(dma)
            self.dcnt[i] += inc
            key, val, src = ("d", i), self.dcnt[i], None
        waits = []
        seen = self.seen[eng]
        for k, v in deps.items():
            if seen.get(k, 0) < v:
                seen[k] = v
                waits.append((k, v))
        for b in reads:
            if b.r.get(key, (0, None))[0] < val:
                b.r[key] = (val, src)
        for b in writes:
            if b.r:
                b.pr = dict(b.r)
                b.w.clear()
                b.r.clear()
            if b.w.get(key, (0, None))[0] < val:
                b.w[key] = (val, src)
        self.ops[eng].append((waits, fn, key, inc))
        self.n_instr += 1

    def load(self, q, dstbuf, out_ap, in_ap, dram=None, **kw):
        self.op(q, lambda e: e.dma_start(out=out_ap, in_=in_ap, **kw), reads=([dram] if dram is not None else []),
                writes=[dstbuf], dma=dstbuf)

    def store(self, q, srcbuf, out_ap, in_ap, dram=None, **kw):
        self.op(q, lambda e: e.dma_start(out=out_ap, in_=in_ap, **kw), reads=[srcbuf],
                writes=([dram] if dram is not None else []), dma=srcbuf)

    def mm(self, out_ap, lhsT, rhs, start, stop, reads, writes):
        self.op("pe", lambda e: e.matmul(out_ap, lhsT, rhs, start=start, stop=stop), reads=reads, writes=writes)

    def end_phase(self, final=False):
        nc = self.nc
        ops = self.ops
        semobj = self.semobj
        esem = self.esem
        finalwaits = []
        if final:
            for i, c in enumerate(self.dcnt):
                if c > 0:
                    finalwaits.append((("d", i), c))
        else:
            for b in self.pbufs:
                if b.dsem is not None and self.dcnt[b.dsem] > 0:
                    finalwaits.append((("d", b.dsem), self.dcnt[b.dsem]))

        def emit(e, name):
            for waits, fn, key, inc in ops[name]:
                for k, v in waits:
                    e.wait_ge(semobj[k], v)
                ins = fn(e)
                if key[0] == "e":
                    ins.then_inc(esem[key[1]], 1)
                else:
                    ins.then_inc(semobj[key], inc)
            if name == "sp":
                for k, v in finalwaits:
                    e.wait_ge(semobj[k], v)

        with nc.Block() as block:
            if ops["sp"] or finalwaits:
                @block.sync
                def _(e):
                    emit(e, "sp")
            if ops["pe"]:
                @block.tensor
                def _(e):
                    emit(e, "pe")
            if ops["act"]:
                @block.scalar
                def _(e):
                    emit(e, "act")
            if ops["dve"]:
                @block.vector
                def _(e):
                    emit(e, "dve")
            if ops["pool"]:
                @block.gpsimd
                def _(e):
                    emit(e, "pool")
        self.ops = {e: [] for e in ENGS}
        for b in self.pbufs:
            if b.dsem is not None:
                self.dfree.append(b.dsem)
        self.pbufs = []
        self.pstack.close()
        self.pstack = None

    def finish(self):
        self.stack.close()


D = 2048
EPS = 1e-6
NTOK = 1024
NB = 8
O_FQ, O_FK, O_FV, O_FF = 0, 768, 1536, 2304
O_GQ, O_GK, O_GV, O_GR, O_GA = 2310, 2822, 3334, 3846, 4358
O_DQ, O_DK, O_DV = 4374, 5142, 5910
O_IQ, O_IK, O_IW = 6678, 7702, 7766
FM_COLS = (list(range(O_FQ, O_FQ + 768)) + list(range(O_FK, O_FK + 768)) + list(range(O_DQ, O_DQ + 768))
           + list(range(O_DK, O_DK + 768)) + list(range(O_GQ, O_GQ + 512)) + list(range(O_GK, O_GK + 512))
           + list(range(O_IQ, O_IQ + 1024)) + list(range(O_IK, O_IK + 64)) + list(range(O_GA, O_GA + 16)))
NFM = 5248
R_FQ, R_FK, R_DQ, R_DK, R_GQ, R_GK, R_IQ, R_IK, R_GA = 0, 768, 1536, 2304, 3072, 3584, 4096, 5120, 5184
TM_COLS = (list(range(O_FV, O_FV + 768)) + list(range(O_DV, O_DV + 768)) + list(range(O_GK, O_GK + 512))
           + list(range(O_GV, O_GV + 512)) + list(range(O_GR, O_GR + 512)) + list(range(O_FF, O_FF + 6))
           + list(range(O_IW, O_IW + 16)))
NTM = 3200
C_FV, C_DV, C_GK, C_GV = 0, 768, 1536, 2048
C_GR, C_FF, C_IW = 0, 512, 518


def emit_mod(P, c_d, adaw_d, adab_d, ncols, modbc):
    P.begin_phase()
    ct = P.sb([128, 16], F32, "sb_ct")
    ca = P.sb([128, 16], F32, "sb_ca")
    crep = P.sb([128, 16, 128], F32, "sb_crep")
    adab = P.sb([128, ncols], F32, "sb_adab")
    wch = [P.sb([128, 16, 512], F32, "sb_wch") for _ in range(2)]
    pm = [P.ps([128, 512], F32, "ps_mod") for _ in range(2)]
    P.load("sp", ct, ct[:, :], c_d[:, :])
    P.load("act", adab, adab[:, :], adab_d[:, :])
    P.op("act", lambda e: e.activation(out=ca[:, :], in_=ct[:, :], func=AF.Silu), reads=[ct], writes=[ca])
    P.op("dve", lambda e: e.tensor_copy(out=crep[:, :, :], in_=ca[:, :].unsqueeze(2).to_broadcast([128, 16, 128])),
         reads=[ca], writes=[crep])
    wv = adaw_d.t.rearrange("(k p) n -> p k n", p=128)
    for ci in range(ncols // 512):
        w = wch[ci % 2]
        q = "sp" if ci % 2 == 0 else "act"
        for kh in range(2):
            P.load(q, w, w[:, kh * 8:(kh + 1) * 8, :], wv[:, kh * 8:(kh + 1) * 8, ci * 512:(ci + 1) * 512])
        ps = pm[ci % 2]
        for k in range(16):
            P.mm(ps[:, :], crep[:, k, :], w[:, k, :], k == 0, k == 15, reads=[crep, w], writes=[ps])
        P.op("dve", lambda e, ps=ps, ci=ci: e.tensor_tensor(out=modbc[:, ci * 512:(ci + 1) * 512], in0=ps[:, :],
                                                          in1=adab[:, ci * 512:(ci + 1) * 512], op=ALU.add),
             reads=[ps, adab], writes=[modbc])
    P.end_phase()


def emit_norm_T(P, xs_d, A_bc, B_bc, ident, hT, xkeep=None, xdram=None):
    P.begin_phase()
    xt = [P.sb([128, D], F32, "sb_x") for _ in range(2)]
    junk = P.sb([128, D], BF16, "sb_junk")
    tmp = [P.sb([128, D], F32, "sb_tmp") for _ in range(2)]
    hb = [P.sb([128, D], BF16, "sb_hb") for _ in range(2)]
    st = [P.sb([128, 4], F32, "sb_st") for _ in range(2)]
    pT = [P.ps([128, 8, 128], BF16, "ps_T") for _ in range(2)]
    for b in range(NB):
        if xkeep is None:
            x = xt[b % 2]
            P.load("sp" if b % 2 == 0 else "act", x, x[:, :], xs_d[b, :, :], dram=xdram)
            xa = x[:, :]
            xr = [x]
        else:
            xa = xkeep[:, b, :]
            xr = [xkeep]
        s = st[b % 2]
        t = tmp[b % 2]
        h = hb[b % 2]
        P.op("act", lambda e, xa=xa, s=s: e.activation(out=junk[:, :], in_=xa, func=AF.Square, accum_out=s[:, 0:1]),
             reads=xr, writes=[junk, s])
        P.op("act", lambda e, s=s: e.activation(out=s[:, 1:2], in_=s[:, 0:1], func=AF.Sqrt, scale=1.0 / D, bias=EPSB[0][:, 0:1]),
             reads=[s, EPSB[0]], writes=[s])
        P.op("dve", lambda e, s=s: e.reciprocal(out=s[:, 2:3], in_=s[:, 1:2]), reads=[s], writes=[s])
        P.op("dve", lambda e, xa=xa, s=s, t=t: e.scalar_tensor_tensor(out=t[:, :], in0=xa, scalar=s[:, 2:3], in1=A_bc[:, :],
                                                                   op0=ALU.mult, op1=ALU.mult),
             reads=xr + [s, A_bc], writes=[t])
        P.op("pool", lambda e, t=t, h=h: e.tensor_tensor(out=h[:, :], in0=t[:, :], in1=B_bc[:, :], op=ALU.add),
             reads=[t, B_bc], writes=[h])
        for half in range(2):
            pt = pT[half]
            for kk in range(8):
                k = half * 8 + kk
                P.op("pe", lambda e, pt=pt, kk=kk, k=k, h=h: e.transpose(out=pt[:, kk, :], in_=h[:, k * 128:(k + 1) * 128],
                                                                       identity=ident[:, :]),
                     reads=[h, ident], writes=[pt])
            P.op("act", lambda e, pt=pt, half=half, b=b: e.activation(
                out=hT[:, half * 8:(half + 1) * 8, b * 128:(b + 1) * 128], in_=pt[:, :, :], func=AF.Copy),
                 reads=[pt], writes=[hT])
    P.end_phase()


EPSB = [None]


def emit_consts(P, ident_d):
    ident = P.sbp([128, 128], BF16, "sbp_ident")
    epsb = P.sbp([128, 1], F32, "sbp_eps")
    EPSB[0] = epsb
    P.begin_phase()
    P.load("sp", ident, ident[:, :], ident_d[:, :])
    P.op("dve", lambda e: e.memset(epsb[:, :], EPS), writes=[epsb])
    P.end_phase()
    return ident


def emit_proj(P, hT, specs):
    P.begin_phase()
    wf = [P.sb([128, 16, 512], F32, "sb_wf") for _ in range(2)]
    wb = [P.sb([128, 16, 512], BF16, "sb_wb") for _ in range(2)]
    pp = [P.ps([128, 512], F32, "ps_pp") for _ in range(4)]
    ofm = [P.sb([128, 1024], BF16, "sb_ofm") for _ in range(2)]
    otb = [P.sb([128, 512], BF16, "sb_otb") for _ in range(2)]
    otf = [P.sb([128, 512], F32, "sb_otf") for _ in range(2)]
    ci_g = 0
    pi = 0
    oi = 0
    for sp in specs:
        N = sp["w"].t.shape[1]
        wv = sp["w"].t.rearrange("(k p) n -> p k n", p=128)
        for c0 in range(0, N, 512):
            cw = min(512, N - c0)
            w32 = wf[ci_g % 2]
            w16 = wb[ci_g % 2]
            for kh in range(2):
                P.load("sp" if kh == 0 else "act", w32, w32[:, kh * 8:(kh + 1) * 8, 0:cw], wv[:, kh * 8:(kh + 1) * 8, c0:c0 + cw])
            P.op("dve", lambda e, w32=w32, w16=w16, cw=cw: e.tensor_copy(out=w16[:, 0:8, 0:cw], in_=w32[:, 0:8, 0:cw]),
                 reads=[w32], writes=[w16])
            P.op("pool", lambda e, w32=w32, w16=w16, cw=cw: e.tensor_copy(out=w16[:, 8:16, 0:cw], in_=w32[:, 8:16, 0:cw]),
                 reads=[w32], writes=[w16])
            ci_g += 1
            if sp["kind"] == "fm":
                od = sp["outs"][0][0]
                for g0 in range(0, cw, 128):
                    o = ofm[oi % 2]
                    oi += 1
                    for th in range(2):
                        ps = pp[pi % 4]
                        pi += 1
                        for k in range(16):
                            P.mm(ps[:, :], w16[:, k, g0:g0 + 128], hT[:, k, th * 512:(th + 1) * 512], k == 0, k == 15,
                                 reads=[w16, hT], writes=[ps])
                        eng = "act" if th == 0 else "dve"
                        if eng == "act":
                            P.op("act", lambda e, o=o, ps=ps, th=th: e.activation(out=o[:, th * 512:(th + 1) * 512], in_=ps[:, :], func=AF.Copy),
                                 reads=[ps], writes=[o])
                        else:
                            P.op("dve", lambda e, o=o, ps=ps, th=th: e.tensor_copy(out=o[:, th * 512:(th + 1) * 512], in_=ps[:, :]),
                                 reads=[ps], writes=[o])
                    r0 = c0 + g0
                    P.store("pool", o, od.t[r0:r0 + 128, :], o[:, :], dram=od)
            else:
                tgt = None
                for (od, a, b_, dt) in sp["outs"]:
                    if a <= c0 and c0 + cw <= b_:
                        tgt = (od, a, dt)
                od, a, dt = tgt
                for b in range(NB):
                    ps = pp[pi % 4]
                    pi += 1
                    for k in range(16):
                        P.mm(ps[:, 0:cw], hT[:, k, b * 128:(b + 1) * 128], w16[:, k, 0:cw], k == 0, k == 15,
                             reads=[w16, hT], writes=[ps])
                    o = (otb if dt == BF16 else otf)[oi % 2]
                    oi += 1
                    if b % 2 == 0:
                        P.op("act", lambda e, o=o, ps=ps, cw=cw: e.activation(out=o[:, 0:cw], in_=ps[:, 0:cw], func=AF.Copy),
                             reads=[ps], writes=[o])
                    else:
                        P.op("dve", lambda e, o=o, ps=ps, cw=cw: e.tensor_copy(out=o[:, 0:cw], in_=ps[:, 0:cw]),
                             reads=[ps], writes=[o])
                    P.store("pool", o, od.t[b * 128:(b + 1) * 128, c0 - a:c0 - a + cw], o[:, 0:cw], dram=od)
    P.end_phase()


def build_A():
    nc = bass.Bass("TRN2", target_bir_lowering=False)
    P = Prog(nc)
    xs = P.dram("xs", [NB, 128, D], F32, "ExternalInput")
    c_d = P.dram("c_pk", [128, 16], F32, "ExternalInput")
    adaw = P.dram("adaw", [D, 4096], F32, "ExternalInput")
    adab = P.dram("adab", [128, 4096], F32, "ExternalInput")
    g_d = P.dram("g_bc", [128, D], F32, "ExternalInput")
    ident_d = P.dram("ident", [128, 128], BF16, "ExternalInput")
    wfm = P.dram("wfm", [D, NFM], F32, "ExternalInput")
    wtm = P.dram("wtm", [D, NTM], F32, "ExternalInput")
    zfm = P.dram("zfm", [NFM, NTOK], BF16, "ExternalOutput")
    ztb = P.dram("ztb", [NTOK, 2560], BF16, "ExternalOutput")
    ztf = P.dram("ztf", [NTOK, 640], F32, "ExternalOutput")
    ident = emit_consts(P, ident_d)
    modbc = P.sbp([128, 4096], F32, "sbp_mod")
    A1 = P.sbp([128, D], F32, "sbp_A1")
    hT = P.sbp([128, 16, NTOK], BF16, "sbp_hT")
    emit_mod(P, c_d, adaw, adab, 4096, modbc)
    P.begin_phase()
    gb = P.sb([128, D], F32, "sb_g")
    P.load("sp", gb, gb[:, :], g_d[:, :])
    P.op("dve", lambda e: e.scalar_tensor_tensor(out=A1[:, :], in0=modbc[:, D:2 * D], scalar=1.0, in1=gb[:, :],
                                                 op0=ALU.add, op1=ALU.mult), reads=[modbc, gb], writes=[A1])
    P.end_phase()
    emit_norm_T(P, xs, A1, modbc_view(modbc, 0, D), ident, hT)
    emit_proj(P, hT, [dict(w=wfm, kind="fm", outs=[(zfm, 0, NFM, BF16)]),
                      dict(w=wtm, kind="tm", outs=[(ztb, 0, 2560, BF16), (ztf, 2560, 3200, F32)])])
    P.begin_phase()
    P.end_phase(final=True)
    P.finish()
    return nc


class View:
    def __init__(self, buf, a, b):
        self.buf = buf
        self.a = a
        self.b = b


def modbc_view(buf, a, b):
    v = Buf(buf.t[:, a:b], buf.name + "_v")
    v.w = buf.w
    v.r = buf.r
    v.pr = buf.pr
    return v


def own_blocks(arr, c):
    a = arr.reshape((64, 128) + arr.shape[1:])
    return np.ascontiguousarray(a[c::8])


def bc128(v):
    return np.ascontiguousarray(np.broadcast_to(v[None, :], (128, v.shape[0]))).astype(np.float32)


def host_A_inputs(x, c, ada_w_l, ada_b_l, norm_g_l, w_in_l):
    wfm = np.zeros((D, NFM), np.float32)
    wfm[:, :len(FM_COLS)] = w_in_l[:, FM_COLS]
    wtm = np.zeros((D, NTM), np.float32)
    wtm[:, :len(TM_COLS)] = w_in_l[:, TM_COLS]
    common = dict(c_pk=np.ascontiguousarray(c.reshape(16, 128).T), adaw=np.ascontiguousarray(ada_w_l[:, 0:4096]),
                  adab=bc128(ada_b_l[0:4096]), g_bc=bc128(norm_g_l), ident=np.eye(128, dtype=NPBF), wfm=wfm, wtm=wtm)
    x2 = x.reshape(8192, D)
    return [dict(common, xs=own_blocks(x2, cc)) for cc in range(8)]


S = 8192
NKB = 64
SCALE = 128 ** -0.5
NEG = -1.0e30
NBIS = 18
TOPK = 256


def emit_attn(P, KT_d, Vg_d, QT_d, nheads, out_stage, col0, bias=None, maskT=None, cm=None):
    P.begin_phase()
    KT = [P.sb([128, S], BF16, "sb_KT") for _ in range(2)]
    Vg = [P.sb([128, NKB, 129], BF16, "sb_Vg") for _ in range(2)]
    QT = [P.sb([128, 1024], BF16, "sb_QT") for _ in range(2)]
    pO = [P.ps([128, 512], F32, "ps_O") for _ in range(2)]
    pS = []
    for _ in range(2):
        bank = P.ps([128, 4, 128], F32, "ps_S")
        for q in range(4):
            pS.append(Buf(bank.t[:, q, :], bank.name + "_q%d" % q))
    Pt = [P.sb([128, 128], BF16, "sb_Pt") for _ in range(6)]
    rc = [P.sb([128, 1], F32, "sb_rc") for _ in range(2)]
    zero_b = P.sb([128, 1], F32, "sb_zb")
    P.op("dve", lambda e: e.memset(zero_b[:, :], 0.0), writes=[zero_b])
    si = 0
    pi = 0
    oi = 0
    for h in range(nheads):
        kt, vg, qt = KT[h % 2], Vg[h % 2], QT[h % 2]
        for q4 in range(4):
            P.load("sp" if q4 % 2 == 0 else "act", kt, kt[:, q4 * 2048:(q4 + 1) * 2048],
                   KT_d.t[h * 128:(h + 1) * 128, q4 * 2048:(q4 + 1) * 2048])
        for q2 in range(2):
            P.load("sp" if q2 == 0 else "act", vg, vg[:, q2 * 32:(q2 + 1) * 32, :], Vg_d.t[h, :, q2 * 32:(q2 + 1) * 32, :])
        P.load("pool", qt, qt[:, :], QT_d.t[h * 128:(h + 1) * 128, :])
        for j in range(8):
            nkb = 8 * j + 8
            po = pO[oi % 2]
            oi += 1
            for kb in range(nkb):
                ps = pS[si % 8]
                si += 1
                pt = Pt[pi % 6]
                pi += 1
                P.mm(ps[:, :], kt[:, kb * 128:(kb + 1) * 128], qt[:, j * 128:(j + 1) * 128], True, True,
                     reads=[kt, qt], writes=[ps])
                if bias is not None:
                    P.op("act", lambda e, pt=pt, ps=ps, h=h, j=j, kb=kb: e.activation(
                        out=pt[:, :], in_=ps[:, :], func=AF.Exp, scale=SCALE, bias=bias[:, h, j, kb:kb + 1]),
                        reads=[ps, bias], writes=[pt])
                else:
                    P.op("act", lambda e, pt=pt, ps=ps: e.activation(
                        out=pt[:, :], in_=ps[:, :], func=AF.Exp, scale=SCALE, bias=zero_b[:, 0:1]),
                        reads=[ps, zero_b], writes=[pt])
                if maskT is not None:
                    mt = maskT[j]
                    P.op("dve", lambda e, pt=pt, mt=mt, kb=kb: e.tensor_tensor(out=pt[:, :], in0=pt[:, :], in1=mt[:, kb, :], op=ALU.mult),
                         reads=[pt, mt], writes=[pt])
                elif kb >= 8 * j:
                    r = kb - 8 * j
                    P.op("dve", lambda e, pt=pt, r=r: e.tensor_tensor(out=pt[:, :], in0=pt[:, :], in1=cm[:, r, :], op=ALU.mult),
                         reads=[pt, cm], writes=[pt])
                P.mm(po[:, 0:129], pt[:, :], vg[:, kb, :], kb == 0, kb == nkb - 1, reads=[pt, vg], writes=[po])
            r_ = rc[oi % 2]
            P.op("dve", lambda e, r_=r_, po=po: e.reciprocal(out=r_[:, 0:1], in_=po[:, 128:129]), reads=[po], writes=[r_])
            P.op("dve", lambda e, r_=r_, po=po, j=j, h=h: e.tensor_scalar(
                out=out_stage[:, j, col0 + h * 128:col0 + (h + 1) * 128], in0=po[:, 0:128], scalar1=r_[:, 0:1], scalar2=None,
                op0=ALU.mult), reads=[po, r_], writes=[out_stage])
    P.end_phase()


def emit_fox_bias(P, ff_d, fb_d, tri_d, sel_d, bias):
    P.begin_phase()
    ff = P.sb([128, 6, 64], F32, "sb_ff")
    fb = P.sb([128, 6], F32, "sb_fb")
    tri = P.sb([128, 128], F32, "sb_tri")
    ones = P.sb([128, 128], F32, "sb_ones")
    onec = P.sb([128, 1], F32, "sb_onec")
    sel = P.sb([128, 8], F32, "sb_sel")
    nlf = P.sb([128, 6, 64], F32, "sb_nlf")
    tot = P.sb([128, 6, 64], F32, "sb_tot")
    incl = P.sb([128, 6, 64], F32, "sb_incl")
    NF = P.sb([128, 6, 64], F32, "sb_NF")
    tmp = P.sb([128, 6, 8, 8], F32, "sb_tmp")
    nfe = P.sb([128, 6, 8], F32, "sb_nfe")
    pw = P.ps([128, 384], F32, "ps_w")
    pt_ = P.ps([128, 384], F32, "ps_t")
    P.load("sp", ff, ff[:, :, :], ff_d[:, :, :])
    P.load("act", fb, fb[:, :], fb_d[:, :])
    P.load("sp", tri, tri[:, :], tri_d[:, :])
    P.load("act", sel, sel[:, :], sel_d[:, :])
    P.op("dve", lambda e: e.memset(ones[:, :], 1.0), writes=[ones])
    P.op("dve", lambda e: e.memset(onec[:, :], 1.0), writes=[onec])
    P.op("dve", lambda e: e.tensor_tensor(out=nlf[:, :, :], in0=ff[:, :, :], in1=fb[:, :].unsqueeze(2).to_broadcast([128, 6, 64]),
                                          op=ALU.add), reads=[ff, fb], writes=[nlf])
    nlf2 = nlf.t.rearrange("p h k -> p (h k)")
    P.op("act", lambda e: e.activation(out=nlf2, in_=nlf2, func=AF.Exp, scale=-1.0), reads=[nlf], writes=[nlf])
    P.op("act", lambda e: e.activation(out=nlf2, in_=nlf2, func=AF.Ln, bias=onec[:, 0:1]), reads=[nlf, onec], writes=[nlf])
    P.mm(pw[:, :], tri[:, :], nlf2, True, True, reads=[tri, nlf], writes=[pw])
    P.mm(pt_[:, :], ones[:, :], nlf2, True, True, reads=[ones, nlf], writes=[pt_])
    tot2 = tot.t.rearrange("p h k -> p (h k)")
    P.op("dve", lambda e: e.tensor_copy(out=tot2, in_=pt_[:, :]), reads=[pt_], writes=[tot])
    for h in range(6):
        P.op("dve", lambda e, h=h: e.tensor_tensor_scan(out=incl[:, h, :], data0=ones[:, 0:64], data1=tot[:, h, :], initial=0.0,
                                                        op0=ALU.mult, op1=ALU.add), reads=[ones, tot], writes=[incl])
    NF2 = NF.t.rearrange("p h k -> p (h k)")
    incl2 = incl.t.rearrange("p h k -> p (h k)")
    P.op("dve", lambda e: e.tensor_tensor(out=NF2, in0=pw[:, :], in1=incl2, op=ALU.add), reads=[pw, incl], writes=[NF])
    P.op("dve", lambda e: e.tensor_tensor(out=NF2, in0=NF2, in1=tot2, op=ALU.subtract), reads=[NF, tot], writes=[NF])
    P.op("dve", lambda e: e.tensor_tensor(out=tmp[:, :, :, :], in0=incl.t.rearrange("p h (j r) -> p h j r", r=8),
                                          in1=sel[:, :].unsqueeze(1).unsqueeze(1).to_broadcast([128, 6, 8, 8]), op=ALU.mult),
         reads=[incl, sel], writes=[tmp])
    P.op("dve", lambda e: e.tensor_reduce(out=nfe[:, :, :], in_=tmp[:, :, :, :], axis=AX.X, op=ALU.add), reads=[tmp], writes=[nfe])
    for h in range(6):
        for j in range(8):
            P.op("dve", lambda e, h=h, j=j: e.tensor_scalar(out=bias[:, h, j, :], in0=NF[:, h, :], scalar1=nfe[:, h, j:j + 1],
                                                            scalar2=0.0, op0=ALU.subtract, op1=ALU.min),
                 reads=[NF, nfe], writes=[bias])
    P.end_phase()


def vg_layout(v_all, nheads):
    v = v_all.reshape(64, 128, nheads, 128).transpose(2, 1, 0, 3)
    o = np.ones((nheads, 128, 64, 129), NPBF)
    o[:, :, :, :128] = v
    return o


def band_masks(c):
    sp = np.arange(128)[:, None]
    t = np.arange(128)[None, :]
    cm = np.zeros((128, 8, 128), np.float32)
    am = np.full((128, 8, 128), NEG, np.float32)
    for r in range(8):
        if r < c:
            cm[:, r, :] = 1.0
            am[:, r, :] = 0.0
        elif r == c:
            cm[:, r, :] = (sp <= t)
            am[:, r, :] = np.where(sp.T <= t.T, 0.0, NEG)
    sel = np.zeros((128, 8), np.float32)
    sel[:, c] = 1.0
    return cm.astype(NPBF), am, sel


TRI = np.triu(np.ones((128, 128), np.float32))


def assemble_tokens(parts, axis):
    shp = list(parts[0].shape)
    n = shp[axis]
    assert n == 1024
    st = np.stack([np.moveaxis(p, axis, 0).reshape((8, 128) + tuple(np.moveaxis(p, axis, 0).shape[1:])) for p in parts], axis=1)
    g = st.reshape((8192,) + st.shape[3:])
    return np.moveaxis(g, 0, axis)


def emit_dsa_select(P, iqT_d, ikT_d, iw_d, am_d, ident, maskT):
    P.begin_phase()
    score = P.sb([128, S], F32, "sb_score")
    junk = P.sb([128, S], BF16, "sb_junk")
    ikT = P.sb([64, S], BF16, "sb_ikT")
    iq = [P.sb([64, 16, 128], BF16, "sb_iq") for _ in range(2)]
    rr = [P.sb([128, 512], F32, "sb_rr") for _ in range(3)]
    am = P.sb([128, 8, 128], F32, "sb_am")
    iw = P.sb([128, 8, 16], F32, "sb_iw")
    wsc = P.sb([128, 8, 16], F32, "sb_wsc")
    pI = [P.ps([128, 512], F32, "ps_I") for _ in range(4)]
    pT = [P.ps([128, 8, 128], BF16, "ps_mT") for _ in range(2)]
    mch = [P.sb([128, 1024], BF16, "sb_mch") for _ in range(2)]
    sms = [P.sb([128, 8], F32, "sb_sm") for _ in range(2)]
    for q4 in range(4):
        P.load("sp" if q4 % 2 == 0 else "act", ikT, ikT[:, q4 * 2048:(q4 + 1) * 2048], ikT_d.t[:, q4 * 2048:(q4 + 1) * 2048])
    P.load("sp", am, am[:, :, :], am_d[:, :, :])
    P.load("act", iw, iw[:, :, :], iw_d[:, :, :])
    P.op("dve", lambda e: e.tensor_scalar(out=wsc[:, :, :], in0=iw[:, :, :], scalar1=(64 ** -0.5) * (16 ** -0.5), scalar2=None,
                                          op0=ALU.mult), reads=[iw], writes=[wsc])
    ii = 0
    ti = 0
    for j in range(8):
        L = (8 * j + 8) * 128
        iqj = iq[j % 2]
        P.load("pool", iqj, iqj[:, :, :], iqT_d.t[:, :, j * 128:(j + 1) * 128])
        sm = sms[j % 2]
        for ck in range(L // 512):
            sc = score.t[:, ck * 512:(ck + 1) * 512]
            for h in range(16):
                ps = pI[ii % 4]
                r = rr[ii % 3]
                ii += 1
                P.mm(ps[:, :], iqj[:, h, :], ikT[:, ck * 512:(ck + 1) * 512], True, True, reads=[iqj, ikT], writes=[ps])
                P.op("act", lambda e, r=r, ps=ps: e.activation(out=r[:, :], in_=ps[:, :], func=AF.Relu), reads=[ps], writes=[r])
                if h == 0:
                    P.op("dve", lambda e, sc=sc, r=r, j=j: e.tensor_scalar(out=sc, in0=r[:, :], scalar1=wsc[:, j, 0:1], scalar2=None,
                                                                         op0=ALU.mult), reads=[r, wsc], writes=[score])
                else:
                    P.op("dve", lambda e, sc=sc, r=r, j=j, h=h: e.scalar_tensor_tensor(
                        out=sc, in0=r[:, :], scalar=wsc[:, j, h:h + 1], in1=sc, op0=ALU.mult, op1=ALU.add),
                        reads=[r, wsc, score], writes=[score])
        sL = score.t[:, 0:L]
        P.op("dve", lambda e, sm=sm, sL=sL: e.tensor_reduce(out=sm[:, 0:1], in_=sL, axis=AX.X, op=ALU.max, apply_absolute_value=True),
             reads=[score], writes=[sm])
        P.op("dve", lambda e, sm=sm: e.tensor_scalar(out=sm[:, 0:1], in0=sm[:, 0:1], scalar1=1.001, scalar2=1e-3, op0=ALU.mult, op1=ALU.add),
             reads=[sm], writes=[sm])
        P.op("dve", lambda e, sm=sm: e.tensor_scalar(out=sm[:, 1:2], in0=sm[:, 0:1], scalar1=-1.0, scalar2=None, op0=ALU.mult),
             reads=[sm], writes=[sm])
        sB = score.t[:, L - 1024:L]
        P.op("dve", lambda e, sB=sB: e.tensor_tensor(out=sB, in0=sB, in1=am.t.rearrange("p r s -> p (r s)"), op=ALU.add),
             reads=[score, am], writes=[score])
        for k in range(1, NBIS + 1):
            f = 2.0 ** (1 - k)
            P.op("dve", lambda e, sm=sm, f=f: e.tensor_scalar(out=sm[:, 2:3], in0=sm[:, 0:1], scalar1=f, scalar2=sm[:, 1:2],
                                                            op0=ALU.mult, op1=ALU.add), reads=[sm], writes=[sm])
            P.op("dve", lambda e, sm=sm, sL=sL, L=L: e.tensor_scalar(out=junk[:, 0:L], in0=sL, scalar1=sm[:, 2:3], scalar2=0.0,
                                                                   op0=ALU.is_ge, op1=ALU.add, accum_out=sm[:, 3:4]),
                 reads=[score, sm], writes=[junk, sm])
            P.op("dve", lambda e, sm=sm, f=f: e.tensor_scalar(out=sm[:, 4:5], in0=sm[:, 3:4], scalar1=TOPK - 0.5, scalar2=f,
                                                            op0=ALU.is_ge, op1=ALU.mult), reads=[sm], writes=[sm])
            P.op("dve", lambda e, sm=sm: e.scalar_tensor_tensor(out=sm[:, 1:2], in0=sm[:, 4:5], scalar=sm[:, 0:1], in1=sm[:, 1:2],
                                                              op0=ALU.mult, op1=ALU.add), reads=[sm], writes=[sm])
        for g in range(L // 1024):
            mc = mch[ti % 2]
            pt = pT[ti % 2]
            ti += 1
            P.op("dve", lambda e, mc=mc, g=g, sm=sm: e.tensor_scalar(out=mc[:, :], in0=score[:, g * 1024:(g + 1) * 1024],
                                                                   scalar1=sm[:, 1:2], scalar2=None, op0=ALU.is_ge),
                 reads=[score, sm], writes=[mc])
            for q in range(8):
                P.op("pe", lambda e, pt=pt, mc=mc, q=q: e.transpose(out=pt[:, q, :], in_=mc[:, q * 128:(q + 1) * 128], identity=ident[:, :]),
                     reads=[mc, ident], writes=[pt])
            mt = maskT[j]
            P.op("act", lambda e, mt=mt, pt=pt, g=g: e.activation(out=mt[:, g * 8:(g + 1) * 8, :], in_=pt[:, :, :], func=AF.Copy),
                 reads=[pt], writes=[mt])
    P.end_phase()


def emit_gla(P, gqT_d, gkT_d, gktm_d, gvtm_d, grtm_d, gaT_d, wa2_d, nbacol_d, barow_d, gn_d, tri2_d, suf2_d, rmask_d, out_d):
    P.scope_begin()

    def keep(shape, dt, name):
        return P.sbs(shape, dt, name)
    qtT = keep([128, S], BF16, "sbk_qtT")
    ktT = keep([128, S], BF16, "sbk_ktT")
    dn = keep([128, 128], F32, "sbk_dn")
    Sb = keep([128, 128, 128], BF16, "sbk_Sb")
    vtm = keep([128, 64, 128], BF16, "sbk_v")
    gs = keep([128, 64, 128], F32, "sbk_gs")
    wa2b = keep([16, 128], BF16, "sbk_wa2b")
    gaT = keep([16, S], BF16, "sbk_gaT")
    tri2 = keep([128, 128], F32, "sbk_tri2")
    onec = keep([128, 1], F32, "sbk_onec")
    epsc = keep([128, 1], F32, "sbk_epsc")

    P.begin_phase()
    wa2f = P.sb([16, 128], F32, "sb_wa2f")
    nbac = P.sb([128, 1], F32, "sb_nbac")
    rmask = P.sb([128, 512], F32, "sb_rmask")
    gq = [P.sb([128, 512], BF16, "sb_gq") for _ in range(2)]
    gk = [P.sb([128, 512], BF16, "sb_gk") for _ in range(2)]
    e1 = [P.sb([128, 512], F32, "sb_e1") for _ in range(2)]
    cs = [P.sb([128, 512], F32, "sb_cs") for _ in range(2)]
    eg = [P.sb([128, 512], F32, "sb_eg") for _ in range(2)]
    en = [P.sb([128, 512], F32, "sb_en") for _ in range(2)]
    pg = [P.ps([128, 512], F32, "ps_g") for _ in range(2)]
    P.load("sp", wa2f, wa2f[:, :], wa2_d[:, :])
    P.load("act", nbac, nbac[:, :], nbacol_d[:, :])
    P.load("sp", rmask, rmask[:, :], rmask_d[:, :])
    P.load("act", tri2, tri2[:, :], tri2_d[:, :])
    for q4 in range(4):
        P.load("sp" if q4 % 2 == 0 else "act", gaT, gaT[:, q4 * 2048:(q4 + 1) * 2048], gaT_d.t[:, q4 * 2048:(q4 + 1) * 2048])
    for q2 in range(2):
        P.load("pool", vtm, vtm[:, q2 * 32:(q2 + 1) * 32, :], gvtm_d.t[:, q2 * 32:(q2 + 1) * 32, :])
    P.op("dve", lambda e: e.tensor_copy(out=wa2b[:, :], in_=wa2f[:, :]), reads=[wa2f], writes=[wa2b])
    P.op("dve", lambda e: e.memset(onec[:, :], 1.0), writes=[onec])
    P.op("dve", lambda e: e.memset(epsc[:, :], 1e-6), writes=[epsc])
    for tc in range(16):
        sl = slice(tc * 512, (tc + 1) * 512)
        a, b_ = gq[tc % 2], gk[tc % 2]
        P.load("sp", a, a[:, :], gqT_d.t[:, sl])
        P.load("act", b_, b_[:, :], gkT_d.t[:, sl])
        ps = pg[tc % 2]
        x1, c1, g1, n1 = e1[tc % 2], cs[tc % 2], eg[tc % 2], en[tc % 2]
        P.mm(ps[:, :], wa2b[:, :], gaT[:, sl], True, True, reads=[wa2b, gaT], writes=[ps])
        P.op("act", lambda e, x1=x1, ps=ps: e.activation(out=x1[:, :], in_=ps[:, :], func=AF.Exp, scale=-1.0, bias=nbac[:, 0:1]),
             reads=[ps, nbac], writes=[x1])
        P.op("act", lambda e, x1=x1: e.activation(out=x1[:, :], in_=x1[:, :], func=AF.Ln, bias=onec[:, 0:1]), reads=[x1, onec], writes=[x1])
        P.op("dve", lambda e, x1=x1, c1=c1: e.tensor_tensor_scan(out=c1[:, :], data0=rmask[:, :], data1=x1[:, :], initial=0.0,
                                                               op0=ALU.mult, op1=ALU.add), reads=[rmask, x1], writes=[c1])
        P.op("act", lambda e, c1=c1, g1=g1: e.activation(out=g1[:, :], in_=c1[:, :], func=AF.Exp, scale=-1.0 / 16, bias=ZB[0][:, 0:1]),
             reads=[c1, ZB[0]], writes=[g1])
        P.op("act", lambda e, c1=c1, n1=n1: e.activation(out=n1[:, :], in_=c1[:, :], func=AF.Exp, scale=1.0 / 16, bias=ZB[0][:, 0:1]),
             reads=[c1, ZB[0]], writes=[n1])
        P.op("dve", lambda e, a=a, g1=g1, sl=sl: e.scalar_tensor_tensor(out=qtT[:, sl], in0=a[:, :], scalar=SCALE, in1=g1[:, :],
                                                                      op0=ALU.mult, op1=ALU.mult), reads=[a, g1], writes=[qtT])
        P.op("pool", lambda e, b_=b_, n1=n1, sl=sl: e.tensor_tensor(out=ktT[:, sl], in0=b_[:, :], in1=n1[:, :], op=ALU.mult),
             reads=[b_, n1], writes=[ktT])
        P.op("dve", lambda e, g1=g1, tc=tc: e.tensor_copy(out=dn[:, tc * 8:(tc + 1) * 8],
                                                        in_=g1.t.rearrange("p (n c) -> p n c", c=64)[:, :, 63]),
             reads=[g1], writes=[dn])
    P.end_phase()

    P.begin_phase()
    barow = P.sb([128, 128], F32, "sb_barow")
    gn = P.sb([128, 128], F32, "sb_gn")
    suf2 = P.sb([128, 128], F32, "sb_suf2")
    ktm = P.sb([128, 64, 128], BF16, "sb_ktm")
    Sst = P.sb([128, 128], F32, "sb_Sst")
    xg = [P.sb([128, 128], F32, "sb_xg") for _ in range(2)]
    fk = [P.sb([128, 128], F32, "sb_fk") for _ in range(2)]
    kh = [P.sb([128, 128], BF16, "sb_kh") for _ in range(2)]
    grt = [P.sb([128, 8, 128], F32, "sb_grt") for _ in range(2)]
    pl = [P.ps([128, 128], F32, "ps_l") for _ in range(2)]
    pf = [P.ps([128, 128], F32, "ps_f") for _ in range(2)]
    pU = [P.ps([128, 128], F32, "ps_U") for _ in range(4)]
    P.load("sp", barow, barow[:, :], barow_d[:, :])
    P.load("act", gn, gn[:, :], gn_d[:, :])
    P.load("sp", suf2, suf2[:, :], suf2_d[:, :])
    for q2 in range(2):
        P.load("pool", ktm, ktm[:, q2 * 32:(q2 + 1) * 32, :], gktm_d.t[:, q2 * 32:(q2 + 1) * 32, :])
    P.op("dve", lambda e: e.memset(Sst[:, :], 0.0), writes=[Sst])
    for g8 in range(8):
        gt = grt[g8 % 2]
        P.load("sp" if g8 % 2 == 0 else "act", gt, gt[:, :, :], grtm_d.t[:, g8 * 8:(g8 + 1) * 8, :])
        P.op("act", lambda e, gt=gt: e.activation(out=gt[:, :, :], in_=gt[:, :, :], func=AF.Silu), reads=[gt], writes=[gt])
        P.op("pool", lambda e, gt=gt, g8=g8: e.tensor_tensor(out=gs[:, g8 * 8:(g8 + 1) * 8, :], in0=gt[:, :, :],
                                                           in1=gn[:, :].unsqueeze(1).to_broadcast([128, 8, 128]), op=ALU.mult),
             reads=[gt, gn], writes=[gs])
    for blk in range(64):
        x, f, k2 = xg[blk % 2], fk[blk % 2], kh[blk % 2]
        p1, p2 = pl[blk % 2], pf[blk % 2]
        P.mm(p1[:, :], gaT[:, blk * 128:(blk + 1) * 128], wa2b[:, :], True, True, reads=[gaT, wa2b], writes=[p1])
        P.op("dve", lambda e, x=x, p1=p1: e.tensor_tensor(out=x[:, :], in0=p1[:, :], in1=barow[:, :], op=ALU.add),
             reads=[p1, barow], writes=[x])
        P.op("act", lambda e, x=x: e.activation(out=x[:, :], in_=x[:, :], func=AF.Exp, scale=-1.0, bias=ZB[0][:, 0:1]),
             reads=[x, ZB[0]], writes=[x])
        P.op("act", lambda e, x=x: e.activation(out=x[:, :], in_=x[:, :], func=AF.Ln, bias=onec[:, 0:1]), reads=[x, onec], writes=[x])
        P.mm(p2[:, :], suf2[:, :], x[:, :], True, True, reads=[suf2, x], writes=[p2])
        P.op("act", lambda e, f=f, p2=p2: e.activation(out=f[:, :], in_=p2[:, :], func=AF.Exp, scale=-1.0 / 16, bias=ZB[0][:, 0:1]),
             reads=[p2, ZB[0]], writes=[f])
        P.op("pool", lambda e, k2=k2, f=f, blk=blk: e.tensor_tensor(out=k2[:, :], in0=ktm[:, blk, :], in1=f[:, :], op=ALU.mult),
             reads=[ktm, f], writes=[k2])
        for hf in range(2):
            n = 2 * blk + hf
            pu = pU[n % 4]
            P.mm(pu[:, :], k2[hf * 64:(hf + 1) * 64, :], vtm[hf * 64:(hf + 1) * 64, blk, :], True, True, reads=[k2, vtm], writes=[pu])
            P.op("dve", lambda e, pu=pu, n=n: e.scalar_tensor_tensor(out=Sst[:, :], in0=Sst[:, :], scalar=dn[:, n:n + 1], in1=pu[:, :],
                                                                   op0=ALU.mult, op1=ALU.add), reads=[Sst, dn, pu], writes=[Sst])
            P.op("act", lambda e, n=n: e.activation(out=Sb[:, n, :], in_=Sst[:, :], func=AF.Copy), reads=[Sst], writes=[Sb])
    P.end_phase()

    P.begin_phase()
    ost = P.sb([128, 64, 128], BF16, "sb_gost")
    At = [P.sb([128, 128], BF16, "sb_At") for _ in range(2)]
    st = [P.sb([128, 4], F32, "sb_gst") for _ in range(2)]
    jk = P.sb([128, 128], F32, "sb_gjk")
    pA = [P.ps([128, 128], F32, "ps_A") for _ in range(2)]
    pO = [P.ps([128, 128], F32, "ps_GO") for _ in range(2)]
    for blk in range(64):
        sl = slice(blk * 128, (blk + 1) * 128)
        pa, po, at, s = pA[blk % 2], pO[blk % 2], At[blk % 2], st[blk % 2]
        P.mm(pa[:, :], ktT[:, sl], qtT[:, sl], True, True, reads=[ktT, qtT], writes=[pa])
        P.op("dve", lambda e, at=at, pa=pa: e.tensor_tensor(out=at[:, :], in0=pa[:, :], in1=tri2[:, :], op=ALU.mult),
             reads=[pa, tri2], writes=[at])
        P.mm(po[:, :], at[:, :], vtm[:, blk, :], True, False, reads=[at, vtm], writes=[po])
        if blk > 0:
            P.mm(po[0:64, :], qtT[:, blk * 128:blk * 128 + 64], Sb[:, 2 * blk - 1, :], False, False, reads=[qtT, Sb], writes=[po])
        P.mm(po[64:128, :], qtT[:, blk * 128 + 64:blk * 128 + 128], Sb[:, 2 * blk, :], False, True, reads=[qtT, Sb], writes=[po])
        P.op("act", lambda e, po=po, s=s: e.activation(out=jk[:, :], in_=po[:, :], func=AF.Square, accum_out=s[:, 0:1]),
             reads=[po], writes=[jk, s])
        P.op("act", lambda e, s=s: e.activation(out=s[:, 1:2], in_=s[:, 0:1], func=AF.Sqrt, scale=1.0 / 128, bias=epsc[:, 0:1]),
             reads=[s, epsc], writes=[s])
        P.op("dve", lambda e, s=s: e.reciprocal(out=s[:, 2:3], in_=s[:, 1:2]), reads=[s], writes=[s])
        P.op("dve", lambda e, po=po, s=s, blk=blk: e.scalar_tensor_tensor(out=ost[:, blk, :], in0=po[:, :], scalar=s[:, 2:3],
                                                                        in1=gs[:, blk, :], op0=ALU.mult, op1=ALU.mult),
             reads=[po, s, gs], writes=[ost])
    P.store("sp", ost, out_d.t.rearrange("b p e -> p b e"), ost[:, :, :])
    P.end_phase()
    P.scope_end()


ZB = [None]


def emit_zero(P):
    zb = P.sbp([128, 1], F32, "sbp_zero")
    ZB[0] = zb
    P.begin_phase()
    P.op("dve", lambda e: e.memset(zb[:, :], 0.0), writes=[zb])
    P.end_phase()


def gla_consts():
    s = np.arange(128)[:, None]
    t = np.arange(128)[None, :]
    same = (s // 64) == (t // 64)
    tri2 = (same & (s <= t)).astype(np.float32)
    suf2 = (same & (s > t)).astype(np.float32)
    rmask = np.ones((128, 512), np.float32)
    rmask[:, ::64] = 0.0
    return tri2, suf2, rmask


def tm_layout(a):
    return np.ascontiguousarray(a.reshape(64, 128, a.shape[1]).transpose(1, 0, 2))


def gla_inputs(hg, gq, gk, gv, gr, ga, wa2_l, ba_l, gng_l):
    sl = slice(hg * 128, (hg + 1) * 128)
    tri2, suf2, rmask = gla_consts()
    return dict(gqT=np.ascontiguousarray(gq[:, sl].T).astype(NPBF), gkT=np.ascontiguousarray(gk[:, sl].T).astype(NPBF),
                gktm=tm_layout(gk[:, sl]).astype(NPBF), gvtm=tm_layout(gv[:, sl]).astype(NPBF),
                grtm=tm_layout(gr[:, sl]).astype(np.float32), gaT=np.ascontiguousarray(ga.T).astype(NPBF),
                wa2=np.ascontiguousarray(wa2_l[:, sl]), nbacol=np.ascontiguousarray(-ba_l[sl][:, None]),
                barow=bc128(ba_l[sl]), gnb=bc128(gng_l), tri2=tri2, suf2=suf2, rmask=rmask)


def build_B():
    nc = bass.Bass("TRN2", target_bir_lowering=False)
    P = Prog(nc)
    Dm = {}
    for name, shp, dt in [("fkT", [768, S], BF16), ("fvg", [6, 128, NKB, 129], BF16), ("fqT", [768, 1024], BF16), ("ffp", [128, 6, 64], F32),
                          ("fbb", [128, 6], F32), ("tri", [128, 128], F32), ("sel", [128, 8], F32), ("cm", [128, 8, 128], BF16),
                          ("dkT", [768, S], BF16), ("dvg", [6, 128, NKB, 129], BF16), ("dqT", [768, 1024], BF16), ("iqT", [64, 16, 1024], BF16),
                          ("ikT", [64, S], BF16), ("iwp", [128, 8, 16], F32), ("am", [128, 8, 128], F32), ("ident", [128, 128], BF16),
                          ("gqT", [128, S], BF16), ("gkT", [128, S], BF16), ("gktm", [128, 64, 128], BF16), ("gvtm", [128, 64, 128], BF16),
                          ("grtm", [128, 64, 128], F32), ("gaT", [16, S], BF16), ("wa2", [16, 128], F32), ("nbacol", [128, 1], F32),
                          ("barow", [128, 128], F32), ("gnb", [128, 128], F32), ("tri2", [128, 128], F32), ("suf2", [128, 128], F32),
                          ("rmask", [128, 512], F32)]:
        Dm[name] = P.dram(name, shp, dt, "ExternalInput")
    foxo = P.dram("foxo", [8, 128, 768], BF16, "ExternalOutput")
    dsao = P.dram("dsao", [8, 128, 768], BF16, "ExternalOutput")
    glao = P.dram("glao", [64, 128, 128], BF16, "ExternalOutput")
    emit_zero(P)
    ident = P.sbp([128, 128], BF16, "sbp_ident")
    P.begin_phase()
    P.load("sp", ident, ident[:, :], Dm["ident"][:, :])
    P.end_phase()
    P.scope_begin()
    bias = P.sbs([128, 6, 8, 64], F32, "sbs_bias")
    cm = P.sbs([128, 8, 128], BF16, "sbs_cm")
    ostF = P.sbs([128, 8, 768], BF16, "sbs_ostF")
    P.begin_phase()
    P.load("sp", cm, cm[:, :, :], Dm["cm"][:, :, :])
    P.end_phase()
    emit_fox_bias(P, Dm["ffp"], Dm["fbb"], Dm["tri"], Dm["sel"], bias)
    emit_attn(P, Dm["fkT"], Dm["fvg"], Dm["fqT"], 6, ostF, 0, bias=bias, cm=cm)
    P.begin_phase()
    P.store("sp", ostF, foxo.t.rearrange("j p w -> p j w"), ostF[:, :, :])
    P.end_phase()
    P.scope_end()
    P.scope_begin()
    ostD = P.sbs([128, 8, 768], BF16, "sbs_ostD")
    maskT = [P.sbs([128, 8 * j + 8, 128], BF16, "sbs_mT") for j in range(8)]
    emit_dsa_select(P, Dm["iqT"], Dm["ikT"], Dm["iwp"], Dm["am"], ident, maskT)
    emit_attn(P, Dm["dkT"], Dm["dvg"], Dm["dqT"], 6, ostD, 0, maskT=maskT)
    P.begin_phase()
    P.store("sp", ostD, dsao.t.rearrange("j p w -> p j w"), ostD[:, :, :])
    P.end_phase()
    P.scope_end()
    emit_gla(P, Dm["gqT"], Dm["gkT"], Dm["gktm"], Dm["gvtm"], Dm["grtm"], Dm["gaT"], Dm["wa2"], Dm["nbacol"], Dm["barow"], Dm["gnb"],
             Dm["tri2"], Dm["suf2"], Dm["rmask"], glao)
    P.begin_phase()
    P.end_phase(final=True)
    P.finish()
    return nc


NE = 16384


def build_C1():
    nc = bass.Bass("TRN2", target_bir_lowering=False)
    P = Prog(nc)
    xs = P.dram("xs", [NB, 128, D], F32, "ExternalInput")
    mixT_d = P.dram("mixT", [D, NTOK], BF16, "ExternalInput")
    c_d = P.dram("c_pk", [128, 16], F32, "ExternalInput")
    adaw = P.dram("adaw", [D, 8192], F32, "ExternalInput")
    adab = P.dram("adab", [128, 8192], F32, "ExternalInput")
    g_d = P.dram("g_bc", [128, D], F32, "ExternalInput")
    ident_d = P.dram("ident", [128, 128], BF16, "ExternalInput")
    wo_d = P.dram("wo", [D, D], F32, "ExternalInput")
    wq_d = P.dram("wq", [D, D], F32, "ExternalInput")
    kT_d = P.dram("kT", [16, 128, 128], F32, "ExternalInput")
    xmid = P.dram("xmid", [NB, 128, D], F32, "ExternalOutput")
    h2T_d = P.dram("h2T", [16, 128, NTOK], BF16, "ExternalOutput")
    s12_d = P.dram("s12", [NB, 128, 16, 128], F32, "ExternalOutput")
    g2_d = P.dram("g2bc", [128, D], F32, "ExternalOutput")
    ident = emit_consts(P, ident_d)
    emit_C1(P, ident, xs, None, mixT_d, None, c_d, adaw, adab, g_d, wo_d, wq_d, kT_d, xmid, h2T_d, s12_d, g2_d)
    P.begin_phase()
    P.end_phase(final=True)
    P.finish()
    return nc


def emit_C1(P, ident, xs, xs_dram, mixT_d, mixT_sb, c_d, adaw, adab, g_d, wo_d, wq_d, kT_d, xmid, h2T_d, s12_d, g2_d):
    P.scope_begin()
    h2T = P.sbs([128, 16, NTOK], BF16, "sbs_h2T")
    P.scope_begin()
    modbc = P.sbs([128, 8192], F32, "sbs_mod2")
    emit_mod(P, c_d, adaw, adab, 8192, modbc)
    P.begin_phase()
    mixT = mixT_sb if mixT_sb is not None else P.sb([128, 16, NTOK], BF16, "sb_mixT")
    wf = P.sb([128, 16, 512], F32, "sb_wof")
    wb = [P.sb([128, 16, 512], BF16, "sb_wob") for _ in range(2)]
    xt = [P.sb([128, 512], F32, "sb_xt") for _ in range(3)]
    tm = [P.sb([128, 512], F32, "sb_tm") for _ in range(2)]
    pp = [P.ps([128, 512], F32, "ps_wo") for _ in range(3)]
    if mixT_sb is None:
        mv = mixT_d.t.rearrange("(k p) t -> p k t", p=128)
        for kh in range(2):
            P.load("sp" if kh == 0 else "act", mixT, mixT[:, kh * 8:(kh + 1) * 8, :], mv[:, kh * 8:(kh + 1) * 8, :])
    wv = wo_d.t.rearrange("(k p) n -> p k n", p=128)
    i = 0
    for dc in range(4):
        w16 = wb[dc % 2]
        for kh in range(2):
            P.load("sp" if kh == 0 else "act", wf, wf[:, kh * 8:(kh + 1) * 8, :], wv[:, kh * 8:(kh + 1) * 8, dc * 512:(dc + 1) * 512])
        P.op("dve", lambda e, w16=w16: e.tensor_copy(out=w16[:, 0:8, :], in_=wf[:, 0:8, :]), reads=[wf], writes=[w16])
        P.op("pool", lambda e, w16=w16: e.tensor_copy(out=w16[:, 8:16, :], in_=wf[:, 8:16, :]), reads=[wf], writes=[w16])
        for b in range(NB):
            ps = pp[i % 3]
            x = xt[i % 3]
            t = tm[i % 2]
            i += 1
            P.load("pool", x, x[:, :], xs[b, :, dc * 512:(dc + 1) * 512], dram=xs_dram)
            for k in range(16):
                P.mm(ps[:, :], mixT[:, k, b * 128:(b + 1) * 128], w16[:, k, :], k == 0, k == 15, reads=[mixT, w16], writes=[ps])
            P.op("dve", lambda e, t=t, ps=ps, dc=dc: e.tensor_tensor(out=t[:, :], in0=ps[:, :], in1=modbc[:, dc * 512:(dc + 1) * 512], op=ALU.mult),
                 reads=[ps, modbc], writes=[t])
            P.op("dve", lambda e, t=t, x=x: e.tensor_tensor(out=x[:, :], in0=x[:, :], in1=t[:, :], op=ALU.add), reads=[x, t], writes=[x])
            P.store("sp", x, xmid[b, :, dc * 512:(dc + 1) * 512], x[:, :], dram=xmid)
    P.end_phase()

    A2 = P.sbs([128, D], F32, "sbs_A2")
    P.begin_phase()
    gb = P.sb([128, D], F32, "sb_g")
    P.load("sp", gb, gb[:, :], g_d[:, :])
    P.op("dve", lambda e: e.scalar_tensor_tensor(out=A2[:, :], in0=modbc[:, 2 * D:3 * D], scalar=1.0, in1=gb[:, :],
                                                 op0=ALU.add, op1=ALU.mult), reads=[modbc, gb], writes=[A2])
    P.store("act", modbc, g2_d[:, :], modbc[:, 3 * D:4 * D], dram=g2_d)
    P.end_phase()
    emit_norm_T(P, xmid, A2, modbc_view(modbc, D, 2 * D), ident, h2T, xdram=xmid)
    P.scope_end()

    P.begin_phase()
    P.store("sp", h2T, h2T_d.t.rearrange("k p t -> p k t"), h2T[:, :, :], dram=h2T_d)
    qT = P.sb([128, 16, NTOK], BF16, "sb_qT")
    kf = P.sb([128, 16, 128], F32, "sb_kf")
    kb_ = P.sb([128, 16, 128], BF16, "sb_kb")
    wf = P.sb([128, 16, 512], F32, "sb_wqf")
    wb = [P.sb([128, 16, 512], BF16, "sb_wqb") for _ in range(2)]
    pp = [P.ps([128, 512], F32, "ps_q") for _ in range(3)]
    pq = [P.ps([128, 4, 128], F32, "ps_s") for _ in range(2)]
    sst = [P.sb([128, 16, 128], F32, "sb_sst") for _ in range(2)]
    P.load("pool", kf, kf[:, :, :], kT_d.t.rearrange("g d n -> d g n"))
    P.op("dve", lambda e: e.tensor_copy(out=kb_[:, :, :], in_=kf[:, :, :]), reads=[kf], writes=[kb_])
    wv = wq_d.t.rearrange("(k p) n -> p k n", p=128)
    i = 0
    for dc in range(4):
        w16 = wb[dc % 2]
        for kh in range(2):
            P.load("sp" if kh == 0 else "act", wf, wf[:, kh * 8:(kh + 1) * 8, :], wv[:, kh * 8:(kh + 1) * 8, dc * 512:(dc + 1) * 512])
        P.op("dve", lambda e, w16=w16: e.tensor_copy(out=w16[:, 0:8, :], in_=wf[:, 0:8, :]), reads=[wf], writes=[w16])
        P.op("pool", lambda e, w16=w16: e.tensor_copy(out=w16[:, 8:16, :], in_=wf[:, 8:16, :]), reads=[wf], writes=[w16])
        for g in range(4):
            gidx = dc * 4 + g
            for th in range(2):
                ps = pp[i % 3]
                i += 1
                for k in range(16):
                    P.mm(ps[:, :], w16[:, k, g * 128:(g + 1) * 128], h2T[:, k, th * 512:(th + 1) * 512], k == 0, k == 15,
                         reads=[w16, h2T], writes=[ps])
                if th == 0:
                    P.op("act", lambda e, ps=ps, gidx=gidx, th=th: e.activation(out=qT[:, gidx, th * 512:(th + 1) * 512], in_=ps[:, :], func=AF.Copy),
                         reads=[ps], writes=[qT])
                else:
                    P.op("dve", lambda e, ps=ps, gidx=gidx, th=th: e.tensor_copy(out=qT[:, gidx, th * 512:(th + 1) * 512], in_=ps[:, :]),
                         reads=[ps], writes=[qT])
    i = 0
    for b in range(NB):
        st = sst[b % 2]
        for g4 in range(4):
            ps = pq[i % 2]
            i += 1
            for q in range(4):
                gidx = g4 * 4 + q
                P.mm(ps[:, q, :], qT[:, gidx, b * 128:(b + 1) * 128], kb_[:, gidx, :], True, True, reads=[qT, kb_], writes=[ps])
            if g4 % 2 == 0:
                P.op("act", lambda e, ps=ps, st=st, g4=g4: e.activation(out=st[:, g4 * 4:(g4 + 1) * 4, :], in_=ps[:, :, :], func=AF.Copy),
                     reads=[ps], writes=[st])
            else:
                P.op("dve", lambda e, ps=ps, st=st, g4=g4: e.tensor_copy(out=st[:, g4 * 4:(g4 + 1) * 4, :], in_=ps[:, :, :]),
                     reads=[ps], writes=[st])
        P.store("pool", st, s12_d[b, :, :, :], st[:, :, :], dram=s12_d)
    P.end_phase()
    P.scope_end()


def host_C1_inputs(xs_list, mix_list, c, ada_w_l, ada_b_l, norm2_g_l, w_out_l, wq_l, k1_l, k2_l):
    kT = np.zeros((16, 128, 128), np.float32)
    for h in range(8):
        kT[2 * h] = k1_l[h].T
        kT[2 * h + 1] = k2_l[h].T
    common = dict(c_pk=np.ascontiguousarray(c.reshape(16, 128).T), adaw=np.ascontiguousarray(ada_w_l[:, 4096:12288]),
                  adab=bc128(ada_b_l[4096:12288]), g_bc=bc128(norm2_g_l), ident=np.eye(128, dtype=NPBF),
                  wo=np.ascontiguousarray(w_out_l), wq=np.ascontiguousarray(wq_l), kT=kT)
    return [dict(common, xs=xs_list[cc], mixT=np.ascontiguousarray(mix_list[cc].T)) for cc in range(8)]


def build_C2(final):
    nc = bass.Bass("TRN2", target_bir_lowering=False)
    P = Prog(nc)
    h2T_d = P.dram("h2T", [16, 128, NTOK], BF16, "ExternalInput")
    s12_d = P.dram("s12", [NB, 128, 16, 128], F32, "ExternalInput")
    xmid = P.dram("xmid", [NB, 128, D], F32, "ExternalInput")
    g2_d = P.dram("g2bc", [128, D], F32, "ExternalInput")
    uT_d = P.dram("uTt", [128, 128, 16, 128], F32, "ExternalInput")
    v_d = P.dram("v", [NE, D], F32, "ExternalInput")
    ident_d = P.dram("ident", [128, 128], BF16, "ExternalInput")
    fg_d = P.dram("fg_bc", [128, D], F32, "ExternalInput") if final else None
    xo = P.dram("xo", [NB, 128, D], F32, "ExternalOutput")
    ident = emit_consts(P, ident_d)
    emit_C2(P, ident, h2T_d, s12_d, xmid, g2_d, uT_d, v_d, fg_d, xo, final, True)
    P.finish()
    return nc


def emit_C2(P, ident, h2T_d, s12_d, xmid, g2_d, uT_d, v_d, fg_d, xo, final, last):
    P.scope_begin()
    kB_zero = P.sbs([128, 1], F32, "sbs_zero")
    g2 = P.sbs([128, D], F32, "sbs_g2")
    stats = P.sbs([128, 4, 8, 2], F32, "sbs_stats")
    P.begin_phase()
    P.load("sp", g2, g2[:, :], g2_d[:, :], dram=g2_d)
    P.op("dve", lambda e: e.memset(kB_zero[:, :], 0.0), writes=[kB_zero])
    P.end_phase()
    for ps_ in range(2):
        P.begin_phase()
        stt_ = [P.sb([128, 16, 128], F32, "sb_st") for _ in range(2)]
        v16 = [P.sb([128, 2, 16], F32, "sb_v16") for _ in range(2)]
        scr = [P.sb([128, 128], F32, "sb_scr") for _ in range(2)]
        cand = [P.sb([128, 16, 16], F32, "sb_cand") for _ in range(2)]
        scr2 = [P.sb([128, 256], F32, "sb_scr2") for _ in range(2)]
        ez = [P.sb([128, 256], F32, "sb_ez") for _ in range(2)]
        c8 = [P.sb([128, 16], F32, "sb_c8") for _ in range(2)]
        sm = [P.sb([128, 4], F32, "sb_sm") for _ in range(2)]
        i = 0
        for bl in range(4):
            b = ps_ * 4 + bl
            st = stt_[bl % 2]
            P.load("sp" if bl % 2 == 0 else "act", st, st[:, :, :], s12_d[b, :, :, :], dram=s12_d)
            for h in range(8):
                vv, sc, cd, s2_, ez_, c8_, sm_ = v16[i % 2], scr[i % 2], cand[i % 2], scr2[i % 2], ez[i % 2], c8[i % 2], sm[i % 2]
                i += 1
                for half in range(2):
                    src = st.t[:, 2 * h + half, :]
                    P.op("dve", lambda e, vv=vv, src=src, half=half: e.max(out=vv[:, half, 0:8], in_=src), reads=[st], writes=[vv])
                    P.op("dve", lambda e, vv=vv, src=src, half=half, sc=sc: e.match_replace(out=sc[:, :], in_to_replace=vv[:, half, 0:8],
                                                                                        in_values=src, imm_value=-1e30),
                         reads=[st, vv], writes=[sc])
                    P.op("dve", lambda e, vv=vv, half=half, sc=sc: e.max(out=vv[:, half, 8:16], in_=sc[:, :]), reads=[sc], writes=[vv])
                P.op("dve", lambda e, vv=vv, cd=cd: e.tensor_tensor(out=cd[:, :, :], in0=vv[:, 0, :].unsqueeze(2).to_broadcast([128, 16, 16]),
                                                                  in1=vv[:, 1, :].unsqueeze(1).to_broadcast([128, 16, 16]), op=ALU.add),
                     reads=[vv], writes=[cd])
                cf = cd.t.rearrange("p a b -> p (a b)")
                P.op("dve", lambda e, c8_=c8_, cf=cf: e.max(out=c8_[:, 0:8], in_=cf), reads=[cd], writes=[c8_])
                P.op("dve", lambda e, c8_=c8_, cf=cf, s2_=s2_: e.match_replace(out=s2_[:, :], in_to_replace=c8_[:, 0:8], in_values=cf, imm_value=-1e30),
                     reads=[cd, c8_], writes=[s2_])
                P.op("dve", lambda e, c8_=c8_, s2_=s2_: e.max(out=c8_[:, 8:16], in_=s2_[:, :]), reads=[s2_], writes=[c8_])
                P.op("dve", lambda e, c8_=c8_, sm_=sm_: e.tensor_scalar(out=sm_[:, 0:1], in0=c8_[:, 0:1], scalar1=-1.0, scalar2=None, op0=ALU.mult),
                     reads=[c8_], writes=[sm_])
                P.op("act", lambda e, ez_=ez_, cf=cf, sm_=sm_: e.activation(out=ez_[:, :], in_=cf, func=AF.Exp, bias=sm_[:, 0:1]),
                     reads=[cd, sm_], writes=[ez_])
                P.op("dve", lambda e, s2_=s2_, cf=cf, c8_=c8_, ez_=ez_, sm_=sm_: e.scalar_tensor_tensor(
                    out=s2_[:, :], in0=cf, scalar=c8_[:, 15:16], in1=ez_[:, :], op0=ALU.is_ge, op1=ALU.mult, accum_out=sm_[:, 1:2]),
                    reads=[cd, c8_, ez_], writes=[s2_, sm_])
                P.op("act", lambda e, sm_=sm_: e.activation(out=sm_[:, 2:3], in_=sm_[:, 1:2], func=AF.Ln, bias=kB_zero[:, 0:1]),
                     reads=[sm_, kB_zero], writes=[sm_])
                P.op("dve", lambda e, sm_=sm_, c8_=c8_, bl=bl, h=h: e.tensor_scalar(out=stats[:, bl, h, 1:2], in0=sm_[:, 2:3], scalar1=c8_[:, 0:1],
                                                                                  scalar2=-1.0, op0=ALU.add, op1=ALU.mult),
                     reads=[sm_, c8_], writes=[stats])
                P.op("dve", lambda e, c8_=c8_, bl=bl, h=h: e.tensor_copy(out=stats[:, bl, h, 0:1], in_=c8_[:, 15:16]), reads=[c8_], writes=[stats])
        P.end_phase()

        P.scope_begin()
        oacc = P.sbs([128, 4, D], F32, "sbs_oacc")
        P.begin_phase()
        h2p = P.sb([128, 16, 512], BF16, "sb_h2p")
        s12t = [P.sb([128, 16, 128], F32, "sb_s12t") for _ in range(2)]
        Xb = [P.sb([128, 4, 4, 128], F32, "sb_X") for _ in range(2)]
        Eb = [P.sb([128, 4, 4, 128], BF16, "sb_E") for _ in range(2)]
        Mb = [P.sb([128, 4, 4, 128], F32, "sb_Y") for _ in range(2)]
        Tb = [P.sb([128, 8, 4, 128], BF16, "sb_T") for _ in range(2)]
        cexp = P.sb([128, 4, 8], F32, "sb_cexp")
        Dg = P.sb([128, 4, 8, 128], BF16, "sb_Dg")
        GT = [P.sb([128, 4, 512], BF16, "sb_GT") for _ in range(2)]
        GAT = [P.sb([128, 4, 512], BF16, "sb_GAT") for _ in range(2)]
        uf = [P.sb([128, 16, 128], F32, "sb_uf") for _ in range(1)]
        ub = [P.sb([128, 16, 128], BF16, "sb_ub") for _ in range(2)]
        vf = [P.sb([128, D], F32, "sb_vf") for _ in range(2)]
        vb = [P.sb([128, 4, D], BF16, "sb_vb") for _ in range(1)]
        ge = [P.sb([128, 512], F32, "sb_ge") for _ in range(2)]
        pG = [P.ps([128, 4, 128], F32, "ps_G") for _ in range(2)]
        pA = [P.ps([128, 512], F32, "ps_A") for _ in range(2)]
        pD = [P.ps([128, 512], F32, "ps_D") for _ in range(3)]
        hv = h2T_d.t.rearrange("k p t -> p k t")
        for kh in range(2):
            P.load("sp" if kh == 0 else "act", h2p, h2p[:, kh * 8:(kh + 1) * 8, :], hv[:, kh * 8:(kh + 1) * 8, ps_ * 512:(ps_ + 1) * 512], dram=h2T_d)
        P.op("act", lambda e: e.activation(out=cexp[:, :, :], in_=stats[:, :, :, 1], func=AF.Exp, bias=kB_zero[:, 0:1]),
             reads=[stats, kB_zero], writes=[cexp])
        for bl in range(4):
            for h in range(8):
                P.op("dve", lambda e, bl=bl, h=h: e.tensor_scalar(out=Dg[:, bl, h, :], in0=ident[:, :], scalar1=cexp[:, bl, h:h + 1],
                                                                 scalar2=None, op0=ALU.mult), reads=[ident, cexp], writes=[Dg])
        si = 0
        xi = 0
        di = 0
        pend = []

        def stage1(g, bl, hh, st, k):
            X, E, M = Xb[k % 2], Eb[k % 2], Mb[k % 2]
            stv = st.t.rearrange("p (h two) n -> p h two n", two=2)
            hs = slice(hh * 4, hh * 4 + 4)
            P.op("dve", lambda e: e.tensor_tensor(
                out=X[:, :, :, :], in0=stv[:, hs, 1, :].unsqueeze(2).to_broadcast([128, 4, 4, 128]),
                in1=stv[:, hs, 0, 4 * g:4 * g + 4].unsqueeze(3).to_broadcast([128, 4, 4, 128]), op=ALU.add), reads=[st], writes=[X])
            P.op("act", lambda e: e.activation(out=E[:, :, :, :], in_=X[:, :, :, :], func=AF.Exp, bias=kB_zero[:, 0:1]),
                 reads=[X, kB_zero], writes=[E])
            P.op("pool", lambda e: e.tensor_tensor(
                out=M[:, :, :, :], in0=X[:, :, :, :],
                in1=stats[:, bl, hs, 0].unsqueeze(2).unsqueeze(3).to_broadcast([128, 4, 4, 128]), op=ALU.subtract),
                reads=[X, stats], writes=[M])

        def stage2(g, bl, hh, T, pg, gt, k):
            E, M = Eb[k % 2], Mb[k % 2]
            hs = slice(hh * 4, hh * 4 + 4)
            P.op("dve", lambda e: e.scalar_tensor_tensor(out=T[:, hs, :, :], in0=M[:, :, :, :], scalar=0.0, in1=E[:, :, :, :],
                                                         op0=ALU.is_ge, op1=ALU.mult), reads=[E, M], writes=[T])
            if hh == 1:
                for a in range(4):
                    for h in range(8):
                        P.mm(pg[:, a, :], T[:, h, a, :], Dg[:, bl, h, :], h == 0, h == 7, reads=[T, Dg], writes=[pg])
                P.op("act", lambda e: e.activation(out=gt[:, :, bl * 128:(bl + 1) * 128], in_=pg[:, :, :], func=AF.Copy),
                     reads=[pg], writes=[gt])

        kstep = 0
        for g in range(32):
            gt, gat, vb_ = GT[g % 2], GAT[g % 2], vb[0]
            for bl in range(4):
                b = ps_ * 4 + bl
                st = s12t[si % 2]
                T = Tb[si % 2]
                pg = pG[si % 2]
                si += 1
                P.load("sp" if si % 2 == 0 else "act", st, st[:, :, :], s12_d[b, :, :, :], dram=s12_d)
                for hh in range(2):
                    stage1(g, bl, hh, st, kstep)
                    if pend:
                        stage2(*pend.pop())
                    pend.append((g, bl, hh, T, pg, gt, kstep))
                    kstep += 1
            if pend:
                stage2(*pend.pop())
            for a in range(4):
                ec = 4 * g + a
                u32, u16, v32, pa, gel = uf[0], ub[ec % 2], vf[ec % 2], pA[ec % 2], ge[ec % 2]
                P.load("sp", u32, u32[:, :, :], uT_d[ec, :, :, :])
                P.load("act", v32, v32[:, :], v_d[ec * 128:(ec + 1) * 128, :])
                P.op("pool", lambda e, u32=u32, u16=u16: e.tensor_copy(out=u16[:, :, :], in_=u32[:, :, :]), reads=[u32], writes=[u16])
                for k in range(16):
                    P.mm(pa[:, :], u16[:, k, :], h2p[:, k, :], k == 0, k == 15, reads=[u16, h2p], writes=[pa])
                P.op("act", lambda e, gel=gel, pa=pa: e.activation(out=gel[:, :], in_=pa[:, :], func=AF.Gelu_apprx_tanh), reads=[pa], writes=[gel])
                P.op("dve", lambda e, gat=gat, gel=gel, gt=gt, a=a: e.tensor_tensor(out=gat[:, a, :], in0=gel[:, :], in1=gt[:, a, :], op=ALU.mult),
                     reads=[gel, gt], writes=[gat])
                P.op("pool", lambda e, v32=v32, vb_=vb_, a=a: e.tensor_copy(out=vb_[:, a, 0:1024], in_=v32[:, 0:1024]), reads=[v32], writes=[vb_])
                P.op("act", lambda e, v32=v32, vb_=vb_, a=a: e.activation(out=vb_[:, a, 1024:2048], in_=v32[:, 1024:2048], func=AF.Copy),
                     reads=[v32], writes=[vb_])
            for bl in range(4):
                for dc in range(4):
                    pd = pD[di % 3]
                    di += 1
                    for a in range(4):
                        P.mm(pd[:, :], gat[:, a, bl * 128:(bl + 1) * 128], vb_[:, a, dc * 512:(dc + 1) * 512], a == 0, a == 3,
                             reads=[gat, vb_], writes=[pd])
                    if g == 0:
                        P.op("act", lambda e, pd=pd, bl=bl, dc=dc: e.activation(out=oacc[:, bl, dc * 512:(dc + 1) * 512], in_=pd[:, :], func=AF.Copy),
                             reads=[pd], writes=[oacc])
                    else:
                        P.op("dve", lambda e, pd=pd, bl=bl, dc=dc: e.tensor_tensor(out=oacc[:, bl, dc * 512:(dc + 1) * 512],
                                                                                 in0=oacc[:, bl, dc * 512:(dc + 1) * 512], in1=pd[:, :], op=ALU.add),
                             reads=[pd, oacc], writes=[oacc])
        P.end_phase()
        P.begin_phase()
        xt = [P.sb([128, D], F32, "sb_xf") for _ in range(2)]
        jk = P.sb([128, D], BF16, "sb_jk")
        s4 = [P.sb([128, 4], F32, "sb_s4") for _ in range(2)]
        if final:
            fg = P.sb([128, D], F32, "sb_fg")
            P.load("sp", fg, fg[:, :], fg_d[:, :])
        for bl in range(4):
            b = ps_ * 4 + bl
            x = xt[bl % 2]
            s = s4[bl % 2]
            P.load("sp" if bl % 2 == 0 else "act", x, x[:, :], xmid[b, :, :], dram=xmid)
            P.op("dve", lambda e, bl=bl: e.tensor_tensor(out=oacc[:, bl, :], in0=oacc[:, bl, :], in1=g2[:, :], op=ALU.mult),
                 reads=[oacc, g2], writes=[oacc])
            P.op("pool", lambda e, x=x, bl=bl: e.tensor_tensor(out=x[:, :], in0=x[:, :], in1=oacc[:, bl, :], op=ALU.add),
                 reads=[x, oacc], writes=[x])
            if final:
                P.op("act", lambda e, x=x, s=s: e.activation(out=jk[:, :], in_=x[:, :], func=AF.Square, accum_out=s[:, 0:1]),
                     reads=[x], writes=[jk, s])
                P.op("act", lambda e, s=s: e.activation(out=s[:, 1:2], in_=s[:, 0:1], func=AF.Sqrt, scale=1.0 / D, bias=EPSB[0][:, 0:1]),
                     reads=[s, EPSB[0]], writes=[s])
                P.op("dve", lambda e, s=s: e.reciprocal(out=s[:, 2:3], in_=s[:, 1:2]), reads=[s], writes=[s])
                P.op("dve", lambda e, x=x, s=s: e.scalar_tensor_tensor(out=x[:, :], in0=x[:, :], scalar=s[:, 2:3], in1=fg[:, :],
                                                                     op0=ALU.mult, op1=ALU.mult), reads=[x, s, fg], writes=[x])
            P.store("pool", x, xo[b, :, :], x[:, :], dram=xo)
        P.end_phase(final=(last and ps_ == 1))
        P.scope_end()
    P.scope_end()


def uT_tiles(u_l):
    return np.ascontiguousarray(u_l.reshape(128, 128, 16, 128).transpose(0, 3, 2, 1))


NFMR = NFM
RM = "(c r) t -> r c t"


def emit_A_f(P, xs_src, xdram, c_d, adaw, adab, g_d, wfm, wtm, ident, zfm_s, ztb_s, ztf_s):
    P.scope_begin()
    modbc = P.sbs([128, 4096], F32, "sbs_mod")
    A1 = P.sbs([128, D], F32, "sbs_A1")
    hT = P.sbs([128, 16, NTOK], BF16, "sbs_hT")
    emit_mod(P, c_d, adaw, adab, 4096, modbc)
    P.begin_phase()
    gb = P.sb([128, D], F32, "sb_g")
    P.load("sp", gb, gb[:, :], g_d[:, :])
    P.op("dve", lambda e: e.scalar_tensor_tensor(out=A1[:, :], in0=modbc[:, D:2 * D], scalar=1.0, in1=gb[:, :],
                                                 op0=ALU.add, op1=ALU.mult), reads=[modbc, gb], writes=[A1])
    P.end_phase()
    emit_norm_T(P, xs_src, A1, modbc_view(modbc, 0, D), ident, hT, xdram=xdram)
    emit_proj(P, hT, [dict(w=wfm, kind="fm", outs=[(zfm_s, 0, NFM, BF16)]),
                      dict(w=wtm, kind="tm", outs=[(ztb_s, 0, 2560, BF16), (ztf_s, 2560, 3200, F32)])])
    P.scope_end()


def emit_fox_bias_f(P, ztf_all, fb_d, tri_d, sel_d, bias):
    P.begin_phase()
    ff2 = P.sb([128, 64, 6], F32, "sb_ff2")
    fb = P.sb([128, 6], F32, "sb_fb")
    tri = P.sb([128, 128], F32, "sb_tri")
    ones = P.sb([128, 128], F32, "sb_ones")
    onec = P.sb([128, 1], F32, "sb_onec")
    sel = P.sb([128, 8], F32, "sb_sel")
    nlf = P.sb([128, 6, 64], F32, "sb_nlf")
    tot = P.sb([128, 6, 64], F32, "sb_tot")
    incl = P.sb([128, 6, 64], F32, "sb_incl")
    NF = P.sb([128, 6, 64], F32, "sb_NF")
    tmp = P.sb([128, 6, 8, 8], F32, "sb_tmp")
    nfe = P.sb([128, 6, 8], F32, "sb_nfe")
    pw = P.ps([128, 384], F32, "ps_w")
    pt_ = P.ps([128, 384], F32, "ps_t")
    src = ztf_all.t.rearrange("(c j p) w -> p j c w", c=8, j=8, p=128)
    ffv = ff2.t.rearrange("p (j c) h -> p j c h", c=8)
    for jh in range(8):
        P.load("sp" if jh % 2 == 0 else "act", ff2, ffv[:, jh, :, :], src[:, jh, :, 512:518], dram=ztf_all)
    P.load("act", fb, fb[:, :], fb_d[:, :])
    P.load("sp", tri, tri[:, :], tri_d[:, :])
    P.load("act", sel, sel[:, :], sel_d[:, :])
    P.op("dve", lambda e: e.memset(ones[:, :], 1.0), writes=[ones])
    P.op("dve", lambda e: e.memset(onec[:, :], 1.0), writes=[onec])
    P.op("dve", lambda e: e.tensor_tensor(out=nlf[:, :, :], in0=ff2.t.rearrange("p k h -> p h k"),
                                          in1=fb[:, :].unsqueeze(2).to_broadcast([128, 6, 64]), op=ALU.add), reads=[ff2, fb], writes=[nlf])
    nlf2 = nlf.t.rearrange("p h k -> p (h k)")
    P.op("act", lambda e: e.activation(out=nlf2, in_=nlf2, func=AF.Exp, scale=-1.0), reads=[nlf], writes=[nlf])
    P.op("act", lambda e: e.activation(out=nlf2, in_=nlf2, func=AF.Ln, bias=onec[:, 0:1]), reads=[nlf, onec], writes=[nlf])
    P.mm(pw[:, :], tri[:, :], nlf2, True, True, reads=[tri, nlf], writes=[pw])
    P.mm(pt_[:, :], ones[:, :], nlf2, True, True, reads=[ones, nlf], writes=[pt_])
    tot2 = tot.t.rearrange("p h k -> p (h k)")
    P.op("dve", lambda e: e.tensor_copy(out=tot2, in_=pt_[:, :]), reads=[pt_], writes=[tot])
    for h in range(6):
        P.op("dve", lambda e, h=h: e.tensor_tensor_scan(out=incl[:, h, :], data0=ones[:, 0:64], data1=tot[:, h, :], initial=0.0,
                                                        op0=ALU.mult, op1=ALU.add), reads=[ones, tot], writes=[incl])
    NF2 = NF.t.rearrange("p h k -> p (h k)")
    incl2 = incl.t.rearrange("p h k -> p (h k)")
    P.op("dve", lambda e: e.tensor_tensor(out=NF2, in0=pw[:, :], in1=incl2, op=ALU.add), reads=[pw, incl], writes=[NF])
    P.op("dve", lambda e: e.tensor_tensor(out=NF2, in0=NF2, in1=tot2, op=ALU.subtract), reads=[NF, tot], writes=[NF])
    P.op("dve", lambda e: e.tensor_tensor(out=tmp[:, :, :, :], in0=incl.t.rearrange("p h (j r) -> p h j r", r=8),
                                          in1=sel[:, :].unsqueeze(1).unsqueeze(1).to_broadcast([128, 6, 8, 8]), op=ALU.mult),
         reads=[incl, sel], writes=[tmp])
    P.op("dve", lambda e: e.tensor_reduce(out=nfe[:, :, :], in_=tmp[:, :, :, :], axis=AX.X, op=ALU.add), reads=[tmp], writes=[nfe])
    for h in range(6):
        for j in range(8):
            P.op("dve", lambda e, h=h, j=j: e.tensor_scalar(out=bias[:, h, j, :], in0=NF[:, h, :], scalar1=nfe[:, h, j:j + 1],
                                                            scalar2=0.0, op0=ALU.subtract, op1=ALU.min),
                 reads=[NF, nfe], writes=[bias])
    P.end_phase()


def emit_attn_f(P, zfm_all, krow0, ztb_all, vcol0, zfm_s, qrow0, nheads, out_stage, bias=None, maskT=None, cm=None):
    P.begin_phase()
    KT = [P.sb([128, S], BF16, "sb_KT") for _ in range(2)]
    Vg = [P.sb([128, NKB, 129], BF16, "sb_Vg") for _ in range(2)]
    QT = [P.sb([128, 1024], BF16, "sb_QT") for _ in range(2)]
    pO = [P.ps([128, 512], F32, "ps_O") for _ in range(2)]
    pS = []
    for _ in range(2):
        bank = P.ps([128, 4, 128], F32, "ps_S")
        for q in range(4):
            pS.append(Buf(bank.t[:, q, :], bank.name + "_q%d" % q))
    Pt = [P.sb([128, 128], BF16, "sb_Pt") for _ in range(6)]
    rc = [P.sb([128, 1], F32, "sb_rc") for _ in range(2)]
    zero_b = P.sb([128, 1], F32, "sb_zb")
    P.op("dve", lambda e: e.memset(zero_b[:, :], 0.0), writes=[zero_b])
    for vg in Vg:
        P.op("pool", lambda e, vg=vg: e.memset(vg[:, :, 128:129], 1.0), writes=[vg])
    kv = zfm_all.t.rearrange(RM, c=8)
    vv = ztb_all.t.rearrange("(m p) w -> p m w", p=128)
    si = pi = oi = 0
    for h in range(nheads):
        kt, vg, qt = KT[h % 2], Vg[h % 2], QT[h % 2]
        ktv = kt.t.rearrange("d (c t) -> d c t", c=8)
        for q4 in range(4):
            P.load("sp" if q4 % 2 == 0 else "act", kt, ktv[:, q4 * 2:(q4 + 1) * 2, :],
                   kv[krow0 + h * 128:krow0 + (h + 1) * 128, q4 * 2:(q4 + 1) * 2, :], dram=zfm_all)
        for q8 in range(8):
            P.load("sp" if q8 % 2 == 0 else "act", vg, vg[:, q8 * 8:(q8 + 1) * 8, 0:128],
                   vv[:, q8 * 8:(q8 + 1) * 8, vcol0 + h * 128:vcol0 + (h + 1) * 128], dram=ztb_all)
        P.load("sp", qt, qt[:, :], zfm_s.t[qrow0 + h * 128:qrow0 + (h + 1) * 128, :], dram=zfm_s)
        for j in range(8):
            blocks = [(cp * 8 + jp, 8 * jp + cp, (cp if jp == j else None), cp * (j + 1) + jp) for jp in range(j + 1) for cp in range(8)]
            nkb = len(blocks)
            po = pO[oi % 2]
            oi += 1
            for bi, (m, gb, r, q) in enumerate(blocks):
                ps = pS[si % 8]
                si += 1
                pt = Pt[pi % 6]
                pi += 1
                P.mm(ps[:, :], kt[:, m * 128:(m + 1) * 128], qt[:, j * 128:(j + 1) * 128], True, True, reads=[kt, qt], writes=[ps])
                if bias is not None:
                    P.op("act", lambda e, pt=pt, ps=ps, h=h, j=j, gb=gb: e.activation(
                        out=pt[:, :], in_=ps[:, :], func=AF.Exp, scale=SCALE, bias=bias[:, h, j, gb:gb + 1]),
                        reads=[ps, bias], writes=[pt])
                else:
                    P.op("act", lambda e, pt=pt, ps=ps: e.activation(
                        out=pt[:, :], in_=ps[:, :], func=AF.Exp, scale=SCALE, bias=zero_b[:, 0:1]),
                        reads=[ps, zero_b], writes=[pt])
                if maskT is not None:
                    mt = maskT[j]
                    P.op("dve", lambda e, pt=pt, mt=mt, q=q: e.tensor_tensor(out=pt[:, :], in0=pt[:, :], in1=mt[:, q, :], op=ALU.mult),
                         reads=[pt, mt], writes=[pt])
                elif r is not None:
                    P.op("dve", lambda e, pt=pt, r=r: e.tensor_tensor(out=pt[:, :], in0=pt[:, :], in1=cm[:, r, :], op=ALU.mult),
                         reads=[pt, cm], writes=[pt])
                P.mm(po[:, 0:129], pt[:, :], vg[:, m, :], bi == 0, bi == nkb - 1, reads=[pt, vg], writes=[po])
            r_ = rc[oi % 2]
            P.op("dve", lambda e, r_=r_, po=po: e.reciprocal(out=r_[:, 0:1], in_=po[:, 128:129]), reads=[po], writes=[r_])
            P.op("dve", lambda e, r_=r_, po=po, j=j, h=h: e.tensor_scalar(
                out=out_stage[:, j, h * 128:(h + 1) * 128], in0=po[:, 0:128], scalar1=r_[:, 0:1], scalar2=None,
                op0=ALU.mult), reads=[po, r_], writes=[out_stage])
    P.end_phase()


def emit_dsa_select_f(P, zfm_all, zfm_s, ztf_s, am_d, ident, maskT):
    P.begin_phase()
    score = P.sb([128, S], F32, "sb_score")
    junk = P.sb([128, S], BF16, "sb_junk")
    ikT = P.sb([64, S], BF16, "sb_ikT")
    iq = [P.sb([64, 16, 128], BF16, "sb_iq") for _ in range(2)]
    rr = [P.sb([128, 512], F32, "sb_rr") for _ in range(3)]
    am = P.sb([128, 8, 128], F32, "sb_am")
    iw = P.sb([128, 8, 16], F32, "sb_iw")
    wsc = P.sb([128, 8, 16], F32, "sb_wsc")
    pI = [P.ps([128, 512], F32, "ps_I") for _ in range(4)]
    pT = [P.ps([128, 8, 128], BF16, "ps_mT") for _ in range(2)]
    mch = [P.sb([128, 1024], BF16, "sb_mch") for _ in range(2)]
    sms = [P.sb([128, 8], F32, "sb_sm") for _ in range(2)]
    kv = zfm_all.t.rearrange(RM, c=8)
    ikv = ikT.t.rearrange("d (c t) -> d c t", c=8)
    for q4 in range(4):
        P.load("sp" if q4 % 2 == 0 else "act", ikT, ikv[:, q4 * 2:(q4 + 1) * 2, :], kv[R_IK:R_IK + 64, q4 * 2:(q4 + 1) * 2, :], dram=zfm_all)
    P.load("sp", am, am[:, :, :], am_d[:, :, :])
    P.load("act", iw, iw[:, :, :], ztf_s.t.rearrange("(j p) w -> p j w", p=128)[:, :, 518:534], dram=ztf_s)
    P.op("dve", lambda e: e.tensor_scalar(out=wsc[:, :, :], in0=iw[:, :, :], scalar1=(64 ** -0.5) * (16 ** -0.5), scalar2=None,
                                          op0=ALU.mult), reads=[iw], writes=[wsc])
    iqsrc = zfm_s.t[R_IQ:R_IQ + 1024, :].rearrange("(h d) t -> d h t", d=64)
    ii = ti = 0
    for j in range(8):
        W = (j + 1) * 128
        L = 8 * W
        iqj = iq[j % 2]
        P.load("sp", iqj, iqj[:, :, :], iqsrc[:, :, j * 128:(j + 1) * 128], dram=zfm_s)
        sm = sms[j % 2]
        for cp in range(8):
            for w0 in range(0, W, 512):
                ww = min(512, W - w0)
                sc = score.t[:, cp * W + w0:cp * W + w0 + ww]
                kcol = cp * 1024 + w0
                for h in range(16):
                    ps = pI[ii % 4]
                    r = rr[ii % 3]
                    ii += 1
                    P.mm(ps[:, 0:ww], iqj[:, h, :], ikT[:, kcol:kcol + ww], True, True, reads=[iqj, ikT], writes=[ps])
                    P.op("act", lambda e, r=r, ps=ps, ww=ww: e.activation(out=r[:, 0:ww], in_=ps[:, 0:ww], func=AF.Relu), reads=[ps], writes=[r])
                    if h == 0:
                        P.op("dve", lambda e, sc=sc, r=r, j=j, ww=ww: e.tensor_scalar(out=sc, in0=r[:, 0:ww], scalar1=wsc[:, j, 0:1], scalar2=None,
                                                                                    op0=ALU.mult), reads=[r, wsc], writes=[score])
                    else:
                        P.op("dve", lambda e, sc=sc, r=r, j=j, h=h, ww=ww: e.scalar_tensor_tensor(
                            out=sc, in0=r[:, 0:ww], scalar=wsc[:, j, h:h + 1], in1=sc, op0=ALU.mult, op1=ALU.add),
                            reads=[r, wsc, score], writes=[score])
        sL = score.t[:, 0:L]
        P.op("dve", lambda e, sm=sm, sL=sL: e.tensor_reduce(out=sm[:, 0:1], in_=sL, axis=AX.X, op=ALU.max, apply_absolute_value=True),
             reads=[score], writes=[sm])
        P.op("dve", lambda e, sm=sm: e.tensor_scalar(out=sm[:, 0:1], in0=sm[:, 0:1], scalar1=1.001, scalar2=1e-3, op0=ALU.mult, op1=ALU.add),
             reads=[sm], writes=[sm])
        P.op("dve", lambda e, sm=sm: e.tensor_scalar(out=sm[:, 1:2], in0=sm[:, 0:1], scalar1=-1.0, scalar2=None, op0=ALU.mult),
             reads=[sm], writes=[sm])
        sB = score.t[:, 0:L].rearrange("p (c w) -> p c w", c=8)[:, :, j * 128:(j + 1) * 128]
        P.op("dve", lambda e, sB=sB: e.tensor_tensor(out=sB, in0=sB, in1=am[:, :, :], op=ALU.add), reads=[score, am], writes=[score])
        for k in range(1, NBIS + 1):
            f = 2.0 ** (1 - k)
            P.op("dve", lambda e, sm=sm, f=f: e.tensor_scalar(out=sm[:, 2:3], in0=sm[:, 0:1], scalar1=f, scalar2=sm[:, 1:2],
                                                            op0=ALU.mult, op1=ALU.add), reads=[sm], writes=[sm])
            P.op("dve", lambda e, sm=sm, sL=sL, L=L: e.tensor_scalar(out=junk[:, 0:L], in0=sL, scalar1=sm[:, 2:3], scalar2=0.0,
                                                                   op0=ALU.is_ge, op1=ALU.add, accum_out=sm[:, 3:4]),
                 reads=[score, sm], writes=[junk, sm])
            P.op("dve", lambda e, sm=sm, f=f: e.tensor_scalar(out=sm[:, 4:5], in0=sm[:, 3:4], scalar1=TOPK - 0.5, scalar2=f,
                                                            op0=ALU.is_ge, op1=ALU.mult), reads=[sm], writes=[sm])
            P.op("dve", lambda e, sm=sm: e.scalar_tensor_tensor(out=sm[:, 1:2], in0=sm[:, 4:5], scalar=sm[:, 0:1], in1=sm[:, 1:2],
                                                              op0=ALU.mult, op1=ALU.add), reads=[sm], writes=[sm])
        for g in range(L // 1024):
            mc = mch[ti % 2]
            pt = pT[ti % 2]
            ti += 1
            P.op("dve", lambda e, mc=mc, g=g, sm=sm: e.tensor_scalar(out=mc[:, :], in0=score[:, g * 1024:(g + 1) * 1024],
                                                                   scalar1=sm[:, 1:2], scalar2=None, op0=ALU.is_ge),
                 reads=[score, sm], writes=[mc])
            for q in range(8):
                P.op("pe", lambda e, pt=pt, mc=mc, q=q: e.transpose(out=pt[:, q, :], in_=mc[:, q * 128:(q + 1) * 128], identity=ident[:, :]),
                     reads=[mc, ident], writes=[pt])
            mt = maskT[j]
            P.op("act", lambda e, mt=mt, pt=pt, g=g: e.activation(out=mt[:, g * 8:(g + 1) * 8, :], in_=pt[:, :, :], func=AF.Copy),
                 reads=[pt], writes=[mt])
    P.end_phase()


def emit_gla_f(P, zfm_all, ztb_all, ztf_all, sel4_d, wa2_d, nbacol_d, barow_d, gn_d, tri2_d, suf2_d, rmask_d, gla_s):
    P.scope_begin()
    keep = P.sbs
    qtT = keep([128, S], BF16, "sbk_qtT")
    ktT = keep([128, S], BF16, "sbk_ktT")
    dn = keep([128, 128], F32, "sbk_dn")
    Sb = keep([128, 128, 128], BF16, "sbk_Sb")
    vtm = keep([128, 64, 128], BF16, "sbk_v")
    gs = keep([128, 64, 128], BF16, "sbk_gs")
    wa2b = keep([16, 128], BF16, "sbk_wa2b")
    gaT = keep([16, S], BF16, "sbk_gaT")
    tri2 = keep([128, 128], F32, "sbk_tri2")
    onec = keep([128, 1], F32, "sbk_onec")
    epsc = keep([128, 1], F32, "sbk_epsc")
    sel4 = keep([128, 4], F32, "sbk_sel4")
    kv = zfm_all.t.rearrange(RM, c=8)

    P.begin_phase()
    wa2f = P.sb([16, 128], F32, "sb_wa2f")
    nbac = P.sb([128, 1], F32, "sb_nbac")
    rmask = P.sb([128, 512], F32, "sb_rmask")
    gq4 = [P.sb([128, 4, 512], BF16, "sb_gq4") for _ in range(2)]
    gk4 = [P.sb([128, 4, 512], BF16, "sb_gk4") for _ in range(2)]
    gq = [P.sb([128, 512], F32, "sb_gq") for _ in range(2)]
    gk = [P.sb([128, 512], F32, "sb_gk") for _ in range(2)]
    e1 = [P.sb([128, 512], F32, "sb_e1") for _ in range(2)]
    cs = [P.sb([128, 512], F32, "sb_cs") for _ in range(2)]
    eg = [P.sb([128, 512], F32, "sb_eg") for _ in range(2)]
    en = [P.sb([128, 512], F32, "sb_en") for _ in range(2)]
    pg = [P.ps([128, 512], F32, "ps_g") for _ in range(2)]
    P.load("sp", wa2f, wa2f[:, :], wa2_d[:, :])
    P.load("act", nbac, nbac[:, :], nbacol_d[:, :])
    P.load("sp", rmask, rmask[:, :], rmask_d[:, :])
    P.load("act", tri2, tri2[:, :], tri2_d[:, :])
    P.load("sp", sel4, sel4[:, :], sel4_d[:, :])
    gav = gaT.t.rearrange("r (j c p) -> r j c p", j=8, c=8)
    gas = kv[R_GA:R_GA + 16, :, :].rearrange("r c (j p) -> r j c p", p=128)
    for jh in range(8):
        P.load("sp" if jh % 2 == 0 else "act", gaT, gav[:, jh, :, :], gas[:, jh, :, :], dram=zfm_all)
    P.op("dve", lambda e: e.tensor_copy(out=wa2b[:, :], in_=wa2f[:, :]), reads=[wa2f], writes=[wa2b])
    P.op("dve", lambda e: e.memset(onec[:, :], 1.0), writes=[onec])
    P.op("dve", lambda e: e.memset(epsc[:, :], 1e-6), writes=[epsc])

    def sel_acc(eng, out_ap, src4, nh_view, outbuf, srcbuf):
        for h in range(4):
            if h == 0:
                P.op(eng, lambda e, h=h: e.tensor_scalar(out=out_ap, in0=nh_view(h), scalar1=sel4[:, 0:1], scalar2=None, op0=ALU.mult),
                     reads=[srcbuf, sel4], writes=[outbuf])
            else:
                P.op("dve", lambda e, h=h: e.scalar_tensor_tensor(out=out_ap, in0=nh_view(h), scalar=sel4[:, h:h + 1], in1=out_ap,
                                                                  op0=ALU.mult, op1=ALU.add), reads=[srcbuf, sel4, outbuf], writes=[outbuf])

    for tc in range(16):
        sl = slice(tc * 512, (tc + 1) * 512)
        j_ = tc // 2
        c0 = (tc % 2) * 4
        a4, b4 = gq4[tc % 2], gk4[tc % 2]
        a, b_ = gq[tc % 2], gk[tc % 2]
        for (dst4, row0, q) in ((a4, R_GQ, "sp"), (b4, R_GK, "act")):
            for hh in range(4):
                srcv = kv[row0 + hh * 128:row0 + (hh + 1) * 128, c0:c0 + 4, j_ * 128:(j_ + 1) * 128]
                P.load(q, dst4, dst4.t[:, hh, :].rearrange("d (c p) -> d c p", p=128), srcv, dram=zfm_all)
        sel_acc("dve", a[:, :], a4, lambda h, a4=a4: a4[:, h, :], a, a4)
        sel_acc("dve", b_[:, :], b4, lambda h, b4=b4: b4[:, h, :], b_, b4)
        ps = pg[tc % 2]
        x1, c1, g1, n1 = e1[tc % 2], cs[tc % 2], eg[tc % 2], en[tc % 2]
        P.mm(ps[:, :], wa2b[:, :], gaT[:, sl], True, True, reads=[wa2b, gaT], writes=[ps])
        P.op("act", lambda e, x1=x1, ps=ps: e.activation(out=x1[:, :], in_=ps[:, :], func=AF.Exp, scale=-1.0, bias=nbac[:, 0:1]),
             reads=[ps, nbac], writes=[x1])
        P.op("act", lambda e, x1=x1: e.activation(out=x1[:, :], in_=x1[:, :], func=AF.Ln, bias=onec[:, 0:1]), reads=[x1, onec], writes=[x1])
        P.op("dve", lambda e, x1=x1, c1=c1: e.tensor_tensor_scan(out=c1[:, :], data0=rmask[:, :], data1=x1[:, :], initial=0.0,
                                                               op0=ALU.mult, op1=ALU.add), reads=[rmask, x1], writes=[c1])
        P.op("act", lambda e, c1=c1, g1=g1: e.activation(out=g1[:, :], in_=c1[:, :], func=AF.Exp, scale=-1.0 / 16, bias=ZB[0][:, 0:1]),
             reads=[c1, ZB[0]], writes=[g1])
        P.op("act", lambda e, c1=c1, n1=n1: e.activation(out=n1[:, :], in_=c1[:, :], func=AF.Exp, scale=1.0 / 16, bias=ZB[0][:, 0:1]),
             reads=[c1, ZB[0]], writes=[n1])
        P.op("dve", lambda e, a=a, g1=g1, sl=sl: e.scalar_tensor_tensor(out=qtT[:, sl], in0=a[:, :], scalar=SCALE, in1=g1[:, :],
                                                                      op0=ALU.mult, op1=ALU.mult), reads=[a, g1], writes=[qtT])
        P.op("pool", lambda e, b_=b_, n1=n1, sl=sl: e.tensor_tensor(out=ktT[:, sl], in0=b_[:, :], in1=n1[:, :], op=ALU.mult),
             reads=[b_, n1], writes=[ktT])
        P.op("dve", lambda e, g1=g1, tc=tc: e.tensor_copy(out=dn[:, tc * 8:(tc + 1) * 8],
                                                        in_=g1.t.rearrange("p (n c) -> p n c", c=64)[:, :, 63]),
             reads=[g1], writes=[dn])
    P.end_phase()

    P.begin_phase()
    barow = P.sb([128, 128], F32, "sb_barow")
    gn = P.sb([128, 128], F32, "sb_gn")
    suf2 = P.sb([128, 128], F32, "sb_suf2")
    ktm = P.sb([128, 64, 128], BF16, "sb_ktm")
    Sst = P.sb([128, 128], F32, "sb_Sst")
    xg = [P.sb([128, 128], F32, "sb_xg") for _ in range(2)]
    fk = [P.sb([128, 128], F32, "sb_fk") for _ in range(2)]
    kh = [P.sb([128, 128], BF16, "sb_kh") for _ in range(2)]
    t4 = [P.sb([128, 4, 1024], BF16, "sb_t4") for _ in range(2)]
    g4 = [P.sb([128, 4, 512], F32, "sb_g4") for _ in range(2)]
    grt = [P.sb([128, 4, 128], F32, "sb_grt") for _ in range(2)]
    pl = [P.ps([128, 128], F32, "ps_l") for _ in range(2)]
    pf = [P.ps([128, 128], F32, "ps_f") for _ in range(2)]
    pU = [P.ps([128, 128], F32, "ps_U") for _ in range(4)]
    P.load("sp", barow, barow[:, :], barow_d[:, :])
    P.load("act", gn, gn[:, :], gn_d[:, :])
    P.load("sp", suf2, suf2[:, :], suf2_d[:, :])
    P.op("dve", lambda e: e.memset(Sst[:, :], 0.0), writes=[Sst])
    tbv = ztb_all.t.rearrange("(c j p) w -> p j c w", c=8, j=8, p=128)
    tfv = ztf_all.t.rearrange("(c j p) w -> p j c w", c=8, j=8, p=128)
    for jc in range(16):
        j_, ch_ = jc // 2, jc % 2
        tt, gg, gt = t4[jc % 2], g4[jc % 2], grt[jc % 2]
        P.load("sp", tt, tt[:, :, :], tbv[:, j_, ch_ * 4:(ch_ + 1) * 4, C_GK:C_GK + 1024], dram=ztb_all)
        P.load("act", gg, gg[:, :, :], tfv[:, j_, ch_ * 4:(ch_ + 1) * 4, 0:512], dram=ztf_all)
        bs = slice(j_ * 8 + ch_ * 4, j_ * 8 + ch_ * 4 + 4)
        sel_acc("dve", ktm[:, bs, :], tt, lambda h, tt=tt: tt[:, :, h * 128:(h + 1) * 128], ktm, tt)
        sel_acc("dve", vtm[:, bs, :], tt, lambda h, tt=tt: tt[:, :, 512 + h * 128:512 + (h + 1) * 128], vtm, tt)
        sel_acc("dve", gt[:, :, :], gg, lambda h, gg=gg: gg[:, :, h * 128:(h + 1) * 128], gt, gg)
        P.op("act", lambda e, gt=gt: e.activation(out=gt[:, :, :], in_=gt[:, :, :], func=AF.Silu), reads=[gt], writes=[gt])
        P.op("pool", lambda e, gt=gt, bs=bs: e.tensor_tensor(out=gs[:, bs, :], in0=gt[:, :, :],
                                                           in1=gn[:, :].unsqueeze(1).to_broadcast([128, 4, 128]), op=ALU.mult),
             reads=[gt, gn], writes=[gs])
    for blk in range(64):
        x, f, k2 = xg[blk % 2], fk[blk % 2], kh[blk % 2]
        p1, p2 = pl[blk % 2], pf[blk % 2]
        P.mm(p1[:, :], gaT[:, blk * 128:(blk + 1) * 128], wa2b[:, :], True, True, reads=[gaT, wa2b], writes=[p1])
        P.op("dve", lambda e, x=x, p1=p1: e.tensor_tensor(out=x[:, :], in0=p1[:, :], in1=barow[:, :], op=ALU.add),
             reads=[p1, barow], writes=[x])
        P.op("act", lambda e, x=x: e.activation(out=x[:, :], in_=x[:, :], func=AF.Exp, scale=-1.0, bias=ZB[0][:, 0:1]),
             reads=[x, ZB[0]], writes=[x])
        P.op("act", lambda e, x=x: e.activation(out=x[:, :], in_=x[:, :], func=AF.Ln, bias=onec[:, 0:1]), reads=[x, onec], writes=[x])
        P.mm(p2[:, :], suf2[:, :], x[:, :], True, True, reads=[suf2, x], writes=[p2])
        P.op("act", lambda e, f=f, p2=p2: e.activation(out=f[:, :], in_=p2[:, :], func=AF.Exp, scale=-1.0 / 16, bias=ZB[0][:, 0:1]),
             reads=[p2, ZB[0]], writes=[f])
        P.op("pool", lambda e, k2=k2, f=f, blk=blk: e.tensor_tensor(out=k2[:, :], in0=ktm[:, blk, :], in1=f[:, :], op=ALU.mult),
             reads=[ktm, f], writes=[k2])
        for hf in range(2):
            n = 2 * blk + hf
            pu = pU[n % 4]
            P.mm(pu[:, :], k2[hf * 64:(hf + 1) * 64, :], vtm[hf * 64:(hf + 1) * 64, blk, :], True, True, reads=[k2, vtm], writes=[pu])
            P.op("dve", lambda e, pu=pu, n=n: e.scalar_tensor_tensor(out=Sst[:, :], in0=Sst[:, :], scalar=dn[:, n:n + 1], in1=pu[:, :],
                                                                   op0=ALU.mult, op1=ALU.add), reads=[Sst, dn, pu], writes=[Sst])
            P.op("act", lambda e, n=n: e.activation(out=Sb[:, n, :], in_=Sst[:, :], func=AF.Copy), reads=[Sst], writes=[Sb])
    P.end_phase()

    P.begin_phase()
    ost = P.sb([128, 64, 128], BF16, "sb_gost")
    At = [P.sb([128, 128], BF16, "sb_At") for _ in range(2)]
    st = [P.sb([128, 4], F32, "sb_gst") for _ in range(2)]
    jk = P.sb([128, 128], F32, "sb_gjk")
    pA = [P.ps([128, 128], F32, "ps_A") for _ in range(2)]
    pO = [P.ps([128, 128], F32, "ps_GO") for _ in range(2)]
    for blk in range(64):
        sl = slice(blk * 128, (blk + 1) * 128)
        pa, po, at, s = pA[blk % 2], pO[blk % 2], At[blk % 2], st[blk % 2]
        P.mm(pa[:, :], ktT[:, sl], qtT[:, sl], True, True, reads=[ktT, qtT], writes=[pa])
        P.op("dve", lambda e, at=at, pa=pa: e.tensor_tensor(out=at[:, :], in0=pa[:, :], in1=tri2[:, :], op=ALU.mult),
             reads=[pa, tri2], writes=[at])
        P.mm(po[:, :], at[:, :], vtm[:, blk, :], True, False, reads=[at, vtm], writes=[po])
        if blk > 0:
            P.mm(po[0:64, :], qtT[:, blk * 128:blk * 128 + 64], Sb[:, 2 * blk - 1, :], False, False, reads=[qtT, Sb], writes=[po])
        P.mm(po[64:128, :], qtT[:, blk * 128 + 64:blk * 128 + 128], Sb[:, 2 * blk, :], False, True, reads=[qtT, Sb], writes=[po])
        P.op("act", lambda e, po=po, s=s: e.activation(out=jk[:, :], in_=po[:, :], func=AF.Square, accum_out=s[:, 0:1]),
             reads=[po], writes=[jk, s])
        P.op("act", lambda e, s=s: e.activation(out=s[:, 1:2], in_=s[:, 0:1], func=AF.Sqrt, scale=1.0 / 128, bias=epsc[:, 0:1]),
             reads=[s, epsc], writes=[s])
        P.op("dve", lambda e, s=s: e.reciprocal(out=s[:, 2:3], in_=s[:, 1:2]), reads=[s], writes=[s])
        P.op("dve", lambda e, po=po, s=s, blk=blk: e.scalar_tensor_tensor(out=ost[:, blk, :], in0=po[:, :], scalar=s[:, 2:3],
                                                                        in1=gs[:, blk, :], op0=ALU.mult, op1=ALU.mult),
             reads=[po, s, gs], writes=[ost])
    P.store("sp", ost, gla_s.t.rearrange("(b p) e -> p b e", p=128), ost[:, :, :], dram=gla_s)
    P.end_phase()
    P.scope_end()


def emit_mixT(P, foxo, dsao, gla_all, sel_d, ident, mixT):
    P.begin_phase()
    sel = P.sb([128, 8], F32, "sb_sel8")
    gch = [P.sb([128, 4, 8, 128], BF16, "sb_gch") for _ in range(2)]
    mg = [P.sb([128, 4, 128], BF16, "sb_mg") for _ in range(2)]
    fo = [P.sb([128, 768], BF16, "sb_fo") for _ in range(2)]
    do = [P.sb([128, 768], BF16, "sb_do") for _ in range(2)]
    pT = [P.ps([128, 8, 128], BF16, "ps_xT") for _ in range(2)]
    P.load("sp", sel, sel[:, :], sel_d[:, :])
    gv_ = gla_all.t.rearrange("(r j c p) e -> p j r c e", r=8, j=8, c=8, p=128)
    for j in range(8):
        ch = gch[j % 2]
        m = mg[j % 2]
        ostF, ostD = fo[j % 2], do[j % 2]
        P.load("sp", ostF, ostF[:, :], foxo[j, :, :], dram=foxo)
        P.load("act", ostD, ostD[:, :], dsao[j, :, :], dram=dsao)
        for hh in range(4):
            P.load("sp" if hh % 2 == 0 else "act", ch, ch[:, hh, :, :], gv_[:, j, hh, :, :], dram=gla_all)
        for cp in range(8):
            if cp == 0:
                P.op("dve", lambda e, m=m, ch=ch: e.tensor_scalar(out=m[:, :, :], in0=ch[:, :, 0, :], scalar1=sel[:, 0:1], scalar2=None, op0=ALU.mult),
                     reads=[ch, sel], writes=[m])
            else:
                P.op("dve", lambda e, m=m, ch=ch, cp=cp: e.scalar_tensor_tensor(out=m[:, :, :], in0=ch[:, :, cp, :], scalar=sel[:, cp:cp + 1],
                                                                              in1=m[:, :, :], op0=ALU.mult, op1=ALU.add),
                     reads=[ch, sel, m], writes=[m])
        for half in range(2):
            pt = pT[half]
            for kk in range(8):
                k = half * 8 + kk
                if k < 6:
                    src, sb_ = ostF[:, k * 128:(k + 1) * 128], ostF
                elif k < 10:
                    src, sb_ = m[:, k - 6, :], m
                else:
                    src, sb_ = ostD[:, (k - 10) * 128:(k - 9) * 128], ostD
                P.op("pe", lambda e, pt=pt, kk=kk, src=src: e.transpose(out=pt[:, kk, :], in_=src, identity=ident[:, :]),
                     reads=[sb_, ident], writes=[pt])
            P.op("act", lambda e, pt=pt, half=half, j=j: e.activation(
                out=mixT[:, half * 8:(half + 1) * 8, j * 128:(j + 1) * 128], in_=pt[:, :, :], func=AF.Copy), reads=[pt], writes=[mixT])
    P.end_phase()


def build_fused():
    nc = bass.Bass("TRN2", target_bir_lowering=False)
    P = Prog(nc)
    IN = {}

    def inp(name, shp, dt):
        IN[name] = P.dram(name, shp, dt, "ExternalInput")
        return IN[name]
    xs = inp("xs", [NB, 128, D], F32)
    c_d = inp("c_pk", [128, 16], F32)
    ident_d = inp("ident", [128, 128], BF16)
    tri_d = inp("tri", [128, 128], F32)
    sel_d = inp("sel", [128, 8], F32)
    sel4_d = inp("sel4", [128, 4], F32)
    cm_d = inp("cm", [128, 8, 128], BF16)
    am_d = inp("am", [128, 8, 128], F32)
    tri2_d = inp("tri2", [128, 128], F32)
    suf2_d = inp("suf2", [128, 128], F32)
    rmask_d = inp("rmask", [128, 512], F32)
    fg_d = inp("fg_bc", [128, D], F32)
    L = []
    for l in range(2):
        s_ = "_%d" % l
        L.append(dict(
            adawA=inp("adawA" + s_, [D, 4096], F32), adabA=inp("adabA" + s_, [128, 4096], F32), g1=inp("g1" + s_, [128, D], F32),
            wfm=inp("wfm" + s_, [D, NFM], F32), wtm=inp("wtm" + s_, [D, NTM], F32), fbb=inp("fbb" + s_, [128, 6], F32),
            wa2=inp("wa2" + s_, [16, 128], F32), nbacol=inp("nbacol" + s_, [128, 1], F32), barow=inp("barow" + s_, [128, 128], F32),
            gnb=inp("gnb" + s_, [128, 128], F32), adawC=inp("adawC" + s_, [D, 8192], F32), adabC=inp("adabC" + s_, [128, 8192], F32),
            g2n=inp("g2n" + s_, [128, D], F32), wo=inp("wo" + s_, [D, D], F32), wq=inp("wq" + s_, [D, D], F32),
            kT=inp("kT" + s_, [16, 128, 128], F32), uTt=inp("uTt" + s_, [128, 128, 16, 128], F32), v=inp("v" + s_, [NE, D], F32),
            zfm_s=P.scratch("zfm_s" + s_, [NFM, NTOK], BF16), zfm_all=P.scratch("zfm_all" + s_, [8 * NFM, NTOK], BF16),
            ztb_s=P.scratch("ztb_s" + s_, [NTOK, 2560], BF16), ztb_all=P.scratch("ztb_all" + s_, [8 * NTOK, 2560], BF16),
            ztf_s=P.scratch("ztf_s" + s_, [NTOK, 640], F32), ztf_all=P.scratch("ztf_all" + s_, [8 * NTOK, 640], F32),
            gla_s=P.scratch("gla_s" + s_, [S, 128], BF16), gla_all=P.scratch("gla_all" + s_, [8 * S, 128], BF16),
            xmid=P.scratch("xmid" + s_, [NB, 128, D], F32), h2T=P.scratch("h2T" + s_, [16, 128, NTOK], BF16),
            s12=P.scratch("s12" + s_, [NB, 128, 16, 128], F32), g2=P.scratch("g2" + s_, [128, D], F32),
            xcur=P.scratch("xcur" + s_, [NB, 128, D], F32), foxo=P.scratch("foxo" + s_, [NB, 128, 768], BF16),
            dsao=P.scratch("dsao" + s_, [NB, 128, 768], BF16)))
    xo = P.dram("xo", [NB, 128, D], F32, "ExternalOutput")
    ident = emit_consts(P, ident_d)
    emit_zero(P)
    for l in range(2):
        W = L[l]
        xsrc = xs if l == 0 else L[0]["xcur"]
        xdr = None if l == 0 else L[0]["xcur"]
        emit_A_f(P, xsrc, xdr, c_d, W["adawA"], W["adabA"], W["g1"], W["wfm"], W["wtm"], ident, W["zfm_s"], W["ztb_s"], W["ztf_s"])
        P.begin_phase()
        P.allgather(W["zfm_s"], W["zfm_all"])
        P.allgather(W["ztb_s"], W["ztb_all"])
        P.allgather(W["ztf_s"], W["ztf_all"])
        P.end_phase()
        P.scope_begin()
        bias = P.sbs([128, 6, 8, 64], F32, "sbs_bias")
        cm = P.sbs([128, 8, 128], BF16, "sbs_cm")
        ostF = P.sbs([128, 8, 768], BF16, "sbs_ostF")
        P.begin_phase()
        P.load("sp", cm, cm[:, :, :], cm_d[:, :, :])
        P.end_phase()
        emit_fox_bias_f(P, W["ztf_all"], W["fbb"], tri_d, sel_d, bias)
        emit_attn_f(P, W["zfm_all"], R_FK, W["ztb_all"], C_FV, W["zfm_s"], R_FQ, 6, ostF, bias=bias, cm=cm)
        P.begin_phase()
        P.store("sp", ostF, W["foxo"].t.rearrange("j p w -> p j w"), ostF[:, :, :], dram=W["foxo"])
        P.end_phase()
        P.scope_end()
        P.scope_begin()
        ostD = P.sbs([128, 8, 768], BF16, "sbs_ostD")
        maskT = [P.sbs([128, 8 * j + 8, 128], BF16, "sbs_mT") for j in range(8)]
        emit_dsa_select_f(P, W["zfm_all"], W["zfm_s"], W["ztf_s"], am_d, ident, maskT)
        emit_attn_f(P, W["zfm_all"], R_DK, W["ztb_all"], C_DV, W["zfm_s"], R_DQ, 6, ostD, maskT=maskT)
        P.begin_phase()
        P.store("sp", ostD, W["dsao"].t.rearrange("j p w -> p j w"), ostD[:, :, :], dram=W["dsao"])
        P.end_phase()
        P.scope_end()
        emit_gla_f(P, W["zfm_all"], W["ztb_all"], W["ztf_all"], sel4_d, W["wa2"], W["nbacol"], W["barow"], W["gnb"], tri2_d, suf2_d,
                   rmask_d, W["gla_s"])
        P.begin_phase()
        P.allgather(W["gla_s"], W["gla_all"])
        P.end_phase()
        P.scope_begin()
        mixT = P.sbs([128, 16, NTOK], BF16, "sbs_mixT")
        emit_mixT(P, W["foxo"], W["dsao"], W["gla_all"], sel_d, ident, mixT)
        emit_C1(P, ident, xsrc, xdr, None, mixT, c_d, W["adawC"], W["adabC"], W["g2n"], W["wo"], W["wq"], W["kT"],
                W["xmid"], W["h2T"], W["s12"], W["g2"])
        P.scope_end()
        final = (l == 1)
        emit_C2(P, ident, W["h2T"], W["s12"], W["xmid"], W["g2"], W["uTt"], W["v"], fg_d if final else None,
                xo if final else W["xcur"], final, final)
    P.finish()
    return nc


def fused_inputs(x, c, ada_w, ada_b, norm1_g, norm2_g, final_g, w_in, fox_fbias, gla_wa2, gla_ba, gla_norm_g, w_out,
                 peer_wq, peer_k1, peer_k2, peer_u, peer_v):
    f32 = np.float32
    x2 = np.asarray(x, f32).reshape(S, D)
    c1 = np.asarray(c, f32).reshape(D)
    tri2, suf2, rmask = gla_consts()
    shared = dict(c_pk=np.ascontiguousarray(c1.reshape(16, 128).T), ident=np.eye(128, dtype=NPBF), tri=TRI, tri2=tri2, suf2=suf2,
                  rmask=rmask, fg_bc=bc128(np.asarray(final_g, f32)))
    per_head = [dict() for _ in range(4)]
    for l in range(2):
        s_ = "_%d" % l
        w_in_l = np.asarray(w_in[l], f32)
        wfm = np.zeros((D, NFM), f32)
        wfm[:, :len(FM_COLS)] = w_in_l[:, FM_COLS]
        wtm = np.zeros((D, NTM), f32)
        wtm[:, :len(TM_COLS)] = w_in_l[:, TM_COLS]
        kT = np.zeros((16, 128, 128), f32)
        for h in range(8):
            kT[2 * h] = np.asarray(peer_k1[l][h], f32).T
            kT[2 * h + 1] = np.asarray(peer_k2[l][h], f32).T
        shared.update({
            "adawA" + s_: np.ascontiguousarray(ada_w[l][:, 0:4096]), "adabA" + s_: bc128(np.asarray(ada_b[l][0:4096], f32)),
            "g1" + s_: bc128(np.asarray(norm1_g[l], f32)), "wfm" + s_: wfm, "wtm" + s_: wtm, "fbb" + s_: bc128(np.asarray(fox_fbias[l], f32)),
            "gnb" + s_: bc128(np.asarray(gla_norm_g[l], f32)), "adawC" + s_: np.ascontiguousarray(ada_w[l][:, 4096:12288]),
            "adabC" + s_: bc128(np.asarray(ada_b[l][4096:12288], f32)), "g2n" + s_: bc128(np.asarray(norm2_g[l], f32)),
            "wo" + s_: np.ascontiguousarray(w_out[l], dtype=f32), "wq" + s_: np.ascontiguousarray(peer_wq[l], dtype=f32), "kT" + s_: kT,
            "uTt" + s_: uT_tiles(np.asarray(peer_u[l], f32)), "v" + s_: np.ascontiguousarray(peer_v[l], dtype=f32)})
        wa2_l = np.asarray(gla_wa2[l], f32)
        ba_l = np.asarray(gla_ba[l], f32)
        for hg in range(4):
            sl = slice(hg * 128, (hg + 1) * 128)
            per_head[hg].update({"wa2" + s_: np.ascontiguousarray(wa2_l[:, sl]), "nbacol" + s_: np.ascontiguousarray(-ba_l[sl][:, None]),
                                 "barow" + s_: bc128(ba_l[sl])})
    ins = []
    for cc in range(8):
        cm, am, sel = band_masks(cc)
        sel4 = np.zeros((128, 4), f32)
        sel4[:, cc % 4] = 1.0
        dct = dict(shared, xs=own_blocks(x2, cc), sel=sel, sel4=sel4, cm=cm, am=am)
        dct.update(per_head[cc % 4])
        ins.append(dct)
    return ins


def build_CA(with_next_A, final):
    nc = bass.Bass("TRN2", target_bir_lowering=False)
    P = Prog(nc)
    I = lambda name, shp, dt: P.dram(name, shp, dt, "ExternalInput")
    xs = I("xs", [NB, 128, D], F32)
    mixT_d = I("mixT", [D, NTOK], BF16)
    c_d = I("c_pk", [128, 16], F32)
    adawC = I("adaw", [D, 8192], F32)
    adabC = I("adab", [128, 8192], F32)
    g2n = I("g_bc", [128, D], F32)
    ident_d = I("ident", [128, 128], BF16)
    wo_d = I("wo", [D, D], F32)
    wq_d = I("wq", [D, D], F32)
    kT_d = I("kT", [16, 128, 128], F32)
    uT_d = I("uTt", [128, 128, 16, 128], F32)
    v_d = I("v", [NE, D], F32)
    fg_d = I("fg_bc", [128, D], F32) if final else None
    if with_next_A:
        adawA = I("adawA", [D, 4096], F32)
        adabA = I("adabA", [128, 4096], F32)
        g1 = I("g1_bc", [128, D], F32)
        wfm = I("wfm", [D, NFM], F32)
        wtm = I("wtm", [D, NTM], F32)
        zfm = P.dram("zfm", [NFM, NTOK], BF16, "ExternalOutput")
        ztb = P.dram("ztb", [NTOK, 2560], BF16, "ExternalOutput")
        ztf = P.dram("ztf", [NTOK, 640], F32, "ExternalOutput")
    xo = P.dram("xo", [NB, 128, D], F32, "ExternalOutput")
    xmid = P.scratch("xmid_s", [NB, 128, D], F32)
    h2T = P.scratch("h2T_s", [16, 128, NTOK], BF16)
    s12 = P.scratch("s12_s", [NB, 128, 16, 128], F32)
    g2 = P.scratch("g2_s", [128, D], F32)
    ident = emit_consts(P, ident_d)
    emit_C1(P, ident, xs, None, mixT_d, None, c_d, adawC, adabC, g2n, wo_d, wq_d, kT_d, xmid, h2T, s12, g2)
    emit_C2(P, ident, h2T, s12, xmid, g2, uT_d, v_d, fg_d, xo, final, not with_next_A)
    if with_next_A:
        emit_A_f(P, xo, xo, c_d, adawA, adabA, g1, wfm, wtm, ident, zfm, ztb, ztf)
        P.begin_phase()
        P.end_phase(final=True)
    P.finish()
    return nc


_NC_CACHE = {}


def _get_nc(name, fn):
    if name not in _NC_CACHE:
        _NC_CACHE[name] = fn()
    return _NC_CACHE[name]


def _run(nc, ins):
    return run_bass_kernel_spmd(nc, ins, core_ids=list(range(8))).results


def _a_weights(l, ada_w, ada_b, norm1_g, w_in, f32):
    w_in_l = np.asarray(w_in[l], f32)
    wfm = np.zeros((D, NFM), f32)
    wfm[:, :len(FM_COLS)] = w_in_l[:, FM_COLS]
    wtm = np.zeros((D, NTM), f32)
    wtm[:, :len(TM_COLS)] = w_in_l[:, TM_COLS]
    return dict(adaw=np.ascontiguousarray(ada_w[l][:, 0:4096]), adab=bc128(np.asarray(ada_b[l][0:4096], f32)),
                g_bc=bc128(np.asarray(norm1_g[l], f32)), wfm=wfm, wtm=wtm)


def kernel(x, c, ada_w, ada_b, norm1_g, norm2_g, final_g, w_in, fox_fbias, gla_wa2, gla_ba, gla_norm_g, w_out,
           peer_wq, peer_k1, peer_k2, peer_u, peer_v):
    f32 = np.float32
    x2 = np.asarray(x, f32).reshape(S, D)
    c1 = np.asarray(c, f32).reshape(D)
    xs_list = [own_blocks(x2, cc) for cc in range(8)]
    ident = np.eye(128, dtype=NPBF)
    tri2, suf2, rmask = gla_consts()
    masks = [band_masks(cc) for cc in range(8)]
    c_pk = np.ascontiguousarray(c1.reshape(16, 128).T)
    aw = _a_weights(0, ada_w, ada_b, norm1_g, w_in, f32)
    rA = _run(_get_nc("A", build_A), [dict(aw, c_pk=c_pk, ident=ident, xs=xs_list[cc]) for cc in range(8)])
    del aw
    for l in range(2):
        ZFM = assemble_tokens([r["zfm"] for r in rA], 1)
        ZTB = assemble_tokens([r["ztb"] for r in rA], 0)
        ZTF = assemble_tokens([r["ztf"] for r in rA], 0)
        fkT = np.ascontiguousarray(ZFM[R_FK:R_FK + 768])
        dkT = np.ascontiguousarray(ZFM[R_DK:R_DK + 768])
        ikT = np.ascontiguousarray(ZFM[R_IK:R_IK + 64])
        gaT = np.ascontiguousarray(ZFM[R_GA:R_GA + 16])
        fvg = vg_layout(ZTB[:, C_FV:C_FV + 768], 6)
        dvg = vg_layout(ZTB[:, C_DV:C_DV + 768], 6)
        ffp = np.ascontiguousarray(ZTF[:, 512:518].reshape(64, 128, 6).transpose(1, 2, 0))
        fbb = bc128(np.asarray(fox_fbias[l], f32))
        wa2_l = np.asarray(gla_wa2[l], f32)
        ba_l = np.asarray(gla_ba[l], f32)
        gnb = bc128(np.asarray(gla_norm_g[l], f32))
        gl = []
        for hg in range(4):
            sl = slice(hg * 128, (hg + 1) * 128)
            gl.append(dict(gqT=np.ascontiguousarray(ZFM[R_GQ + hg * 128:R_GQ + (hg + 1) * 128]),
                           gkT=np.ascontiguousarray(ZFM[R_GK + hg * 128:R_GK + (hg + 1) * 128]),
                           gktm=tm_layout(ZTB[:, C_GK + hg * 128:C_GK + (hg + 1) * 128]),
                           gvtm=tm_layout(ZTB[:, C_GV + hg * 128:C_GV + (hg + 1) * 128]),
                           grtm=tm_layout(ZTF[:, hg * 128:(hg + 1) * 128]), gaT=gaT,
                           wa2=np.ascontiguousarray(wa2_l[:, sl]), nbacol=np.ascontiguousarray(-ba_l[sl][:, None]),
                           barow=bc128(ba_l[sl]), gnb=gnb, tri2=tri2, suf2=suf2, rmask=rmask))
        insB = []
        for cc in range(8):
            cm, am, sel = masks[cc]
            zo = rA[cc]["zfm"]
            iq = zo[R_IQ:R_IQ + 1024].reshape(16, 64, 1024).transpose(1, 0, 2)
            iw = rA[cc]["ztf"][:, 518:534].reshape(8, 128, 16).transpose(1, 0, 2)
            dct = dict(fkT=fkT, fvg=fvg, fqT=np.ascontiguousarray(zo[R_FQ:R_FQ + 768]), ffp=ffp, fbb=fbb, tri=TRI, sel=sel, cm=cm,
                       dkT=dkT, dvg=dvg, dqT=np.ascontiguousarray(zo[R_DQ:R_DQ + 768]), iqT=np.ascontiguousarray(iq), ikT=ikT,
                       iwp=np.ascontiguousarray(iw), am=am, ident=ident)
            dct.update(gl[cc % 4])
            insB.append(dct)
        rB = _run(_get_nc("B", build_B), insB)
        del insB, fvg, dvg, gl, ZFM, ZTB, ZTF
        gla_full = np.concatenate([rB[hg]["glao"].reshape(S, 128) for hg in range(4)], axis=1)
        mix_list = []
        for cc in range(8):
            g_own = own_blocks(gla_full, cc)
            mix = np.concatenate([rB[cc]["foxo"], g_own, rB[cc]["dsao"]], axis=2)
            mix_list.append(mix.reshape(NTOK, D))
        final = (l == 1)
        kT = np.zeros((16, 128, 128), f32)
        for h in range(8):
            kT[2 * h] = np.asarray(peer_k1[l][h], f32).T
            kT[2 * h + 1] = np.asarray(peer_k2[l][h], f32).T
        common = dict(c_pk=c_pk, adaw=np.ascontiguousarray(ada_w[l][:, 4096:12288]), adab=bc128(np.asarray(ada_b[l][4096:12288], f32)),
                      g_bc=bc128(np.asarray(norm2_g[l], f32)), ident=ident, wo=np.ascontiguousarray(w_out[l], dtype=f32),
                      wq=np.ascontiguousarray(peer_wq[l], dtype=f32), kT=kT, uTt=uT_tiles(np.asarray(peer_u[l], f32)),
                      v=np.ascontiguousarray(peer_v[l], dtype=f32))
        if final:
            common["fg_bc"] = bc128(np.asarray(final_g, f32))
            nc = _get_nc("Cf", lambda: build_CA(False, True))
        else:
            aw = _a_weights(1, ada_w, ada_b, norm1_g, w_in, f32)
            common.update(adawA=aw["adaw"], adabA=aw["adab"], g1_bc=aw["g_bc"], wfm=aw["wfm"], wtm=aw["wtm"])
            nc = _get_nc("CA", lambda: build_CA(True, False))
        rC = _run(nc, [dict(common, xs=xs_list[cc], mixT=np.ascontiguousarray(mix_list[cc].T)) for cc in range(8)])
        del common
        xs_list = [r["xo"] for r in rC]
        rA = rC
    out = assemble_tokens([xx.reshape(NTOK, D) for xx in xs_list], 0)
    return np.ascontiguousarray(out.reshape(1, S, D).astype(np.float32))
```
